# Optimizing a Trainium2 kernel written in Bass

```python
import jax, jax.numpy as jnp
from jax import lax
import numpy as np

D_MODEL = 1024
BATCH = 32
SEQ = 2048
DEPTH = 1

ATT_HEADS = 8
ATT_HEAD_DIM = 64
ATT_WIDTH = ATT_HEADS * ATT_HEAD_DIM
Q_BLOCK = 128
FORGET_BIAS_INIT = 3.0
RNN_WIDTH = D_MODEL
RNN_BLOCKS = 16
RNN_BLOCK_DIM = RNN_WIDTH // RNN_BLOCKS
CONV_WIDTH = 4
RGLRU_C = 8.0
N_EXPERTS = 64
TOP_K = 6
N_GROUPS = 8
TOPK_GROUPS = 4
EXPERT_FF = D_MODEL // 4
SHARED_FF = EXPERT_FF
ROUTE_SCALE = 2.5
DISPATCH_ROWS = 256
NORM_EPS = 1e-6
IN_SPLITS = (ATT_WIDTH, ATT_WIDTH, ATT_WIDTH, ATT_HEADS, RNN_WIDTH, RNN_WIDTH, D_MODEL, D_MODEL)
IN_COLS = sum(IN_SPLITS)
IN_OFFSETS = tuple(int(o) for o in np.cumsum(IN_SPLITS)[:-1])

kernel_name = 'hybrid_fox_rglru_moe_block'


def rmsnorm(x, g):
    xf = x.astype(jnp.float32)
    y = xf * lax.rsqrt(jnp.mean(xf * xf, axis=-1, keepdims=True) + NORM_EPS)
    return (y * g.astype(jnp.float32)).astype(x.dtype)


def swiglu(x, w_gate, w_up, w_down):
    return (jax.nn.silu(x @ w_gate) * (x @ w_up)) @ w_down


def forgetting_attention(q, k, v, f_logit):
    S = q.shape[1]
    log_f = jax.nn.log_sigmoid(f_logit.astype(jnp.float32))
    cum = jnp.transpose(jnp.cumsum(log_f, axis=1), (0, 2, 1))
    scale = ATT_HEAD_DIM ** -0.5
    outs = []
    for i in range(S // Q_BLOCK):
        q0, q1 = i * Q_BLOCK, (i + 1) * Q_BLOCK
        kb, vb = k[:, :q1], v[:, :q1]
        s = jnp.einsum('bqhd,bkhd->bhqk', q[:, q0:q1], kb, preferred_element_type=jnp.float32) * scale
        s = s + cum[:, :, q0:q1, None] - cum[:, :, None, :q1]
        causal = jnp.arange(q1)[None, :] <= jnp.arange(q0, q1)[:, None]
        s = jnp.where(causal, s, -jnp.inf)
        p = jax.nn.softmax(s, axis=-1)
        outs.append(jnp.einsum('bhqk,bkhd->bqhd', p.astype(v.dtype), vb))
    return jnp.concatenate(outs, axis=1)


def causal_depthwise_conv(x, w, b):
    S = x.shape[1]
    xp = jnp.pad(x, ((0, 0), (CONV_WIDTH - 1, 0), (0, 0)))
    y = b
    for j in range(CONV_WIDTH):
        y = y + w[j] * xp[:, j:j + S]
    return y


def rg_lru(u, w_rg, b_rg, w_ig, b_ig, lam):
    B, S, _ = u.shape
    ub = u.reshape(B, S, RNN_BLOCKS, RNN_BLOCK_DIM)
    r = jax.nn.sigmoid((jnp.einsum('bsni,nij->bsnj', ub, w_rg).reshape(B, S, RNN_WIDTH) + b_rg).astype(jnp.float32))
    ig = jax.nn.sigmoid((jnp.einsum('bsni,nij->bsnj', ub, w_ig).reshape(B, S, RNN_WIDTH) + b_ig).astype(jnp.float32))
    log_a = -RGLRU_C * r * jax.nn.softplus(-lam.astype(jnp.float32))
    a = jnp.exp(log_a)
    mult = jnp.sqrt(-jnp.expm1(2.0 * log_a))
    b_in = mult * ig * u.astype(jnp.float32)

    def combine(left, right):
        a1, b1 = left
        a2, b2 = right
        return a1 * a2, a2 * b1 + b2

    _, h = lax.associative_scan(combine, (a, b_in), axis=1)
    return h.astype(u.dtype)


def route(h_flat, w_router, router_bias):
    T = h_flat.shape[0]
    scores = jax.nn.sigmoid(h_flat.astype(jnp.float32) @ w_router.astype(jnp.float32))
    sel = scores + router_bias.astype(jnp.float32)
    grp = sel.reshape(T, N_GROUPS, N_EXPERTS // N_GROUPS)
    grp_score = lax.top_k(grp, 2)[0].sum(-1)
    _, gidx = lax.top_k(grp_score, TOPK_GROUPS)
    gmask = jnp.any(gidx[..., None] == jnp.arange(N_GROUPS)[None, None, :], axis=1)
    emask = jnp.repeat(gmask, N_EXPERTS // N_GROUPS, axis=1)
    _, eidx = lax.top_k(jnp.where(emask, sel, -jnp.inf), TOP_K)
    w = jnp.take_along_axis(scores, eidx, axis=-1)
    w = w / jnp.sum(w, axis=-1, keepdims=True) * ROUTE_SCALE
    return eidx.astype(jnp.int32), w


def routed_experts(h_flat, eidx, ew, w_gate, w_up, w_down):
    T, D = h_flat.shape
    P = T * TOP_K
    C = DISPATCH_ROWS
    n_blocks = -(-P // C) + N_EXPERTS
    flat_e = eidx.reshape(-1)
    flat_tok = jnp.repeat(jnp.arange(T, dtype=jnp.int32), TOP_K)
    flat_w = ew.reshape(-1)
    order = jnp.argsort(flat_e)
    se, stok, sw = flat_e[order], flat_tok[order], flat_w[order]
    counts = jnp.bincount(flat_e, length=N_EXPERTS).astype(jnp.int32)
    start = jnp.cumsum(counts) - counts
    padded = ((counts + C - 1) // C) * C
    pend = jnp.cumsum(padded)
    pstart = pend - padded
    dest = pstart[se] + (jnp.arange(P, dtype=jnp.int32) - start[se])
    buf_tok = jnp.full((n_blocks * C,), T, jnp.int32).at[dest].set(stok)
    buf_w = jnp.zeros((n_blocks * C,), jnp.float32).at[dest].set(sw)
    block_e = jnp.minimum(jnp.searchsorted(pend, jnp.arange(n_blocks, dtype=jnp.int32) * C, side='right'),
                          N_EXPERTS - 1).astype(jnp.int32)
    h_pad = jnp.concatenate([h_flat, jnp.zeros((1, D), h_flat.dtype)], axis=0)

    def expert_block(args):
        tok, wt, e = args
        y = swiglu(h_pad[tok], w_gate[e], w_up[e], w_down[e])
        return y * wt[:, None].astype(y.dtype)

    y_buf = lax.map(expert_block, (buf_tok.reshape(n_blocks, C), buf_w.reshape(n_blocks, C), block_e))
    return jax.ops.segment_sum(y_buf.reshape(-1, D), buf_tok, num_segments=T + 1)[:T]


def setup_inputs(seed: int = 0) -> dict:
    key = jax.random.key(seed)
    ks = jax.random.split(key, 28)
    f32 = jnp.float32
    L, D = DEPTH, D_MODEL

    def nrm(k, shape, scale):
        return jax.random.normal(k, shape, f32) * scale

    a0 = jax.random.uniform(ks[16], (L, RNN_WIDTH), f32, 0.9, 0.999)
    return {
        'x': nrm(ks[0], (BATCH, SEQ, D), 1.0),
        'c': nrm(ks[1], (BATCH, D), 1.0),
        'w_ada': nrm(ks[2], (L, D, 6 * D), 0.5 * D ** -0.5),
        'b_ada': nrm(ks[3], (L, 6 * D), 0.02),
        'g_pre_mix': 1.0 + nrm(ks[4], (L, D), 0.1),
        'g_post_mix': 1.0 + nrm(ks[5], (L, D), 0.1),
        'g_pre_ffn': 1.0 + nrm(ks[6], (L, D), 0.1),
        'g_post_ffn': 1.0 + nrm(ks[7], (L, D), 0.1),
        'w_in': nrm(ks[8], (L, D, IN_COLS), D ** -0.5),
        'b_forget': FORGET_BIAS_INIT + nrm(ks[9], (L, ATT_HEADS), 0.5),
        'w_conv': nrm(ks[10], (L, CONV_WIDTH, RNN_WIDTH), CONV_WIDTH ** -0.5),
        'b_conv': nrm(ks[11], (L, RNN_WIDTH), 0.02),
        'w_rg': nrm(ks[12], (L, RNN_BLOCKS, RNN_BLOCK_DIM, RNN_BLOCK_DIM), RNN_BLOCK_DIM ** -0.5),
        'b_rg': nrm(ks[13], (L, RNN_WIDTH), 0.02),
        'w_ig': nrm(ks[14], (L, RNN_BLOCKS, RNN_BLOCK_DIM, RNN_BLOCK_DIM), RNN_BLOCK_DIM ** -0.5),
        'b_ig': nrm(ks[15], (L, RNN_WIDTH), 0.02),
        'rglru_lambda': jnp.log(a0) - jnp.log1p(-a0),
        'w_branch_attn': nrm(ks[17], (L, ATT_WIDTH, D), ATT_WIDTH ** -0.5),
        'w_branch_rnn': nrm(ks[18], (L, RNN_WIDTH, D), RNN_WIDTH ** -0.5),
        'w_out': nrm(ks[19], (L, D, D), D ** -0.5),
        'w_router': nrm(ks[20], (L, D, N_EXPERTS), D ** -0.5),
        'router_bias': nrm(ks[21], (L, N_EXPERTS), 0.01),
        'w_exp_gate': nrm(ks[22], (L, N_EXPERTS, D, EXPERT_FF), D ** -0.5),
        'w_exp_up': nrm(ks[23], (L, N_EXPERTS, D, EXPERT_FF), D ** -0.5),
        'w_exp_down': nrm(ks[24], (L, N_EXPERTS, EXPERT_FF, D), EXPERT_FF ** -0.5),
        'w_sh_gate': nrm(ks[25], (L, D, SHARED_FF), D ** -0.5),
        'w_sh_up': nrm(ks[26], (L, D, SHARED_FF), D ** -0.5),
        'w_sh_down': nrm(ks[27], (L, SHARED_FF, D), SHARED_FF ** -0.5),
    }


def reference(x, c, w_ada, b_ada, g_pre_mix, g_post_mix, g_pre_ffn, g_post_ffn, w_in, b_forget,
              w_conv, b_conv, w_rg, b_rg, w_ig, b_ig, rglru_lambda, w_branch_attn, w_branch_rnn, w_out,
              w_router, router_bias, w_exp_gate, w_exp_up, w_exp_down, w_sh_gate, w_sh_up, w_sh_down):
    B, S, _ = x.shape
    for l in range(DEPTH):
        mod = jax.nn.silu(c) @ w_ada[l] + b_ada[l]
        shift_m, scale_m, gate_m, shift_f, scale_f, gate_f = jnp.split(mod[:, None, :], 6, axis=-1)

        h = rmsnorm(x, g_pre_mix[l]) * (1.0 + scale_m) + shift_m
        proj = h @ w_in[l]
        q, k, v, f_logit, x_rnn, g_rnn, gate_a, gate_r = jnp.split(proj, IN_OFFSETS, axis=-1)
        y_attn = forgetting_attention(q.reshape(B, S, ATT_HEADS, ATT_HEAD_DIM),
                                      k.reshape(B, S, ATT_HEADS, ATT_HEAD_DIM),
                                      v.reshape(B, S, ATT_HEADS, ATT_HEAD_DIM),
                                      f_logit + b_forget[l]).reshape(B, S, ATT_WIDTH)
        u = causal_depthwise_conv(x_rnn, w_conv[l], b_conv[l])
        y_rnn = rg_lru(u, w_rg[l], b_rg[l], w_ig[l], b_ig[l], rglru_lambda[l]) * jax.nn.gelu(g_rnn)
        merged = (jax.nn.sigmoid(gate_a) * (y_attn @ w_branch_attn[l])
                  + jax.nn.sigmoid(gate_r) * (y_rnn @ w_branch_rnn[l]))
        x = x + gate_m * rmsnorm(merged @ w_out[l], g_post_mix[l])

        h = rmsnorm(x, g_pre_ffn[l]) * (1.0 + scale_f) + shift_f
        h_flat = h.reshape(B * S, D_MODEL)
        eidx, ew = route(h_flat, w_router[l], router_bias[l])
        routed = routed_experts(h_flat, eidx, ew, w_exp_gate[l], w_exp_up[l], w_exp_down[l])
        shared = swiglu(h_flat, w_sh_gate[l], w_sh_up[l], w_sh_down[l])
        x = x + gate_f * rmsnorm((routed + shared).reshape(B, S, D_MODEL), g_post_ffn[l])
    return x
```

```python
import numpy as np
import concourse.bass as bass
import concourse.mybir as mybir
from concourse.bass_utils import run_bass_kernel_spmd
from contextlib import ExitStack

F32 = mybir.dt.float32
BF16 = mybir.dt.bfloat16
I32 = mybir.dt.int32
U32 = mybir.dt.uint32
AF = mybir.ActivationFunctionType
ALU = mybir.AluOpType
AX = mybir.AxisListType

D = 1024
NH = 8
E = 64
TOPK = 6
FF = 256
CB = 256
INC = 5640
OQ, OK_, OV, OF_, OX, OG, OGA, OGR = 0, 512, 1024, 1536, 1544, 2568, 3592, 4616
EPS = 1e-6
BIG = 1.0e4
NCORES = 8
ARENA_SHIFT = [0]
ARENA_MAX = [0]


class Buf:
    __slots__ = ("w", "r", "name")

    def __init__(self, name=""):
        self.w = {}
        self.r = {}
        self.name = name


class Eng:
    def __init__(self, name, e, sem, key):
        self.name = name
        self.e = e
        self.sem = sem
        self.key = key
        self.n = 0
        self.seen = {}
        self.pending = False


class DQ:
    def __init__(self, eng, sems):
        self.eng = eng
        self.sems = sems
        self.cnt = [0] * len(sems)
        self.next = 0


def _merge(d, s):
    for k, v in s.items():
        if d.get(k, 0) < v:
            d[k] = v


class KB:
    def __init__(self, nc, stack):
        self.nc = nc
        self.semtab = {}
        self.engs = []
        for nm, e in (("pe", nc.tensor), ("act", nc.scalar), ("dve", nc.vector),
                      ("pool", nc.gpsimd), ("sp", nc.sync)):
            sem = stack.enter_context(nc.semaphore("s_" + nm))
            eng = Eng(nm, e, sem, "c_" + nm)
            self.semtab[eng.key] = sem
            setattr(self, nm, eng)
            self.engs.append(eng)
        self.queues = []
        for nm, eng, n in (("qsp", self.sp, 8), ("qpool", self.pool, 6)):
            sems = []
            for i in range(n):
                key = "d_%s%d" % (nm, i)
                sem = stack.enter_context(nc.semaphore(key))
                self.semtab[key] = sem
                sems.append((sem, key))
            q = DQ(eng, sems)
            setattr(self, nm, q)
            self.queues.append(q)

    def _wait(self, E_, deps):
        for k, v in deps.items():
            if E_.seen.get(k, 0) < v:
                E_.e.wait_ge(self.semtab[k], v)
                E_.seen[k] = v

    def op(self, E_, fn, reads=(), writes=(), inc=True):
        deps = {}
        for b in reads:
            _merge(deps, b.w)
        for b in writes:
            _merge(deps, b.w)
            _merge(deps, b.r)
        if E_.name == "pe":
            deps.pop(E_.key, None)
        self._wait(E_, deps)
        ins = fn()
        ev = E_.n + 1
        for b in reads:
            if b.r.get(E_.key, 0) < ev:
                b.r[E_.key] = ev
        for b in writes:
            b.w = {E_.key: ev}
            b.r = {}
        if inc:
            E_.n = ev
            ins.then_inc(E_.sem, 1)
            E_.pending = False
        else:
            E_.pending = True
        return ins

    def dma(self, Q, fn, reads=(), writes=(), swrites=()):
        E_ = Q.eng
        deps = {}
        for b in reads:
            _merge(deps, b.w)
        for b in writes:
            _merge(deps, b.w)
            _merge(deps, b.r)
        for b in swrites:
            _merge(deps, b.r)
        slot = Q.next
        Q.next = (Q.next + 1) % len(Q.sems)
        sem, key = Q.sems[slot]
        if Q.cnt[slot] > 0:
            if deps.get(key, 0) < 16 * Q.cnt[slot]:
                deps[key] = 16 * Q.cnt[slot]
        self._wait(E_, deps)
        ins = fn()
        Q.cnt[slot] += 1
        v = 16 * Q.cnt[slot]
        ins.then_inc(sem, 16)
        for b in reads:
            if b.r.get(key, 0) < v:
                b.r[key] = v
        for b in writes:
            b.w = {key: v}
            b.r = {}
        for b in swrites:
            if b.w.get(key, 0) < v:
                b.w[key] = v
        return ins

    def barrier(self):
        tot = {}
        for E_ in self.engs:
            assert not E_.pending
            if E_.n > 0:
                tot[E_.key] = E_.n
        for Q in self.queues:
            for i, (sem, key) in enumerate(Q.sems):
                if Q.cnt[i] > 0:
                    tot[key] = 16 * Q.cnt[i]
        for E_ in self.engs:
            self._wait(E_, dict(tot))


class Arena:
    def __init__(self, nc, limit):
        self.nc = nc
        self.off = 0
        self.limit = limit
        self.n = 0

    def alloc(self, name, shape, dtype):
        sz = 1
        for s in shape[1:]:
            sz *= s
        sz *= {F32: 4, BF16: 2, I32: 4, U32: 4}[dtype]
        sz = (sz + 63) // 64 * 64
        off = self.off
        assert off + sz <= self.limit, ("SBUF arena overflow", name, off, sz)
        self.off += sz
        self.n += 1
        ARENA_MAX[0] = max(ARENA_MAX[0], self.off)
        return self.nc.alloc_sbuf_tensor_at("%s_%d" % (name, self.n), list(shape), dtype, offset=off)

    def mark(self):
        return self.off

    def release(self, m):
        self.off = m


def build(NSEQ, S, skip_reload=True):
    NT_S = S // 128
    NTOK = NSEQ * S
    NT = NTOK // 128
    QB = min(512, S)
    TQ = QB // 128
    NQ = S // QB
    NB5 = S // QB
    NBLK = (NTOK * TOPK) // CB + E
    NSLOT = NBLK * CB

    nc = bass.Bass("TRN2", target_bir_lowering=False)
    dt = nc.dram_tensor

    def ein(name, shape, dtype=F32):
        return dt(name, list(shape), dtype, kind="ExternalInput")

    x_d = ein("x", [NTOK, D])
    cs_d = ein("csT", [128, 8, NSEQ])
    wada_d = ein("w_ada", [D, 6 * D])
    bada_d = ein("b_ada_rep", [NSEQ, 6 * D])
    fm_d = ein("fm", [128, 9, 8])
    bcv_d = ein("bcv", [128, 3, D])
    rb_d = ein("rbias", [128, E])
    bf_d = ein("b_forget", [NH, 1])
    win_d = ein("w_in", [D, INC])
    wrg_d = ein("wrg_bd", [8, 128, 128])
    wig_d = ein("wig_bd", [8, 128, 128])
    wba_d = ein("w_ba", [512, D])
    wbr_d = ein("w_br", [D, D])
    wout_d = ein("w_out", [D, D])
    wr_d = ein("w_router", [D, E])
    wsg_d = ein("w_sg", [D, FF])
    wsu_d = ein("w_su", [D, FF])
    wsd_d = ein("w_sd", [FF, D])
    wpg_d = ein("wpg", [E * 128, 2048])
    wpu_d = ein("wpu", [E * 128, 2048])
    wpd_d = ein("wpd", [E * 128, 2048])
    NCF = 128 + 128 + 64 + NBLK + 2
    cf_d = ein("cf", [128, NCF])
    cb_d = ein("cb", [128, 1792], BF16)
    out_d = dt("out", [NTOK, D], F32, kind="ExternalOutput")
    modd = dt("modd", [NSEQ, 6 * D], F32)
    h2_d = dt("h2s", [NTOK, D], BF16)
    x1_d = dt("x1s", [NTOK, D], F32)
    sh_d = dt("shs", [NTOK, D], BF16)
    xs_d = dt("xss", [NSLOT, D], BF16)
    ys_d = dt("yss", [NSLOT, D], BF16)
    wpb_d = [dt("wpb%d" % m_, [E * 128, 2048], BF16) for m_ in range(3)]

    stack = ExitStack()
    with stack:
        K = KB(nc, stack)
        al = Arena(nc, int(nc._sbuf_addr_for_side("right")) - 64)
        al.off = (int(nc._sbuf_addr_for_side("left")) + 63) // 64 * 64 + ARENA_SHIFT[0]
        ps = [stack.enter_context(nc.psum_tensor("ps%d" % i, [128, 512], F32)) for i in range(8)]
        pb = [Buf("pb%d" % i) for i in range(8)]
        rot = {"l": list(range(8)), "i": 0}

        def nb():
            i = rot["l"][rot["i"] % len(rot["l"])]
            rot["i"] += 1
            return i

        PE, ACT, DVE, POOL = K.pe, K.act, K.dve, K.pool
        reg_slot = nc.gpsimd.alloc_register("bc_slot")
        nc.gpsimd.reg_mov(reg_slot, NSLOT - 1)
        reg_w = nc.gpsimd.alloc_register("bc_w")
        nc.gpsimd.reg_mov(reg_w, E * 128 - 1)
        v_, a_, g_, t_ = nc.vector, nc.scalar, nc.gpsimd, nc.tensor

        def mm(out, lhsT, rhs, start, stop, r, w, inc):
            return K.op(PE, lambda: t_.matmul(out, lhsT, rhs, start=start, stop=stop), r, w, inc)

        def tr(out, in_, ident, r, w, inc):
            return K.op(PE, lambda: t_.transpose(out, in_, ident), r, w, inc)

        def sp_dma(out, in_, r=(), w=(), sw=()):
            return K.dma(K.qsp, lambda: nc.sync.dma_start(out=out, in_=in_), r, w, sw)

        def pl_dma(out, in_, r=(), w=(), sw=()):
            return K.dma(K.qpool, lambda: nc.gpsimd.dma_start(out=out, in_=in_), r, w, sw)

        cf = al.alloc("cf", [128, NCF], F32)
        cbt = al.alloc("cb", [128, 1792], BF16)
        b_const = Buf("const")
        ident_f = cf[:, 0:128]
        sel127 = cf[:, 128:256]
        iota64 = cf[:, 256:320]
        iotab = cf[:, 320:320 + NBLK]
        pidx = cf[:, 320 + NBLK:321 + NBLK]
        ones_c = cf[:, 321 + NBLK:322 + NBLK]
        ident_b = cbt[:, 0:128]
        triu_b = cbt[:, 128:256]
        trim_b = cbt[:, 256:384]
        ones_b = cbt[:, 384:512]
        negm_b = cbt[:, 1536:1664]
        swap_b = cbt[:, 1664:1792]
        fm = al.alloc("fm", [128, 9, 8], F32)
        sm = al.alloc("sm", [128, 6, 8], F32)
        rbias = al.alloc("rbias", [128, E], F32)
        nbf = al.alloc("nbf", [128, 2], F32)
        b_nbf = Buf()
        gsmT = al.alloc("gsmT", [128, 8, NSEQ], F32)
        shmT = al.alloc("shmT", [128, 8, NSEQ], F32)
        run = al.alloc("run", [128, E], F32)
        rankm = al.alloc("rankm", [128, NT, E], F32)
        eidx = al.alloc("eidx", [128, NT, 8], F32)
        wk = al.alloc("wk", [128, NT, 8], F32)
        wkn = al.alloc("wkn", [128, NT, 8], F32)
        desti = al.alloc("desti", [128, NT, 8], I32)
        widx = al.alloc("widx", [128, NBLK], I32)
        b_fm, b_sm, b_gs, b_run, b_route = Buf(), Buf(), Buf(), Buf(), Buf()
        b_widx = Buf()
        zt = al.alloc("zt", [128, 2, D], BF16)
        b_zt, b_xs0 = Buf(), Buf()
        K.op(POOL, lambda: g_.memset(zt[:], 0.0), [], [b_zt])
        zf = {"n": 0}
        ZF_PER = -(-NBLK // NT)

        b_wcast = Buf()
        pcast = {"n": 0}
        PC_PER = -(-(3 * E) // (NSEQ * 4 * NQ))

        def precast(cnt):
            for _ in range(cnt):
                if pcast["n"] < 3 * E:
                    e_, m_ = pcast["n"] // 3, pcast["n"] % 3
                    src = (wpg_d, wpu_d, wpd_d)[m_]
                    pl_dma(wpb_d[m_][e_ * 128:(e_ + 1) * 128, :], src[e_ * 128:(e_ + 1) * 128, :], sw=[b_wcast])
                    pcast["n"] += 1

        def zero_fill(cnt):
            for _ in range(cnt):
                if zf["n"] < NBLK:
                    r0_ = zf["n"] * CB
                    sp_dma(xs_d[r0_:r0_ + CB, :].rearrange("(s p) d -> p s d", p=128), zt[:], r=[b_zt], sw=[b_xs0])
                    zf["n"] += 1

        sp_dma(cf[:], cf_d.ap(), w=[b_const])
        sp_dma(cbt[:], cb_d.ap(), w=[b_const])
        sp_dma(fm[:], fm_d.ap(), w=[b_fm])
        sp_dma(rbias[:], rb_d.ap(), w=[b_const])
        K.op(DVE, lambda: v_.memset(nbf[:], 0.0), [], [b_nbf])
        for g3 in range(3):
            sp_dma(nbf[g3 * 32:g3 * 32 + NH, 0:1], bf_d.ap(), w=[b_nbf])
        K.op(DVE, lambda: v_.memset(run[:], 0.0), [], [b_run])

        m0 = al.mark()
        cs = al.alloc("cs", [128, 8, NSEQ], F32)
        th0 = al.alloc("th0", [128, 8, NSEQ], F32)
        siluT = al.alloc("siluT", [128, 8, NSEQ], BF16)
        modt = al.alloc("modt", [NSEQ, 6 * D], F32)
        bada = al.alloc("bada", [NSEQ, 6 * D], F32)
        wada = [al.alloc("wada", [128, 8, 512], BF16) for _ in range(2)]
        b_cs, b_th0, b_silu, b_modt, b_bada = Buf(), Buf(), Buf(), Buf(), Buf()
        b_wada = [Buf(), Buf()]

        K.op(DVE, lambda: v_.tensor_scalar(out=nbf[0:72, 1:2], in0=nbf[0:72, 0:1], scalar1=-1.0, scalar2=None,
                                           op0=ALU.mult), [b_nbf], [b_nbf])
        K.op(ACT, lambda: a_.activation(out=sm[:, 4, :], in_=fm[:, 8, :], func=AF.Exp, scale=-1.0), [b_fm], [b_sm])
        K.op(ACT, lambda: a_.activation(out=sm[:, 5, :], in_=sm[:, 4, :], func=AF.Ln, bias=1.0, scale=1.0), [b_sm], [b_sm])
        K.op(DVE, lambda: v_.tensor_scalar(out=sm[:, 0, :], in0=sm[:, 5, :], scalar1=-8.0, scalar2=None, op0=ALU.mult), [b_sm], [b_sm])
        K.op(DVE, lambda: v_.tensor_scalar(out=sm[:, 1, :], in0=sm[:, 5, :], scalar1=-4.0, scalar2=None, op0=ALU.mult), [b_sm], [b_sm])
        K.op(DVE, lambda: v_.tensor_scalar(out=sm[:, 2, :], in0=fm[:, 6, :], scalar1=0.5, scalar2=None, op0=ALU.mult), [b_fm, b_sm], [b_sm])
        K.op(DVE, lambda: v_.tensor_scalar(out=sm[:, 3, :], in0=fm[:, 7, :], scalar1=0.5, scalar2=None, op0=ALU.mult), [b_fm, b_sm], [b_sm])
        cneg = sm[:, 0, :]
        hcneg = sm[:, 1, :]
        hbrg = sm[:, 2, :]
        hbig = sm[:, 3, :]

        sp_dma(cs[:], cs_d.ap(), w=[b_cs])
        sp_dma(bada[:], bada_d.ap(), w=[b_bada])
        K.op(ACT, lambda: a_.activation(out=th0[:], in_=cs[:], func=AF.Tanh, scale=0.5), [b_cs], [b_th0])
        K.op(DVE, lambda: v_.scalar_tensor_tensor(out=th0[:], in0=th0[:], scalar=1.0, in1=cs[:], op0=ALU.add, op1=ALU.mult),
             [b_cs, b_th0], [b_th0])
        K.op(DVE, lambda: v_.tensor_scalar(out=siluT[:], in0=th0[:], scalar1=0.5, scalar2=None, op0=ALU.mult), [b_th0], [b_silu])
        for g in range(12):
            wb = wada[g % 2]
            bw = b_wada[g % 2]
            pl_dma(wb[:], wada_d[:, g * 512:(g + 1) * 512].rearrange("(kc p) n -> p kc n", p=128), w=[bw])
            i = nb()
            for kc in range(8):
                mm(ps[i][0:NSEQ, :], siluT[:, kc, :], wb[:, kc, :], kc == 0, kc == 7, [b_silu, bw], [pb[i]], kc == 7)
            K.op(DVE, lambda: v_.tensor_tensor(out=modt[:, g * 512:(g + 1) * 512], in0=ps[i][0:NSEQ, :],
                                               in1=bada[:, g * 512:(g + 1) * 512], op=ALU.add), [pb[i], b_bada], [b_modt])
        b_modd = Buf()
        sp_dma(modd.ap(), modt[:], r=[b_modt], w=[b_modd])
        i = nb()
        pT0 = ps[i][:, 0:16 * NSEQ].rearrange("p (a b) -> p a b", b=NSEQ)
        for kc in range(16):
            col = (D + kc * 128) if kc < 8 else ((kc - 8) * 128)
            tr(pT0[:, kc, :], modt[0:NSEQ, col:col + 128], ident_f[0:NSEQ, 0:NSEQ], [b_modt, b_const], [pb[i]], kc == 15)
        K.op(DVE, lambda: v_.scalar_tensor_tensor(out=gsmT[:], in0=pT0[:, 0:8, :], scalar=1.0,
                                                  in1=fm[:, 0, :].unsqueeze(2).to_broadcast([128, 8, NSEQ]),
                                                  op0=ALU.add, op1=ALU.mult), [pb[i], b_fm], [b_gs])
        K.op(DVE, lambda: v_.tensor_copy(out=shmT[:], in_=pT0[:, 8:16, :]), [pb[i]], [b_gs])
        K.barrier()
        al.release(m0)

        off_hT = al.off
        hT = al.alloc("hT", [128, 8, S], BF16)
        yaT = al.alloc("yaT", [128, 4, S], BF16)
        off_yrT = al.off
        yrT = al.alloc("yrT", [128, 8, S], BF16)
        LIMIT = al.limit
        b_hT = [Buf() for _ in range(NT_S)]
        b_yaT, b_yrT = Buf(), Buf()
        mU = al.mark()

        for b in range(NSEQ):
            tok0 = b * S
            al.release(mU)
            x_sb = [al.alloc("x_sb", [128, D], F32) for _ in range(4)]
            xn = [al.alloc("xn", [128, D], BF16) for _ in range(2)]
            jk = al.alloc("jk", [128, D], BF16)
            st = [al.alloc("st", [128, 4], F32) for _ in range(2)]
            b_x, b_xn, b_st, b_jk = [Buf() for _ in range(4)], [Buf(), Buf()], [Buf(), Buf()], Buf()
            rot["l"] = list(range(8))
            def m1_L(t):
                sp_dma(x_sb[t % 4][:], x_d[tok0 + t * 128: tok0 + (t + 1) * 128, :], w=[b_x[t % 4]])

            def m1_A(t):
                xb, xnb, stb = x_sb[t % 4], xn[t % 2], st[t % 2]
                bx, bxn, bst = b_x[t % 4], b_xn[t % 2], b_st[t % 2]
                zero_fill(ZF_PER)
                K.op(ACT, lambda: a_.activation(out=jk[:], in_=xb[:], func=AF.Square, accum_out=stb[:, 0:1]), [bx], [b_jk, bst])
                K.op(ACT, lambda: a_.activation(out=stb[:, 1:2], in_=stb[:, 0:1], func=AF.Sqrt, bias=EPS, scale=1.0 / D), [bst], [bst])
                K.op(DVE, lambda: v_.reciprocal(out=stb[:, 2:3], in_=stb[:, 1:2]), [bst], [bst])
                K.op(ACT, lambda: a_.activation(out=xnb[:], in_=xb[:], func=AF.Identity, scale=stb[:, 2:3]), [bx, bst], [bxn])

            def m1_B(t):
                xnb, bxn = xn[t % 2], b_xn[t % 2]
                i = nb()
                pT = ps[i][:, :].bitcast(BF16)
                for kc in range(8):
                    tr(pT[:, kc * 128:(kc + 1) * 128], xnb[:, kc * 128:(kc + 1) * 128], ident_b, [bxn, b_const], [pb[i]], kc == 7)
                for kc in range(8):
                    o_ap = hT[:, kc, t * 128:(t + 1) * 128]
                    i_ap = pT[:, kc * 128:(kc + 1) * 128]
                    if kc % 2 == 0:
                        K.op(ACT, lambda: a_.activation(out=o_ap, in_=i_ap, func=AF.Identity, bias=shmT[:, kc, b:b + 1],
                                                        scale=gsmT[:, kc, b:b + 1]), [pb[i], b_gs], [b_hT[t]])
                    else:
                        K.op(DVE, lambda: v_.tensor_scalar(out=o_ap, in0=i_ap, scalar1=gsmT[:, kc, b:b + 1],
                                                           scalar2=shmT[:, kc, b:b + 1], op0=ALU.mult, op1=ALU.add),
                             [pb[i], b_gs], [b_hT[t]])
            for t0_ in range(min(3, NT_S)):
                m1_L(t0_)
            m1_A(0)
            for t in range(NT_S):
                if t + 3 < NT_S:
                    m1_L(t + 3)
                if t + 1 < NT_S:
                    m1_A(t + 1)
                m1_B(t)
            K.barrier()

            al.release(mU)
            qT = al.alloc("qT", [128, 4, 2, S], BF16)
            kT = al.alloc("kT", [128, 4, S], BF16)
            sv_ = al.off
            al.off = off_yrT
            vS = al.alloc("vS", [128, NT_S, 4, 192], BF16)
            ef = al.alloc("ef", [72, S], F32)
            assert al.off <= mU
            al.off = sv_
            off_wq = al.off
            wq = al.alloc("wq", [128, 8, 512], BF16)
            wkk = al.alloc("wk", [128, 8, 512], BF16)
            wv = al.alloc("wv", [128, 8, 512], BF16)
            wf = al.alloc("wf", [128, 8, 72], BF16)
            caug = al.alloc("caug", [128, S], BF16)
            tmpb = al.alloc("tmpb", [72, S], BF16)
            b_caug, b_tmpb = Buf(), Buf()
            Lf = al.alloc("Lf", [72, S], F32)
            cumLT = al.alloc("cumLT", [128, NT_S, NH], F32)
            b_Rb, b_Rs = Buf(), Buf()
            b_q, b_k, b_v, b_wq, b_wk, b_wv, b_wf = Buf(), Buf(), Buf(), Buf(), Buf(), Buf(), Buf()
            b_ef, b_L, b_cumLT = Buf(), Buf(), Buf()
            b_PT = [Buf() for _ in range(6)]

            def wview(c0, n):
                return win_d[:, c0:c0 + n].rearrange("(kc p) n -> p kc n", p=128)
            K.op(POOL, lambda: g_.memset(vS[:, :, :, 64:128], 1.0), [], [b_v])
            K.op(POOL, lambda: g_.memset(qT[:], 0.0), [], [b_q])
            pl_dma(wq[:], wview(OQ, 512), w=[b_wq])
            pl_dma(wkk[:], wview(OK_, 512), w=[b_wk])
            pl_dma(wv[:], wview(OV, 512), w=[b_wv])
            K.op(POOL, lambda: g_.memset(wf[:], 0.0), [], [b_wf])
            K.op(POOL, lambda: g_.memset(caug[:], 0.0), [], [b_caug])
            for g3 in range(3):
                pl_dma(wf[:, :, g3 * 32:g3 * 32 + NH], wview(OF_, 8), w=[b_wf])
            rot["l"] = list(range(8))
            cnt = 0
            for (wt, bw, dst, bd, isq) in ((wq, b_wq, qT, b_q, True), (wkk, b_wk, kT, b_k, False)):
                for j in range(4):
                    for n in range(NQ):
                        i = nb()
                        for kc in range(8):
                            mm(ps[i][:, 0:QB], wt[:, kc, j * 128:(j + 1) * 128], hT[:, kc, n * QB:(n + 1) * QB], kc == 0, kc == 7,
                               [bw] + b_hT[n * TQ:(n + 1) * TQ], [pb[i]], kc == 7)
                        cols = slice(n * QB, (n + 1) * QB)
                        if isq:
                            K.op(ACT, lambda: a_.copy(out=qT[0:64, j, 0, cols], in_=ps[i][0:64, 0:QB]), [pb[i]], [bd])
                            K.op(DVE, lambda: v_.tensor_copy(out=qT[64:128, j, 1, cols], in_=ps[i][64:128, 0:QB]), [pb[i]], [bd])
                        elif cnt % 2 == 0:
                            K.op(ACT, lambda: a_.copy(out=kT[:, j, cols], in_=ps[i][:, 0:QB]), [pb[i]], [bd])
                        else:
                            K.op(DVE, lambda: v_.tensor_copy(out=kT[:, j, cols], in_=ps[i][:, 0:QB]), [pb[i]], [bd])
                        cnt += 1
            for t in range(NT_S):
                i = nb()
                for kc in range(8):
                    mm(ps[i][:, :], hT[:, kc, t * 128:(t + 1) * 128], wv[:, kc, :], kc == 0, kc == 7, [b_wv, b_hT[t]], [pb[i]], kc == 7)
                o_v = vS[:, t, :, :].rearrange("p j (c w) -> p j c w", w=64)[:, :, 0::2, :]
                i_v = ps[i][:, :].rearrange("p (j c w) -> p j c w", c=2, w=64)
                if t % 2 == 0:
                    K.op(ACT, lambda: a_.copy(out=o_v, in_=i_v), [pb[i]], [b_v])
                else:
                    K.op(DVE, lambda: v_.tensor_copy(out=o_v, in_=i_v), [pb[i]], [b_v])
            for n in range(NQ):
                i = nb()
                for kc in range(8):
                    mm(ps[i][0:72, 0:QB], wf[:, kc, :], hT[:, kc, n * QB:(n + 1) * QB], kc == 0, kc == 7,
                       [b_wf] + b_hT[n * TQ:(n + 1) * TQ], [pb[i]], kc == 7)
                K.op(ACT, lambda: a_.activation(out=ef[:, n * QB:(n + 1) * QB], in_=ps[i][0:72, 0:QB], func=AF.Exp,
                                                bias=nbf[0:72, 1:2], scale=-1.0), [pb[i], b_nbf], [b_ef])
            K.op(ACT, lambda: a_.activation(out=Lf[:], in_=ef[:], func=AF.Ln, bias=1.0, scale=1.0), [b_ef], [b_L])
            K.op(DVE, lambda: v_.tensor_tensor_scan(out=ef[:], data0=ones_c[0:72, 0:1].to_broadcast([72, S]), data1=Lf[:],
                                                    initial=0.0, op0=ALU.mult, op1=ALU.add), [b_L, b_const, b_ef], [b_ef])
            K.op(DVE, lambda: v_.tensor_scalar(out=Lf[:], in0=ef[:], scalar1=-8.0, scalar2=None, op0=ALU.mult), [b_ef, b_L], [b_L])
            K.op(DVE, lambda: v_.tensor_copy(out=tmpb[:], in_=Lf[:]), [b_L], [b_tmpb])
            K.op(DVE, lambda: v_.tensor_copy(out=caug[0:NH, :], in_=tmpb[0:NH, :]), [b_tmpb], [b_caug])
            K.op(DVE, lambda: v_.tensor_tensor(out=Lf[:], in0=Lf[:], in1=tmpb[:], op=ALU.subtract), [b_L, b_tmpb], [b_L])
            K.op(DVE, lambda: v_.tensor_copy(out=tmpb[:], in_=Lf[:]), [b_L, b_caug], [b_tmpb])
            K.op(DVE, lambda: v_.tensor_copy(out=caug[32:32 + NH, :], in_=tmpb[32:32 + NH, :]), [b_tmpb], [b_caug])
            K.op(DVE, lambda: v_.tensor_tensor(out=Lf[:], in0=Lf[:], in1=tmpb[:], op=ALU.subtract), [b_L, b_tmpb], [b_L])
            K.op(DVE, lambda: v_.tensor_copy(out=caug[64:64 + NH, :], in_=Lf[64:64 + NH, :]), [b_L], [b_caug])
            i = nb()
            pT1 = ps[i][:, 0:NT_S * NH].rearrange("p (a b) -> p a b", b=NH)
            for t in range(NT_S):
                tr(pT1[:, t, :], ef[0:NH, t * 128:(t + 1) * 128], ident_f[0:NH, 0:NH], [b_ef, b_const], [pb[i]], t == NT_S - 1)
            K.op(DVE, lambda: v_.tensor_copy(out=cumLT[:], in_=pT1), [pb[i]], [b_cumLT])

            K.barrier()
            sv3 = al.off
            al.off = off_wq
            PT = [al.alloc("PT", [128, QB], BF16) for _ in range(6)]
            Rb = al.alloc("Rb", [128, QB], BF16)
            Rs = al.alloc("Rs", [128, QB], F32)
            al.off = sv3
            rot["l"] = [4, 5, 6, 7]
            pcount = 0
            LAG = 3
            grp = {"n": 0}
            pend_backs = []

            def att_front(j, q, t, half, nt, gpar):
                nonlocal pcount
                d = t - q * TQ
                q0 = max(d, 0) * 128
                h = 2 * j + half
                rows = slice(half * 64, half * 64 + 64)
                i = nb()
                mm(ps[i][:, q0:QB], kT[:, j, t * 128:(t + 1) * 128], qT[:, j, half, q * QB + q0:(q + 1) * QB],
                   True, False, [b_q, b_k], [pb[i]], False)
                mm(ps[i][:, q0:QB], cbt[:, 512 + h * 128:512 + (h + 1) * 128], caug[:, q * QB + q0:(q + 1) * QB],
                   False, d < 0, [b_const, b_caug], [pb[i]], d < 0)
                if d >= 0:
                    mm(ps[i][:, q0:q0 + 128], ident_b, negm_b, False, True, [b_const], [pb[i]], True)
                pt = PT[pcount % 6]
                bpt = b_PT[pcount % 6]
                pcount += 1
                K.op(ACT, lambda: a_.activation(out=pt[:, q0:QB], in_=ps[i][:, q0:QB], func=AF.Exp,
                                                bias=cumLT[:, t, h:h + 1], scale=0.125), [pb[i], b_cumLT], [bpt])

                def back():
                    yi = half + 2 * gpar
                    ya_, yb_ = 2 * gpar, 2 * gpar + 1
                    lo = 0 if half == 0 else 64
                    mm(ps[yi][:, q0:QB], vS[:, t, j, lo:lo + 128], pt[:, q0:QB], t == 0, t == nt - 1,
                       [b_v, bpt], [pb[yi]], True)
                    if t == nt - 1 and half == 1:
                        K.op(DVE, lambda: v_.reciprocal(out=Rs[64:128, :], in_=ps[ya_][64:128, 0:QB]), [pb[ya_], b_Rs], [b_Rs])
                        K.op(DVE, lambda: v_.reciprocal(out=Rs[0:64, :], in_=ps[yb_][0:64, 0:QB]), [pb[yb_], b_Rs], [b_Rs])
                        K.op(DVE, lambda: v_.tensor_copy(out=Rb[:], in_=Rs[:]), [b_Rs, b_Rb], [b_Rb])
                        isw = nb()
                        mm(ps[isw][:, 0:QB], swap_b, Rb[:, :], True, True, [b_const, b_Rb], [pb[isw]], True)
                        K.op(ACT, lambda: a_.copy(out=Rs[:], in_=ps[isw][:, 0:QB]), [pb[isw]], [b_Rs])
                        K.op(DVE, lambda: v_.tensor_tensor(out=yaT[0:64, j, q * QB:(q + 1) * QB], in0=ps[ya_][0:64, 0:QB],
                                                           in1=Rs[0:64, :], op=ALU.mult), [pb[ya_], b_Rs], [b_yaT])
                        K.op(DVE, lambda: v_.tensor_tensor(out=yaT[64:128, j, q * QB:(q + 1) * QB], in0=ps[yb_][64:128, 0:QB],
                                                           in1=Rs[64:128, :], op=ALU.mult), [pb[yb_], b_Rs], [b_yaT])
                return back

            for j in range(4):
                for q in range(NQ):
                    nt = q * TQ + TQ
                    gpar = grp["n"] % 2
                    grp["n"] += 1
                    precast(PC_PER)
                    for t in range(nt):
                        for half in range(2):
                            pend_backs.append(att_front(j, q, t, half, nt, gpar))
                            if len(pend_backs) > LAG:
                                pend_backs.pop(0)()
            while pend_backs:
                pend_backs.pop(0)()
            K.barrier()

            al.release(mU)
            xp = [al.alloc("xp", [128, S + 4], F32) for _ in range(2)]
            uu = [al.alloc("uu", [128, S], F32) for _ in range(2)]
            ub = [al.alloc("ub", [128, S], BF16) for _ in range(2)]
            gg = [al.alloc("gg", [128, S], BF16) for _ in range(2)]
            thr = al.alloc("thr", [128, S], F32)
            thi = al.alloc("thi", [128, S], F32)
            e2 = al.alloc("e2", [128, S], F32)
            wx = [al.alloc("wx", [128, 8, 128], BF16) for _ in range(2)]
            wg = [al.alloc("wg", [128, 8, 128], BF16) for _ in range(2)]
            wrg = [al.alloc("wrg", [128, 128], BF16) for _ in range(2)]
            wig = [al.alloc("wig", [128, 128], BF16) for _ in range(2)]
            gt = [[al.alloc("gt", [128, QB], F32) for _ in range(2)] for _ in range(2)]
            b_xp, b_uu, b_ub, b_gg = [Buf(), Buf()], [Buf(), Buf()], [Buf(), Buf()], [Buf(), Buf()]
            b_thr, b_thi, b_e2 = Buf(), Buf(), Buf()
            b_w4 = [[Buf() for _ in range(4)] for _ in range(2)]
            b_gt = [[Buf() for _ in range(2)] for _ in range(2)]
            rot["l"] = list(range(8))
            for s2_ in range(2):
                K.op(DVE, lambda: v_.memset(xp[s2_][:, 0:4], 0.0), [], [b_xp[s2_]])

            def load_w4(c):
                s_ = c % 2
                pl_dma(wx[s_][:], wview(OX + c * 128, 128), w=[b_w4[s_][0]])
                pl_dma(wg[s_][:], wview(OG + c * 128, 128), w=[b_w4[s_][1]])
                pl_dma(wrg[s_][:], wrg_d[c, :, :], w=[b_w4[s_][2]])
                pl_dma(wig[s_][:], wig_d[c, :, :], w=[b_w4[s_][3]])

            gcnt4 = {"n": 0}

            def m4_A(c):
                s_ = c % 2
                bwx, bwg_, bwrg, bwig = b_w4[s_]
                xp_, uu_, ub_, gg_ = xp[s_], uu[s_], ub[s_], gg[s_]
                bxp, buu, bub, bgg = b_xp[s_], b_uu[s_], b_ub[s_], b_gg[s_]
                for n in range(NQ):
                    i = nb()
                    for kc in range(8):
                        mm(ps[i][:, 0:QB], wx[s_][:, kc, :], hT[:, kc, n * QB:(n + 1) * QB], kc == 0, kc == 7,
                           [bwx] + b_hT[n * TQ:(n + 1) * TQ], [pb[i]], kc == 7)
                    K.op(ACT, lambda: a_.copy(out=xp_[:, 3 + n * QB:3 + (n + 1) * QB], in_=ps[i][:, 0:QB]), [pb[i]], [bxp])
                K.op(DVE, lambda: v_.tensor_scalar(out=uu_[:], in0=xp_[:, 3:3 + S], scalar1=fm[:, 4, c:c + 1], scalar2=fm[:, 5, c:c + 1],
                                                   op0=ALU.mult, op1=ALU.add), [bxp, b_fm], [buu])
                for jj in range(3):
                    K.op(DVE, lambda: v_.scalar_tensor_tensor(out=uu_[:], in0=xp_[:, jj:jj + S], scalar=fm[:, 1 + jj, c:c + 1], in1=uu_[:],
                                                              op0=ALU.mult, op1=ALU.add), [bxp, b_fm, buu], [buu])
                K.op(POOL, lambda: g_.tensor_copy(out=ub_[:], in_=uu_[:]), [buu], [bub])
                for n in range(NQ):
                    i = nb()
                    for kc in range(8):
                        mm(ps[i][:, 0:QB], wg[s_][:, kc, :], hT[:, kc, n * QB:(n + 1) * QB], kc == 0, kc == 7,
                           [bwg_] + b_hT[n * TQ:(n + 1) * TQ], [pb[i]], kc == 7)
                    g0, g1 = gt[gcnt4["n"] % 2]
                    bg0, bg1 = b_gt[gcnt4["n"] % 2]
                    gcnt4["n"] += 1
                    pg = ps[i][:, 0:QB]
                    K.op(ACT, lambda: a_.activation(out=g0[:], in_=pg, func=AF.Square), [pb[i]], [bg0])
                    K.op(DVE, lambda: v_.tensor_scalar(out=g0[:], in0=g0[:], scalar1=0.044715, scalar2=1.0, op0=ALU.mult, op1=ALU.add),
                         [bg0], [bg0])
                    K.op(DVE, lambda: v_.tensor_tensor(out=g0[:], in0=g0[:], in1=pg, op=ALU.mult), [bg0, pb[i]], [bg0])
                    K.op(ACT, lambda: a_.activation(out=g1[:], in_=g0[:], func=AF.Tanh, scale=0.7978845608028654), [bg0], [bg1])
                    K.op(DVE, lambda: v_.scalar_tensor_tensor(out=gg_[:, n * QB:(n + 1) * QB], in0=g1[:], scalar=1.0, in1=pg,
                                                              op0=ALU.add, op1=ALU.mult), [bg1, pb[i]], [bgg])

            def m4_B(c):
                s_ = c % 2
                bwx, bwg_, bwrg, bwig = b_w4[s_]
                uu_, ub_, gg_ = uu[s_], ub[s_], gg[s_]
                buu, bub, bgg = b_uu[s_], b_ub[s_], b_gg[s_]
                for n in range(NQ):
                    i = nb()
                    mm(ps[i][:, 0:QB], wrg[s_][:, :], ub_[:, n * QB:(n + 1) * QB], True, True, [bwrg, bub], [pb[i]], True)
                    K.op(ACT, lambda: a_.activation(out=thr[:, n * QB:(n + 1) * QB], in_=ps[i][:, 0:QB], func=AF.Tanh,
                                                    bias=hbrg[:, c:c + 1], scale=0.5), [pb[i], b_sm], [b_thr])
                    i2 = nb()
                    mm(ps[i2][:, 0:QB], wig[s_][:, :], ub_[:, n * QB:(n + 1) * QB], True, True, [bwig, bub], [pb[i2]], True)
                    K.op(ACT, lambda: a_.activation(out=thi[:, n * QB:(n + 1) * QB], in_=ps[i2][:, 0:QB], func=AF.Tanh,
                                                    bias=hbig[:, c:c + 1], scale=0.5), [pb[i2], b_sm], [b_thi])
                K.op(ACT, lambda: a_.activation(out=e2[:], in_=thr[:], func=AF.Exp, bias=cneg[:, c:c + 1], scale=cneg[:, c:c + 1]),
                     [b_thr, b_sm], [b_e2])
                K.op(ACT, lambda: a_.activation(out=thr[:], in_=thr[:], func=AF.Exp, bias=hcneg[:, c:c + 1], scale=hcneg[:, c:c + 1]),
                     [b_thr, b_sm], [b_thr])
                K.op(DVE, lambda: v_.tensor_scalar(out=e2[:], in0=e2[:], scalar1=1.0 - 1.0e-7, scalar2=None, op0=ALU.min), [b_e2], [b_e2])
                K.op(ACT, lambda: a_.activation(out=e2[:], in_=e2[:], func=AF.Sqrt, bias=1.0, scale=-1.0), [b_e2], [b_e2])
                K.op(DVE, lambda: v_.scalar_tensor_tensor(out=thi[:], in0=thi[:], scalar=1.0, in1=e2[:], op0=ALU.add, op1=ALU.mult),
                     [b_thi, b_e2], [b_thi])
                K.op(DVE, lambda: v_.scalar_tensor_tensor(out=thi[:], in0=thi[:], scalar=0.5, in1=uu_[:], op0=ALU.mult, op1=ALU.mult),
                     [b_thi, buu], [b_thi])
                K.op(DVE, lambda: v_.tensor_tensor_scan(out=e2[:], data0=thr[:], data1=thi[:], initial=0.0, op0=ALU.mult, op1=ALU.add),
                     [b_thr, b_thi, b_e2], [b_e2])
                K.op(DVE, lambda: v_.scalar_tensor_tensor(out=yrT[:, c, :], in0=gg_[:], scalar=0.5, in1=e2[:], op0=ALU.mult, op1=ALU.mult),
                     [bgg, b_e2], [b_yrT])

            load_w4(0)
            load_w4(1)
            m4_A(0)
            for c in range(8):
                if c + 1 < 8:
                    m4_A(c + 1)
                m4_B(c)
                if c + 2 < 8:
                    load_w4(c + 2)
            K.barrier()

            al.release(mU)
            mgT = al.alloc("mgT", [128, 8, S], BF16)
            b_mg = [Buf() for _ in range(NT_S)]
            m5 = al.mark()
            w5 = [al.alloc("w5", [128, 28, 128], BF16) for _ in range(2)]
            b_w5 = [[Buf() for _ in range(4)] for _ in range(2)]
            sa = [[al.alloc("sa", [128, QB], F32) for _ in range(4)] for _ in range(2)]
            b_sa = [[Buf() for _ in range(4)] for _ in range(2)]
            rot["l"] = list(range(8))

            def load_w5(m):
                s_ = m % 2
                cs_ = slice(m * 128, (m + 1) * 128)
                pl_dma(w5[s_][:, 0:4, :], wba_d[:, cs_].rearrange("(kc p) n -> p kc n", p=128), w=[b_w5[s_][0]])
                pl_dma(w5[s_][:, 4:12, :], wbr_d[:, cs_].rearrange("(kc p) n -> p kc n", p=128), w=[b_w5[s_][1]])
                pl_dma(w5[s_][:, 12:20, :], wview(OGA + m * 128, 128), w=[b_w5[s_][2]])
                pl_dma(w5[s_][:, 20:28, :], wview(OGR + m * 128, 128), w=[b_w5[s_][3]])
            load_w5(0)
            scount = 0
            for m in range(8):
                s_ = m % 2
                bwa, bwr_, bwga, bwgr = b_w5[s_]
                if m + 1 < 8:
                    load_w5(m + 1)
                for n in range(NQ):
                    cols = slice(n * QB, (n + 1) * QB)
                    hb = b_hT[n * TQ:(n + 1) * TQ]
                    iA, iR, iGA, iGR = nb(), nb(), nb(), nb()
                    for kc in range(4):
                        mm(ps[iA][:, 0:QB], w5[s_][:, kc, :], yaT[:, kc, cols], kc == 0, kc == 3, [bwa, b_yaT], [pb[iA]], kc == 3)
                    for kc in range(8):
                        mm(ps[iGA][:, 0:QB], w5[s_][:, 12 + kc, :], hT[:, kc, cols], kc == 0, kc == 7, [bwga] + hb, [pb[iGA]], kc == 7)
                    for kc in range(8):
                        mm(ps[iR][:, 0:QB], w5[s_][:, 4 + kc, :], yrT[:, kc, cols], kc == 0, kc == 7, [bwr_, b_yrT], [pb[iR]], kc == 7)
                    for kc in range(8):
                        mm(ps[iGR][:, 0:QB], w5[s_][:, 20 + kc, :], hT[:, kc, cols], kc == 0, kc == 7, [bwgr] + hb, [pb[iGR]], kc == 7)
                    s0, s1, s2, s3 = sa[scount % 2]
                    c0, c1, c2, c3 = b_sa[scount % 2]
                    scount += 1
                    K.op(ACT, lambda: a_.activation(out=s0[:], in_=ps[iGA][:, 0:QB], func=AF.Sigmoid), [pb[iGA]], [c0])
                    K.op(DVE, lambda: v_.tensor_tensor(out=s1[:], in0=s0[:], in1=ps[iA][:, 0:QB], op=ALU.mult), [c0, pb[iA]], [c1])
                    K.op(ACT, lambda: a_.activation(out=s2[:], in_=ps[iGR][:, 0:QB], func=AF.Sigmoid), [pb[iGR]], [c2])
                    K.op(DVE, lambda: v_.tensor_tensor(out=s3[:], in0=s2[:], in1=ps[iR][:, 0:QB], op=ALU.mult), [c2, pb[iR]], [c3])
                    K.op(POOL, lambda: g_.tensor_tensor(out=mgT[:, m, cols], in0=s1[:], in1=s3[:], op=ALU.add), [c1, c3],
                         b_mg[n * TQ:(n + 1) * TQ])
            K.barrier()

            al.release(m5)
            offB = al.off
            useA = (mU - off_hT) >= 80 * 1024
            if useA:
                al.off = off_hT
                al.limit = mU
            wo = al.alloc("wo", [128, 8, D], BF16)
            h2Tb = [al.alloc("h2Tb", [128, 8, QB], BF16) for _ in range(2)]
            xs2 = [al.alloc("xs2", [128, D], F32) for _ in range(2)]
            x1 = [al.alloc("x1", [128, D], F32) for _ in range(2)]
            h2 = [al.alloc("h2", [128, D], F32) for _ in range(2)]
            h2T = [al.alloc("h2T", [128, 8, 128], F32) for _ in range(2)]
            shs = [al.alloc("shs", [128, D], BF16) for _ in range(2)]
            h2b = [al.alloc("h2b", [128, D], BF16) for _ in range(2)]
            if useA:
                al.off = offB
                al.limit = LIMIT
            wsg = al.alloc("wsg", [128, 8, FF], BF16)
            wsu = al.alloc("wsu", [128, 8, FF], BF16)
            wsd = al.alloc("wsd", [128, 2, D], BF16)
            wr = al.alloc("wr", [128, 8, E], F32)
            bcv5 = al.alloc("bcv5", [128, 2, D], F32)
            b_bcv5 = Buf()
            gmb = al.alloc("gmb", [128, D], F32)
            gsfb = al.alloc("gsfb", [128, D], F32)
            shfb = al.alloc("shfb", [128, D], F32)
            sg = [al.alloc("sg", [128, QB], F32) for _ in range(2)]
            actT = al.alloc("actT", [128, 2, QB], BF16)
            rs = [al.alloc("rs", [128, 8], F32) for _ in range(2)]
            rt_ = [al.alloc("rt", [128, 6, E], F32) for _ in range(2)]
            g8 = [al.alloc("g8", [128, 8, 8], F32) for _ in range(2)]
            r8 = [al.alloc("r8", [128, 4, 8], F32) for _ in range(2)]
            i8 = [al.alloc("i8", [128, 8], U32) for _ in range(2)]
            mkb = [al.alloc("mkb", [128, E], BF16) for _ in range(2)]
            b_wo, b_ws, b_wr, b_bc5 = Buf(), Buf(), Buf(), Buf()
            b_xs2, b_x1, b_h2, b_h2b, b_h2T = [Buf(), Buf()], [Buf(), Buf()], [Buf(), Buf()], [Buf(), Buf()], [Buf(), Buf()]
            b_h2Tb, b_shs, b_sg, b_act, b_rs, b_rt = [Buf(), Buf()], [Buf(), Buf()], [Buf(), Buf()], Buf(), [Buf(), Buf()], [Buf(), Buf()]
            b_jk5 = Buf()
            jk5 = al.alloc("jk5", [128, D], BF16)
            pl_dma(wo[:], wout_d.ap().rearrange("(kc p) n -> p kc n", p=128), w=[b_wo])
            b_wsg, b_wsu, b_wsd = Buf(), Buf(), Buf()
            pl_dma(wsg[:], wsg_d.ap().rearrange("(kc p) n -> p kc n", p=128), w=[b_wsg])
            pl_dma(wsu[:], wsu_d.ap().rearrange("(kc p) n -> p kc n", p=128), w=[b_wsu])
            pl_dma(wsd[:], wsd_d.ap().rearrange("(kc p) n -> p kc n", p=128), w=[b_wsd])
            sp_dma(wr[:], wr_d.ap().rearrange("(kc p) n -> p kc n", p=128), w=[b_wr])
            sp_dma(bcv5[:], bcv_d[:, 0:2, :], w=[b_bcv5])
            sp_dma(gmb[:], modd[b:b + 1, 2 * D:3 * D].partition_broadcast(128), r=[b_modd], w=[b_bc5])
            sp_dma(gsfb[:], modd[b:b + 1, 4 * D:5 * D].partition_broadcast(128), r=[b_modd], w=[b_bc5])
            sp_dma(shfb[:], modd[b:b + 1, 3 * D:4 * D].partition_broadcast(128), r=[b_modd], w=[b_bc5])
            K.op(DVE, lambda: v_.tensor_tensor(out=gmb[:], in0=gmb[:], in1=bcv5[:, 0, :], op=ALU.mult), [b_bc5, b_bcv5], [b_bc5])
            K.op(DVE, lambda: v_.scalar_tensor_tensor(out=gsfb[:], in0=gsfb[:], scalar=1.0, in1=bcv5[:, 1, :], op0=ALU.add, op1=ALU.mult),
                 [b_bc5, b_bcv5], [b_bc5])
            rot["l"] = list(range(8))
            def m5_A(t):
                n, tt = t // TQ, t % TQ
                T = b * NT_S + t
                r0 = tok0 + t * 128
                p_ = t % 2
                xb, x1b, h2_, h2b_, h2T_, rs_ = xs2[p_], x1[p_], h2[p_], h2b[p_], h2T[p_], rs[p_]
                bxb, bx1, bh2, bh2b, bh2T, brs = b_xs2[p_], b_x1[p_], b_h2[p_], b_h2b[p_], b_h2T[p_], b_rs[p_]
                sp_dma(xb[:], x_d[r0:r0 + 128, :], w=[bxb])
                io = [nb(), nb()]
                for hf in range(2):
                    for kc in range(8):
                        mm(ps[io[hf]][:, :], mgT[:, kc, t * 128:(t + 1) * 128], wo[:, kc, hf * 512:(hf + 1) * 512], kc == 0, kc == 7,
                           [b_mg[t], b_wo], [pb[io[hf]]], kc == 7)
                for hf in range(2):
                    K.op(ACT, lambda: a_.activation(out=jk5[:, hf * 512:(hf + 1) * 512], in_=ps[io[hf]][:, :], func=AF.Square,
                                                    accum_out=rs_[:, hf:hf + 1]), [pb[io[hf]]], [b_jk5, brs])
                K.op(DVE, lambda: v_.tensor_tensor(out=rs_[:, 2:3], in0=rs_[:, 0:1], in1=rs_[:, 1:2], op=ALU.add), [brs], [brs])
                K.op(ACT, lambda: a_.activation(out=rs_[:, 3:4], in_=rs_[:, 2:3], func=AF.Sqrt, bias=EPS, scale=1.0 / D), [brs], [brs])
                K.op(DVE, lambda: v_.reciprocal(out=rs_[:, 4:5], in_=rs_[:, 3:4]), [brs], [brs])
                for hf in range(2):
                    cs_ = slice(hf * 512, (hf + 1) * 512)
                    K.op(DVE, lambda: v_.scalar_tensor_tensor(out=x1b[:, cs_], in0=ps[io[hf]][:, :], scalar=rs_[:, 4:5], in1=gmb[:, cs_],
                                                              op0=ALU.mult, op1=ALU.mult), [pb[io[hf]], brs, b_bc5], [bx1])
                K.op(POOL, lambda: g_.tensor_tensor(out=x1b[:], in0=x1b[:], in1=xb[:], op=ALU.add), [bx1, bxb], [bx1])
                sp_dma(x1_d[r0:r0 + 128, :], x1b[:], r=[bx1])
                K.op(ACT, lambda: a_.activation(out=jk5[:], in_=x1b[:], func=AF.Square, accum_out=rs_[:, 5:6]), [bx1], [b_jk5, brs])
                K.op(ACT, lambda: a_.activation(out=rs_[:, 6:7], in_=rs_[:, 5:6], func=AF.Sqrt, bias=EPS, scale=1.0 / D), [brs], [brs])
                K.op(DVE, lambda: v_.reciprocal(out=rs_[:, 7:8], in_=rs_[:, 6:7]), [brs], [brs])
                K.op(DVE, lambda: v_.scalar_tensor_tensor(out=h2_[:], in0=x1b[:], scalar=rs_[:, 7:8], in1=gsfb[:], op0=ALU.mult, op1=ALU.mult),
                     [bx1, brs, b_bc5], [bh2])
                K.op(POOL, lambda: g_.tensor_tensor(out=h2_[:], in0=h2_[:], in1=shfb[:], op=ALU.add), [bh2, b_bc5], [bh2])
                K.op(POOL, lambda: g_.tensor_copy(out=h2b_[:], in_=h2_[:]), [bh2], [bh2b])
                sp_dma(h2_d[r0:r0 + 128, :], h2b_[:], r=[bh2b])

            def m5_B(t):
                n, tt = t // TQ, t % TQ
                hb_ = h2Tb[n % 2]
                bhb = b_h2Tb[n % 2]
                T = b * NT_S + t
                p_ = t % 2
                h2_, h2T_ = h2[p_], h2T[p_]
                bh2, bh2T = b_h2[p_], b_h2T[p_]
                it = [nb(), nb()]
                for kc in range(8):
                    ib = it[kc // 4]
                    tr(ps[ib][:, (kc % 4) * 128:(kc % 4 + 1) * 128], h2_[:, kc * 128:(kc + 1) * 128], ident_f, [bh2, b_const], [pb[ib]],
                       kc % 4 == 3)
                for hf in range(2):
                    K.op(ACT, lambda: a_.copy(out=h2T_[:, hf * 4:(hf + 1) * 4, :], in_=ps[it[hf]][:, :].rearrange("p (a b) -> p a b", b=128)),
                         [pb[it[hf]]], [bh2T])
                K.op(POOL, lambda: g_.tensor_copy(out=hb_[:, :, tt * 128:(tt + 1) * 128], in_=h2T_[:]), [bh2T], [bhb])
                il = nb()
                for kc in range(8):
                    mm(ps[il][:, 0:E], h2T_[:, kc, :], wr[:, kc, :], kc == 0, kc == 7, [bh2T, b_wr], [pb[il]], kc == 7)
                R_, g8_, r8_, i8_, mk_ = rt_[p_], g8[p_], r8[p_], i8[p_], mkb[p_]
                brt = b_rt[p_]
                sc, sel, selm, mkf, wfull, tmp = (R_[:, k_, :] for k_ in range(6))
                K.op(ACT, lambda: a_.activation(out=sc, in_=ps[il][:, 0:E], func=AF.Sigmoid), [pb[il]], [brt])
                K.op(DVE, lambda: v_.tensor_tensor(out=sel, in0=sc, in1=rbias[:], op=ALU.add), [brt, b_const], [brt])
                for gg in range(8):
                    K.op(DVE, lambda: v_.max(out=g8_[:, gg, :], in_=sel[:, gg * 8:(gg + 1) * 8]), [brt], [brt])
                K.op(DVE, lambda: v_.tensor_tensor(out=r8_[:, 0, :], in0=g8_[:, :, 0], in1=g8_[:, :, 1], op=ALU.add), [brt], [brt])
                K.op(DVE, lambda: v_.max(out=r8_[:, 1, :], in_=r8_[:, 0, :]), [brt], [brt])
                K.op(DVE, lambda: v_.tensor_scalar(out=r8_[:, 2, :], in0=r8_[:, 0, :], scalar1=r8_[:, 1, 3:4], scalar2=-BIG,
                                                   op0=ALU.is_lt, op1=ALU.mult), [brt], [brt])
                K.op(DVE, lambda: v_.tensor_tensor(out=selm.rearrange("p (a b) -> p a b", b=8), in0=sel.rearrange("p (a b) -> p a b", b=8),
                                                   in1=r8_[:, 2, :].unsqueeze(2).to_broadcast([128, 8, 8]), op=ALU.add), [brt], [brt])
                K.op(DVE, lambda: v_.max(out=r8_[:, 3, :], in_=selm), [brt], [brt])
                K.op(DVE, lambda: v_.max_index(out=i8_[:], in_max=r8_[:, 3, :], in_values=selm), [brt], [brt])
                K.op(DVE, lambda: v_.tensor_scalar(out=mkf, in0=selm, scalar1=r8_[:, 3, 5:6], scalar2=None, op0=ALU.is_ge), [brt], [brt])
                K.op(DVE, lambda: v_.tensor_tensor(out=wfull, in0=sc, in1=mkf, op=ALU.mult), [brt], [brt])
                K.op(DVE, lambda: v_.tensor_reduce(out=r8_[:, 2, 0:1], in_=wfull, axis=AX.X, op=ALU.add), [brt], [brt])
                K.op(DVE, lambda: v_.reciprocal(out=r8_[:, 2, 1:2], in_=r8_[:, 2, 0:1]), [brt], [brt])
                K.op(POOL, lambda: g_.tensor_copy(out=mk_[:], in_=mkf), [brt], [brt])
                ik = nb()
                mm(ps[ik][:, 0:E], triu_b, mk_[:], True, True, [brt, b_const], [pb[ik]], False)
                mm(ps[ik][:, E:2 * E], ones_b, mk_[:], True, True, [brt, b_const], [pb[ik]], True)
                K.op(DVE, lambda: v_.tensor_tensor(out=tmp, in0=ps[ik][:, 0:E], in1=run[:], op=ALU.add), [pb[ik], b_run, brt], [brt])
                K.op(DVE, lambda: v_.tensor_tensor(out=rankm[:, T, :], in0=tmp, in1=mkf, op=ALU.mult), [brt], [b_route])
                K.op(DVE, lambda: v_.tensor_tensor(out=run[:], in0=run[:], in1=ps[ik][:, E:2 * E], op=ALU.add), [pb[ik], b_run], [b_run])
                K.op(DVE, lambda: v_.tensor_copy(out=eidx[:, T, :], in_=i8_[:]), [brt], [b_route])
                for k_ in range(TOPK):
                    K.op(DVE, lambda: v_.scalar_tensor_tensor(out=tmp, in0=iota64, scalar=eidx[:, T, k_:k_ + 1], in1=wfull,
                                                              op0=ALU.is_equal, op1=ALU.mult, accum_out=wk[:, T, k_:k_ + 1]),
                         [brt, b_route, b_const], [brt, b_route])
                K.op(DVE, lambda: v_.tensor_scalar(out=wkn[:, T, 0:TOPK], in0=wk[:, T, 0:TOPK], scalar1=r8_[:, 2, 1:2], scalar2=2.5,
                                                   op0=ALU.mult, op1=ALU.mult), [brt, b_route], [b_route])

            def m5_SH(n):
                hb_ = h2Tb[n % 2]
                bhb = b_h2Tb[n % 2]
                ig = [nb(), nb()]
                iu = [nb(), nb()]
                for c in range(2):
                    for kc in range(8):
                        mm(ps[ig[c]][:, 0:QB], wsg[:, kc, c * 128:(c + 1) * 128], hb_[:, kc, :], kc == 0, kc == 7, [b_wsg, bhb], [pb[ig[c]]], kc == 7)
                    for kc in range(8):
                        mm(ps[iu[c]][:, 0:QB], wsu[:, kc, c * 128:(c + 1) * 128], hb_[:, kc, :], kc == 0, kc == 7, [b_wsu, bhb], [pb[iu[c]]], kc == 7)
                    sg_ = sg[c]
                    K.op(ACT, lambda: a_.activation(out=sg_[:], in_=ps[ig[c]][:, 0:QB], func=AF.Sigmoid), [pb[ig[c]]], [b_sg[c]])
                    K.op(DVE, lambda: v_.tensor_tensor(out=sg_[:], in0=sg_[:], in1=ps[ig[c]][:, 0:QB], op=ALU.mult), [b_sg[c], pb[ig[c]]], [b_sg[c]])
                    K.op(DVE, lambda: v_.tensor_tensor(out=actT[:, c, :], in0=sg_[:], in1=ps[iu[c]][:, 0:QB], op=ALU.mult),
                         [b_sg[c], pb[iu[c]]], [b_act])
                for tt in range(TQ):
                    t = n * TQ + tt
                    r0 = tok0 + t * 128
                    iy = [nb(), nb()]
                    for hf in range(2):
                        for c in range(2):
                            mm(ps[iy[hf]][:, :], actT[:, c, tt * 128:(tt + 1) * 128], wsd[:, c, hf * 512:(hf + 1) * 512], c == 0, c == 1,
                               [b_act, b_wsd], [pb[iy[hf]]], c == 1)
                    sh_ = shs[t % 2]
                    K.op(ACT, lambda: a_.copy(out=sh_[:, 0:512], in_=ps[iy[0]][:, :]), [pb[iy[0]]], [b_shs[t % 2]])
                    K.op(DVE, lambda: v_.tensor_copy(out=sh_[:, 512:1024], in_=ps[iy[1]][:, :]), [pb[iy[1]]], [b_shs[t % 2]])
                    sp_dma(sh_d[r0:r0 + 128, :], sh_[:], r=[b_shs[t % 2]])

            m5_A(0)
            for t in range(NT_S):
                if t + 1 < NT_S:
                    m5_A(t + 1)
                m5_B(t)
                if t % TQ == TQ - 1:
                    m5_SH(t // TQ)
            K.barrier()

        al.release(mU)
        al.off = mU
        pe_ = al.alloc("pend", [128, 4, E], F32)
        pei = al.alloc("pei", [128, E], I32)
        ebf = al.alloc("ebf", [128, 2, NBLK], F32)
        djk = al.alloc("djk", [128, E], F32)
        rkp = al.alloc("rkp", [128, E], F32)
        destf = al.alloc("destf", [128, NT, 8], F32)
        b_pe, b_eb, b_dj, b_rkp, b_destf, b_desti = Buf(), Buf(), Buf(), Buf(), Buf(), Buf()
        b_desti_t = [Buf() for _ in range(NT)]
        K.op(DVE, lambda: v_.tensor_scalar(out=pe_[:, 3, :], in0=run[:], scalar1=float(CB - 1), scalar2=None, op0=ALU.add), [b_run], [b_pe])
        K.op(DVE, lambda: v_.tensor_copy(out=pei[:], in_=pe_[:, 3, :]), [b_pe], [b_pe])
        K.op(DVE, lambda: v_.tensor_single_scalar(out=pei[:], in_=pei[:], scalar=8, op=ALU.arith_shift_right), [b_pe], [b_pe])
        K.op(DVE, lambda: v_.tensor_copy(out=pe_[:, 0, :], in_=pei[:]), [b_pe], [b_pe])
        K.op(DVE, lambda: v_.tensor_tensor_scan(out=pe_[:, 1, :], data0=ones_c[:, 0:1].to_broadcast([128, E]), data1=pe_[:, 0, :],
                                                initial=0.0, op0=ALU.mult, op1=ALU.add), [b_pe, b_const], [b_pe])
        K.op(DVE, lambda: v_.tensor_tensor(out=pe_[:, 2, :], in0=pe_[:, 1, :], in1=pe_[:, 0, :], op=ALU.subtract), [b_pe], [b_pe])
        K.op(DVE, lambda: v_.tensor_scalar(out=pe_[:, 2, :], in0=pe_[:, 2, :], scalar1=float(CB), scalar2=None, op0=ALU.mult), [b_pe], [b_pe])
        K.op(DVE, lambda: v_.memset(ebf[:, 0, :], 0.0), [], [b_eb])
        for e_ in range(E):
            K.op(DVE, lambda: v_.scalar_tensor_tensor(out=ebf[:, 0, :], in0=iotab, scalar=pe_[:, 1, e_:e_ + 1], in1=ebf[:, 0, :],
                                                      op0=ALU.is_ge, op1=ALU.add), [b_pe, b_const, b_eb], [b_eb])
        K.op(DVE, lambda: v_.tensor_scalar(out=ebf[:, 0, :], in0=ebf[:, 0, :], scalar1=float(E - 1), scalar2=128.0, op0=ALU.min, op1=ALU.mult),
             [b_eb], [b_eb])
        K.op(DVE, lambda: v_.tensor_scalar(out=ebf[:, 1, :], in0=ebf[:, 0, :], scalar1=pidx, scalar2=None, op0=ALU.add), [b_eb, b_const], [b_eb])
        if skip_reload and NBLK > 2:
            K.op(DVE, lambda: v_.tensor_tensor(out=ebf[:, 0, 2:NBLK], in0=ebf[:, 0, 2:NBLK], in1=ebf[:, 1, 0:NBLK - 2], op=ALU.subtract),
                 [b_eb], [b_eb])
            K.op(DVE, lambda: v_.tensor_scalar(out=ebf[:, 0, 2:NBLK], in0=ebf[:, 0, 2:NBLK], scalar1=pidx, scalar2=None, op0=ALU.add),
                 [b_eb, b_const], [b_eb])
            K.op(DVE, lambda: v_.tensor_scalar(out=ebf[:, 0, 2:NBLK], in0=ebf[:, 0, 2:NBLK], scalar1=0.0, scalar2=1.0e6,
                                               op0=ALU.is_equal, op1=ALU.mult), [b_eb], [b_eb])
            K.op(DVE, lambda: v_.tensor_tensor(out=ebf[:, 1, 2:NBLK], in0=ebf[:, 1, 2:NBLK], in1=ebf[:, 0, 2:NBLK], op=ALU.add), [b_eb], [b_eb])
        K.op(DVE, lambda: v_.tensor_copy(out=widx[:], in_=ebf[:, 1, :]), [b_eb], [b_widx])
        for T in range(NT):
            K.op(DVE, lambda: v_.tensor_tensor(out=rkp[:], in0=rankm[:, T, :], in1=pe_[:, 2, :], op=ALU.add), [b_route, b_pe, b_rkp], [b_rkp])
            for k_ in range(TOPK):
                K.op(DVE, lambda: v_.scalar_tensor_tensor(out=djk[:], in0=iota64, scalar=eidx[:, T, k_:k_ + 1], in1=rkp[:],
                                                          op0=ALU.is_equal, op1=ALU.mult, accum_out=destf[:, T, k_:k_ + 1]),
                     [b_route, b_rkp, b_const], [b_dj, b_destf])
            K.op(DVE, lambda: v_.tensor_copy(out=desti[:, T, 0:TOPK], in_=destf[:, T, 0:TOPK]), [b_destf], [b_desti_t[T]])
        hg = [al.alloc("hg", [128, D], BF16) for _ in range(3)]
        b_hg = [Buf() for _ in range(3)]
        b_xs = Buf()
        for T in range(NT):
            hb_ = hg[T % 3]
            sp_dma(hb_[:], h2_d[T * 128:(T + 1) * 128, :], w=[b_hg[T % 3]])
            for k_ in range(TOPK):
                K.dma(K.qpool, lambda: g_.indirect_dma_start(out=xs_d[:, :], out_offset=bass.IndirectOffsetOnAxis(ap=desti[:, T, k_:k_ + 1], axis=0),
                                                             in_=hb_[:], in_offset=None, bounds_check=reg_slot, oob_is_err=False),
                      [b_hg[T % 3], b_desti_t[T], b_xs0], [], [b_xs])
        K.barrier()

        al.off = mU
        wE = [[al.alloc("wE", [128, 2048], BF16) for _ in range(3)] for _ in range(2)]
        b_wE = [[Buf() for _ in range(3)] for _ in range(2)]
        xsb = [al.alloc("xsb", [128, 2, D], BF16) for _ in range(3)]
        xTe = [al.alloc("xTe", [128, 8, CB], BF16) for _ in range(3)]
        sge = [al.alloc("sge", [128, 2 * CB], F32) for _ in range(2)]
        acte = [al.alloc("acte", [128, 2 * CB], BF16) for _ in range(2)]
        ysb = [al.alloc("ysb", [128, D], BF16) for _ in range(4)]
        b_xsb, b_xTe, b_sge, b_acte = [Buf(), Buf(), Buf()], [Buf(), Buf(), Buf()], [Buf(), Buf()], [Buf(), Buf()]
        b_ysb = [Buf() for _ in range(4)]
        b_ys = Buf()
        rot["l"] = list(range(8))
        wsrc = wpb_d

        def load_wE(blk, which):
            s_ = blk % 2
            for m in which:
                K.dma(K.qpool, lambda: g_.indirect_dma_start(out=wE[s_][m][:], out_offset=None, in_=wsrc[m][:, :],
                                                             in_offset=bass.IndirectOffsetOnAxis(ap=widx[:, blk:blk + 1], axis=0),
                                                             bounds_check=reg_w, oob_is_err=False),
                      [b_widx, b_wcast], [b_wE[s_][m]])

        def load_xs(blk):
            p_ = blk % 3
            sp_dma(xsb[p_][:], xs_d[blk * CB:(blk + 1) * CB, :].rearrange("(s p) d -> p s d", p=128), r=[b_xs], w=[b_xsb[p_]])

        def stage_T(blk):
            p_ = blk % 3
            for s2 in range(2):
                i = nb()
                pT = ps[i][:, :].bitcast(BF16)
                for kc in range(8):
                    tr(pT[:, kc * 128:(kc + 1) * 128], xsb[p_][:, s2, kc * 128:(kc + 1) * 128], ident_b, [b_xsb[p_], b_const], [pb[i]], kc == 7)
                o_ap = xTe[p_][:, :, s2 * 128:(s2 + 1) * 128]
                i_ap = pT.rearrange("p (a b) -> p a b", b=128)
                if s2 == 0:
                    K.op(ACT, lambda: a_.copy(out=o_ap, in_=i_ap), [pb[i]], [b_xTe[p_]])
                else:
                    K.op(DVE, lambda: v_.tensor_copy(out=o_ap, in_=i_ap), [pb[i]], [b_xTe[p_]])

        def stage_GU(blk):
            s_ = blk % 2
            p_ = blk % 2
            x_ = blk % 3
            wgE, wuE, wdE = wE[s_]
            bwg, bwu, bwd = b_wE[s_]
            ig_, iu_ = nb(), nb()
            for c in range(2):
                for kc in range(8):
                    mm(ps[ig_][:, c * CB:(c + 1) * CB], wgE[:, kc * FF + c * 128: kc * FF + (c + 1) * 128], xTe[x_][:, kc, :], kc == 0, kc == 7,
                       [bwg, b_xTe[x_]], [pb[ig_]], kc == 7)
            for c in range(2):
                for kc in range(8):
                    mm(ps[iu_][:, c * CB:(c + 1) * CB], wuE[:, kc * FF + c * 128: kc * FF + (c + 1) * 128], xTe[x_][:, kc, :], kc == 0, kc == 7,
                       [bwu, b_xTe[x_]], [pb[iu_]], kc == 7)
            K.op(ACT, lambda: a_.activation(out=sge[p_][:], in_=ps[ig_][:, :], func=AF.Sigmoid), [pb[ig_]], [b_sge[p_]])
            K.op(DVE, lambda: v_.tensor_tensor(out=sge[p_][:], in0=sge[p_][:], in1=ps[ig_][:, :], op=ALU.mult), [b_sge[p_], pb[ig_]], [b_sge[p_]])
            K.op(DVE, lambda: v_.tensor_tensor(out=acte[p_][:], in0=sge[p_][:], in1=ps[iu_][:, :], op=ALU.mult), [b_sge[p_], pb[iu_]], [b_acte[p_]])

        ycnt = {"n": 0}

        def stage_D(blk):
            s_ = blk % 2
            p_ = blk % 2
            wdE = wE[s_][2]
            bwd = b_wE[s_][2]
            for s2 in range(2):
                yb = ysb[ycnt["n"] % 4]
                byb = b_ysb[ycnt["n"] % 4]
                ycnt["n"] += 1
                for hf in range(2):
                    i = nb()
                    for c in range(2):
                        mm(ps[i][:, :], acte[p_][:, c * CB + s2 * 128: c * CB + (s2 + 1) * 128], wdE[:, c * D + hf * 512: c * D + (hf + 1) * 512],
                           c == 0, c == 1, [b_acte[p_], bwd], [pb[i]], c == 1)
                    if hf == 0:
                        K.op(ACT, lambda: a_.copy(out=yb[:, 0:512], in_=ps[i][:, :]), [pb[i]], [byb])
                    else:
                        K.op(DVE, lambda: v_.tensor_copy(out=yb[:, 512:1024], in_=ps[i][:, :]), [pb[i]], [byb])
                r0 = blk * CB + s2 * 128
                sp_dma(ys_d[r0:r0 + 128, :], yb[:], r=[byb], sw=[b_ys])

        precast(3 * E)
        load_wE(0, (0, 1, 2))
        if NBLK > 1:
            load_wE(1, (0, 1, 2))
        for b0 in range(min(3, NBLK)):
            load_xs(b0)
        stage_T(0)
        if NBLK > 1:
            stage_T(1)
        stage_GU(0)
        for blk in range(NBLK):
            if blk + 2 < NBLK:
                load_wE(blk + 2, (0, 1))
                stage_T(blk + 2)
                if blk + 3 < NBLK:
                    load_xs(blk + 3)
            if blk >= 1:
                stage_D(blk - 1)
                if blk + 1 < NBLK:
                    load_wE(blk + 1, (2,))
            if blk + 1 < NBLK:
                stage_GU(blk + 1)
        stage_D(NBLK - 1)
        K.barrier()

        al.off = mU
        gfb = al.alloc("gfb", [128, NSEQ, D], F32)
        bcvc = al.alloc("bcvc", [128, D], F32)
        b_bcvc = Buf()
        sp_dma(bcvc[:], bcv_d[:, 2, :], w=[b_bcvc])
        b_gfb = Buf()
        for b in range(NSEQ):
            sp_dma(gfb[:, b, :], modd[b:b + 1, 5 * D:6 * D].partition_broadcast(128), r=[b_modd], w=[b_gfb])
            K.op(DVE, lambda: v_.tensor_tensor(out=gfb[:, b, :], in0=gfb[:, b, :], in1=bcvc[:], op=ALU.mult), [b_gfb, b_bcvc], [b_gfb])
        x1c = [al.alloc("x1c", [128, D], F32) for _ in range(2)]
        zc = [al.alloc("zc", [128, D], F32) for _ in range(2)]
        yg = [al.alloc("yg", [128, D], BF16) for _ in range(12)]
        shc = [al.alloc("shc", [128, D], BF16) for _ in range(2)]
        b_shc = [Buf(), Buf()]
        jkc = al.alloc("jkc", [128, D], BF16)
        rc_ = [al.alloc("rc", [128, 4], F32) for _ in range(2)]
        b_x1c, b_zc, b_rc = [Buf(), Buf()], [Buf(), Buf()], [Buf(), Buf()]
        b_yg = [Buf() for _ in range(12)]
        b_jkc = Buf()
        b_out = Buf()
        z2 = [al.alloc("z2", [128, D], F32) for _ in range(2)]
        b_z2 = [Buf(), Buf()]
        z3 = [al.alloc("z3", [128, D], F32) for _ in range(2)]
        b_z3 = [Buf(), Buf()]
        gc = {"n": 0}

        def c_A(T):
            p_ = T % 2
            r0 = T * 128
            sp_dma(x1c[p_][:], x1_d[r0:r0 + 128, :], w=[b_x1c[p_]])
            sp_dma(shc[p_][:], sh_d[r0:r0 + 128, :], w=[b_shc[p_]])
            ys_ = []
            for k_ in range(TOPK):
                y_ = yg[gc["n"] % 12]
                by = b_yg[gc["n"] % 12]
                gc["n"] += 1
                K.dma(K.qpool, lambda: g_.indirect_dma_start(out=y_[:], out_offset=None, in_=ys_d[:, :],
                                                             in_offset=bass.IndirectOffsetOnAxis(ap=desti[:, T, k_:k_ + 1], axis=0),
                                                             bounds_check=reg_slot, oob_is_err=False),
                      [b_ys, b_desti_t[T]], [by])
                ys_.append((y_, by))
            for k_ in range(4):
                y_, by = ys_[k_]
                acc_in = shc[p_][:] if k_ == 0 else zc[p_][:]
                K.op(DVE, lambda: v_.scalar_tensor_tensor(out=zc[p_][:], in0=y_[:], scalar=wkn[:, T, k_:k_ + 1], in1=acc_in,
                                                          op0=ALU.mult, op1=ALU.add), [by, b_route, b_zc[p_], b_shc[p_]], [b_zc[p_]])
            y_, by = ys_[4]
            K.op(ACT, lambda: a_.activation(out=z2[p_][:], in_=y_[:], func=AF.Identity, scale=wkn[:, T, 4:5]), [by, b_route], [b_z2[p_]])
            y_, by = ys_[5]
            K.op(ACT, lambda: a_.activation(out=z3[p_][:], in_=y_[:], func=AF.Identity, scale=wkn[:, T, 5:6]), [by, b_route], [b_z3[p_]])
            K.op(POOL, lambda: g_.tensor_tensor(out=z2[p_][:], in0=z2[p_][:], in1=z3[p_][:], op=ALU.add), [b_z2[p_], b_z3[p_]], [b_z2[p_]])
            K.op(DVE, lambda: v_.tensor_tensor(out=zc[p_][:], in0=zc[p_][:], in1=z2[p_][:], op=ALU.add), [b_zc[p_], b_z2[p_]], [b_zc[p_]])

        def c_B(T):
            b = T // NT_S
            p_ = T % 2
            r0 = T * 128
            K.op(ACT, lambda: a_.activation(out=jkc[:], in_=zc[p_][:], func=AF.Square, accum_out=rc_[p_][:, 0:1]), [b_zc[p_]], [b_jkc, b_rc[p_]])
            K.op(ACT, lambda: a_.activation(out=rc_[p_][:, 1:2], in_=rc_[p_][:, 0:1], func=AF.Sqrt, bias=EPS, scale=1.0 / D), [b_rc[p_]], [b_rc[p_]])
            K.op(DVE, lambda: v_.reciprocal(out=rc_[p_][:, 2:3], in_=rc_[p_][:, 1:2]), [b_rc[p_]], [b_rc[p_]])
            K.op(DVE, lambda: v_.scalar_tensor_tensor(out=zc[p_][:], in0=zc[p_][:], scalar=rc_[p_][:, 2:3], in1=gfb[:, b, :],
                                                      op0=ALU.mult, op1=ALU.mult), [b_zc[p_], b_rc[p_], b_gfb], [b_zc[p_]])
            K.op(DVE, lambda: v_.tensor_tensor(out=zc[p_][:], in0=zc[p_][:], in1=x1c[p_][:], op=ALU.add), [b_zc[p_], b_x1c[p_]], [b_zc[p_]])
            sp_dma(out_d[r0:r0 + 128, :], zc[p_][:], r=[b_zc[p_]], sw=[b_out])

        c_A(0)
        for T in range(NT):
            if T + 1 < NT:
                c_A(T + 1)
            c_B(T)
        K.barrier()
    return nc


def _bf16():
    import ml_dtypes
    return ml_dtypes.bfloat16


def make_consts(NBLK):
    NCF = 128 + 128 + 64 + NBLK + 2
    cf = np.zeros((128, NCF), np.float32)
    cf[:, 0:128] = np.eye(128, dtype=np.float32)
    cf[127, 128:256] = 1.0
    cf[:, 256:320] = np.arange(64, dtype=np.float32)[None, :]
    cf[:, 320:320 + NBLK] = np.arange(NBLK, dtype=np.float32)[None, :]
    cf[:, 320 + NBLK] = np.arange(128, dtype=np.float32)
    cf[:, 321 + NBLK] = 1.0
    cb = np.zeros((128, 1792), np.float32)
    cb[:, 0:128] = np.eye(128)
    k = np.arange(128)[:, None]
    m = np.arange(128)[None, :]
    cb[:, 128:256] = (k < m)
    cb[:, 256:384] = (m >= k)
    cb[:, 384:512] = 1.0
    for h in range(NH):
        for g3 in range(3):
            cb[g3 * 32 + h, 512 + h * 128:512 + (h + 1) * 128] = 1.0
    cb[:, 1536:1664] = np.where(m < k, -30000.0, 0.0)
    cb[:, 1664:1792] = (m == (k + 64) % 128)
    return cf, cb.astype(_bf16())


def prep_shared(inp):
    f = np.float32
    sh = {}
    sh["w_ada"] = np.ascontiguousarray(inp["w_ada"][0], f)
    fm = np.zeros((128, 9, 8), f)

    def fmaj(v):
        return np.asarray(v, f).reshape(8, 128).T
    fm[:, 0, :] = fmaj(inp["g_pre_mix"][0])
    for j in range(4):
        fm[:, 1 + j, :] = fmaj(inp["w_conv"][0, j])
    fm[:, 5, :] = fmaj(inp["b_conv"][0])
    fm[:, 6, :] = fmaj(inp["b_rg"][0])
    fm[:, 7, :] = fmaj(inp["b_ig"][0])
    fm[:, 8, :] = fmaj(inp["rglru_lambda"][0])
    sh["fm"] = fm
    bcv = np.zeros((128, 3, D), f)
    bcv[:, 0, :] = np.asarray(inp["g_post_mix"][0], f)[None, :]
    bcv[:, 1, :] = np.asarray(inp["g_pre_ffn"][0], f)[None, :]
    bcv[:, 2, :] = np.asarray(inp["g_post_ffn"][0], f)[None, :]
    sh["bcv"] = bcv
    sh["rbias"] = np.ascontiguousarray(np.broadcast_to(np.asarray(inp["router_bias"][0], f)[None, :], (128, E)))
    sh["b_forget"] = np.asarray(inp["b_forget"][0], f).reshape(NH, 1)
    sh["w_in"] = np.ascontiguousarray(inp["w_in"][0], f)
    for nm, src in (("wrg_bd", inp["w_rg"][0]), ("wig_bd", inp["w_ig"][0])):
        bd = np.zeros((8, 128, 128), f)
        for c in range(8):
            bd[c, 0:64, 0:64] = src[2 * c]
            bd[c, 64:128, 64:128] = src[2 * c + 1]
        sh[nm] = bd
    sh["w_ba"] = np.ascontiguousarray(inp["w_branch_attn"][0], f)
    sh["w_br"] = np.ascontiguousarray(inp["w_branch_rnn"][0], f)
    sh["w_out"] = np.ascontiguousarray(inp["w_out"][0], f)
    sh["w_router"] = np.ascontiguousarray(inp["w_router"][0], f)
    sh["w_sg"] = np.ascontiguousarray(inp["w_sh_gate"][0], f)
    sh["w_su"] = np.ascontiguousarray(inp["w_sh_up"][0], f)
    sh["w_sd"] = np.ascontiguousarray(inp["w_sh_down"][0], f)
    wg = np.asarray(inp["w_exp_gate"][0], f).reshape(E, 8, 128, FF).transpose(0, 2, 1, 3)
    sh["wpg"] = np.ascontiguousarray(wg).reshape(E * 128, 2048)
    wu = np.asarray(inp["w_exp_up"][0], f).reshape(E, 8, 128, FF).transpose(0, 2, 1, 3)
    sh["wpu"] = np.ascontiguousarray(wu).reshape(E * 128, 2048)
    wd = np.asarray(inp["w_exp_down"][0], f).reshape(E, 2, 128, D).transpose(0, 2, 1, 3)
    sh["wpd"] = np.ascontiguousarray(wd).reshape(E * 128, 2048)
    return sh


def prep_core(inp, sh, core, NSEQ, S, NBLK):
    f = np.float32
    m = dict(sh)
    xs = np.asarray(inp["x"][core * NSEQ:(core + 1) * NSEQ], f).reshape(NSEQ * S, D)
    m["x"] = np.ascontiguousarray(xs)
    c = np.asarray(inp["c"][core * NSEQ:(core + 1) * NSEQ], f)
    m["csT"] = np.ascontiguousarray(c.T.reshape(8, 128, NSEQ).transpose(1, 0, 2))
    m["b_ada_rep"] = np.ascontiguousarray(np.broadcast_to(np.asarray(inp["b_ada"][0], f)[None, :], (NSEQ, 6 * D)))
    cf, cb = make_consts(NBLK)
    m["cf"] = cf
    m["cb"] = cb
    return m


def kernel(**inputs):
    B, S = inputs["x"].shape[0], inputs["x"].shape[1]
    NSEQ = B // NCORES
    NTOK = NSEQ * S
    NBLK = (NTOK * TOPK) // CB + E
    nc = build(NSEQ, S, skip_reload=True)
    sh = prep_shared(inputs)
    in_maps = [prep_core(inputs, sh, i, NSEQ, S, NBLK) for i in range(NCORES)]
    res = run_bass_kernel_spmd(nc, in_maps, core_ids=list(range(NCORES)))
    outs = [np.asarray(r["out"], np.float32).reshape(NSEQ, S, D) for r in res.results]
    return np.concatenate(outs, axis=0)
```

```python
import numpy as np
import concourse.bass as bass
import concourse.mybir as mybir
from concourse.bass_utils import run_bass_kernel_spmd
from contextlib import ExitStack

F32 = mybir.dt.float32
BF16 = mybir.dt.bfloat16
I32 = mybir.dt.int32
U32 = mybir.dt.uint32
AF = mybir.ActivationFunctionType
ALU = mybir.AluOpType
AX = mybir.AxisListType

D = 1024
NH = 8
E = 64
TOPK = 6
FF = 256
CB = 256
INC = 5640
OQ, OK_, OV, OF_, OX, OG, OGA, OGR = 0, 512, 1024, 1536, 1544, 2568, 3592, 4616
EPS = 1e-6
BIG = 1.0e4
NCORES = 8
ARENA_SHIFT = [0]
ARENA_MAX = [0]


class Buf:
    __slots__ = ("w", "r", "name")

    def __init__(self, name=""):
        self.w = {}
        self.r = {}
        self.name = name


class Eng:
    def __init__(self, name, e, sem, key):
        self.name = name
        self.e = e
        self.sem = sem
        self.key = key
        self.n = 0
        self.seen = {}
        self.pending = False


class DQ:
    def __init__(self, eng, sems):
        self.eng = eng
        self.sems = sems
        self.cnt = [0] * len(sems)
        self.next = 0


def _merge(d, s):
    for k, v in s.items():
        if d.get(k, 0) < v:
            d[k] = v


class KB:
    def __init__(self, nc, stack):
        self.nc = nc
        self.semtab = {}
        self.engs = []
        for nm, e in (("pe", nc.tensor), ("act", nc.scalar), ("dve", nc.vector),
                      ("pool", nc.gpsimd), ("sp", nc.sync)):
            sem = stack.enter_context(nc.semaphore("s_" + nm))
            eng = Eng(nm, e, sem, "c_" + nm)
            self.semtab[eng.key] = sem
            setattr(self, nm, eng)
            self.engs.append(eng)
        self.queues = []
        for nm, eng, n in (("qsp", self.sp, 8), ("qpool", self.pool, 6)):
            sems = []
            for i in range(n):
                key = "d_%s%d" % (nm, i)
                sem = stack.enter_context(nc.semaphore(key))
                self.semtab[key] = sem
                sems.append((sem, key))
            q = DQ(eng, sems)
            setattr(self, nm, q)
            self.queues.append(q)

    def _wait(self, E_, deps):
        for k, v in deps.items():
            if E_.seen.get(k, 0) < v:
                E_.e.wait_ge(self.semtab[k], v)
                E_.seen[k] = v

    def op(self, E_, fn, reads=(), writes=(), inc=True):
        deps = {}
        for b in reads:
            _merge(deps, b.w)
        for b in writes:
            _merge(deps, b.w)
            _merge(deps, b.r)
        if E_.name == "pe":
            deps.pop(E_.key, None)
        self._wait(E_, deps)
        ins = fn()
        ev = E_.n + 1
        for b in reads:
            if b.r.get(E_.key, 0) < ev:
                b.r[E_.key] = ev
        for b in writes:
            b.w = {E_.key: ev}
            b.r = {}
        if inc:
            E_.n = ev
            ins.then_inc(E_.sem, 1)
            E_.pending = False
        else:
            E_.pending = True
        return ins

    def dma(self, Q, fn, reads=(), writes=(), swrites=()):
        E_ = Q.eng
        deps = {}
        for b in reads:
            _merge(deps, b.w)
        for b in writes:
            _merge(deps, b.w)
            _merge(deps, b.r)
        for b in swrites:
            _merge(deps, b.r)
        slot = Q.next
        Q.next = (Q.next + 1) % len(Q.sems)
        sem, key = Q.sems[slot]
        if Q.cnt[slot] > 0:
            if deps.get(key, 0) < 16 * Q.cnt[slot]:
                deps[key] = 16 * Q.cnt[slot]
        self._wait(E_, deps)
        ins = fn()
        Q.cnt[slot] += 1
        v = 16 * Q.cnt[slot]
        ins.then_inc(sem, 16)
        for b in reads:
            if b.r.get(key, 0) < v:
                b.r[key] = v
        for b in writes:
            b.w = {key: v}
            b.r = {}
        for b in swrites:
            if b.w.get(key, 0) < v:
                b.w[key] = v
        return ins

    def barrier(self):
        tot = {}
        for E_ in self.engs:
            assert not E_.pending
            if E_.n > 0:
                tot[E_.key] = E_.n
        for Q in self.queues:
            for i, (sem, key) in enumerate(Q.sems):
                if Q.cnt[i] > 0:
                    tot[key] = 16 * Q.cnt[i]
        for E_ in self.engs:
            self._wait(E_, dict(tot))


class Arena:
    def __init__(self, nc, limit):
        self.nc = nc
        self.off = 0
        self.limit = limit
        self.n = 0

    def alloc(self, name, shape, dtype):
        sz = 1
        for s in shape[1:]:
            sz *= s
        sz *= {F32: 4, BF16: 2, I32: 4, U32: 4}[dtype]
        sz = (sz + 63) // 64 * 64
        off = self.off
        assert off + sz <= self.limit, ("SBUF arena overflow", name, off, sz)
        self.off += sz
        self.n += 1
        ARENA_MAX[0] = max(ARENA_MAX[0], self.off)
        return self.nc.alloc_sbuf_tensor_at("%s_%d" % (name, self.n), list(shape), dtype, offset=off)

    def mark(self):
        return self.off

    def release(self, m):
        self.off = m


def build(NSEQ, S, skip_reload=True):
    NT_S = S // 128
    NTOK = NSEQ * S
    NT = NTOK // 128
    QB = min(512, S)
    TQ = QB // 128
    NQ = S // QB
    NB5 = S // QB
    NBLK = (NTOK * TOPK) // CB + E
    NSLOT = NBLK * CB

    nc = bass.Bass("TRN2", target_bir_lowering=False)
    dt = nc.dram_tensor

    def ein(name, shape, dtype=F32):
        return dt(name, list(shape), dtype, kind="ExternalInput")

    x_d = ein("x", [NTOK, D])
    cs_d = ein("csT", [128, 8, NSEQ])
    wada_d = ein("w_ada", [D, 6 * D])
    bada_d = ein("b_ada_rep", [NSEQ, 6 * D])
    fm_d = ein("fm", [128, 9, 8])
    bcv_d = ein("bcv", [128, 3, D])
    rb_d = ein("rbias", [128, E])
    bf_d = ein("b_forget", [NH, 1])
    win_d = ein("w_in", [D, INC])
    wrg_d = ein("wrg_bd", [8, 128, 128])
    wig_d = ein("wig_bd", [8, 128, 128])
    wba_d = ein("w_ba", [512, D])
    wbr_d = ein("w_br", [D, D])
    wout_d = ein("w_out", [D, D])
    wr_d = ein("w_router", [D, E])
    wsg_d = ein("w_sg", [D, FF])
    wsu_d = ein("w_su", [D, FF])
    wsd_d = ein("w_sd", [FF, D])
    wpg_d = ein("wpg", [E * 128, 2048])
    wpu_d = ein("wpu", [E * 128, 2048])
    wpd_d = ein("wpd", [E * 128, 2048])
    NCF = 128 + 128 + 64 + NBLK + 2
    cf_d = ein("cf", [128, NCF])
    cb_d = ein("cb", [128, 1792], BF16)
    out_d = dt("out", [NTOK, D], F32, kind="ExternalOutput")
    modd = dt("modd", [NSEQ, 6 * D], F32)
    h2_d = dt("h2s", [NTOK, D], BF16)
    x1_d = dt("x1s", [NTOK, D], F32)
    sh_d = dt("shs", [NTOK, D], BF16)
    xs_d = dt("xss", [NSLOT, D], BF16)
    ys_d = dt("yss", [NSLOT, D], BF16)
    wpb_d = [dt("wpb%d" % m_, [E * 128, 2048], BF16) for m_ in range(3)]

    stack = ExitStack()
    with stack:
        K = KB(nc, stack)
        al = Arena(nc, int(nc._sbuf_addr_for_side("right")) - 64)
        al.off = (int(nc._sbuf_addr_for_side("left")) + 63) // 64 * 64 + ARENA_SHIFT[0]
        ps = [stack.enter_context(nc.psum_tensor("ps%d" % i, [128, 512], F32)) for i in range(8)]
        pb = [Buf("pb%d" % i) for i in range(8)]
        rot = {"l": list(range(8)), "i": 0}

        def nb():
            i = rot["l"][rot["i"] % len(rot["l"])]
            rot["i"] += 1
            return i

        PE, ACT, DVE, POOL = K.pe, K.act, K.dve, K.pool
        reg_slot = nc.gpsimd.alloc_register("bc_slot")
        nc.gpsimd.reg_mov(reg_slot, NSLOT - 1)
        reg_w = nc.gpsimd.alloc_register("bc_w")
        nc.gpsimd.reg_mov(reg_w, E * 128 - 1)
        v_, a_, g_, t_ = nc.vector, nc.scalar, nc.gpsimd, nc.tensor

        def mm(out, lhsT, rhs, start, stop, r, w, inc):
            return K.op(PE, lambda: t_.matmul(out, lhsT, rhs, start=start, stop=stop), r, w, inc)

        def tr(out, in_, ident, r, w, inc):
            return K.op(PE, lambda: t_.transpose(out, in_, ident), r, w, inc)

        def sp_dma(out, in_, r=(), w=(), sw=()):
            return K.dma(K.qsp, lambda: nc.sync.dma_start(out=out, in_=in_), r, w, sw)

        def pl_dma(out, in_, r=(), w=(), sw=()):
            return K.dma(K.qpool, lambda: nc.gpsimd.dma_start(out=out, in_=in_), r, w, sw)

        cf = al.alloc("cf", [128, NCF], F32)
        cbt = al.alloc("cb", [128, 1792], BF16)
        b_const = Buf("const")
        ident_f = cf[:, 0:128]
        sel127 = cf[:, 128:256]
        iota64 = cf[:, 256:320]
        iotab = cf[:, 320:320 + NBLK]
        pidx = cf[:, 320 + NBLK:321 + NBLK]
        ones_c = cf[:, 321 + NBLK:322 + NBLK]
        ident_b = cbt[:, 0:128]
        triu_b = cbt[:, 128:256]
        trim_b = cbt[:, 256:384]
        ones_b = cbt[:, 384:512]
        negm_b = cbt[:, 1536:1664]
        swap_b = cbt[:, 1664:1792]
        fm = al.alloc("fm", [128, 9, 8], F32)
        sm = al.alloc("sm", [128, 6, 8], F32)
        rbias = al.alloc("rbias", [128, E], F32)
        nbf = al.alloc("nbf", [128, 2], F32)
        b_nbf = Buf()
        gsmT = al.alloc("gsmT", [128, 8, NSEQ], F32)
        shmT = al.alloc("shmT", [128, 8, NSEQ], F32)
        run = al.alloc("run", [128, E], F32)
        rankm = al.alloc("rankm", [128, NT, E], F32)
        eidx = al.alloc("eidx", [128, NT, 8], F32)
        wk = al.alloc("wk", [128, NT, 8], F32)
        wkn = al.alloc("wkn", [128, NT, 8], F32)
        desti = al.alloc("desti", [128, NT, 8], I32)
        widx = al.alloc("widx", [128, NBLK], I32)
        b_fm, b_sm, b_gs, b_run, b_route = Buf(), Buf(), Buf(), Buf(), Buf()
        b_widx = Buf()
        zt = al.alloc("zt", [128, 2, D], BF16)
        b_zt, b_xs0 = Buf(), Buf()
        K.op(POOL, lambda: g_.memset(zt[:], 0.0), [], [b_zt])
        zf = {"n": 0}
        ZF_PER = -(-NBLK // NT)

        b_wcast = Buf()
        pcast = {"n": 0}
        PC_PER = -(-(3 * E) // (NSEQ * 4 * NQ))

        def precast(cnt):
            for _ in range(cnt):
                if pcast["n"] < 3 * E:
                    e_, m_ = pcast["n"] // 3, pcast["n"] % 3
                    src = (wpg_d, wpu_d, wpd_d)[m_]
                    pl_dma(wpb_d[m_][e_ * 128:(e_ + 1) * 128, :], src[e_ * 128:(e_ + 1) * 128, :], sw=[b_wcast])
                    pcast["n"] += 1

        def zero_fill(cnt):
            for _ in range(cnt):
                if zf["n"] < NBLK:
                    r0_ = zf["n"] * CB
                    sp_dma(xs_d[r0_:r0_ + CB, :].rearrange("(s p) d -> p s d", p=128), zt[:], r=[b_zt], sw=[b_xs0])
                    zf["n"] += 1

        sp_dma(cf[:], cf_d.ap(), w=[b_const])
        sp_dma(cbt[:], cb_d.ap(), w=[b_const])
        sp_dma(fm[:], fm_d.ap(), w=[b_fm])
        sp_dma(rbias[:], rb_d.ap(), w=[b_const])
        K.op(DVE, lambda: v_.memset(nbf[:], 0.0), [], [b_nbf])
        for g3 in range(3):
            sp_dma(nbf[g3 * 32:g3 * 32 + NH, 0:1], bf_d.ap(), w=[b_nbf])
        K.op(DVE, lambda: v_.memset(run[:], 0.0), [], [b_run])

        m0 = al.mark()
        cs = al.alloc("cs", [128, 8, NSEQ], F32)
        th0 = al.alloc("th0", [128, 8, NSEQ], F32)
        siluT = al.alloc("siluT", [128, 8, NSEQ], BF16)
        modt = al.alloc("modt", [NSEQ, 6 * D], F32)
        bada = al.alloc("bada", [NSEQ, 6 * D], F32)
        wada = [al.alloc("wada", [128, 8, 512], BF16) for _ in range(2)]
        b_cs, b_th0, b_silu, b_modt, b_bada = Buf(), Buf(), Buf(), Buf(), Buf()
        b_wada = [Buf(), Buf()]

        K.op(DVE, lambda: v_.tensor_scalar(out=nbf[0:72, 1:2], in0=nbf[0:72, 0:1], scalar1=-1.0, scalar2=None,
                                           op0=ALU.mult), [b_nbf], [b_nbf])
        K.op(ACT, lambda: a_.activation(out=sm[:, 4, :], in_=fm[:, 8, :], func=AF.Exp, scale=-1.0), [b_fm], [b_sm])
        K.op(ACT, lambda: a_.activation(out=sm[:, 5, :], in_=sm[:, 4, :], func=AF.Ln, bias=1.0, scale=1.0), [b_sm], [b_sm])
        K.op(DVE, lambda: v_.tensor_scalar(out=sm[:, 0, :], in0=sm[:, 5, :], scalar1=-8.0, scalar2=None, op0=ALU.mult), [b_sm], [b_sm])
        K.op(DVE, lambda: v_.tensor_scalar(out=sm[:, 1, :], in0=sm[:, 5, :], scalar1=-4.0, scalar2=None, op0=ALU.mult), [b_sm], [b_sm])
        K.op(DVE, lambda: v_.tensor_scalar(out=sm[:, 2, :], in0=fm[:, 6, :], scalar1=0.5, scalar2=None, op0=ALU.mult), [b_fm, b_sm], [b_sm])
        K.op(DVE, lambda: v_.tensor_scalar(out=sm[:, 3, :], in0=fm[:, 7, :], scalar1=0.5, scalar2=None, op0=ALU.mult), [b_fm, b_sm], [b_sm])
        cneg = sm[:, 0, :]
        hcneg = sm[:, 1, :]
        hbrg = sm[:, 2, :]
        hbig = sm[:, 3, :]

        sp_dma(cs[:], cs_d.ap(), w=[b_cs])
        sp_dma(bada[:], bada_d.ap(), w=[b_bada])
        K.op(ACT, lambda: a_.activation(out=th0[:], in_=cs[:], func=AF.Tanh, scale=0.5), [b_cs], [b_th0])
        K.op(DVE, lambda: v_.scalar_tensor_tensor(out=th0[:], in0=th0[:], scalar=1.0, in1=cs[:], op0=ALU.add, op1=ALU.mult),
             [b_cs, b_th0], [b_th0])
        K.op(DVE, lambda: v_.tensor_scalar(out=siluT[:], in0=th0[:], scalar1=0.5, scalar2=None, op0=ALU.mult), [b_th0], [b_silu])
        for g in range(12):
            wb = wada[g % 2]
            bw = b_wada[g % 2]
            pl_dma(wb[:], wada_d[:, g * 512:(g + 1) * 512].rearrange("(kc p) n -> p kc n", p=128), w=[bw])
            i = nb()
            for kc in range(8):
                mm(ps[i][0:NSEQ, :], siluT[:, kc, :], wb[:, kc, :], kc == 0, kc == 7, [b_silu, bw], [pb[i]], kc == 7)
            K.op(DVE, lambda: v_.tensor_tensor(out=modt[:, g * 512:(g + 1) * 512], in0=ps[i][0:NSEQ, :],
                                               in1=bada[:, g * 512:(g + 1) * 512], op=ALU.add), [pb[i], b_bada], [b_modt])
        b_modd = Buf()
        sp_dma(modd.ap(), modt[:], r=[b_modt], w=[b_modd])
        i = nb()
        pT0 = ps[i][:, 0:16 * NSEQ].rearrange("p (a b) -> p a b", b=NSEQ)
        for kc in range(16):
            col = (D + kc * 128) if kc < 8 else ((kc - 8) * 128)
            tr(pT0[:, kc, :], modt[0:NSEQ, col:col + 128], ident_f[0:NSEQ, 0:NSEQ], [b_modt, b_const], [pb[i]], kc == 15)
        K.op(DVE, lambda: v_.scalar_tensor_tensor(out=gsmT[:], in0=pT0[:, 0:8, :], scalar=1.0,
                                                  in1=fm[:, 0, :].unsqueeze(2).to_broadcast([128, 8, NSEQ]),
                                                  op0=ALU.add, op1=ALU.mult), [pb[i], b_fm], [b_gs])
        K.op(DVE, lambda: v_.tensor_copy(out=shmT[:], in_=pT0[:, 8:16, :]), [pb[i]], [b_gs])
        K.barrier()
        al.release(m0)

        off_hT = al.off
        hT = al.alloc("hT", [128, 8, S], BF16)
        yaT = al.alloc("yaT", [128, 4, S], BF16)
        off_yrT = al.off
        yrT = al.alloc("yrT", [128, 8, S], BF16)
        LIMIT = al.limit
        b_hT = [Buf() for _ in range(NT_S)]
        b_yaT, b_yrT = Buf(), Buf()
        mU = al.mark()

        for b in range(NSEQ):
            tok0 = b * S
            al.release(mU)
            x_sb = [al.alloc("x_sb", [128, D], F32) for _ in range(2)]
            xn = [al.alloc("xn", [128, D], BF16) for _ in range(2)]
            jk = al.alloc("jk", [128, D], BF16)
            st = [al.alloc("st", [128, 4], F32) for _ in range(2)]
            b_x, b_xn, b_st, b_jk = [Buf(), Buf()], [Buf(), Buf()], [Buf(), Buf()], Buf()
            rot["l"] = list(range(8))
            def m1_A(t):
                xb, xnb, stb = x_sb[t % 2], xn[t % 2], st[t % 2]
                bx, bxn, bst = b_x[t % 2], b_xn[t % 2], b_st[t % 2]
                sp_dma(xb[:], x_d[tok0 + t * 128: tok0 + (t + 1) * 128, :], w=[bx])
                zero_fill(ZF_PER)
                K.op(ACT, lambda: a_.activation(out=jk[:], in_=xb[:], func=AF.Square, accum_out=stb[:, 0:1]), [bx], [b_jk, bst])
                K.op(ACT, lambda: a_.activation(out=stb[:, 1:2], in_=stb[:, 0:1], func=AF.Sqrt, bias=EPS, scale=1.0 / D), [bst], [bst])
                K.op(DVE, lambda: v_.reciprocal(out=stb[:, 2:3], in_=stb[:, 1:2]), [bst], [bst])
                K.op(ACT, lambda: a_.activation(out=xnb[:], in_=xb[:], func=AF.Identity, scale=stb[:, 2:3]), [bx, bst], [bxn])

            def m1_B(t):
                xnb, bxn = xn[t % 2], b_xn[t % 2]
                i = nb()
                pT = ps[i][:, :].bitcast(BF16)
                for kc in range(8):
                    tr(pT[:, kc * 128:(kc + 1) * 128], xnb[:, kc * 128:(kc + 1) * 128], ident_b, [bxn, b_const], [pb[i]], kc == 7)
                for kc in range(8):
                    o_ap = hT[:, kc, t * 128:(t + 1) * 128]
                    i_ap = pT[:, kc * 128:(kc + 1) * 128]
                    if kc % 2 == 0:
                        K.op(ACT, lambda: a_.activation(out=o_ap, in_=i_ap, func=AF.Identity, bias=shmT[:, kc, b:b + 1],
                                                        scale=gsmT[:, kc, b:b + 1]), [pb[i], b_gs], [b_hT[t]])
                    else:
                        K.op(DVE, lambda: v_.tensor_scalar(out=o_ap, in0=i_ap, scalar1=gsmT[:, kc, b:b + 1],
                                                           scalar2=shmT[:, kc, b:b + 1], op0=ALU.mult, op1=ALU.add),
                             [pb[i], b_gs], [b_hT[t]])
            m1_A(0)
            for t in range(NT_S):
                if t + 1 < NT_S:
                    m1_A(t + 1)
                m1_B(t)
            K.barrier()

            al.release(mU)
            qT = al.alloc("qT", [128, 4, 2, S], BF16)
            kT = al.alloc("kT", [128, 4, S], BF16)
            sv_ = al.off
            al.off = off_yrT
            vS = al.alloc("vS", [128, NT_S, 4, 192], BF16)
            ef = al.alloc("ef", [72, S], F32)
            assert al.off <= mU
            al.off = sv_
            off_wq = al.off
            wq = al.alloc("wq", [128, 8, 512], BF16)
            wkk = al.alloc("wk", [128, 8, 512], BF16)
            wv = al.alloc("wv", [128, 8, 512], BF16)
            wf = al.alloc("wf", [128, 8, 72], BF16)
            caug = al.alloc("caug", [128, S], BF16)
            tmpb = al.alloc("tmpb", [72, S], BF16)
            b_caug, b_tmpb = Buf(), Buf()
            Lf = al.alloc("Lf", [72, S], F32)
            cumLT = al.alloc("cumLT", [128, NT_S, NH], F32)
            b_Rb, b_Rs = Buf(), Buf()
            b_q, b_k, b_v, b_wq, b_wk, b_wv, b_wf = Buf(), Buf(), Buf(), Buf(), Buf(), Buf(), Buf()
            b_ef, b_L, b_cumLT = Buf(), Buf(), Buf()
            b_PT = [Buf() for _ in range(6)]

            def wview(c0, n):
                return win_d[:, c0:c0 + n].rearrange("(kc p) n -> p kc n", p=128)
            K.op(POOL, lambda: g_.memset(vS[:, :, :, 64:128], 1.0), [], [b_v])
            K.op(POOL, lambda: g_.memset(qT[:], 0.0), [], [b_q])
            pl_dma(wq[:], wview(OQ, 512), w=[b_wq])
            pl_dma(wkk[:], wview(OK_, 512), w=[b_wk])
            pl_dma(wv[:], wview(OV, 512), w=[b_wv])
            K.op(POOL, lambda: g_.memset(wf[:], 0.0), [], [b_wf])
            K.op(POOL, lambda: g_.memset(caug[:], 0.0), [], [b_caug])
            for g3 in range(3):
                pl_dma(wf[:, :, g3 * 32:g3 * 32 + NH], wview(OF_, 8), w=[b_wf])
            rot["l"] = list(range(8))
            cnt = 0
            for (wt, bw, dst, bd, isq) in ((wq, b_wq, qT, b_q, True), (wkk, b_wk, kT, b_k, False)):
                for j in range(4):
                    for n in range(NQ):
                        i = nb()
                        for kc in range(8):
                            mm(ps[i][:, 0:QB], wt[:, kc, j * 128:(j + 1) * 128], hT[:, kc, n * QB:(n + 1) * QB], kc == 0, kc == 7,
                               [bw] + b_hT[n * TQ:(n + 1) * TQ], [pb[i]], kc == 7)
                        cols = slice(n * QB, (n + 1) * QB)
                        if isq:
                            K.op(ACT, lambda: a_.copy(out=qT[0:64, j, 0, cols], in_=ps[i][0:64, 0:QB]), [pb[i]], [bd])
                            K.op(DVE, lambda: v_.tensor_copy(out=qT[64:128, j, 1, cols], in_=ps[i][64:128, 0:QB]), [pb[i]], [bd])
                        elif cnt % 2 == 0:
                            K.op(ACT, lambda: a_.copy(out=kT[:, j, cols], in_=ps[i][:, 0:QB]), [pb[i]], [bd])
                        else:
                            K.op(DVE, lambda: v_.tensor_copy(out=kT[:, j, cols], in_=ps[i][:, 0:QB]), [pb[i]], [bd])
                        cnt += 1
            for t in range(NT_S):
                i = nb()
                for kc in range(8):
                    mm(ps[i][:, :], hT[:, kc, t * 128:(t + 1) * 128], wv[:, kc, :], kc == 0, kc == 7, [b_wv, b_hT[t]], [pb[i]], kc == 7)
                o_v = vS[:, t, :, :].rearrange("p j (c w) -> p j c w", w=64)[:, :, 0::2, :]
                i_v = ps[i][:, :].rearrange("p (j c w) -> p j c w", c=2, w=64)
                if t % 2 == 0:
                    K.op(ACT, lambda: a_.copy(out=o_v, in_=i_v), [pb[i]], [b_v])
                else:
                    K.op(DVE, lambda: v_.tensor_copy(out=o_v, in_=i_v), [pb[i]], [b_v])
            for n in range(NQ):
                i = nb()
                for kc in range(8):
                    mm(ps[i][0:72, 0:QB], wf[:, kc, :], hT[:, kc, n * QB:(n + 1) * QB], kc == 0, kc == 7,
                       [b_wf] + b_hT[n * TQ:(n + 1) * TQ], [pb[i]], kc == 7)
                K.op(ACT, lambda: a_.activation(out=ef[:, n * QB:(n + 1) * QB], in_=ps[i][0:72, 0:QB], func=AF.Exp,
                                                bias=nbf[0:72, 1:2], scale=-1.0), [pb[i], b_nbf], [b_ef])
            K.op(ACT, lambda: a_.activation(out=Lf[:], in_=ef[:], func=AF.Ln, bias=1.0, scale=1.0), [b_ef], [b_L])
            K.op(DVE, lambda: v_.tensor_tensor_scan(out=ef[:], data0=ones_c[0:72, 0:1].to_broadcast([72, S]), data1=Lf[:],
                                                    initial=0.0, op0=ALU.mult, op1=ALU.add), [b_L, b_const, b_ef], [b_ef])
            K.op(DVE, lambda: v_.tensor_scalar(out=Lf[:], in0=ef[:], scalar1=-8.0, scalar2=None, op0=ALU.mult), [b_ef, b_L], [b_L])
            K.op(DVE, lambda: v_.tensor_copy(out=tmpb[:], in_=Lf[:]), [b_L], [b_tmpb])
            K.op(DVE, lambda: v_.tensor_copy(out=caug[0:NH, :], in_=tmpb[0:NH, :]), [b_tmpb], [b_caug])
            K.op(DVE, lambda: v_.tensor_tensor(out=Lf[:], in0=Lf[:], in1=tmpb[:], op=ALU.subtract), [b_L, b_tmpb], [b_L])
            K.op(DVE, lambda: v_.tensor_copy(out=tmpb[:], in_=Lf[:]), [b_L, b_caug], [b_tmpb])
            K.op(DVE, lambda: v_.tensor_copy(out=caug[32:32 + NH, :], in_=tmpb[32:32 + NH, :]), [b_tmpb], [b_caug])
            K.op(DVE, lambda: v_.tensor_tensor(out=Lf[:], in0=Lf[:], in1=tmpb[:], op=ALU.subtract), [b_L, b_tmpb], [b_L])
            K.op(DVE, lambda: v_.tensor_copy(out=caug[64:64 + NH, :], in_=Lf[64:64 + NH, :]), [b_L], [b_caug])
            i = nb()
            pT1 = ps[i][:, 0:NT_S * NH].rearrange("p (a b) -> p a b", b=NH)
            for t in range(NT_S):
                tr(pT1[:, t, :], ef[0:NH, t * 128:(t + 1) * 128], ident_f[0:NH, 0:NH], [b_ef, b_const], [pb[i]], t == NT_S - 1)
            K.op(DVE, lambda: v_.tensor_copy(out=cumLT[:], in_=pT1), [pb[i]], [b_cumLT])

            K.barrier()
            sv3 = al.off
            al.off = off_wq
            PT = [al.alloc("PT", [128, QB], BF16) for _ in range(6)]
            Rb = al.alloc("Rb", [128, QB], BF16)
            Rs = al.alloc("Rs", [128, QB], F32)
            al.off = sv3
            rot["l"] = [4, 5, 6, 7]
            pcount = 0
            LAG = 3
            grp = {"n": 0}
            pend_backs = []

            def att_front(j, q, t, half, nt, gpar):
                nonlocal pcount
                d = t - q * TQ
                q0 = max(d, 0) * 128
                h = 2 * j + half
                rows = slice(half * 64, half * 64 + 64)
                i = nb()
                mm(ps[i][:, q0:QB], kT[:, j, t * 128:(t + 1) * 128], qT[:, j, half, q * QB + q0:(q + 1) * QB],
                   True, False, [b_q, b_k], [pb[i]], False)
                mm(ps[i][:, q0:QB], cbt[:, 512 + h * 128:512 + (h + 1) * 128], caug[:, q * QB + q0:(q + 1) * QB],
                   False, d < 0, [b_const, b_caug], [pb[i]], d < 0)
                if d >= 0:
                    mm(ps[i][:, q0:q0 + 128], ident_b, negm_b, False, True, [b_const], [pb[i]], True)
                pt = PT[pcount % 6]
                bpt = b_PT[pcount % 6]
                pcount += 1
                K.op(ACT, lambda: a_.activation(out=pt[:, q0:QB], in_=ps[i][:, q0:QB], func=AF.Exp,
                                                bias=cumLT[:, t, h:h + 1], scale=0.125), [pb[i], b_cumLT], [bpt])

                def back():
                    yi = half + 2 * gpar
                    ya_, yb_ = 2 * gpar, 2 * gpar + 1
                    lo = 0 if half == 0 else 64
                    mm(ps[yi][:, q0:QB], vS[:, t, j, lo:lo + 128], pt[:, q0:QB], t == 0, t == nt - 1,
                       [b_v, bpt], [pb[yi]], True)
                    if t == nt - 1 and half == 1:
                        K.op(DVE, lambda: v_.reciprocal(out=Rs[64:128, :], in_=ps[ya_][64:128, 0:QB]), [pb[ya_], b_Rs], [b_Rs])
                        K.op(DVE, lambda: v_.reciprocal(out=Rs[0:64, :], in_=ps[yb_][0:64, 0:QB]), [pb[yb_], b_Rs], [b_Rs])
                        K.op(DVE, lambda: v_.tensor_copy(out=Rb[:], in_=Rs[:]), [b_Rs, b_Rb], [b_Rb])
                        isw = nb()
                        mm(ps[isw][:, 0:QB], swap_b, Rb[:, :], True, True, [b_const, b_Rb], [pb[isw]], True)
                        K.op(ACT, lambda: a_.copy(out=Rs[:], in_=ps[isw][:, 0:QB]), [pb[isw]], [b_Rs])
                        K.op(DVE, lambda: v_.tensor_tensor(out=yaT[0:64, j, q * QB:(q + 1) * QB], in0=ps[ya_][0:64, 0:QB],
                                                           in1=Rs[0:64, :], op=ALU.mult), [pb[ya_], b_Rs], [b_yaT])
                        K.op(DVE, lambda: v_.tensor_tensor(out=yaT[64:128, j, q * QB:(q + 1) * QB], in0=ps[yb_][64:128, 0:QB],
                                                           in1=Rs[64:128, :], op=ALU.mult), [pb[yb_], b_Rs], [b_yaT])
                return back

            for j in range(4):
                for q in range(NQ):
                    nt = q * TQ + TQ
                    gpar = grp["n"] % 2
                    grp["n"] += 1
                    precast(PC_PER)
                    for t in range(nt):
                        for half in range(2):
                            pend_backs.append(att_front(j, q, t, half, nt, gpar))
                            if len(pend_backs) > LAG:
                                pend_backs.pop(0)()
            while pend_backs:
                pend_backs.pop(0)()
            K.barrier()

            al.release(mU)
            xp = [al.alloc("xp", [128, S + 4], F32) for _ in range(2)]
            uu = [al.alloc("uu", [128, S], F32) for _ in range(2)]
            ub = [al.alloc("ub", [128, S], BF16) for _ in range(2)]
            gg = [al.alloc("gg", [128, S], BF16) for _ in range(2)]
            thr = al.alloc("thr", [128, S], F32)
            thi = al.alloc("thi", [128, S], F32)
            e2 = al.alloc("e2", [128, S], F32)
            wx = [al.alloc("wx", [128, 8, 128], BF16) for _ in range(2)]
            wg = [al.alloc("wg", [128, 8, 128], BF16) for _ in range(2)]
            wrg = [al.alloc("wrg", [128, 128], BF16) for _ in range(2)]
            wig = [al.alloc("wig", [128, 128], BF16) for _ in range(2)]
            gt = [[al.alloc("gt", [128, QB], F32) for _ in range(2)] for _ in range(2)]
            b_xp, b_uu, b_ub, b_gg = [Buf(), Buf()], [Buf(), Buf()], [Buf(), Buf()], [Buf(), Buf()]
            b_thr, b_thi, b_e2 = Buf(), Buf(), Buf()
            b_w4 = [[Buf() for _ in range(4)] for _ in range(2)]
            b_gt = [[Buf() for _ in range(2)] for _ in range(2)]
            rot["l"] = list(range(8))
            for s2_ in range(2):
                K.op(DVE, lambda: v_.memset(xp[s2_][:, 0:4], 0.0), [], [b_xp[s2_]])

            def load_w4(c):
                s_ = c % 2
                pl_dma(wx[s_][:], wview(OX + c * 128, 128), w=[b_w4[s_][0]])
                pl_dma(wg[s_][:], wview(OG + c * 128, 128), w=[b_w4[s_][1]])
                pl_dma(wrg[s_][:], wrg_d[c, :, :], w=[b_w4[s_][2]])
                pl_dma(wig[s_][:], wig_d[c, :, :], w=[b_w4[s_][3]])

            gcnt4 = {"n": 0}

            def m4_A(c):
                s_ = c % 2
                bwx, bwg_, bwrg, bwig = b_w4[s_]
                xp_, uu_, ub_, gg_ = xp[s_], uu[s_], ub[s_], gg[s_]
                bxp, buu, bub, bgg = b_xp[s_], b_uu[s_], b_ub[s_], b_gg[s_]
                for n in range(NQ):
                    i = nb()
                    for kc in range(8):
                        mm(ps[i][:, 0:QB], wx[s_][:, kc, :], hT[:, kc, n * QB:(n + 1) * QB], kc == 0, kc == 7,
                           [bwx] + b_hT[n * TQ:(n + 1) * TQ], [pb[i]], kc == 7)
                    K.op(ACT, lambda: a_.copy(out=xp_[:, 3 + n * QB:3 + (n + 1) * QB], in_=ps[i][:, 0:QB]), [pb[i]], [bxp])
                K.op(DVE, lambda: v_.tensor_scalar(out=uu_[:], in0=xp_[:, 3:3 + S], scalar1=fm[:, 4, c:c + 1], scalar2=fm[:, 5, c:c + 1],
                                                   op0=ALU.mult, op1=ALU.add), [bxp, b_fm], [buu])
                for jj in range(3):
                    K.op(DVE, lambda: v_.scalar_tensor_tensor(out=uu_[:], in0=xp_[:, jj:jj + S], scalar=fm[:, 1 + jj, c:c + 1], in1=uu_[:],
                                                              op0=ALU.mult, op1=ALU.add), [bxp, b_fm, buu], [buu])
                K.op(POOL, lambda: g_.tensor_copy(out=ub_[:], in_=uu_[:]), [buu], [bub])
                for n in range(NQ):
                    i = nb()
                    for kc in range(8):
                        mm(ps[i][:, 0:QB], wg[s_][:, kc, :], hT[:, kc, n * QB:(n + 1) * QB], kc == 0, kc == 7,
                           [bwg_] + b_hT[n * TQ:(n + 1) * TQ], [pb[i]], kc == 7)
                    g0, g1 = gt[gcnt4["n"] % 2]
                    bg0, bg1 = b_gt[gcnt4["n"] % 2]
                    gcnt4["n"] += 1
                    pg = ps[i][:, 0:QB]
                    K.op(ACT, lambda: a_.activation(out=g0[:], in_=pg, func=AF.Square), [pb[i]], [bg0])
                    K.op(DVE, lambda: v_.tensor_scalar(out=g0[:], in0=g0[:], scalar1=0.044715, scalar2=1.0, op0=ALU.mult, op1=ALU.add),
                         [bg0], [bg0])
                    K.op(DVE, lambda: v_.tensor_tensor(out=g0[:], in0=g0[:], in1=pg, op=ALU.mult), [bg0, pb[i]], [bg0])
                    K.op(ACT, lambda: a_.activation(out=g1[:], in_=g0[:], func=AF.Tanh, scale=0.7978845608028654), [bg0], [bg1])
                    K.op(DVE, lambda: v_.scalar_tensor_tensor(out=gg_[:, n * QB:(n + 1) * QB], in0=g1[:], scalar=1.0, in1=pg,
                                                              op0=ALU.add, op1=ALU.mult), [bg1, pb[i]], [bgg])

            def m4_B(c):
                s_ = c % 2
                bwx, bwg_, bwrg, bwig = b_w4[s_]
                uu_, ub_, gg_ = uu[s_], ub[s_], gg[s_]
                buu, bub, bgg = b_uu[s_], b_ub[s_], b_gg[s_]
                for n in range(NQ):
                    i = nb()
                    mm(ps[i][:, 0:QB], wrg[s_][:, :], ub_[:, n * QB:(n + 1) * QB], True, True, [bwrg, bub], [pb[i]], True)
                    K.op(ACT, lambda: a_.activation(out=thr[:, n * QB:(n + 1) * QB], in_=ps[i][:, 0:QB], func=AF.Tanh,
                                                    bias=hbrg[:, c:c + 1], scale=0.5), [pb[i], b_sm], [b_thr])
                    i2 = nb()
                    mm(ps[i2][:, 0:QB], wig[s_][:, :], ub_[:, n * QB:(n + 1) * QB], True, True, [bwig, bub], [pb[i2]], True)
                    K.op(ACT, lambda: a_.activation(out=thi[:, n * QB:(n + 1) * QB], in_=ps[i2][:, 0:QB], func=AF.Tanh,
                                                    bias=hbig[:, c:c + 1], scale=0.5), [pb[i2], b_sm], [b_thi])
                K.op(ACT, lambda: a_.activation(out=e2[:], in_=thr[:], func=AF.Exp, bias=cneg[:, c:c + 1], scale=cneg[:, c:c + 1]),
                     [b_thr, b_sm], [b_e2])
                K.op(ACT, lambda: a_.activation(out=thr[:], in_=thr[:], func=AF.Exp, bias=hcneg[:, c:c + 1], scale=hcneg[:, c:c + 1]),
                     [b_thr, b_sm], [b_thr])
                K.op(DVE, lambda: v_.tensor_scalar(out=e2[:], in0=e2[:], scalar1=1.0 - 1.0e-7, scalar2=None, op0=ALU.min), [b_e2], [b_e2])
                K.op(ACT, lambda: a_.activation(out=e2[:], in_=e2[:], func=AF.Sqrt, bias=1.0, scale=-1.0), [b_e2], [b_e2])
                K.op(DVE, lambda: v_.scalar_tensor_tensor(out=thi[:], in0=thi[:], scalar=1.0, in1=e2[:], op0=ALU.add, op1=ALU.mult),
                     [b_thi, b_e2], [b_thi])
                K.op(DVE, lambda: v_.scalar_tensor_tensor(out=thi[:], in0=thi[:], scalar=0.5, in1=uu_[:], op0=ALU.mult, op1=ALU.mult),
                     [b_thi, buu], [b_thi])
                K.op(DVE, lambda: v_.tensor_tensor_scan(out=e2[:], data0=thr[:], data1=thi[:], initial=0.0, op0=ALU.mult, op1=ALU.add),
                     [b_thr, b_thi, b_e2], [b_e2])
                K.op(DVE, lambda: v_.scalar_tensor_tensor(out=yrT[:, c, :], in0=gg_[:], scalar=0.5, in1=e2[:], op0=ALU.mult, op1=ALU.mult),
                     [bgg, b_e2], [b_yrT])

            load_w4(0)
            load_w4(1)
            m4_A(0)
            for c in range(8):
                if c + 1 < 8:
                    m4_A(c + 1)
                m4_B(c)
                if c + 2 < 8:
                    load_w4(c + 2)
            K.barrier()

            al.release(mU)
            mgT = al.alloc("mgT", [128, 8, S], BF16)
            b_mg = [Buf() for _ in range(NT_S)]
            m5 = al.mark()
            w5 = [al.alloc("w5", [128, 28, 128], BF16) for _ in range(2)]
            b_w5 = [[Buf() for _ in range(4)] for _ in range(2)]
            sa = [[al.alloc("sa", [128, QB], F32) for _ in range(4)] for _ in range(2)]
            b_sa = [[Buf() for _ in range(4)] for _ in range(2)]
            rot["l"] = list(range(8))

            def load_w5(m):
                s_ = m % 2
                cs_ = slice(m * 128, (m + 1) * 128)
                pl_dma(w5[s_][:, 0:4, :], wba_d[:, cs_].rearrange("(kc p) n -> p kc n", p=128), w=[b_w5[s_][0]])
                pl_dma(w5[s_][:, 4:12, :], wbr_d[:, cs_].rearrange("(kc p) n -> p kc n", p=128), w=[b_w5[s_][1]])
                pl_dma(w5[s_][:, 12:20, :], wview(OGA + m * 128, 128), w=[b_w5[s_][2]])
                pl_dma(w5[s_][:, 20:28, :], wview(OGR + m * 128, 128), w=[b_w5[s_][3]])
            load_w5(0)
            scount = 0
            for m in range(8):
                s_ = m % 2
                bwa, bwr_, bwga, bwgr = b_w5[s_]
                if m + 1 < 8:
                    load_w5(m + 1)
                for n in range(NQ):
                    cols = slice(n * QB, (n + 1) * QB)
                    hb = b_hT[n * TQ:(n + 1) * TQ]
                    iA, iR, iGA, iGR = nb(), nb(), nb(), nb()
                    for kc in range(4):
                        mm(ps[iA][:, 0:QB], w5[s_][:, kc, :], yaT[:, kc, cols], kc == 0, kc == 3, [bwa, b_yaT], [pb[iA]], kc == 3)
                    for kc in range(8):
                        mm(ps[iGA][:, 0:QB], w5[s_][:, 12 + kc, :], hT[:, kc, cols], kc == 0, kc == 7, [bwga] + hb, [pb[iGA]], kc == 7)
                    for kc in range(8):
                        mm(ps[iR][:, 0:QB], w5[s_][:, 4 + kc, :], yrT[:, kc, cols], kc == 0, kc == 7, [bwr_, b_yrT], [pb[iR]], kc == 7)
                    for kc in range(8):
                        mm(ps[iGR][:, 0:QB], w5[s_][:, 20 + kc, :], hT[:, kc, cols], kc == 0, kc == 7, [bwgr] + hb, [pb[iGR]], kc == 7)
                    s0, s1, s2, s3 = sa[scount % 2]
                    c0, c1, c2, c3 = b_sa[scount % 2]
                    scount += 1
                    K.op(ACT, lambda: a_.activation(out=s0[:], in_=ps[iGA][:, 0:QB], func=AF.Sigmoid), [pb[iGA]], [c0])
                    K.op(DVE, lambda: v_.tensor_tensor(out=s1[:], in0=s0[:], in1=ps[iA][:, 0:QB], op=ALU.mult), [c0, pb[iA]], [c1])
                    K.op(ACT, lambda: a_.activation(out=s2[:], in_=ps[iGR][:, 0:QB], func=AF.Sigmoid), [pb[iGR]], [c2])
                    K.op(DVE, lambda: v_.tensor_tensor(out=s3[:], in0=s2[:], in1=ps[iR][:, 0:QB], op=ALU.mult), [c2, pb[iR]], [c3])
                    K.op(POOL, lambda: g_.tensor_tensor(out=mgT[:, m, cols], in0=s1[:], in1=s3[:], op=ALU.add), [c1, c3],
                         b_mg[n * TQ:(n + 1) * TQ])
            K.barrier()

            al.release(m5)
            offB = al.off
            useA = (mU - off_hT) >= 80 * 1024
            if useA:
                al.off = off_hT
                al.limit = mU
            wo = al.alloc("wo", [128, 8, D], BF16)
            h2Tb = [al.alloc("h2Tb", [128, 8, QB], BF16) for _ in range(2)]
            xs2 = [al.alloc("xs2", [128, D], F32) for _ in range(2)]
            x1 = [al.alloc("x1", [128, D], F32) for _ in range(2)]
            h2 = [al.alloc("h2", [128, D], F32) for _ in range(2)]
            h2T = [al.alloc("h2T", [128, 8, 128], F32) for _ in range(2)]
            shs = [al.alloc("shs", [128, D], BF16) for _ in range(2)]
            h2b = [al.alloc("h2b", [128, D], BF16) for _ in range(2)]
            if useA:
                al.off = offB
                al.limit = LIMIT
            wsg = al.alloc("wsg", [128, 8, FF], BF16)
            wsu = al.alloc("wsu", [128, 8, FF], BF16)
            wsd = al.alloc("wsd", [128, 2, D], BF16)
            wr = al.alloc("wr", [128, 8, E], F32)
            bcv5 = al.alloc("bcv5", [128, 2, D], F32)
            b_bcv5 = Buf()
            gmb = al.alloc("gmb", [128, D], F32)
            gsfb = al.alloc("gsfb", [128, D], F32)
            shfb = al.alloc("shfb", [128, D], F32)
            sg = [al.alloc("sg", [128, QB], F32) for _ in range(2)]
            actT = al.alloc("actT", [128, 2, QB], BF16)
            rs = [al.alloc("rs", [128, 8], F32) for _ in range(2)]
            rt_ = [al.alloc("rt", [128, 6, E], F32) for _ in range(2)]
            g8 = [al.alloc("g8", [128, 8, 8], F32) for _ in range(2)]
            r8 = [al.alloc("r8", [128, 4, 8], F32) for _ in range(2)]
            i8 = [al.alloc("i8", [128, 8], U32) for _ in range(2)]
            mkb = [al.alloc("mkb", [128, E], BF16) for _ in range(2)]
            b_wo, b_ws, b_wr, b_bc5 = Buf(), Buf(), Buf(), Buf()
            b_xs2, b_x1, b_h2, b_h2b, b_h2T = [Buf(), Buf()], [Buf(), Buf()], [Buf(), Buf()], [Buf(), Buf()], [Buf(), Buf()]
            b_h2Tb, b_shs, b_sg, b_act, b_rs, b_rt = [Buf(), Buf()], [Buf(), Buf()], [Buf(), Buf()], Buf(), [Buf(), Buf()], [Buf(), Buf()]
            b_jk5 = Buf()
            jk5 = al.alloc("jk5", [128, D], BF16)
            pl_dma(wo[:], wout_d.ap().rearrange("(kc p) n -> p kc n", p=128), w=[b_wo])
            b_wsg, b_wsu, b_wsd = Buf(), Buf(), Buf()
            pl_dma(wsg[:], wsg_d.ap().rearrange("(kc p) n -> p kc n", p=128), w=[b_wsg])
            pl_dma(wsu[:], wsu_d.ap().rearrange("(kc p) n -> p kc n", p=128), w=[b_wsu])
            pl_dma(wsd[:], wsd_d.ap().rearrange("(kc p) n -> p kc n", p=128), w=[b_wsd])
            sp_dma(wr[:], wr_d.ap().rearrange("(kc p) n -> p kc n", p=128), w=[b_wr])
            sp_dma(bcv5[:], bcv_d[:, 0:2, :], w=[b_bcv5])
            sp_dma(gmb[:], modd[b:b + 1, 2 * D:3 * D].partition_broadcast(128), r=[b_modd], w=[b_bc5])
            sp_dma(gsfb[:], modd[b:b + 1, 4 * D:5 * D].partition_broadcast(128), r=[b_modd], w=[b_bc5])
            sp_dma(shfb[:], modd[b:b + 1, 3 * D:4 * D].partition_broadcast(128), r=[b_modd], w=[b_bc5])
            K.op(DVE, lambda: v_.tensor_tensor(out=gmb[:], in0=gmb[:], in1=bcv5[:, 0, :], op=ALU.mult), [b_bc5, b_bcv5], [b_bc5])
            K.op(DVE, lambda: v_.scalar_tensor_tensor(out=gsfb[:], in0=gsfb[:], scalar=1.0, in1=bcv5[:, 1, :], op0=ALU.add, op1=ALU.mult),
                 [b_bc5, b_bcv5], [b_bc5])
            rot["l"] = list(range(8))
            def m5_A(t):
                n, tt = t // TQ, t % TQ
                T = b * NT_S + t
                r0 = tok0 + t * 128
                p_ = t % 2
                xb, x1b, h2_, h2b_, h2T_, rs_ = xs2[p_], x1[p_], h2[p_], h2b[p_], h2T[p_], rs[p_]
                bxb, bx1, bh2, bh2b, bh2T, brs = b_xs2[p_], b_x1[p_], b_h2[p_], b_h2b[p_], b_h2T[p_], b_rs[p_]
                sp_dma(xb[:], x_d[r0:r0 + 128, :], w=[bxb])
                io = [nb(), nb()]
                for hf in range(2):
                    for kc in range(8):
                        mm(ps[io[hf]][:, :], mgT[:, kc, t * 128:(t + 1) * 128], wo[:, kc, hf * 512:(hf + 1) * 512], kc == 0, kc == 7,
                           [b_mg[t], b_wo], [pb[io[hf]]], kc == 7)
                for hf in range(2):
                    K.op(ACT, lambda: a_.activation(out=jk5[:, hf * 512:(hf + 1) * 512], in_=ps[io[hf]][:, :], func=AF.Square,
                                                    accum_out=rs_[:, hf:hf + 1]), [pb[io[hf]]], [b_jk5, brs])
                K.op(DVE, lambda: v_.tensor_tensor(out=rs_[:, 2:3], in0=rs_[:, 0:1], in1=rs_[:, 1:2], op=ALU.add), [brs], [brs])
                K.op(ACT, lambda: a_.activation(out=rs_[:, 3:4], in_=rs_[:, 2:3], func=AF.Sqrt, bias=EPS, scale=1.0 / D), [brs], [brs])
                K.op(DVE, lambda: v_.reciprocal(out=rs_[:, 4:5], in_=rs_[:, 3:4]), [brs], [brs])
                for hf in range(2):
                    cs_ = slice(hf * 512, (hf + 1) * 512)
                    K.op(DVE, lambda: v_.scalar_tensor_tensor(out=x1b[:, cs_], in0=ps[io[hf]][:, :], scalar=rs_[:, 4:5], in1=gmb[:, cs_],
                                                              op0=ALU.mult, op1=ALU.mult), [pb[io[hf]], brs, b_bc5], [bx1])
                K.op(POOL, lambda: g_.tensor_tensor(out=x1b[:], in0=x1b[:], in1=xb[:], op=ALU.add), [bx1, bxb], [bx1])
                sp_dma(x1_d[r0:r0 + 128, :], x1b[:], r=[bx1])
                K.op(ACT, lambda: a_.activation(out=jk5[:], in_=x1b[:], func=AF.Square, accum_out=rs_[:, 5:6]), [bx1], [b_jk5, brs])
                K.op(ACT, lambda: a_.activation(out=rs_[:, 6:7], in_=rs_[:, 5:6], func=AF.Sqrt, bias=EPS, scale=1.0 / D), [brs], [brs])
                K.op(DVE, lambda: v_.reciprocal(out=rs_[:, 7:8], in_=rs_[:, 6:7]), [brs], [brs])
                K.op(DVE, lambda: v_.scalar_tensor_tensor(out=h2_[:], in0=x1b[:], scalar=rs_[:, 7:8], in1=gsfb[:], op0=ALU.mult, op1=ALU.mult),
                     [bx1, brs, b_bc5], [bh2])
                K.op(POOL, lambda: g_.tensor_tensor(out=h2_[:], in0=h2_[:], in1=shfb[:], op=ALU.add), [bh2, b_bc5], [bh2])
                K.op(POOL, lambda: g_.tensor_copy(out=h2b_[:], in_=h2_[:]), [bh2], [bh2b])
                sp_dma(h2_d[r0:r0 + 128, :], h2b_[:], r=[bh2b])

            def m5_B(t):
                n, tt = t // TQ, t % TQ
                hb_ = h2Tb[n % 2]
                bhb = b_h2Tb[n % 2]
                T = b * NT_S + t
                p_ = t % 2
                h2_, h2T_ = h2[p_], h2T[p_]
                bh2, bh2T = b_h2[p_], b_h2T[p_]
                it = [nb(), nb()]
                for kc in range(8):
                    ib = it[kc // 4]
                    tr(ps[ib][:, (kc % 4) * 128:(kc % 4 + 1) * 128], h2_[:, kc * 128:(kc + 1) * 128], ident_f, [bh2, b_const], [pb[ib]],
                       kc % 4 == 3)
                for hf in range(2):
                    K.op(ACT, lambda: a_.copy(out=h2T_[:, hf * 4:(hf + 1) * 4, :], in_=ps[it[hf]][:, :].rearrange("p (a b) -> p a b", b=128)),
                         [pb[it[hf]]], [bh2T])
                K.op(POOL, lambda: g_.tensor_copy(out=hb_[:, :, tt * 128:(tt + 1) * 128], in_=h2T_[:]), [bh2T], [bhb])
                il = nb()
                for kc in range(8):
                    mm(ps[il][:, 0:E], h2T_[:, kc, :], wr[:, kc, :], kc == 0, kc == 7, [bh2T, b_wr], [pb[il]], kc == 7)
                R_, g8_, r8_, i8_, mk_ = rt_[p_], g8[p_], r8[p_], i8[p_], mkb[p_]
                brt = b_rt[p_]
                sc, sel, selm, mkf, wfull, tmp = (R_[:, k_, :] for k_ in range(6))
                K.op(ACT, lambda: a_.activation(out=sc, in_=ps[il][:, 0:E], func=AF.Sigmoid), [pb[il]], [brt])
                K.op(DVE, lambda: v_.tensor_tensor(out=sel, in0=sc, in1=rbias[:], op=ALU.add), [brt, b_const], [brt])
                for gg in range(8):
                    K.op(DVE, lambda: v_.max(out=g8_[:, gg, :], in_=sel[:, gg * 8:(gg + 1) * 8]), [brt], [brt])
                K.op(DVE, lambda: v_.tensor_tensor(out=r8_[:, 0, :], in0=g8_[:, :, 0], in1=g8_[:, :, 1], op=ALU.add), [brt], [brt])
                K.op(DVE, lambda: v_.max(out=r8_[:, 1, :], in_=r8_[:, 0, :]), [brt], [brt])
                K.op(DVE, lambda: v_.tensor_scalar(out=r8_[:, 2, :], in0=r8_[:, 0, :], scalar1=r8_[:, 1, 3:4], scalar2=-BIG,
                                                   op0=ALU.is_lt, op1=ALU.mult), [brt], [brt])
                K.op(DVE, lambda: v_.tensor_tensor(out=selm.rearrange("p (a b) -> p a b", b=8), in0=sel.rearrange("p (a b) -> p a b", b=8),
                                                   in1=r8_[:, 2, :].unsqueeze(2).to_broadcast([128, 8, 8]), op=ALU.add), [brt], [brt])
                K.op(DVE, lambda: v_.max(out=r8_[:, 3, :], in_=selm), [brt], [brt])
                K.op(DVE, lambda: v_.max_index(out=i8_[:], in_max=r8_[:, 3, :], in_values=selm), [brt], [brt])
                K.op(DVE, lambda: v_.tensor_scalar(out=mkf, in0=selm, scalar1=r8_[:, 3, 5:6], scalar2=None, op0=ALU.is_ge), [brt], [brt])
                K.op(DVE, lambda: v_.tensor_tensor(out=wfull, in0=sc, in1=mkf, op=ALU.mult), [brt], [brt])
                K.op(DVE, lambda: v_.tensor_reduce(out=r8_[:, 2, 0:1], in_=wfull, axis=AX.X, op=ALU.add), [brt], [brt])
                K.op(DVE, lambda: v_.reciprocal(out=r8_[:, 2, 1:2], in_=r8_[:, 2, 0:1]), [brt], [brt])
                K.op(POOL, lambda: g_.tensor_copy(out=mk_[:], in_=mkf), [brt], [brt])
                ik = nb()
                mm(ps[ik][:, 0:E], triu_b, mk_[:], True, True, [brt, b_const], [pb[ik]], False)
                mm(ps[ik][:, E:2 * E], ones_b, mk_[:], True, True, [brt, b_const], [pb[ik]], True)
                K.op(DVE, lambda: v_.tensor_tensor(out=tmp, in0=ps[ik][:, 0:E], in1=run[:], op=ALU.add), [pb[ik], b_run, brt], [brt])
                K.op(DVE, lambda: v_.tensor_tensor(out=rankm[:, T, :], in0=tmp, in1=mkf, op=ALU.mult), [brt], [b_route])
                K.op(DVE, lambda: v_.tensor_tensor(out=run[:], in0=run[:], in1=ps[ik][:, E:2 * E], op=ALU.add), [pb[ik], b_run], [b_run])
                K.op(DVE, lambda: v_.tensor_copy(out=eidx[:, T, :], in_=i8_[:]), [brt], [b_route])
                for k_ in range(TOPK):
                    K.op(DVE, lambda: v_.scalar_tensor_tensor(out=tmp, in0=iota64, scalar=eidx[:, T, k_:k_ + 1], in1=wfull,
                                                              op0=ALU.is_equal, op1=ALU.mult, accum_out=wk[:, T, k_:k_ + 1]),
                         [brt, b_route, b_const], [brt, b_route])
                K.op(DVE, lambda: v_.tensor_scalar(out=wkn[:, T, 0:TOPK], in0=wk[:, T, 0:TOPK], scalar1=r8_[:, 2, 1:2], scalar2=2.5,
                                                   op0=ALU.mult, op1=ALU.mult), [brt, b_route], [b_route])

            def m5_SH(n):
                hb_ = h2Tb[n % 2]
                bhb = b_h2Tb[n % 2]
                ig = [nb(), nb()]
                iu = [nb(), nb()]
                for c in range(2):
                    for kc in range(8):
                        mm(ps[ig[c]][:, 0:QB], wsg[:, kc, c * 128:(c + 1) * 128], hb_[:, kc, :], kc == 0, kc == 7, [b_wsg, bhb], [pb[ig[c]]], kc == 7)
                    for kc in range(8):
                        mm(ps[iu[c]][:, 0:QB], wsu[:, kc, c * 128:(c + 1) * 128], hb_[:, kc, :], kc == 0, kc == 7, [b_wsu, bhb], [pb[iu[c]]], kc == 7)
                    sg_ = sg[c]
                    K.op(ACT, lambda: a_.activation(out=sg_[:], in_=ps[ig[c]][:, 0:QB], func=AF.Sigmoid), [pb[ig[c]]], [b_sg[c]])
                    K.op(DVE, lambda: v_.tensor_tensor(out=sg_[:], in0=sg_[:], in1=ps[ig[c]][:, 0:QB], op=ALU.mult), [b_sg[c], pb[ig[c]]], [b_sg[c]])
                    K.op(DVE, lambda: v_.tensor_tensor(out=actT[:, c, :], in0=sg_[:], in1=ps[iu[c]][:, 0:QB], op=ALU.mult),
                         [b_sg[c], pb[iu[c]]], [b_act])
                for tt in range(TQ):
                    t = n * TQ + tt
                    r0 = tok0 + t * 128
                    iy = [nb(), nb()]
                    for hf in range(2):
                        for c in range(2):
                            mm(ps[iy[hf]][:, :], actT[:, c, tt * 128:(tt + 1) * 128], wsd[:, c, hf * 512:(hf + 1) * 512], c == 0, c == 1,
                               [b_act, b_wsd], [pb[iy[hf]]], c == 1)
                    sh_ = shs[t % 2]
                    K.op(ACT, lambda: a_.copy(out=sh_[:, 0:512], in_=ps[iy[0]][:, :]), [pb[iy[0]]], [b_shs[t % 2]])
                    K.op(DVE, lambda: v_.tensor_copy(out=sh_[:, 512:1024], in_=ps[iy[1]][:, :]), [pb[iy[1]]], [b_shs[t % 2]])
                    sp_dma(sh_d[r0:r0 + 128, :], sh_[:], r=[b_shs[t % 2]])

            m5_A(0)
            for t in range(NT_S):
                if t + 1 < NT_S:
                    m5_A(t + 1)
                m5_B(t)
                if t % TQ == TQ - 1:
                    m5_SH(t // TQ)
            K.barrier()

        al.release(mU)
        al.off = mU
        pe_ = al.alloc("pend", [128, 4, E], F32)
        pei = al.alloc("pei", [128, E], I32)
        ebf = al.alloc("ebf", [128, 2, NBLK], F32)
        djk = al.alloc("djk", [128, E], F32)
        rkp = al.alloc("rkp", [128, E], F32)
        destf = al.alloc("destf", [128, NT, 8], F32)
        b_pe, b_eb, b_dj, b_rkp, b_destf, b_desti = Buf(), Buf(), Buf(), Buf(), Buf(), Buf()
        b_desti_t = [Buf() for _ in range(NT)]
        K.op(DVE, lambda: v_.tensor_scalar(out=pe_[:, 3, :], in0=run[:], scalar1=float(CB - 1), scalar2=None, op0=ALU.add), [b_run], [b_pe])
        K.op(DVE, lambda: v_.tensor_copy(out=pei[:], in_=pe_[:, 3, :]), [b_pe], [b_pe])
        K.op(DVE, lambda: v_.tensor_single_scalar(out=pei[:], in_=pei[:], scalar=8, op=ALU.arith_shift_right), [b_pe], [b_pe])
        K.op(DVE, lambda: v_.tensor_copy(out=pe_[:, 0, :], in_=pei[:]), [b_pe], [b_pe])
        K.op(DVE, lambda: v_.tensor_tensor_scan(out=pe_[:, 1, :], data0=ones_c[:, 0:1].to_broadcast([128, E]), data1=pe_[:, 0, :],
                                                initial=0.0, op0=ALU.mult, op1=ALU.add), [b_pe, b_const], [b_pe])
        K.op(DVE, lambda: v_.tensor_tensor(out=pe_[:, 2, :], in0=pe_[:, 1, :], in1=pe_[:, 0, :], op=ALU.subtract), [b_pe], [b_pe])
        K.op(DVE, lambda: v_.tensor_scalar(out=pe_[:, 2, :], in0=pe_[:, 2, :], scalar1=float(CB), scalar2=None, op0=ALU.mult), [b_pe], [b_pe])
        K.op(DVE, lambda: v_.memset(ebf[:, 0, :], 0.0), [], [b_eb])
        for e_ in range(E):
            K.op(DVE, lambda: v_.scalar_tensor_tensor(out=ebf[:, 0, :], in0=iotab, scalar=pe_[:, 1, e_:e_ + 1], in1=ebf[:, 0, :],
                                                      op0=ALU.is_ge, op1=ALU.add), [b_pe, b_const, b_eb], [b_eb])
        K.op(DVE, lambda: v_.tensor_scalar(out=ebf[:, 0, :], in0=ebf[:, 0, :], scalar1=float(E - 1), scalar2=128.0, op0=ALU.min, op1=ALU.mult),
             [b_eb], [b_eb])
        K.op(DVE, lambda: v_.tensor_scalar(out=ebf[:, 1, :], in0=ebf[:, 0, :], scalar1=pidx, scalar2=None, op0=ALU.add), [b_eb, b_const], [b_eb])
        if skip_reload and NBLK > 2:
            K.op(DVE, lambda: v_.tensor_tensor(out=ebf[:, 0, 2:NBLK], in0=ebf[:, 0, 2:NBLK], in1=ebf[:, 1, 0:NBLK - 2], op=ALU.subtract),
                 [b_eb], [b_eb])
            K.op(DVE, lambda: v_.tensor_scalar(out=ebf[:, 0, 2:NBLK], in0=ebf[:, 0, 2:NBLK], scalar1=pidx, scalar2=None, op0=ALU.add),
                 [b_eb, b_const], [b_eb])
            K.op(DVE, lambda: v_.tensor_scalar(out=ebf[:, 0, 2:NBLK], in0=ebf[:, 0, 2:NBLK], scalar1=0.0, scalar2=1.0e6,
                                               op0=ALU.is_equal, op1=ALU.mult), [b_eb], [b_eb])
            K.op(DVE, lambda: v_.tensor_tensor(out=ebf[:, 1, 2:NBLK], in0=ebf[:, 1, 2:NBLK], in1=ebf[:, 0, 2:NBLK], op=ALU.add), [b_eb], [b_eb])
        K.op(DVE, lambda: v_.tensor_copy(out=widx[:], in_=ebf[:, 1, :]), [b_eb], [b_widx])
        for T in range(NT):
            K.op(DVE, lambda: v_.tensor_tensor(out=rkp[:], in0=rankm[:, T, :], in1=pe_[:, 2, :], op=ALU.add), [b_route, b_pe, b_rkp], [b_rkp])
            for k_ in range(TOPK):
                K.op(DVE, lambda: v_.scalar_tensor_tensor(out=djk[:], in0=iota64, scalar=eidx[:, T, k_:k_ + 1], in1=rkp[:],
                                                          op0=ALU.is_equal, op1=ALU.mult, accum_out=destf[:, T, k_:k_ + 1]),
                     [b_route, b_rkp, b_const], [b_dj, b_destf])
            K.op(DVE, lambda: v_.tensor_copy(out=desti[:, T, 0:TOPK], in_=destf[:, T, 0:TOPK]), [b_destf], [b_desti_t[T]])
        hg = [al.alloc("hg", [128, D], BF16) for _ in range(3)]
        b_hg = [Buf() for _ in range(3)]
        b_xs = Buf()
        for T in range(NT):
            hb_ = hg[T % 3]
            sp_dma(hb_[:], h2_d[T * 128:(T + 1) * 128, :], w=[b_hg[T % 3]])
            for k_ in range(TOPK):
                K.dma(K.qpool, lambda: g_.indirect_dma_start(out=xs_d[:, :], out_offset=bass.IndirectOffsetOnAxis(ap=desti[:, T, k_:k_ + 1], axis=0),
                                                             in_=hb_[:], in_offset=None, bounds_check=reg_slot, oob_is_err=False),
                      [b_hg[T % 3], b_desti_t[T], b_xs0], [], [b_xs])
        K.barrier()

        al.off = mU
        wE = [[al.alloc("wE", [128, 2048], BF16) for _ in range(3)] for _ in range(2)]
        b_wE = [[Buf() for _ in range(3)] for _ in range(2)]
        xsb = [al.alloc("xsb", [128, 2, D], BF16) for _ in range(3)]
        xTe = [al.alloc("xTe", [128, 8, CB], BF16) for _ in range(3)]
        sge = [al.alloc("sge", [128, 2 * CB], F32) for _ in range(2)]
        acte = [al.alloc("acte", [128, 2 * CB], BF16) for _ in range(2)]
        ysb = [al.alloc("ysb", [128, D], BF16) for _ in range(4)]
        b_xsb, b_xTe, b_sge, b_acte = [Buf(), Buf(), Buf()], [Buf(), Buf(), Buf()], [Buf(), Buf()], [Buf(), Buf()]
        b_ysb = [Buf() for _ in range(4)]
        b_ys = Buf()
        rot["l"] = list(range(8))
        wsrc = wpb_d

        def load_wE(blk, which):
            s_ = blk % 2
            for m in which:
                K.dma(K.qpool, lambda: g_.indirect_dma_start(out=wE[s_][m][:], out_offset=None, in_=wsrc[m][:, :],
                                                             in_offset=bass.IndirectOffsetOnAxis(ap=widx[:, blk:blk + 1], axis=0),
                                                             bounds_check=reg_w, oob_is_err=False),
                      [b_widx, b_wcast], [b_wE[s_][m]])

        def load_xs(blk):
            p_ = blk % 3
            sp_dma(xsb[p_][:], xs_d[blk * CB:(blk + 1) * CB, :].rearrange("(s p) d -> p s d", p=128), r=[b_xs], w=[b_xsb[p_]])

        def stage_T(blk):
            p_ = blk % 3
            for s2 in range(2):
                i = nb()
                pT = ps[i][:, :].bitcast(BF16)
                for kc in range(8):
                    tr(pT[:, kc * 128:(kc + 1) * 128], xsb[p_][:, s2, kc * 128:(kc + 1) * 128], ident_b, [b_xsb[p_], b_const], [pb[i]], kc == 7)
                o_ap = xTe[p_][:, :, s2 * 128:(s2 + 1) * 128]
                i_ap = pT.rearrange("p (a b) -> p a b", b=128)
                if s2 == 0:
                    K.op(ACT, lambda: a_.copy(out=o_ap, in_=i_ap), [pb[i]], [b_xTe[p_]])
                else:
                    K.op(DVE, lambda: v_.tensor_copy(out=o_ap, in_=i_ap), [pb[i]], [b_xTe[p_]])

        def stage_GU(blk):
            s_ = blk % 2
            p_ = blk % 2
            x_ = blk % 3
            wgE, wuE, wdE = wE[s_]
            bwg, bwu, bwd = b_wE[s_]
            ig_, iu_ = nb(), nb()
            for c in range(2):
                for kc in range(8):
                    mm(ps[ig_][:, c * CB:(c + 1) * CB], wgE[:, kc * FF + c * 128: kc * FF + (c + 1) * 128], xTe[x_][:, kc, :], kc == 0, kc == 7,
                       [bwg, b_xTe[x_]], [pb[ig_]], kc == 7)
            for c in range(2):
                for kc in range(8):
                    mm(ps[iu_][:, c * CB:(c + 1) * CB], wuE[:, kc * FF + c * 128: kc * FF + (c + 1) * 128], xTe[x_][:, kc, :], kc == 0, kc == 7,
                       [bwu, b_xTe[x_]], [pb[iu_]], kc == 7)
            K.op(ACT, lambda: a_.activation(out=sge[p_][:], in_=ps[ig_][:, :], func=AF.Sigmoid), [pb[ig_]], [b_sge[p_]])
            K.op(DVE, lambda: v_.tensor_tensor(out=sge[p_][:], in0=sge[p_][:], in1=ps[ig_][:, :], op=ALU.mult), [b_sge[p_], pb[ig_]], [b_sge[p_]])
            K.op(DVE, lambda: v_.tensor_tensor(out=acte[p_][:], in0=sge[p_][:], in1=ps[iu_][:, :], op=ALU.mult), [b_sge[p_], pb[iu_]], [b_acte[p_]])

        ycnt = {"n": 0}

        def stage_D(blk):
            s_ = blk % 2
            p_ = blk % 2
            wdE = wE[s_][2]
            bwd = b_wE[s_][2]
            for s2 in range(2):
                yb = ysb[ycnt["n"] % 4]
                byb = b_ysb[ycnt["n"] % 4]
                ycnt["n"] += 1
                for hf in range(2):
                    i = nb()
                    for c in range(2):
                        mm(ps[i][:, :], acte[p_][:, c * CB + s2 * 128: c * CB + (s2 + 1) * 128], wdE[:, c * D + hf * 512: c * D + (hf + 1) * 512],
                           c == 0, c == 1, [b_acte[p_], bwd], [pb[i]], c == 1)
                    if hf == 0:
                        K.op(ACT, lambda: a_.copy(out=yb[:, 0:512], in_=ps[i][:, :]), [pb[i]], [byb])
                    else:
                        K.op(DVE, lambda: v_.tensor_copy(out=yb[:, 512:1024], in_=ps[i][:, :]), [pb[i]], [byb])
                r0 = blk * CB + s2 * 128
                sp_dma(ys_d[r0:r0 + 128, :], yb[:], r=[byb], sw=[b_ys])

        precast(3 * E)
        load_wE(0, (0, 1, 2))
        if NBLK > 1:
            load_wE(1, (0, 1, 2))
        for b0 in range(min(3, NBLK)):
            load_xs(b0)
        stage_T(0)
        if NBLK > 1:
            stage_T(1)
        stage_GU(0)
        for blk in range(NBLK):
            if blk + 2 < NBLK:
                load_wE(blk + 2, (0, 1))
                stage_T(blk + 2)
                if blk + 3 < NBLK:
                    load_xs(blk + 3)
            if blk >= 1:
                stage_D(blk - 1)
                if blk + 1 < NBLK:
                    load_wE(blk + 1, (2,))
            if blk + 1 < NBLK:
                stage_GU(blk + 1)
        stage_D(NBLK - 1)
        K.barrier()

        al.off = mU
        gfb = al.alloc("gfb", [128, NSEQ, D], F32)
        bcvc = al.alloc("bcvc", [128, D], F32)
        b_bcvc = Buf()
        sp_dma(bcvc[:], bcv_d[:, 2, :], w=[b_bcvc])
        b_gfb = Buf()
        for b in range(NSEQ):
            sp_dma(gfb[:, b, :], modd[b:b + 1, 5 * D:6 * D].partition_broadcast(128), r=[b_modd], w=[b_gfb])
            K.op(DVE, lambda: v_.tensor_tensor(out=gfb[:, b, :], in0=gfb[:, b, :], in1=bcvc[:], op=ALU.mult), [b_gfb, b_bcvc], [b_gfb])
        x1c = [al.alloc("x1c", [128, D], F32) for _ in range(2)]
        zc = [al.alloc("zc", [128, D], F32) for _ in range(2)]
        yg = [al.alloc("yg", [128, D], BF16) for _ in range(12)]
        shc = [al.alloc("shc", [128, D], BF16) for _ in range(2)]
        b_shc = [Buf(), Buf()]
        jkc = al.alloc("jkc", [128, D], BF16)
        rc_ = [al.alloc("rc", [128, 4], F32) for _ in range(2)]
        b_x1c, b_zc, b_rc = [Buf(), Buf()], [Buf(), Buf()], [Buf(), Buf()]
        b_yg = [Buf() for _ in range(12)]
        b_jkc = Buf()
        b_out = Buf()
        z2 = [al.alloc("z2", [128, D], F32) for _ in range(2)]
        b_z2 = [Buf(), Buf()]
        z3 = [al.alloc("z3", [128, D], F32) for _ in range(2)]
        b_z3 = [Buf(), Buf()]
        gc = {"n": 0}

        dg = [al.alloc("dg", [128, 128], BF16) for _ in range(12)]
        b_dg = [Buf() for _ in range(12)]
        rot["l"] = list(range(8))
        cps = {}

        def c_A(T):
            p_ = T % 2
            r0 = T * 128
            sp_dma(x1c[p_][:], x1_d[r0:r0 + 128, :], w=[b_x1c[p_]])
            sp_dma(shc[p_][:], sh_d[r0:r0 + 128, :], w=[b_shc[p_]])
            ys_ = []
            for k_ in range(TOPK):
                y_ = yg[gc["n"] % 12]
                by = b_yg[gc["n"] % 12]
                d_ = dg[gc["n"] % 12]
                bd = b_dg[gc["n"] % 12]
                gc["n"] += 1
                K.dma(K.qpool, lambda: g_.indirect_dma_start(out=y_[:], out_offset=None, in_=ys_d[:, :],
                                                             in_offset=bass.IndirectOffsetOnAxis(ap=desti[:, T, k_:k_ + 1], axis=0),
                                                             bounds_check=reg_slot, oob_is_err=False),
                      [b_ys, b_desti_t[T]], [by])
                K.op(ACT, lambda: a_.activation(out=d_[:], in_=ident_f, func=AF.Identity, scale=wkn[:, T, k_:k_ + 1]),
                     [b_const, b_route], [bd])
                ys_.append((y_, by, d_, bd))
            banks = [nb(), nb()]
            cps[T] = banks
            for hf in range(2):
                i = banks[hf]
                cs_ = slice(hf * 512, (hf + 1) * 512)
                mm(ps[i][:, :], ident_b, shc[p_][:, cs_], True, False, [b_const, b_shc[p_]], [pb[i]], False)
                for k_ in range(TOPK):
                    y_, by, d_, bd = ys_[k_]
                    mm(ps[i][:, :], d_[:], y_[:, cs_], False, k_ == TOPK - 1, [bd, by], [pb[i]], k_ == TOPK - 1)

        def c_B(T):
            b = T // NT_S
            p_ = T % 2
            r0 = T * 128
            banks = cps.pop(T)
            for hf in range(2):
                K.op(ACT, lambda: a_.activation(out=jkc[:, hf * 512:(hf + 1) * 512], in_=ps[banks[hf]][:, :], func=AF.Square,
                                                accum_out=rc_[p_][:, hf:hf + 1]), [pb[banks[hf]]], [b_jkc, b_rc[p_]])
            K.op(DVE, lambda: v_.tensor_tensor(out=rc_[p_][:, 3:4], in0=rc_[p_][:, 0:1], in1=rc_[p_][:, 1:2], op=ALU.add), [b_rc[p_]], [b_rc[p_]])
            K.op(ACT, lambda: a_.activation(out=rc_[p_][:, 1:2], in_=rc_[p_][:, 3:4], func=AF.Sqrt, bias=EPS, scale=1.0 / D), [b_rc[p_]], [b_rc[p_]])
            K.op(DVE, lambda: v_.reciprocal(out=rc_[p_][:, 2:3], in_=rc_[p_][:, 1:2]), [b_rc[p_]], [b_rc[p_]])
            for hf in range(2):
                cs_ = slice(hf * 512, (hf + 1) * 512)
                K.op(DVE, lambda: v_.scalar_tensor_tensor(out=zc[p_][:, cs_], in0=ps[banks[hf]][:, :], scalar=rc_[p_][:, 2:3], in1=gfb[:, b, cs_],
                                                          op0=ALU.mult, op1=ALU.mult), [pb[banks[hf]], b_rc[p_], b_gfb], [b_zc[p_]])
            K.op(DVE, lambda: v_.tensor_tensor(out=zc[p_][:], in0=zc[p_][:], in1=x1c[p_][:], op=ALU.add), [b_zc[p_], b_x1c[p_]], [b_zc[p_]])
            sp_dma(out_d[r0:r0 + 128, :], zc[p_][:], r=[b_zc[p_]], sw=[b_out])

        c_A(0)
        for T in range(NT):
            if T + 1 < NT:
                c_A(T + 1)
            c_B(T)
        K.barrier()
    return nc


def _bf16():
    import ml_dtypes
    return ml_dtypes.bfloat16


def make_consts(NBLK):
    NCF = 128 + 128 + 64 + NBLK + 2
    cf = np.zeros((128, NCF), np.float32)
    cf[:, 0:128] = np.eye(128, dtype=np.float32)
    cf[127, 128:256] = 1.0
    cf[:, 256:320] = np.arange(64, dtype=np.float32)[None, :]
    cf[:, 320:320 + NBLK] = np.arange(NBLK, dtype=np.float32)[None, :]
    cf[:, 320 + NBLK] = np.arange(128, dtype=np.float32)
    cf[:, 321 + NBLK] = 1.0
    cb = np.zeros((128, 1792), np.float32)
    cb[:, 0:128] = np.eye(128)
    k = np.arange(128)[:, None]
    m = np.arange(128)[None, :]
    cb[:, 128:256] = (k < m)
    cb[:, 256:384] = (m >= k)
    cb[:, 384:512] = 1.0
    for h in range(NH):
        for g3 in range(3):
            cb[g3 * 32 + h, 512 + h * 128:512 + (h + 1) * 128] = 1.0
    cb[:, 1536:1664] = np.where(m < k, -30000.0, 0.0)
    cb[:, 1664:1792] = (m == (k + 64) % 128)
    return cf, cb.astype(_bf16())


def prep_shared(inp):
    f = np.float32
    sh = {}
    sh["w_ada"] = np.ascontiguousarray(inp["w_ada"][0], f)
    fm = np.zeros((128, 9, 8), f)

    def fmaj(v):
        return np.asarray(v, f).reshape(8, 128).T
    fm[:, 0, :] = fmaj(inp["g_pre_mix"][0])
    for j in range(4):
        fm[:, 1 + j, :] = fmaj(inp["w_conv"][0, j])
    fm[:, 5, :] = fmaj(inp["b_conv"][0])
    fm[:, 6, :] = fmaj(inp["b_rg"][0])
    fm[:, 7, :] = fmaj(inp["b_ig"][0])
    fm[:, 8, :] = fmaj(inp["rglru_lambda"][0])
    sh["fm"] = fm
    bcv = np.zeros((128, 3, D), f)
    bcv[:, 0, :] = np.asarray(inp["g_post_mix"][0], f)[None, :]
    bcv[:, 1, :] = np.asarray(inp["g_pre_ffn"][0], f)[None, :]
    bcv[:, 2, :] = np.asarray(inp["g_post_ffn"][0], f)[None, :]
    sh["bcv"] = bcv
    sh["rbias"] = np.ascontiguousarray(np.broadcast_to(np.asarray(inp["router_bias"][0], f)[None, :], (128, E)))
    sh["b_forget"] = np.asarray(inp["b_forget"][0], f).reshape(NH, 1)
    sh["w_in"] = np.ascontiguousarray(inp["w_in"][0], f)
    for nm, src in (("wrg_bd", inp["w_rg"][0]), ("wig_bd", inp["w_ig"][0])):
        bd = np.zeros((8, 128, 128), f)
        for c in range(8):
            bd[c, 0:64, 0:64] = src[2 * c]
            bd[c, 64:128, 64:128] = src[2 * c + 1]
        sh[nm] = bd
    sh["w_ba"] = np.ascontiguousarray(inp["w_branch_attn"][0], f)
    sh["w_br"] = np.ascontiguousarray(inp["w_branch_rnn"][0], f)
    sh["w_out"] = np.ascontiguousarray(inp["w_out"][0], f)
    sh["w_router"] = np.ascontiguousarray(inp["w_router"][0], f)
    sh["w_sg"] = np.ascontiguousarray(inp["w_sh_gate"][0], f)
    sh["w_su"] = np.ascontiguousarray(inp["w_sh_up"][0], f)
    sh["w_sd"] = np.ascontiguousarray(inp["w_sh_down"][0], f)
    wg = np.asarray(inp["w_exp_gate"][0], f).reshape(E, 8, 128, FF).transpose(0, 2, 1, 3)
    sh["wpg"] = np.ascontiguousarray(wg).reshape(E * 128, 2048)
    wu = np.asarray(inp["w_exp_up"][0], f).reshape(E, 8, 128, FF).transpose(0, 2, 1, 3)
    sh["wpu"] = np.ascontiguousarray(wu).reshape(E * 128, 2048)
    wd = np.asarray(inp["w_exp_down"][0], f).reshape(E, 2, 128, D).transpose(0, 2, 1, 3)
    sh["wpd"] = np.ascontiguousarray(wd).reshape(E * 128, 2048)
    return sh


def prep_core(inp, sh, core, NSEQ, S, NBLK):
    f = np.float32
    m = dict(sh)
    xs = np.asarray(inp["x"][core * NSEQ:(core + 1) * NSEQ], f).reshape(NSEQ * S, D)
    m["x"] = np.ascontiguousarray(xs)
    c = np.asarray(inp["c"][core * NSEQ:(core + 1) * NSEQ], f)
    m["csT"] = np.ascontiguousarray(c.T.reshape(8, 128, NSEQ).transpose(1, 0, 2))
    m["b_ada_rep"] = np.ascontiguousarray(np.broadcast_to(np.asarray(inp["b_ada"][0], f)[None, :], (NSEQ, 6 * D)))
    cf, cb = make_consts(NBLK)
    m["cf"] = cf
    m["cb"] = cb
    return m


def kernel(**inputs):
    B, S = inputs["x"].shape[0], inputs["x"].shape[1]
    NSEQ = B // NCORES
    NTOK = NSEQ * S
    NBLK = (NTOK * TOPK) // CB + E
    nc = build(NSEQ, S, skip_reload=True)
    sh = prep_shared(inputs)
    in_maps = [prep_core(inputs, sh, i, NSEQ, S, NBLK) for i in range(NCORES)]
    res = run_bass_kernel_spmd(nc, in_maps, core_ids=list(range(NCORES)))
    outs = [np.asarray(r["out"], np.float32).reshape(NSEQ, S, D) for r in res.results]
    return np.concatenate(outs, axis=0)
```

```python
import numpy as np
import concourse.bass as bass
import concourse.mybir as mybir
from concourse.bass_utils import run_bass_kernel_spmd
from contextlib import ExitStack

F32 = mybir.dt.float32
BF16 = mybir.dt.bfloat16
I32 = mybir.dt.int32
U32 = mybir.dt.uint32
AF = mybir.ActivationFunctionType
ALU = mybir.AluOpType
AX = mybir.AxisListType

D = 1024
NH = 8
E = 64
TOPK = 6
FF = 256
CB = 256
INC = 5640
OQ, OK_, OV, OF_, OX, OG, OGA, OGR = 0, 512, 1024, 1536, 1544, 2568, 3592, 4616
EPS = 1e-6
BIG = 1.0e4
NCORES = 8
ARENA_SHIFT = [0]
ARENA_MAX = [0]


class Buf:
    __slots__ = ("w", "r", "name")

    def __init__(self, name=""):
        self.w = {}
        self.r = {}
        self.name = name


class Eng:
    def __init__(self, name, e, sem, key):
        self.name = name
        self.e = e
        self.sem = sem
        self.key = key
        self.n = 0
        self.seen = {}
        self.pending = False


class DQ:
    def __init__(self, eng, sems):
        self.eng = eng
        self.sems = sems
        self.cnt = [0] * len(sems)
        self.next = 0


def _merge(d, s):
    for k, v in s.items():
        if d.get(k, 0) < v:
            d[k] = v


class KB:
    def __init__(self, nc, stack):
        self.nc = nc
        self.semtab = {}
        self.engs = []
        for nm, e in (("pe", nc.tensor), ("act", nc.scalar), ("dve", nc.vector),
                      ("pool", nc.gpsimd), ("sp", nc.sync)):
            sem = stack.enter_context(nc.semaphore("s_" + nm))
            eng = Eng(nm, e, sem, "c_" + nm)
            self.semtab[eng.key] = sem
            setattr(self, nm, eng)
            self.engs.append(eng)
        self.queues = []
        for nm, eng, n in (("qsp", self.sp, 8), ("qpool", self.pool, 6)):
            sems = []
            for i in range(n):
                key = "d_%s%d" % (nm, i)
                sem = stack.enter_context(nc.semaphore(key))
                self.semtab[key] = sem
                sems.append((sem, key))
            q = DQ(eng, sems)
            setattr(self, nm, q)
            self.queues.append(q)

    def _wait(self, E_, deps):
        for k, v in deps.items():
            if E_.seen.get(k, 0) < v:
                E_.e.wait_ge(self.semtab[k], v)
                E_.seen[k] = v

    def op(self, E_, fn, reads=(), writes=(), inc=True):
        deps = {}
        for b in reads:
            _merge(deps, b.w)
        for b in writes:
            _merge(deps, b.w)
            _merge(deps, b.r)
        if E_.name == "pe":
            deps.pop(E_.key, None)
        self._wait(E_, deps)
        ins = fn()
        ev = E_.n + 1
        for b in reads:
            if b.r.get(E_.key, 0) < ev:
                b.r[E_.key] = ev
        for b in writes:
            b.w = {E_.key: ev}
            b.r = {}
        if inc:
            E_.n = ev
            ins.then_inc(E_.sem, 1)
            E_.pending = False
        else:
            E_.pending = True
        return ins

    def dma(self, Q, fn, reads=(), writes=(), swrites=()):
        E_ = Q.eng
        deps = {}
        for b in reads:
            _merge(deps, b.w)
        for b in writes:
            _merge(deps, b.w)
            _merge(deps, b.r)
        for b in swrites:
            _merge(deps, b.r)
        slot = Q.next
        Q.next = (Q.next + 1) % len(Q.sems)
        sem, key = Q.sems[slot]
        if Q.cnt[slot] > 0:
            if deps.get(key, 0) < 16 * Q.cnt[slot]:
                deps[key] = 16 * Q.cnt[slot]
        self._wait(E_, deps)
        ins = fn()
        Q.cnt[slot] += 1
        v = 16 * Q.cnt[slot]
        ins.then_inc(sem, 16)
        for b in reads:
            if b.r.get(key, 0) < v:
                b.r[key] = v
        for b in writes:
            b.w = {key: v}
            b.r = {}
        for b in swrites:
            if b.w.get(key, 0) < v:
                b.w[key] = v
        return ins

    def barrier(self):
        tot = {}
        for E_ in self.engs:
            assert not E_.pending
            if E_.n > 0:
                tot[E_.key] = E_.n
        for Q in self.queues:
            for i, (sem, key) in enumerate(Q.sems):
                if Q.cnt[i] > 0:
                    tot[key] = 16 * Q.cnt[i]
        for E_ in self.engs:
            self._wait(E_, dict(tot))


class Arena:
    def __init__(self, nc, limit):
        self.nc = nc
        self.off = 0
        self.limit = limit
        self.n = 0

    def alloc(self, name, shape, dtype):
        sz = 1
        for s in shape[1:]:
            sz *= s
        sz *= {F32: 4, BF16: 2, I32: 4, U32: 4}[dtype]
        sz = (sz + 63) // 64 * 64
        off = self.off
        assert off + sz <= self.limit, ("SBUF arena overflow", name, off, sz)
        self.off += sz
        self.n += 1
        ARENA_MAX[0] = max(ARENA_MAX[0], self.off)
        return self.nc.alloc_sbuf_tensor_at("%s_%d" % (name, self.n), list(shape), dtype, offset=off)

    def mark(self):
        return self.off

    def release(self, m):
        self.off = m


def build(NSEQ, S, skip_reload=True):
    NT_S = S // 128
    NTOK = NSEQ * S
    NT = NTOK // 128
    QB = min(512, S)
    TQ = QB // 128
    NQ = S // QB
    NB5 = S // QB
    NBLK = (NTOK * TOPK) // CB + E
    NSLOT = NBLK * CB

    nc = bass.Bass("TRN2", target_bir_lowering=False)
    dt = nc.dram_tensor

    def ein(name, shape, dtype=F32):
        return dt(name, list(shape), dtype, kind="ExternalInput")

    x_d = ein("x", [NTOK, D])
    cs_d = ein("csT", [128, 8, NSEQ])
    wada_d = ein("w_ada", [D, 6 * D])
    bada_d = ein("b_ada_rep", [NSEQ, 6 * D])
    fm_d = ein("fm", [128, 9, 8])
    bcv_d = ein("bcv", [128, 3, D])
    rb_d = ein("rbias", [128, E])
    bf_d = ein("b_forget", [NH, 1])
    win_d = ein("w_in", [D, INC])
    wrg_d = ein("wrg_bd", [8, 128, 128])
    wig_d = ein("wig_bd", [8, 128, 128])
    wba_d = ein("w_ba", [512, D])
    wbr_d = ein("w_br", [D, D])
    wout_d = ein("w_out", [D, D])
    wr_d = ein("w_router", [D, E])
    wsg_d = ein("w_sg", [D, FF])
    wsu_d = ein("w_su", [D, FF])
    wsd_d = ein("w_sd", [FF, D])
    wpg_d = ein("wpg", [E * 128, 2048])
    wpu_d = ein("wpu", [E * 128, 2048])
    wpd_d = ein("wpd", [E * 128, 2048])
    NCF = 128 + 128 + 64 + NBLK + 2
    cf_d = ein("cf", [128, NCF])
    cb_d = ein("cb", [128, 1792], BF16)
    out_d = dt("out", [NTOK, D], F32, kind="ExternalOutput")
    modd = dt("modd", [NSEQ, 6 * D], F32)
    h2_d = dt("h2s", [NTOK, D], BF16)
    x1_d = dt("x1s", [NTOK, D], F32)
    sh_d = dt("shs", [NTOK, D], BF16)
    xs_d = dt("xss", [NSLOT, D], BF16)
    ys_d = dt("yss", [NSLOT, D], BF16)
    wpb_d = [dt("wpb%d" % m_, [E * 128, 2048], BF16) for m_ in range(3)]

    stack = ExitStack()
    with stack:
        K = KB(nc, stack)
        al = Arena(nc, int(nc._sbuf_addr_for_side("right")) - 64)
        al.off = (int(nc._sbuf_addr_for_side("left")) + 63) // 64 * 64 + ARENA_SHIFT[0]
        ps = [stack.enter_context(nc.psum_tensor("ps%d" % i, [128, 512], F32)) for i in range(8)]
        pb = [Buf("pb%d" % i) for i in range(8)]
        rot = {"l": list(range(8)), "i": 0}

        def nb():
            i = rot["l"][rot["i"] % len(rot["l"])]
            rot["i"] += 1
            return i

        PE, ACT, DVE, POOL = K.pe, K.act, K.dve, K.pool
        reg_slot = nc.gpsimd.alloc_register("bc_slot")
        nc.gpsimd.reg_mov(reg_slot, NSLOT - 1)
        reg_w = nc.gpsimd.alloc_register("bc_w")
        nc.gpsimd.reg_mov(reg_w, E * 128 - 1)
        v_, a_, g_, t_ = nc.vector, nc.scalar, nc.gpsimd, nc.tensor

        def mm(out, lhsT, rhs, start, stop, r, w, inc):
            return K.op(PE, lambda: t_.matmul(out, lhsT, rhs, start=start, stop=stop), r, w, inc)

        def tr(out, in_, ident, r, w, inc):
            return K.op(PE, lambda: t_.transpose(out, in_, ident), r, w, inc)

        def sp_dma(out, in_, r=(), w=(), sw=()):
            return K.dma(K.qsp, lambda: nc.sync.dma_start(out=out, in_=in_), r, w, sw)

        def pl_dma(out, in_, r=(), w=(), sw=()):
            return K.dma(K.qpool, lambda: nc.gpsimd.dma_start(out=out, in_=in_), r, w, sw)

        cf = al.alloc("cf", [128, NCF], F32)
        cbt = al.alloc("cb", [128, 1792], BF16)
        b_const = Buf("const")
        ident_f = cf[:, 0:128]
        sel127 = cf[:, 128:256]
        iota64 = cf[:, 256:320]
        iotab = cf[:, 320:320 + NBLK]
        pidx = cf[:, 320 + NBLK:321 + NBLK]
        ones_c = cf[:, 321 + NBLK:322 + NBLK]
        ident_b = cbt[:, 0:128]
        triu_b = cbt[:, 128:256]
        trim_b = cbt[:, 256:384]
        ones_b = cbt[:, 384:512]
        negm_b = cbt[:, 1536:1664]
        swap_b = cbt[:, 1664:1792]
        fm = al.alloc("fm", [128, 9, 8], F32)
        sm = al.alloc("sm", [128, 6, 8], F32)
        rbias = al.alloc("rbias", [128, E], F32)
        nbf = al.alloc("nbf", [128, 2], F32)
        b_nbf = Buf()
        gsmT = al.alloc("gsmT", [128, 8, NSEQ], F32)
        shmT = al.alloc("shmT", [128, 8, NSEQ], F32)
        run = al.alloc("run", [128, E], F32)
        rankm = al.alloc("rankm", [128, NT, E], F32)
        eidx = al.alloc("eidx", [128, NT, 8], F32)
        wk = al.alloc("wk", [128, NT, 8], F32)
        wkn = al.alloc("wkn", [128, NT, 8], F32)
        desti = al.alloc("desti", [128, NT, 8], I32)
        widx = al.alloc("widx", [128, NBLK], I32)
        b_fm, b_sm, b_gs, b_run, b_route = Buf(), Buf(), Buf(), Buf(), Buf()
        b_widx = Buf()
        zt = al.alloc("zt", [128, 2, D], BF16)
        b_zt, b_xs0 = Buf(), Buf()
        K.op(POOL, lambda: g_.memset(zt[:], 0.0), [], [b_zt])
        zf = {"n": 0}
        ZF_PER = -(-NBLK // NT)

        b_wcast = Buf()
        pcast = {"n": 0}
        PC_PER = -(-(3 * E) // (NSEQ * 4 * NQ))

        def precast(cnt):
            for _ in range(cnt):
                if pcast["n"] < 3 * E:
                    e_, m_ = pcast["n"] // 3, pcast["n"] % 3
                    src = (wpg_d, wpu_d, wpd_d)[m_]
                    pl_dma(wpb_d[m_][e_ * 128:(e_ + 1) * 128, :], src[e_ * 128:(e_ + 1) * 128, :], sw=[b_wcast])
                    pcast["n"] += 1

        def zero_fill(cnt):
            for _ in range(cnt):
                if zf["n"] < NBLK:
                    r0_ = zf["n"] * CB
                    sp_dma(xs_d[r0_:r0_ + CB, :].rearrange("(s p) d -> p s d", p=128), zt[:], r=[b_zt], sw=[b_xs0])
                    zf["n"] += 1

        sp_dma(cf[:], cf_d.ap(), w=[b_const])
        sp_dma(cbt[:], cb_d.ap(), w=[b_const])
        sp_dma(fm[:], fm_d.ap(), w=[b_fm])
        sp_dma(rbias[:], rb_d.ap(), w=[b_const])
        K.op(DVE, lambda: v_.memset(nbf[:], 0.0), [], [b_nbf])
        for g3 in range(3):
            sp_dma(nbf[g3 * 32:g3 * 32 + NH, 0:1], bf_d.ap(), w=[b_nbf])
        K.op(DVE, lambda: v_.memset(run[:], 0.0), [], [b_run])

        m0 = al.mark()
        cs = al.alloc("cs", [128, 8, NSEQ], F32)
        th0 = al.alloc("th0", [128, 8, NSEQ], F32)
        siluT = al.alloc("siluT", [128, 8, NSEQ], BF16)
        modt = al.alloc("modt", [NSEQ, 6 * D], F32)
        bada = al.alloc("bada", [NSEQ, 6 * D], F32)
        wada = [al.alloc("wada", [128, 8, 512], BF16) for _ in range(2)]
        b_cs, b_th0, b_silu, b_modt, b_bada = Buf(), Buf(), Buf(), Buf(), Buf()
        b_wada = [Buf(), Buf()]

        K.op(DVE, lambda: v_.tensor_scalar(out=nbf[0:72, 1:2], in0=nbf[0:72, 0:1], scalar1=-1.0, scalar2=None,
                                           op0=ALU.mult), [b_nbf], [b_nbf])
        K.op(ACT, lambda: a_.activation(out=sm[:, 4, :], in_=fm[:, 8, :], func=AF.Exp, scale=-1.0), [b_fm], [b_sm])
        K.op(ACT, lambda: a_.activation(out=sm[:, 5, :], in_=sm[:, 4, :], func=AF.Ln, bias=1.0, scale=1.0), [b_sm], [b_sm])
        K.op(DVE, lambda: v_.tensor_scalar(out=sm[:, 0, :], in0=sm[:, 5, :], scalar1=-8.0, scalar2=None, op0=ALU.mult), [b_sm], [b_sm])
        K.op(DVE, lambda: v_.tensor_scalar(out=sm[:, 1, :], in0=sm[:, 5, :], scalar1=-4.0, scalar2=None, op0=ALU.mult), [b_sm], [b_sm])
        K.op(DVE, lambda: v_.tensor_scalar(out=sm[:, 2, :], in0=fm[:, 6, :], scalar1=0.5, scalar2=None, op0=ALU.mult), [b_fm, b_sm], [b_sm])
        K.op(DVE, lambda: v_.tensor_scalar(out=sm[:, 3, :], in0=fm[:, 7, :], scalar1=0.5, scalar2=None, op0=ALU.mult), [b_fm, b_sm], [b_sm])
        cneg = sm[:, 0, :]
        hcneg = sm[:, 1, :]
        hbrg = sm[:, 2, :]
        hbig = sm[:, 3, :]

        sp_dma(cs[:], cs_d.ap(), w=[b_cs])
        sp_dma(bada[:], bada_d.ap(), w=[b_bada])
        K.op(ACT, lambda: a_.activation(out=th0[:], in_=cs[:], func=AF.Tanh, scale=0.5), [b_cs], [b_th0])
        K.op(DVE, lambda: v_.scalar_tensor_tensor(out=th0[:], in0=th0[:], scalar=1.0, in1=cs[:], op0=ALU.add, op1=ALU.mult),
             [b_cs, b_th0], [b_th0])
        K.op(DVE, lambda: v_.tensor_scalar(out=siluT[:], in0=th0[:], scalar1=0.5, scalar2=None, op0=ALU.mult), [b_th0], [b_silu])
        for g in range(12):
            wb = wada[g % 2]
            bw = b_wada[g % 2]
            pl_dma(wb[:], wada_d[:, g * 512:(g + 1) * 512].rearrange("(kc p) n -> p kc n", p=128), w=[bw])
            i = nb()
            for kc in range(8):
                mm(ps[i][0:NSEQ, :], siluT[:, kc, :], wb[:, kc, :], kc == 0, kc == 7, [b_silu, bw], [pb[i]], kc == 7)
            K.op(DVE, lambda: v_.tensor_tensor(out=modt[:, g * 512:(g + 1) * 512], in0=ps[i][0:NSEQ, :],
                                               in1=bada[:, g * 512:(g + 1) * 512], op=ALU.add), [pb[i], b_bada], [b_modt])
        b_modd = Buf()
        sp_dma(modd.ap(), modt[:], r=[b_modt], w=[b_modd])
        i = nb()
        pT0 = ps[i][:, 0:16 * NSEQ].rearrange("p (a b) -> p a b", b=NSEQ)
        for kc in range(16):
            col = (D + kc * 128) if kc < 8 else ((kc - 8) * 128)
            tr(pT0[:, kc, :], modt[0:NSEQ, col:col + 128], ident_f[0:NSEQ, 0:NSEQ], [b_modt, b_const], [pb[i]], kc == 15)
        K.op(DVE, lambda: v_.scalar_tensor_tensor(out=gsmT[:], in0=pT0[:, 0:8, :], scalar=1.0,
                                                  in1=fm[:, 0, :].unsqueeze(2).to_broadcast([128, 8, NSEQ]),
                                                  op0=ALU.add, op1=ALU.mult), [pb[i], b_fm], [b_gs])
        K.op(DVE, lambda: v_.tensor_copy(out=shmT[:], in_=pT0[:, 8:16, :]), [pb[i]], [b_gs])
        K.barrier()
        al.release(m0)

        off_hT = al.off
        hT = al.alloc("hT", [128, 8, S], BF16)
        yaT = al.alloc("yaT", [128, 4, S], BF16)
        off_yrT = al.off
        yrT = al.alloc("yrT", [128, 8, S], BF16)
        LIMIT = al.limit
        b_hT = [Buf() for _ in range(NT_S)]
        b_yaT, b_yrT = Buf(), Buf()
        mU = al.mark()

        for b in range(NSEQ):
            tok0 = b * S
            al.release(mU)
            x_sb = [al.alloc("x_sb", [128, D], F32) for _ in range(2)]
            xn = [al.alloc("xn", [128, D], BF16) for _ in range(2)]
            jk = al.alloc("jk", [128, D], BF16)
            st = [al.alloc("st", [128, 4], F32) for _ in range(2)]
            b_x, b_xn, b_st, b_jk = [Buf(), Buf()], [Buf(), Buf()], [Buf(), Buf()], Buf()
            rot["l"] = list(range(8))
            def m1_A(t):
                xb, xnb, stb = x_sb[t % 2], xn[t % 2], st[t % 2]
                bx, bxn, bst = b_x[t % 2], b_xn[t % 2], b_st[t % 2]
                sp_dma(xb[:], x_d[tok0 + t * 128: tok0 + (t + 1) * 128, :], w=[bx])
                zero_fill(ZF_PER)
                K.op(ACT, lambda: a_.activation(out=jk[:], in_=xb[:], func=AF.Square, accum_out=stb[:, 0:1]), [bx], [b_jk, bst])
                K.op(ACT, lambda: a_.activation(out=stb[:, 1:2], in_=stb[:, 0:1], func=AF.Sqrt, bias=EPS, scale=1.0 / D), [bst], [bst])
                K.op(DVE, lambda: v_.reciprocal(out=stb[:, 2:3], in_=stb[:, 1:2]), [bst], [bst])
                K.op(ACT, lambda: a_.activation(out=xnb[:], in_=xb[:], func=AF.Identity, scale=stb[:, 2:3]), [bx, bst], [bxn])

            def m1_B(t):
                xnb, bxn = xn[t % 2], b_xn[t % 2]
                i = nb()
                pT = ps[i][:, :].bitcast(BF16)
                for kc in range(8):
                    tr(pT[:, kc * 128:(kc + 1) * 128], xnb[:, kc * 128:(kc + 1) * 128], ident_b, [bxn, b_const], [pb[i]], kc == 7)
                for kc in range(8):
                    o_ap = hT[:, kc, t * 128:(t + 1) * 128]
                    i_ap = pT[:, kc * 128:(kc + 1) * 128]
                    if kc % 2 == 0:
                        K.op(ACT, lambda: a_.activation(out=o_ap, in_=i_ap, func=AF.Identity, bias=shmT[:, kc, b:b + 1],
                                                        scale=gsmT[:, kc, b:b + 1]), [pb[i], b_gs], [b_hT[t]])
                    else:
                        K.op(DVE, lambda: v_.tensor_scalar(out=o_ap, in0=i_ap, scalar1=gsmT[:, kc, b:b + 1],
                                                           scalar2=shmT[:, kc, b:b + 1], op0=ALU.mult, op1=ALU.add),
                             [pb[i], b_gs], [b_hT[t]])
            m1_A(0)
            for t in range(NT_S):
                if t + 1 < NT_S:
                    m1_A(t + 1)
                m1_B(t)
            K.barrier()

            al.release(mU)
            qT = al.alloc("qT", [128, 4, 2, S], BF16)
            kT = al.alloc("kT", [128, 4, S], BF16)
            sv_ = al.off
            al.off = off_yrT
            vS = al.alloc("vS", [128, NT_S, 4, 192], BF16)
            ef = al.alloc("ef", [72, S], F32)
            assert al.off <= mU
            al.off = sv_
            off_wq = al.off
            wq = al.alloc("wq", [128, 8, 512], BF16)
            wkk = al.alloc("wk", [128, 8, 512], BF16)
            wv = al.alloc("wv", [128, 8, 512], BF16)
            wf = al.alloc("wf", [128, 8, 72], BF16)
            caug = al.alloc("caug", [128, S], BF16)
            tmpb = al.alloc("tmpb", [72, S], BF16)
            b_caug, b_tmpb = Buf(), Buf()
            Lf = al.alloc("Lf", [72, S], F32)
            cumLT = al.alloc("cumLT", [128, NT_S, NH], F32)
            b_Rb, b_Rs = Buf(), Buf()
            b_q, b_k, b_v, b_wq, b_wk, b_wv, b_wf = Buf(), Buf(), Buf(), Buf(), Buf(), Buf(), Buf()
            b_ef, b_L, b_cumLT = Buf(), Buf(), Buf()
            b_PT = [Buf() for _ in range(6)]

            def wview(c0, n):
                return win_d[:, c0:c0 + n].rearrange("(kc p) n -> p kc n", p=128)
            K.op(POOL, lambda: g_.memset(vS[:, :, :, 64:128], 1.0), [], [b_v])
            K.op(POOL, lambda: g_.memset(qT[:], 0.0), [], [b_q])
            pl_dma(wq[:], wview(OQ, 512), w=[b_wq])
            pl_dma(wkk[:], wview(OK_, 512), w=[b_wk])
            pl_dma(wv[:], wview(OV, 512), w=[b_wv])
            K.op(POOL, lambda: g_.memset(wf[:], 0.0), [], [b_wf])
            K.op(POOL, lambda: g_.memset(caug[:], 0.0), [], [b_caug])
            for g3 in range(3):
                pl_dma(wf[:, :, g3 * 32:g3 * 32 + NH], wview(OF_, 8), w=[b_wf])
            rot["l"] = list(range(8))
            cnt = 0
            for n in range(NQ):
                i = nb()
                for kc in range(8):
                    mm(ps[i][0:72, 0:QB], wf[:, kc, :], hT[:, kc, n * QB:(n + 1) * QB], kc == 0, kc == 7,
                       [b_wf] + b_hT[n * TQ:(n + 1) * TQ], [pb[i]], kc == 7)
                K.op(ACT, lambda: a_.activation(out=ef[:, n * QB:(n + 1) * QB], in_=ps[i][0:72, 0:QB], func=AF.Exp,
                                                bias=nbf[0:72, 1:2], scale=-1.0), [pb[i], b_nbf], [b_ef])
            K.op(ACT, lambda: a_.activation(out=Lf[:], in_=ef[:], func=AF.Ln, bias=1.0, scale=1.0), [b_ef], [b_L])
            K.op(DVE, lambda: v_.tensor_tensor_scan(out=ef[:], data0=ones_c[0:72, 0:1].to_broadcast([72, S]), data1=Lf[:],
                                                    initial=0.0, op0=ALU.mult, op1=ALU.add), [b_L, b_const, b_ef], [b_ef])
            K.op(DVE, lambda: v_.tensor_scalar(out=Lf[:], in0=ef[:], scalar1=-8.0, scalar2=None, op0=ALU.mult), [b_ef, b_L], [b_L])
            K.op(DVE, lambda: v_.tensor_copy(out=tmpb[:], in_=Lf[:]), [b_L], [b_tmpb])
            K.op(DVE, lambda: v_.tensor_copy(out=caug[0:NH, :], in_=tmpb[0:NH, :]), [b_tmpb], [b_caug])
            K.op(DVE, lambda: v_.tensor_tensor(out=Lf[:], in0=Lf[:], in1=tmpb[:], op=ALU.subtract), [b_L, b_tmpb], [b_L])
            K.op(DVE, lambda: v_.tensor_copy(out=tmpb[:], in_=Lf[:]), [b_L, b_caug], [b_tmpb])
            K.op(DVE, lambda: v_.tensor_copy(out=caug[32:32 + NH, :], in_=tmpb[32:32 + NH, :]), [b_tmpb], [b_caug])
            K.op(DVE, lambda: v_.tensor_tensor(out=Lf[:], in0=Lf[:], in1=tmpb[:], op=ALU.subtract), [b_L, b_tmpb], [b_L])
            K.op(DVE, lambda: v_.tensor_copy(out=caug[64:64 + NH, :], in_=Lf[64:64 + NH, :]), [b_L], [b_caug])
            for (wt, bw, dst, bd, isq) in ((wq, b_wq, qT, b_q, True), (wkk, b_wk, kT, b_k, False)):
                for j in range(4):
                    for n in range(NQ):
                        i = nb()
                        for kc in range(8):
                            mm(ps[i][:, 0:QB], wt[:, kc, j * 128:(j + 1) * 128], hT[:, kc, n * QB:(n + 1) * QB], kc == 0, kc == 7,
                               [bw] + b_hT[n * TQ:(n + 1) * TQ], [pb[i]], kc == 7)
                        cols = slice(n * QB, (n + 1) * QB)
                        if isq:
                            K.op(ACT, lambda: a_.copy(out=qT[0:64, j, 0, cols], in_=ps[i][0:64, 0:QB]), [pb[i]], [bd])
                            K.op(DVE, lambda: v_.tensor_copy(out=qT[64:128, j, 1, cols], in_=ps[i][64:128, 0:QB]), [pb[i]], [bd])
                        elif cnt % 2 == 0:
                            K.op(ACT, lambda: a_.copy(out=kT[:, j, cols], in_=ps[i][:, 0:QB]), [pb[i]], [bd])
                        else:
                            K.op(DVE, lambda: v_.tensor_copy(out=kT[:, j, cols], in_=ps[i][:, 0:QB]), [pb[i]], [bd])
                        cnt += 1
            for t in range(NT_S):
                i = nb()
                for kc in range(8):
                    mm(ps[i][:, :], hT[:, kc, t * 128:(t + 1) * 128], wv[:, kc, :], kc == 0, kc == 7, [b_wv, b_hT[t]], [pb[i]], kc == 7)
                o_v = vS[:, t, :, :].rearrange("p j (c w) -> p j c w", w=64)[:, :, 0::2, :]
                i_v = ps[i][:, :].rearrange("p (j c w) -> p j c w", c=2, w=64)
                if t % 2 == 0:
                    K.op(ACT, lambda: a_.copy(out=o_v, in_=i_v), [pb[i]], [b_v])
                else:
                    K.op(DVE, lambda: v_.tensor_copy(out=o_v, in_=i_v), [pb[i]], [b_v])
            i = nb()
            pT1 = ps[i][:, 0:NT_S * NH].rearrange("p (a b) -> p a b", b=NH)
            for t in range(NT_S):
                tr(pT1[:, t, :], ef[0:NH, t * 128:(t + 1) * 128], ident_f[0:NH, 0:NH], [b_ef, b_const], [pb[i]], t == NT_S - 1)
            K.op(DVE, lambda: v_.tensor_copy(out=cumLT[:], in_=pT1), [pb[i]], [b_cumLT])

            K.barrier()
            sv3 = al.off
            al.off = off_wq
            PT = [al.alloc("PT", [128, QB], BF16) for _ in range(6)]
            Rb = al.alloc("Rb", [128, QB], BF16)
            Rs = al.alloc("Rs", [128, QB], F32)
            al.off = sv3
            rot["l"] = [4, 5, 6, 7]
            pcount = 0
            LAG = 3
            grp = {"n": 0}
            pend_backs = []

            def att_front(j, q, t, half, nt, gpar):
                nonlocal pcount
                d = t - q * TQ
                q0 = max(d, 0) * 128
                h = 2 * j + half
                rows = slice(half * 64, half * 64 + 64)
                i = nb()
                mm(ps[i][:, q0:QB], kT[:, j, t * 128:(t + 1) * 128], qT[:, j, half, q * QB + q0:(q + 1) * QB],
                   True, False, [b_q, b_k], [pb[i]], False)
                mm(ps[i][:, q0:QB], cbt[:, 512 + h * 128:512 + (h + 1) * 128], caug[:, q * QB + q0:(q + 1) * QB],
                   False, d < 0, [b_const, b_caug], [pb[i]], d < 0)
                if d >= 0:
                    mm(ps[i][:, q0:q0 + 128], ident_b, negm_b, False, True, [b_const], [pb[i]], True)
                pt = PT[pcount % 6]
                bpt = b_PT[pcount % 6]
                pcount += 1
                K.op(ACT, lambda: a_.activation(out=pt[:, q0:QB], in_=ps[i][:, q0:QB], func=AF.Exp,
                                                bias=cumLT[:, t, h:h + 1], scale=0.125), [pb[i], b_cumLT], [bpt])

                def back():
                    yi = half + 2 * gpar
                    ya_, yb_ = 2 * gpar, 2 * gpar + 1
                    lo = 0 if half == 0 else 64
                    mm(ps[yi][:, q0:QB], vS[:, t, j, lo:lo + 128], pt[:, q0:QB], t == 0, t == nt - 1,
                       [b_v, bpt], [pb[yi]], True)
                    if t == nt - 1 and half == 1:
                        K.op(DVE, lambda: v_.reciprocal(out=Rs[64:128, :], in_=ps[ya_][64:128, 0:QB]), [pb[ya_], b_Rs], [b_Rs])
                        K.op(DVE, lambda: v_.reciprocal(out=Rs[0:64, :], in_=ps[yb_][0:64, 0:QB]), [pb[yb_], b_Rs], [b_Rs])
                        K.op(DVE, lambda: v_.tensor_copy(out=Rb[:], in_=Rs[:]), [b_Rs, b_Rb], [b_Rb])
                        isw = nb()
                        mm(ps[isw][:, 0:QB], swap_b, Rb[:, :], True, True, [b_const, b_Rb], [pb[isw]], True)
                        K.op(ACT, lambda: a_.copy(out=Rs[:], in_=ps[isw][:, 0:QB]), [pb[isw]], [b_Rs])
                        K.op(DVE, lambda: v_.tensor_tensor(out=yaT[0:64, j, q * QB:(q + 1) * QB], in0=ps[ya_][0:64, 0:QB],
                                                           in1=Rs[0:64, :], op=ALU.mult), [pb[ya_], b_Rs], [b_yaT])
                        K.op(DVE, lambda: v_.tensor_tensor(out=yaT[64:128, j, q * QB:(q + 1) * QB], in0=ps[yb_][64:128, 0:QB],
                                                           in1=Rs[64:128, :], op=ALU.mult), [pb[yb_], b_Rs], [b_yaT])
                return back

            for j in range(4):
                for q in range(NQ):
                    nt = q * TQ + TQ
                    gpar = grp["n"] % 2
                    grp["n"] += 1
                    precast(PC_PER)
                    for t in range(nt):
                        for half in range(2):
                            pend_backs.append(att_front(j, q, t, half, nt, gpar))
                            if len(pend_backs) > LAG:
                                pend_backs.pop(0)()
            while pend_backs:
                pend_backs.pop(0)()
            K.barrier()

            al.release(mU)
            xp = [al.alloc("xp", [128, S + 4], F32) for _ in range(2)]
            uu = [al.alloc("uu", [128, S], F32) for _ in range(2)]
            ub = [al.alloc("ub", [128, S], BF16) for _ in range(2)]
            gg = [al.alloc("gg", [128, S], BF16) for _ in range(2)]
            thr = al.alloc("thr", [128, S], F32)
            thi = al.alloc("thi", [128, S], F32)
            e2 = al.alloc("e2", [128, S], F32)
            wx = [al.alloc("wx", [128, 8, 128], BF16) for _ in range(2)]
            wg = [al.alloc("wg", [128, 8, 128], BF16) for _ in range(2)]
            wrg = [al.alloc("wrg", [128, 128], BF16) for _ in range(2)]
            wig = [al.alloc("wig", [128, 128], BF16) for _ in range(2)]
            gt = [[al.alloc("gt", [128, QB], F32) for _ in range(2)] for _ in range(2)]
            b_xp, b_uu, b_ub, b_gg = [Buf(), Buf()], [Buf(), Buf()], [Buf(), Buf()], [Buf(), Buf()]
            b_thr, b_thi, b_e2 = Buf(), Buf(), Buf()
            b_w4 = [[Buf() for _ in range(4)] for _ in range(2)]
            b_gt = [[Buf() for _ in range(2)] for _ in range(2)]
            rot["l"] = list(range(8))
            for s2_ in range(2):
                K.op(DVE, lambda: v_.memset(xp[s2_][:, 0:4], 0.0), [], [b_xp[s2_]])

            def load_w4(c):
                s_ = c % 2
                pl_dma(wx[s_][:], wview(OX + c * 128, 128), w=[b_w4[s_][0]])
                pl_dma(wg[s_][:], wview(OG + c * 128, 128), w=[b_w4[s_][1]])
                pl_dma(wrg[s_][:], wrg_d[c, :, :], w=[b_w4[s_][2]])
                pl_dma(wig[s_][:], wig_d[c, :, :], w=[b_w4[s_][3]])

            gcnt4 = {"n": 0}

            def m4_A(c):
                s_ = c % 2
                bwx, bwg_, bwrg, bwig = b_w4[s_]
                xp_, uu_, ub_, gg_ = xp[s_], uu[s_], ub[s_], gg[s_]
                bxp, buu, bub, bgg = b_xp[s_], b_uu[s_], b_ub[s_], b_gg[s_]
                for n in range(NQ):
                    i = nb()
                    for kc in range(8):
                        mm(ps[i][:, 0:QB], wx[s_][:, kc, :], hT[:, kc, n * QB:(n + 1) * QB], kc == 0, kc == 7,
                           [bwx] + b_hT[n * TQ:(n + 1) * TQ], [pb[i]], kc == 7)
                    K.op(ACT, lambda: a_.copy(out=xp_[:, 3 + n * QB:3 + (n + 1) * QB], in_=ps[i][:, 0:QB]), [pb[i]], [bxp])
                K.op(DVE, lambda: v_.tensor_scalar(out=uu_[:], in0=xp_[:, 3:3 + S], scalar1=fm[:, 4, c:c + 1], scalar2=fm[:, 5, c:c + 1],
                                                   op0=ALU.mult, op1=ALU.add), [bxp, b_fm], [buu])
                for jj in range(3):
                    K.op(DVE, lambda: v_.scalar_tensor_tensor(out=uu_[:], in0=xp_[:, jj:jj + S], scalar=fm[:, 1 + jj, c:c + 1], in1=uu_[:],
                                                              op0=ALU.mult, op1=ALU.add), [bxp, b_fm, buu], [buu])
                K.op(POOL, lambda: g_.tensor_copy(out=ub_[:], in_=uu_[:]), [buu], [bub])
                for n in range(NQ):
                    i = nb()
                    for kc in range(8):
                        mm(ps[i][:, 0:QB], wg[s_][:, kc, :], hT[:, kc, n * QB:(n + 1) * QB], kc == 0, kc == 7,
                           [bwg_] + b_hT[n * TQ:(n + 1) * TQ], [pb[i]], kc == 7)
                    g0, g1 = gt[gcnt4["n"] % 2]
                    bg0, bg1 = b_gt[gcnt4["n"] % 2]
                    gcnt4["n"] += 1
                    pg = ps[i][:, 0:QB]
                    K.op(ACT, lambda: a_.activation(out=g0[:], in_=pg, func=AF.Square), [pb[i]], [bg0])
                    K.op(DVE, lambda: v_.tensor_scalar(out=g0[:], in0=g0[:], scalar1=0.044715, scalar2=1.0, op0=ALU.mult, op1=ALU.add),
                         [bg0], [bg0])
                    K.op(DVE, lambda: v_.tensor_tensor(out=g0[:], in0=g0[:], in1=pg, op=ALU.mult), [bg0, pb[i]], [bg0])
                    K.op(ACT, lambda: a_.activation(out=g1[:], in_=g0[:], func=AF.Tanh, scale=0.7978845608028654), [bg0], [bg1])
                    K.op(DVE, lambda: v_.scalar_tensor_tensor(out=gg_[:, n * QB:(n + 1) * QB], in0=g1[:], scalar=1.0, in1=pg,
                                                              op0=ALU.add, op1=ALU.mult), [bg1, pb[i]], [bgg])

            def m4_B(c):
                s_ = c % 2
                bwx, bwg_, bwrg, bwig = b_w4[s_]
                uu_, ub_, gg_ = uu[s_], ub[s_], gg[s_]
                buu, bub, bgg = b_uu[s_], b_ub[s_], b_gg[s_]
                for n in range(NQ):
                    i = nb()
                    mm(ps[i][:, 0:QB], wrg[s_][:, :], ub_[:, n * QB:(n + 1) * QB], True, True, [bwrg, bub], [pb[i]], True)
                    K.op(ACT, lambda: a_.activation(out=thr[:, n * QB:(n + 1) * QB], in_=ps[i][:, 0:QB], func=AF.Tanh,
                                                    bias=hbrg[:, c:c + 1], scale=0.5), [pb[i], b_sm], [b_thr])
                    i2 = nb()
                    mm(ps[i2][:, 0:QB], wig[s_][:, :], ub_[:, n * QB:(n + 1) * QB], True, True, [bwig, bub], [pb[i2]], True)
                    K.op(ACT, lambda: a_.activation(out=thi[:, n * QB:(n + 1) * QB], in_=ps[i2][:, 0:QB], func=AF.Tanh,
                                                    bias=hbig[:, c:c + 1], scale=0.5), [pb[i2], b_sm], [b_thi])
                K.op(ACT, lambda: a_.activation(out=e2[:], in_=thr[:], func=AF.Exp, bias=cneg[:, c:c + 1], scale=cneg[:, c:c + 1]),
                     [b_thr, b_sm], [b_e2])
                K.op(ACT, lambda: a_.activation(out=thr[:], in_=thr[:], func=AF.Exp, bias=hcneg[:, c:c + 1], scale=hcneg[:, c:c + 1]),
                     [b_thr, b_sm], [b_thr])
                K.op(DVE, lambda: v_.tensor_scalar(out=e2[:], in0=e2[:], scalar1=1.0 - 1.0e-7, scalar2=None, op0=ALU.min), [b_e2], [b_e2])
                K.op(ACT, lambda: a_.activation(out=e2[:], in_=e2[:], func=AF.Sqrt, bias=1.0, scale=-1.0), [b_e2], [b_e2])
                K.op(DVE, lambda: v_.scalar_tensor_tensor(out=thi[:], in0=thi[:], scalar=1.0, in1=e2[:], op0=ALU.add, op1=ALU.mult),
                     [b_thi, b_e2], [b_thi])
                K.op(DVE, lambda: v_.scalar_tensor_tensor(out=thi[:], in0=thi[:], scalar=0.5, in1=uu_[:], op0=ALU.mult, op1=ALU.mult),
                     [b_thi, buu], [b_thi])
                K.op(DVE, lambda: v_.tensor_tensor_scan(out=e2[:], data0=thr[:], data1=thi[:], initial=0.0, op0=ALU.mult, op1=ALU.add),
                     [b_thr, b_thi, b_e2], [b_e2])
                K.op(DVE, lambda: v_.scalar_tensor_tensor(out=yrT[:, c, :], in0=gg_[:], scalar=0.5, in1=e2[:], op0=ALU.mult, op1=ALU.mult),
                     [bgg, b_e2], [b_yrT])

            load_w4(0)
            load_w4(1)
            m4_A(0)
            for c in range(8):
                if c + 1 < 8:
                    m4_A(c + 1)
                m4_B(c)
                if c + 2 < 8:
                    load_w4(c + 2)
            K.barrier()

            al.release(mU)
            mgT = al.alloc("mgT", [128, 8, S], BF16)
            b_mg = [Buf() for _ in range(NT_S)]
            m5 = al.mark()
            w5 = [al.alloc("w5", [128, 28, 128], BF16) for _ in range(2)]
            b_w5 = [[Buf() for _ in range(4)] for _ in range(2)]
            sa = [[al.alloc("sa", [128, QB], F32) for _ in range(4)] for _ in range(2)]
            b_sa = [[Buf() for _ in range(4)] for _ in range(2)]
            rot["l"] = list(range(8))

            def load_w5(m):
                s_ = m % 2
                cs_ = slice(m * 128, (m + 1) * 128)
                pl_dma(w5[s_][:, 0:4, :], wba_d[:, cs_].rearrange("(kc p) n -> p kc n", p=128), w=[b_w5[s_][0]])
                pl_dma(w5[s_][:, 4:12, :], wbr_d[:, cs_].rearrange("(kc p) n -> p kc n", p=128), w=[b_w5[s_][1]])
                pl_dma(w5[s_][:, 12:20, :], wview(OGA + m * 128, 128), w=[b_w5[s_][2]])
                pl_dma(w5[s_][:, 20:28, :], wview(OGR + m * 128, 128), w=[b_w5[s_][3]])
            load_w5(0)
            scount = 0
            for m in range(8):
                s_ = m % 2
                bwa, bwr_, bwga, bwgr = b_w5[s_]
                if m + 1 < 8:
                    load_w5(m + 1)
                for n in range(NQ):
                    cols = slice(n * QB, (n + 1) * QB)
                    hb = b_hT[n * TQ:(n + 1) * TQ]
                    iA, iR, iGA, iGR = nb(), nb(), nb(), nb()
                    for kc in range(4):
                        mm(ps[iA][:, 0:QB], w5[s_][:, kc, :], yaT[:, kc, cols], kc == 0, kc == 3, [bwa, b_yaT], [pb[iA]], kc == 3)
                    for kc in range(8):
                        mm(ps[iGA][:, 0:QB], w5[s_][:, 12 + kc, :], hT[:, kc, cols], kc == 0, kc == 7, [bwga] + hb, [pb[iGA]], kc == 7)
                    for kc in range(8):
                        mm(ps[iR][:, 0:QB], w5[s_][:, 4 + kc, :], yrT[:, kc, cols], kc == 0, kc == 7, [bwr_, b_yrT], [pb[iR]], kc == 7)
                    for kc in range(8):
                        mm(ps[iGR][:, 0:QB], w5[s_][:, 20 + kc, :], hT[:, kc, cols], kc == 0, kc == 7, [bwgr] + hb, [pb[iGR]], kc == 7)
                    s0, s1, s2, s3 = sa[scount % 2]
                    c0, c1, c2, c3 = b_sa[scount % 2]
                    scount += 1
                    K.op(ACT, lambda: a_.activation(out=s0[:], in_=ps[iGA][:, 0:QB], func=AF.Sigmoid), [pb[iGA]], [c0])
                    K.op(DVE, lambda: v_.tensor_tensor(out=s1[:], in0=s0[:], in1=ps[iA][:, 0:QB], op=ALU.mult), [c0, pb[iA]], [c1])
                    K.op(ACT, lambda: a_.activation(out=s2[:], in_=ps[iGR][:, 0:QB], func=AF.Sigmoid), [pb[iGR]], [c2])
                    K.op(DVE, lambda: v_.tensor_tensor(out=s3[:], in0=s2[:], in1=ps[iR][:, 0:QB], op=ALU.mult), [c2, pb[iR]], [c3])
                    K.op(POOL, lambda: g_.tensor_tensor(out=mgT[:, m, cols], in0=s1[:], in1=s3[:], op=ALU.add), [c1, c3],
                         b_mg[n * TQ:(n + 1) * TQ])
            K.barrier()

            al.release(m5)
            offB = al.off
            useA = (mU - off_hT) >= 80 * 1024
            if useA:
                al.off = off_hT
                al.limit = mU
            wo = al.alloc("wo", [128, 8, D], BF16)
            h2Tb = [al.alloc("h2Tb", [128, 8, QB], BF16) for _ in range(2)]
            xs2 = [al.alloc("xs2", [128, D], F32) for _ in range(2)]
            x1 = [al.alloc("x1", [128, D], F32) for _ in range(2)]
            h2 = [al.alloc("h2", [128, D], F32) for _ in range(2)]
            h2T = [al.alloc("h2T", [128, 8, 128], F32) for _ in range(2)]
            shs = [al.alloc("shs", [128, D], BF16) for _ in range(2)]
            h2b = [al.alloc("h2b", [128, D], BF16) for _ in range(2)]
            if useA:
                al.off = offB
                al.limit = LIMIT
            wsg = al.alloc("wsg", [128, 8, FF], BF16)
            wsu = al.alloc("wsu", [128, 8, FF], BF16)
            wsd = al.alloc("wsd", [128, 2, D], BF16)
            wr = al.alloc("wr", [128, 8, E], F32)
            bcv5 = al.alloc("bcv5", [128, 2, D], F32)
            b_bcv5 = Buf()
            gmb = al.alloc("gmb", [128, D], F32)
            gsfb = al.alloc("gsfb", [128, D], F32)
            shfb = al.alloc("shfb", [128, D], F32)
            sg = [al.alloc("sg", [128, QB], F32) for _ in range(2)]
            actT = al.alloc("actT", [128, 2, QB], BF16)
            rs = [al.alloc("rs", [128, 8], F32) for _ in range(2)]
            rt_ = [al.alloc("rt", [128, 6, E], F32) for _ in range(2)]
            g8 = [al.alloc("g8", [128, 8, 8], F32) for _ in range(2)]
            r8 = [al.alloc("r8", [128, 4, 8], F32) for _ in range(2)]
            i8 = [al.alloc("i8", [128, 8], U32) for _ in range(2)]
            mkb = [al.alloc("mkb", [128, E], BF16) for _ in range(2)]
            b_wo, b_ws, b_wr, b_bc5 = Buf(), Buf(), Buf(), Buf()
            b_xs2, b_x1, b_h2, b_h2b, b_h2T = [Buf(), Buf()], [Buf(), Buf()], [Buf(), Buf()], [Buf(), Buf()], [Buf(), Buf()]
            b_h2Tb, b_shs, b_sg, b_act, b_rs, b_rt = [Buf(), Buf()], [Buf(), Buf()], [Buf(), Buf()], Buf(), [Buf(), Buf()], [Buf(), Buf()]
            b_jk5 = Buf()
            jk5 = al.alloc("jk5", [128, D], BF16)
            pl_dma(wo[:], wout_d.ap().rearrange("(kc p) n -> p kc n", p=128), w=[b_wo])
            b_wsg, b_wsu, b_wsd = Buf(), Buf(), Buf()
            pl_dma(wsg[:], wsg_d.ap().rearrange("(kc p) n -> p kc n", p=128), w=[b_wsg])
            pl_dma(wsu[:], wsu_d.ap().rearrange("(kc p) n -> p kc n", p=128), w=[b_wsu])
            pl_dma(wsd[:], wsd_d.ap().rearrange("(kc p) n -> p kc n", p=128), w=[b_wsd])
            sp_dma(wr[:], wr_d.ap().rearrange("(kc p) n -> p kc n", p=128), w=[b_wr])
            sp_dma(bcv5[:], bcv_d[:, 0:2, :], w=[b_bcv5])
            sp_dma(gmb[:], modd[b:b + 1, 2 * D:3 * D].partition_broadcast(128), r=[b_modd], w=[b_bc5])
            sp_dma(gsfb[:], modd[b:b + 1, 4 * D:5 * D].partition_broadcast(128), r=[b_modd], w=[b_bc5])
            sp_dma(shfb[:], modd[b:b + 1, 3 * D:4 * D].partition_broadcast(128), r=[b_modd], w=[b_bc5])
            K.op(DVE, lambda: v_.tensor_tensor(out=gmb[:], in0=gmb[:], in1=bcv5[:, 0, :], op=ALU.mult), [b_bc5, b_bcv5], [b_bc5])
            K.op(DVE, lambda: v_.scalar_tensor_tensor(out=gsfb[:], in0=gsfb[:], scalar=1.0, in1=bcv5[:, 1, :], op0=ALU.add, op1=ALU.mult),
                 [b_bc5, b_bcv5], [b_bc5])
            rot["l"] = list(range(8))
            def m5_A(t):
                n, tt = t // TQ, t % TQ
                T = b * NT_S + t
                r0 = tok0 + t * 128
                p_ = t % 2
                xb, x1b, h2_, h2b_, h2T_, rs_ = xs2[p_], x1[p_], h2[p_], h2b[p_], h2T[p_], rs[p_]
                bxb, bx1, bh2, bh2b, bh2T, brs = b_xs2[p_], b_x1[p_], b_h2[p_], b_h2b[p_], b_h2T[p_], b_rs[p_]
                sp_dma(xb[:], x_d[r0:r0 + 128, :], w=[bxb])
                io = [nb(), nb()]
                for hf in range(2):
                    for kc in range(8):
                        mm(ps[io[hf]][:, :], mgT[:, kc, t * 128:(t + 1) * 128], wo[:, kc, hf * 512:(hf + 1) * 512], kc == 0, kc == 7,
                           [b_mg[t], b_wo], [pb[io[hf]]], kc == 7)
                for hf in range(2):
                    K.op(ACT, lambda: a_.activation(out=jk5[:, hf * 512:(hf + 1) * 512], in_=ps[io[hf]][:, :], func=AF.Square,
                                                    accum_out=rs_[:, hf:hf + 1]), [pb[io[hf]]], [b_jk5, brs])
                K.op(DVE, lambda: v_.tensor_tensor(out=rs_[:, 2:3], in0=rs_[:, 0:1], in1=rs_[:, 1:2], op=ALU.add), [brs], [brs])
                K.op(ACT, lambda: a_.activation(out=rs_[:, 3:4], in_=rs_[:, 2:3], func=AF.Sqrt, bias=EPS, scale=1.0 / D), [brs], [brs])
                K.op(DVE, lambda: v_.reciprocal(out=rs_[:, 4:5], in_=rs_[:, 3:4]), [brs], [brs])
                for hf in range(2):
                    cs_ = slice(hf * 512, (hf + 1) * 512)
                    K.op(DVE, lambda: v_.scalar_tensor_tensor(out=x1b[:, cs_], in0=ps[io[hf]][:, :], scalar=rs_[:, 4:5], in1=gmb[:, cs_],
                                                              op0=ALU.mult, op1=ALU.mult), [pb[io[hf]], brs, b_bc5], [bx1])
                K.op(POOL, lambda: g_.tensor_tensor(out=x1b[:], in0=x1b[:], in1=xb[:], op=ALU.add), [bx1, bxb], [bx1])
                sp_dma(x1_d[r0:r0 + 128, :], x1b[:], r=[bx1])
                K.op(ACT, lambda: a_.activation(out=jk5[:], in_=x1b[:], func=AF.Square, accum_out=rs_[:, 5:6]), [bx1], [b_jk5, brs])
                K.op(ACT, lambda: a_.activation(out=rs_[:, 6:7], in_=rs_[:, 5:6], func=AF.Sqrt, bias=EPS, scale=1.0 / D), [brs], [brs])
                K.op(DVE, lambda: v_.reciprocal(out=rs_[:, 7:8], in_=rs_[:, 6:7]), [brs], [brs])
                K.op(DVE, lambda: v_.scalar_tensor_tensor(out=h2_[:], in0=x1b[:], scalar=rs_[:, 7:8], in1=gsfb[:], op0=ALU.mult, op1=ALU.mult),
                     [bx1, brs, b_bc5], [bh2])
                K.op(POOL, lambda: g_.tensor_tensor(out=h2_[:], in0=h2_[:], in1=shfb[:], op=ALU.add), [bh2, b_bc5], [bh2])
                K.op(POOL, lambda: g_.tensor_copy(out=h2b_[:], in_=h2_[:]), [bh2], [bh2b])
                sp_dma(h2_d[r0:r0 + 128, :], h2b_[:], r=[bh2b])

            def m5_B(t):
                n, tt = t // TQ, t % TQ
                hb_ = h2Tb[n % 2]
                bhb = b_h2Tb[n % 2]
                T = b * NT_S + t
                p_ = t % 2
                h2_, h2T_ = h2[p_], h2T[p_]
                bh2, bh2T = b_h2[p_], b_h2T[p_]
                it = [nb(), nb()]
                for kc in range(8):
                    ib = it[kc // 4]
                    tr(ps[ib][:, (kc % 4) * 128:(kc % 4 + 1) * 128], h2_[:, kc * 128:(kc + 1) * 128], ident_f, [bh2, b_const], [pb[ib]],
                       kc % 4 == 3)
                for hf in range(2):
                    K.op(ACT, lambda: a_.copy(out=h2T_[:, hf * 4:(hf + 1) * 4, :], in_=ps[it[hf]][:, :].rearrange("p (a b) -> p a b", b=128)),
                         [pb[it[hf]]], [bh2T])
                K.op(POOL, lambda: g_.tensor_copy(out=hb_[:, :, tt * 128:(tt + 1) * 128], in_=h2T_[:]), [bh2T], [bhb])
                il = nb()
                for kc in range(8):
                    mm(ps[il][:, 0:E], h2T_[:, kc, :], wr[:, kc, :], kc == 0, kc == 7, [bh2T, b_wr], [pb[il]], kc == 7)
                R_, g8_, r8_, i8_, mk_ = rt_[p_], g8[p_], r8[p_], i8[p_], mkb[p_]
                brt = b_rt[p_]
                sc, sel, selm, mkf, wfull, tmp = (R_[:, k_, :] for k_ in range(6))
                K.op(ACT, lambda: a_.activation(out=sc, in_=ps[il][:, 0:E], func=AF.Sigmoid), [pb[il]], [brt])
                K.op(DVE, lambda: v_.tensor_tensor(out=sel, in0=sc, in1=rbias[:], op=ALU.add), [brt, b_const], [brt])
                for gg in range(8):
                    K.op(DVE, lambda: v_.max(out=g8_[:, gg, :], in_=sel[:, gg * 8:(gg + 1) * 8]), [brt], [brt])
                K.op(DVE, lambda: v_.tensor_tensor(out=r8_[:, 0, :], in0=g8_[:, :, 0], in1=g8_[:, :, 1], op=ALU.add), [brt], [brt])
                K.op(DVE, lambda: v_.max(out=r8_[:, 1, :], in_=r8_[:, 0, :]), [brt], [brt])
                K.op(DVE, lambda: v_.tensor_scalar(out=r8_[:, 2, :], in0=r8_[:, 0, :], scalar1=r8_[:, 1, 3:4], scalar2=-BIG,
                                                   op0=ALU.is_lt, op1=ALU.mult), [brt], [brt])
                K.op(DVE, lambda: v_.tensor_tensor(out=selm.rearrange("p (a b) -> p a b", b=8), in0=sel.rearrange("p (a b) -> p a b", b=8),
                                                   in1=r8_[:, 2, :].unsqueeze(2).to_broadcast([128, 8, 8]), op=ALU.add), [brt], [brt])
                K.op(DVE, lambda: v_.max(out=r8_[:, 3, :], in_=selm), [brt], [brt])
                K.op(DVE, lambda: v_.max_index(out=i8_[:], in_max=r8_[:, 3, :], in_values=selm), [brt], [brt])
                K.op(DVE, lambda: v_.tensor_scalar(out=mkf, in0=selm, scalar1=r8_[:, 3, 5:6], scalar2=None, op0=ALU.is_ge), [brt], [brt])
                K.op(DVE, lambda: v_.tensor_tensor(out=wfull, in0=sc, in1=mkf, op=ALU.mult), [brt], [brt])
                K.op(DVE, lambda: v_.tensor_reduce(out=r8_[:, 2, 0:1], in_=wfull, axis=AX.X, op=ALU.add), [brt], [brt])
                K.op(DVE, lambda: v_.reciprocal(out=r8_[:, 2, 1:2], in_=r8_[:, 2, 0:1]), [brt], [brt])
                K.op(POOL, lambda: g_.tensor_copy(out=mk_[:], in_=mkf), [brt], [brt])
                ik = nb()
                mm(ps[ik][:, 0:E], triu_b, mk_[:], True, True, [brt, b_const], [pb[ik]], False)
                mm(ps[ik][:, E:2 * E], ones_b, mk_[:], True, True, [brt, b_const], [pb[ik]], True)
                K.op(DVE, lambda: v_.tensor_tensor(out=tmp, in0=ps[ik][:, 0:E], in1=run[:], op=ALU.add), [pb[ik], b_run, brt], [brt])
                K.op(DVE, lambda: v_.tensor_tensor(out=rankm[:, T, :], in0=tmp, in1=mkf, op=ALU.mult), [brt], [b_route])
                K.op(DVE, lambda: v_.tensor_tensor(out=run[:], in0=run[:], in1=ps[ik][:, E:2 * E], op=ALU.add), [pb[ik], b_run], [b_run])
                K.op(DVE, lambda: v_.tensor_copy(out=eidx[:, T, :], in_=i8_[:]), [brt], [b_route])
                for k_ in range(TOPK):
                    K.op(DVE, lambda: v_.scalar_tensor_tensor(out=tmp, in0=iota64, scalar=eidx[:, T, k_:k_ + 1], in1=wfull,
                                                              op0=ALU.is_equal, op1=ALU.mult, accum_out=wk[:, T, k_:k_ + 1]),
                         [brt, b_route, b_const], [brt, b_route])
                K.op(DVE, lambda: v_.tensor_scalar(out=wkn[:, T, 0:TOPK], in0=wk[:, T, 0:TOPK], scalar1=r8_[:, 2, 1:2], scalar2=2.5,
                                                   op0=ALU.mult, op1=ALU.mult), [brt, b_route], [b_route])

            def m5_SH(n):
                hb_ = h2Tb[n % 2]
                bhb = b_h2Tb[n % 2]
                ig = [nb(), nb()]
                iu = [nb(), nb()]
                for c in range(2):
                    for kc in range(8):
                        mm(ps[ig[c]][:, 0:QB], wsg[:, kc, c * 128:(c + 1) * 128], hb_[:, kc, :], kc == 0, kc == 7, [b_wsg, bhb], [pb[ig[c]]], kc == 7)
                    for kc in range(8):
                        mm(ps[iu[c]][:, 0:QB], wsu[:, kc, c * 128:(c + 1) * 128], hb_[:, kc, :], kc == 0, kc == 7, [b_wsu, bhb], [pb[iu[c]]], kc == 7)
                    sg_ = sg[c]
                    K.op(ACT, lambda: a_.activation(out=sg_[:], in_=ps[ig[c]][:, 0:QB], func=AF.Sigmoid), [pb[ig[c]]], [b_sg[c]])
                    K.op(DVE, lambda: v_.tensor_tensor(out=sg_[:], in0=sg_[:], in1=ps[ig[c]][:, 0:QB], op=ALU.mult), [b_sg[c], pb[ig[c]]], [b_sg[c]])
                    K.op(DVE, lambda: v_.tensor_tensor(out=actT[:, c, :], in0=sg_[:], in1=ps[iu[c]][:, 0:QB], op=ALU.mult),
                         [b_sg[c], pb[iu[c]]], [b_act])
                for tt in range(TQ):
                    t = n * TQ + tt
                    r0 = tok0 + t * 128
                    iy = [nb(), nb()]
                    for hf in range(2):
                        for c in range(2):
                            mm(ps[iy[hf]][:, :], actT[:, c, tt * 128:(tt + 1) * 128], wsd[:, c, hf * 512:(hf + 1) * 512], c == 0, c == 1,
                               [b_act, b_wsd], [pb[iy[hf]]], c == 1)
                    sh_ = shs[t % 2]
                    K.op(ACT, lambda: a_.copy(out=sh_[:, 0:512], in_=ps[iy[0]][:, :]), [pb[iy[0]]], [b_shs[t % 2]])
                    K.op(DVE, lambda: v_.tensor_copy(out=sh_[:, 512:1024], in_=ps[iy[1]][:, :]), [pb[iy[1]]], [b_shs[t % 2]])
                    sp_dma(sh_d[r0:r0 + 128, :], sh_[:], r=[b_shs[t % 2]])

            m5_A(0)
            for t in range(NT_S):
                if t + 1 < NT_S:
                    m5_A(t + 1)
                m5_B(t)
                if t % TQ == TQ - 1:
                    m5_SH(t // TQ)
            K.barrier()

        al.release(mU)
        al.off = mU
        pe_ = al.alloc("pend", [128, 4, E], F32)
        pei = al.alloc("pei", [128, E], I32)
        ebf = al.alloc("ebf", [128, 2, NBLK], F32)
        djk = al.alloc("djk", [128, E], F32)
        rkp = al.alloc("rkp", [128, E], F32)
        destf = al.alloc("destf", [128, NT, 8], F32)
        b_pe, b_eb, b_dj, b_rkp, b_destf, b_desti = Buf(), Buf(), Buf(), Buf(), Buf(), Buf()
        b_desti_t = [Buf() for _ in range(NT)]
        K.op(DVE, lambda: v_.tensor_scalar(out=pe_[:, 3, :], in0=run[:], scalar1=float(CB - 1), scalar2=None, op0=ALU.add), [b_run], [b_pe])
        K.op(DVE, lambda: v_.tensor_copy(out=pei[:], in_=pe_[:, 3, :]), [b_pe], [b_pe])
        K.op(DVE, lambda: v_.tensor_single_scalar(out=pei[:], in_=pei[:], scalar=8, op=ALU.arith_shift_right), [b_pe], [b_pe])
        K.op(DVE, lambda: v_.tensor_copy(out=pe_[:, 0, :], in_=pei[:]), [b_pe], [b_pe])
        K.op(DVE, lambda: v_.tensor_tensor_scan(out=pe_[:, 1, :], data0=ones_c[:, 0:1].to_broadcast([128, E]), data1=pe_[:, 0, :],
                                                initial=0.0, op0=ALU.mult, op1=ALU.add), [b_pe, b_const], [b_pe])
        K.op(DVE, lambda: v_.tensor_tensor(out=pe_[:, 2, :], in0=pe_[:, 1, :], in1=pe_[:, 0, :], op=ALU.subtract), [b_pe], [b_pe])
        K.op(DVE, lambda: v_.tensor_scalar(out=pe_[:, 2, :], in0=pe_[:, 2, :], scalar1=float(CB), scalar2=None, op0=ALU.mult), [b_pe], [b_pe])
        for T in range(NT):
            K.op(DVE, lambda: v_.tensor_tensor(out=rkp[:], in0=rankm[:, T, :], in1=pe_[:, 2, :], op=ALU.add), [b_route, b_pe, b_rkp], [b_rkp])
            for k_ in range(TOPK):
                K.op(DVE, lambda: v_.scalar_tensor_tensor(out=djk[:], in0=iota64, scalar=eidx[:, T, k_:k_ + 1], in1=rkp[:],
                                                          op0=ALU.is_equal, op1=ALU.mult, accum_out=destf[:, T, k_:k_ + 1]),
                     [b_route, b_rkp, b_const], [b_dj, b_destf])
            K.op(DVE, lambda: v_.tensor_copy(out=desti[:, T, 0:TOPK], in_=destf[:, T, 0:TOPK]), [b_destf], [b_desti_t[T]])
        hg = [al.alloc("hg", [128, D], BF16) for _ in range(3)]
        b_hg = [Buf() for _ in range(3)]
        b_xs = Buf()
        for T in range(NT):
            hb_ = hg[T % 3]
            sp_dma(hb_[:], h2_d[T * 128:(T + 1) * 128, :], w=[b_hg[T % 3]])
            for k_ in range(TOPK):
                K.dma(K.qpool, lambda: g_.indirect_dma_start(out=xs_d[:, :], out_offset=bass.IndirectOffsetOnAxis(ap=desti[:, T, k_:k_ + 1], axis=0),
                                                             in_=hb_[:], in_offset=None, bounds_check=reg_slot, oob_is_err=False),
                      [b_hg[T % 3], b_desti_t[T], b_xs0], [], [b_xs])
        K.op(DVE, lambda: v_.memset(ebf[:, 0, :], 0.0), [], [b_eb])
        for e_ in range(E):
            K.op(DVE, lambda: v_.scalar_tensor_tensor(out=ebf[:, 0, :], in0=iotab, scalar=pe_[:, 1, e_:e_ + 1], in1=ebf[:, 0, :],
                                                      op0=ALU.is_ge, op1=ALU.add), [b_pe, b_const, b_eb], [b_eb])
        K.op(DVE, lambda: v_.tensor_scalar(out=ebf[:, 0, :], in0=ebf[:, 0, :], scalar1=float(E - 1), scalar2=128.0, op0=ALU.min, op1=ALU.mult),
             [b_eb], [b_eb])
        K.op(DVE, lambda: v_.tensor_scalar(out=ebf[:, 1, :], in0=ebf[:, 0, :], scalar1=pidx, scalar2=None, op0=ALU.add), [b_eb, b_const], [b_eb])
        if skip_reload and NBLK > 2:
            K.op(DVE, lambda: v_.tensor_tensor(out=ebf[:, 0, 2:NBLK], in0=ebf[:, 0, 2:NBLK], in1=ebf[:, 1, 0:NBLK - 2], op=ALU.subtract),
                 [b_eb], [b_eb])
            K.op(DVE, lambda: v_.tensor_scalar(out=ebf[:, 0, 2:NBLK], in0=ebf[:, 0, 2:NBLK], scalar1=pidx, scalar2=None, op0=ALU.add),
                 [b_eb, b_const], [b_eb])
            K.op(DVE, lambda: v_.tensor_scalar(out=ebf[:, 0, 2:NBLK], in0=ebf[:, 0, 2:NBLK], scalar1=0.0, scalar2=1.0e6,
                                               op0=ALU.is_equal, op1=ALU.mult), [b_eb], [b_eb])
            K.op(DVE, lambda: v_.tensor_tensor(out=ebf[:, 1, 2:NBLK], in0=ebf[:, 1, 2:NBLK], in1=ebf[:, 0, 2:NBLK], op=ALU.add), [b_eb], [b_eb])
        K.op(DVE, lambda: v_.tensor_copy(out=widx[:], in_=ebf[:, 1, :]), [b_eb], [b_widx])
        K.barrier()

        al.off = mU
        wE = [[al.alloc("wE", [128, 2048], BF16) for _ in range(3)] for _ in range(2)]
        b_wE = [[Buf() for _ in range(3)] for _ in range(2)]
        xsb = [al.alloc("xsb", [128, 2, D], BF16) for _ in range(3)]
        xTe = [al.alloc("xTe", [128, 8, CB], BF16) for _ in range(3)]
        sge = [al.alloc("sge", [128, 2 * CB], F32) for _ in range(2)]
        acte = [al.alloc("acte", [128, 2 * CB], BF16) for _ in range(2)]
        ysb = [al.alloc("ysb", [128, D], BF16) for _ in range(4)]
        b_xsb, b_xTe, b_sge, b_acte = [Buf(), Buf(), Buf()], [Buf(), Buf(), Buf()], [Buf(), Buf()], [Buf(), Buf()]
        b_ysb = [Buf() for _ in range(4)]
        b_ys = Buf()
        rot["l"] = list(range(8))
        wsrc = wpb_d

        def load_wE(blk, which):
            s_ = blk % 2
            for m in which:
                K.dma(K.qpool, lambda: g_.indirect_dma_start(out=wE[s_][m][:], out_offset=None, in_=wsrc[m][:, :],
                                                             in_offset=bass.IndirectOffsetOnAxis(ap=widx[:, blk:blk + 1], axis=0),
                                                             bounds_check=reg_w, oob_is_err=False),
                      [b_widx, b_wcast], [b_wE[s_][m]])

        def load_xs(blk):
            p_ = blk % 3
            sp_dma(xsb[p_][:], xs_d[blk * CB:(blk + 1) * CB, :].rearrange("(s p) d -> p s d", p=128), r=[b_xs], w=[b_xsb[p_]])

        def stage_T(blk):
            p_ = blk % 3
            for s2 in range(2):
                i = nb()
                pT = ps[i][:, :].bitcast(BF16)
                for kc in range(8):
                    tr(pT[:, kc * 128:(kc + 1) * 128], xsb[p_][:, s2, kc * 128:(kc + 1) * 128], ident_b, [b_xsb[p_], b_const], [pb[i]], kc == 7)
                o_ap = xTe[p_][:, :, s2 * 128:(s2 + 1) * 128]
                i_ap = pT.rearrange("p (a b) -> p a b", b=128)
                if s2 == 0:
                    K.op(ACT, lambda: a_.copy(out=o_ap, in_=i_ap), [pb[i]], [b_xTe[p_]])
                else:
                    K.op(DVE, lambda: v_.tensor_copy(out=o_ap, in_=i_ap), [pb[i]], [b_xTe[p_]])

        def stage_GU(blk):
            s_ = blk % 2
            p_ = blk % 2
            x_ = blk % 3
            wgE, wuE, wdE = wE[s_]
            bwg, bwu, bwd = b_wE[s_]
            ig_, iu_ = nb(), nb()
            for c in range(2):
                for kc in range(8):
                    mm(ps[ig_][:, c * CB:(c + 1) * CB], wgE[:, kc * FF + c * 128: kc * FF + (c + 1) * 128], xTe[x_][:, kc, :], kc == 0, kc == 7,
                       [bwg, b_xTe[x_]], [pb[ig_]], kc == 7)
            for c in range(2):
                for kc in range(8):
                    mm(ps[iu_][:, c * CB:(c + 1) * CB], wuE[:, kc * FF + c * 128: kc * FF + (c + 1) * 128], xTe[x_][:, kc, :], kc == 0, kc == 7,
                       [bwu, b_xTe[x_]], [pb[iu_]], kc == 7)
            K.op(ACT, lambda: a_.activation(out=sge[p_][:], in_=ps[ig_][:, :], func=AF.Sigmoid), [pb[ig_]], [b_sge[p_]])
            K.op(DVE, lambda: v_.tensor_tensor(out=sge[p_][:], in0=sge[p_][:], in1=ps[ig_][:, :], op=ALU.mult), [b_sge[p_], pb[ig_]], [b_sge[p_]])
            K.op(DVE, lambda: v_.tensor_tensor(out=acte[p_][:], in0=sge[p_][:], in1=ps[iu_][:, :], op=ALU.mult), [b_sge[p_], pb[iu_]], [b_acte[p_]])

        ycnt = {"n": 0}

        def stage_D(blk):
            s_ = blk % 2
            p_ = blk % 2
            wdE = wE[s_][2]
            bwd = b_wE[s_][2]
            for s2 in range(2):
                yb = ysb[ycnt["n"] % 4]
                byb = b_ysb[ycnt["n"] % 4]
                ycnt["n"] += 1
                for hf in range(2):
                    i = nb()
                    for c in range(2):
                        mm(ps[i][:, :], acte[p_][:, c * CB + s2 * 128: c * CB + (s2 + 1) * 128], wdE[:, c * D + hf * 512: c * D + (hf + 1) * 512],
                           c == 0, c == 1, [b_acte[p_], bwd], [pb[i]], c == 1)
                    if hf == 0:
                        K.op(ACT, lambda: a_.copy(out=yb[:, 0:512], in_=ps[i][:, :]), [pb[i]], [byb])
                    else:
                        K.op(DVE, lambda: v_.tensor_copy(out=yb[:, 512:1024], in_=ps[i][:, :]), [pb[i]], [byb])
                r0 = blk * CB + s2 * 128
                sp_dma(ys_d[r0:r0 + 128, :], yb[:], r=[byb], sw=[b_ys])

        precast(3 * E)
        load_wE(0, (0, 1, 2))
        if NBLK > 1:
            load_wE(1, (0, 1, 2))
        for b0 in range(min(3, NBLK)):
            load_xs(b0)
        stage_T(0)
        if NBLK > 1:
            stage_T(1)
        stage_GU(0)
        for blk in range(NBLK):
            if blk + 2 < NBLK:
                load_wE(blk + 2, (0, 1))
                stage_T(blk + 2)
                if blk + 3 < NBLK:
                    load_xs(blk + 3)
            if blk >= 1:
                stage_D(blk - 1)
                if blk + 1 < NBLK:
                    load_wE(blk + 1, (2,))
            if blk + 1 < NBLK:
                stage_GU(blk + 1)
        stage_D(NBLK - 1)
        K.barrier()

        al.off = mU
        gfb = al.alloc("gfb", [128, NSEQ, D], F32)
        bcvc = al.alloc("bcvc", [128, D], F32)
        b_bcvc = Buf()
        sp_dma(bcvc[:], bcv_d[:, 2, :], w=[b_bcvc])
        b_gfb = Buf()
        for b in range(NSEQ):
            sp_dma(gfb[:, b, :], modd[b:b + 1, 5 * D:6 * D].partition_broadcast(128), r=[b_modd], w=[b_gfb])
            K.op(DVE, lambda: v_.tensor_tensor(out=gfb[:, b, :], in0=gfb[:, b, :], in1=bcvc[:], op=ALU.mult), [b_gfb, b_bcvc], [b_gfb])
        x1c = [al.alloc("x1c", [128, D], F32) for _ in range(2)]
        zc = [al.alloc("zc", [128, D], F32) for _ in range(2)]
        yg = [al.alloc("yg", [128, D], BF16) for _ in range(12)]
        shc = [al.alloc("shc", [128, D], BF16) for _ in range(2)]
        b_shc = [Buf(), Buf()]
        jkc = al.alloc("jkc", [128, D], BF16)
        rc_ = [al.alloc("rc", [128, 4], F32) for _ in range(2)]
        b_x1c, b_zc, b_rc = [Buf(), Buf()], [Buf(), Buf()], [Buf(), Buf()]
        b_yg = [Buf() for _ in range(12)]
        b_jkc = Buf()
        b_out = Buf()
        z2 = [al.alloc("z2", [128, D], F32) for _ in range(2)]
        b_z2 = [Buf(), Buf()]
        z3 = [al.alloc("z3", [128, D], F32) for _ in range(2)]
        b_z3 = [Buf(), Buf()]
        gc = {"n": 0}

        dg = [al.alloc("dg", [128, 128], BF16) for _ in range(12)]
        b_dg = [Buf() for _ in range(12)]
        rot["l"] = list(range(8))
        cps = {}

        def c_A(T):
            p_ = T % 2
            r0 = T * 128
            sp_dma(x1c[p_][:], x1_d[r0:r0 + 128, :], w=[b_x1c[p_]])
            sp_dma(shc[p_][:], sh_d[r0:r0 + 128, :], w=[b_shc[p_]])
            ys_ = []
            for k_ in range(TOPK):
                y_ = yg[gc["n"] % 12]
                by = b_yg[gc["n"] % 12]
                d_ = dg[gc["n"] % 12]
                bd = b_dg[gc["n"] % 12]
                gc["n"] += 1
                K.dma(K.qpool, lambda: g_.indirect_dma_start(out=y_[:], out_offset=None, in_=ys_d[:, :],
                                                             in_offset=bass.IndirectOffsetOnAxis(ap=desti[:, T, k_:k_ + 1], axis=0),
                                                             bounds_check=reg_slot, oob_is_err=False),
                      [b_ys, b_desti_t[T]], [by])
                K.op(ACT, lambda: a_.activation(out=d_[:], in_=ident_f, func=AF.Identity, scale=wkn[:, T, k_:k_ + 1]),
                     [b_const, b_route], [bd])
                ys_.append((y_, by, d_, bd))
            banks = [nb(), nb()]
            cps[T] = banks
            for hf in range(2):
                i = banks[hf]
                cs_ = slice(hf * 512, (hf + 1) * 512)
                mm(ps[i][:, :], ident_b, shc[p_][:, cs_], True, False, [b_const, b_shc[p_]], [pb[i]], False)
                for k_ in range(TOPK):
                    y_, by, d_, bd = ys_[k_]
                    mm(ps[i][:, :], d_[:], y_[:, cs_], False, k_ == TOPK - 1, [bd, by], [pb[i]], k_ == TOPK - 1)

        def c_B(T):
            b = T // NT_S
            p_ = T % 2
            r0 = T * 128
            banks = cps.pop(T)
            for hf in range(2):
                K.op(ACT, lambda: a_.activation(out=jkc[:, hf * 512:(hf + 1) * 512], in_=ps[banks[hf]][:, :], func=AF.Square,
                                                accum_out=rc_[p_][:, hf:hf + 1]), [pb[banks[hf]]], [b_jkc, b_rc[p_]])
            K.op(DVE, lambda: v_.tensor_tensor(out=rc_[p_][:, 3:4], in0=rc_[p_][:, 0:1], in1=rc_[p_][:, 1:2], op=ALU.add), [b_rc[p_]], [b_rc[p_]])
            K.op(ACT, lambda: a_.activation(out=rc_[p_][:, 1:2], in_=rc_[p_][:, 3:4], func=AF.Sqrt, bias=EPS, scale=1.0 / D), [b_rc[p_]], [b_rc[p_]])
            K.op(DVE, lambda: v_.reciprocal(out=rc_[p_][:, 2:3], in_=rc_[p_][:, 1:2]), [b_rc[p_]], [b_rc[p_]])
            for hf in range(2):
                cs_ = slice(hf * 512, (hf + 1) * 512)
                K.op(DVE, lambda: v_.scalar_tensor_tensor(out=zc[p_][:, cs_], in0=ps[banks[hf]][:, :], scalar=rc_[p_][:, 2:3], in1=gfb[:, b, cs_],
                                                          op0=ALU.mult, op1=ALU.mult), [pb[banks[hf]], b_rc[p_], b_gfb], [b_zc[p_]])
            K.op(DVE, lambda: v_.tensor_tensor(out=zc[p_][:], in0=zc[p_][:], in1=x1c[p_][:], op=ALU.add), [b_zc[p_], b_x1c[p_]], [b_zc[p_]])
            sp_dma(out_d[r0:r0 + 128, :], zc[p_][:], r=[b_zc[p_]], sw=[b_out])

        c_A(0)
        for T in range(NT):
            if T + 1 < NT:
                c_A(T + 1)
            c_B(T)
        K.barrier()
    return nc


def _bf16():
    import ml_dtypes
    return ml_dtypes.bfloat16


def make_consts(NBLK):
    NCF = 128 + 128 + 64 + NBLK + 2
    cf = np.zeros((128, NCF), np.float32)
    cf[:, 0:128] = np.eye(128, dtype=np.float32)
    cf[127, 128:256] = 1.0
    cf[:, 256:320] = np.arange(64, dtype=np.float32)[None, :]
    cf[:, 320:320 + NBLK] = np.arange(NBLK, dtype=np.float32)[None, :]
    cf[:, 320 + NBLK] = np.arange(128, dtype=np.float32)
    cf[:, 321 + NBLK] = 1.0
    cb = np.zeros((128, 1792), np.float32)
    cb[:, 0:128] = np.eye(128)
    k = np.arange(128)[:, None]
    m = np.arange(128)[None, :]
    cb[:, 128:256] = (k < m)
    cb[:, 256:384] = (m >= k)
    cb[:, 384:512] = 1.0
    for h in range(NH):
        for g3 in range(3):
            cb[g3 * 32 + h, 512 + h * 128:512 + (h + 1) * 128] = 1.0
    cb[:, 1536:1664] = np.where(m < k, -30000.0, 0.0)
    cb[:, 1664:1792] = (m == (k + 64) % 128)
    return cf, cb.astype(_bf16())


def prep_shared(inp):
    f = np.float32
    sh = {}
    sh["w_ada"] = np.ascontiguousarray(inp["w_ada"][0], f)
    fm = np.zeros((128, 9, 8), f)

    def fmaj(v):
        return np.asarray(v, f).reshape(8, 128).T
    fm[:, 0, :] = fmaj(inp["g_pre_mix"][0])
    for j in range(4):
        fm[:, 1 + j, :] = fmaj(inp["w_conv"][0, j])
    fm[:, 5, :] = fmaj(inp["b_conv"][0])
    fm[:, 6, :] = fmaj(inp["b_rg"][0])
    fm[:, 7, :] = fmaj(inp["b_ig"][0])
    fm[:, 8, :] = fmaj(inp["rglru_lambda"][0])
    sh["fm"] = fm
    bcv = np.zeros((128, 3, D), f)
    bcv[:, 0, :] = np.asarray(inp["g_post_mix"][0], f)[None, :]
    bcv[:, 1, :] = np.asarray(inp["g_pre_ffn"][0], f)[None, :]
    bcv[:, 2, :] = np.asarray(inp["g_post_ffn"][0], f)[None, :]
    sh["bcv"] = bcv
    sh["rbias"] = np.ascontiguousarray(np.broadcast_to(np.asarray(inp["router_bias"][0], f)[None, :], (128, E)))
    sh["b_forget"] = np.asarray(inp["b_forget"][0], f).reshape(NH, 1)
    sh["w_in"] = np.ascontiguousarray(inp["w_in"][0], f)
    for nm, src in (("wrg_bd", inp["w_rg"][0]), ("wig_bd", inp["w_ig"][0])):
        bd = np.zeros((8, 128, 128), f)
        for c in range(8):
            bd[c, 0:64, 0:64] = src[2 * c]
            bd[c, 64:128, 64:128] = src[2 * c + 1]
        sh[nm] = bd
    sh["w_ba"] = np.ascontiguousarray(inp["w_branch_attn"][0], f)
    sh["w_br"] = np.ascontiguousarray(inp["w_branch_rnn"][0], f)
    sh["w_out"] = np.ascontiguousarray(inp["w_out"][0], f)
    sh["w_router"] = np.ascontiguousarray(inp["w_router"][0], f)
    sh["w_sg"] = np.ascontiguousarray(inp["w_sh_gate"][0], f)
    sh["w_su"] = np.ascontiguousarray(inp["w_sh_up"][0], f)
    sh["w_sd"] = np.ascontiguousarray(inp["w_sh_down"][0], f)
    wg = np.asarray(inp["w_exp_gate"][0], f).reshape(E, 8, 128, FF).transpose(0, 2, 1, 3)
    sh["wpg"] = np.ascontiguousarray(wg).reshape(E * 128, 2048)
    wu = np.asarray(inp["w_exp_up"][0], f).reshape(E, 8, 128, FF).transpose(0, 2, 1, 3)
    sh["wpu"] = np.ascontiguousarray(wu).reshape(E * 128, 2048)
    wd = np.asarray(inp["w_exp_down"][0], f).reshape(E, 2, 128, D).transpose(0, 2, 1, 3)
    sh["wpd"] = np.ascontiguousarray(wd).reshape(E * 128, 2048)
    return sh


def prep_core(inp, sh, core, NSEQ, S, NBLK):
    f = np.float32
    m = dict(sh)
    xs = np.asarray(inp["x"][core * NSEQ:(core + 1) * NSEQ], f).reshape(NSEQ * S, D)
    m["x"] = np.ascontiguousarray(xs)
    c = np.asarray(inp["c"][core * NSEQ:(core + 1) * NSEQ], f)
    m["csT"] = np.ascontiguousarray(c.T.reshape(8, 128, NSEQ).transpose(1, 0, 2))
    m["b_ada_rep"] = np.ascontiguousarray(np.broadcast_to(np.asarray(inp["b_ada"][0], f)[None, :], (NSEQ, 6 * D)))
    cf, cb = make_consts(NBLK)
    m["cf"] = cf
    m["cb"] = cb
    return m


def kernel(**inputs):
    B, S = inputs["x"].shape[0], inputs["x"].shape[1]
    NSEQ = B // NCORES
    NTOK = NSEQ * S
    NBLK = (NTOK * TOPK) // CB + E
    nc = build(NSEQ, S, skip_reload=True)
    sh = prep_shared(inputs)
    in_maps = [prep_core(inputs, sh, i, NSEQ, S, NBLK) for i in range(NCORES)]
    res = run_bass_kernel_spmd(nc, in_maps, core_ids=list(range(NCORES)))
    outs = [np.asarray(r["out"], np.float32).reshape(NSEQ, S, D) for r in res.results]
    return np.concatenate(outs, axis=0)
```

```python
import numpy as np
import concourse.bass as bass
import concourse.mybir as mybir
from concourse.bass_utils import run_bass_kernel_spmd
from contextlib import ExitStack

F32 = mybir.dt.float32
BF16 = mybir.dt.bfloat16
I32 = mybir.dt.int32
U32 = mybir.dt.uint32
AF = mybir.ActivationFunctionType
ALU = mybir.AluOpType
AX = mybir.AxisListType

D = 1024
NH = 8
E = 64
TOPK = 6
FF = 256
CB = 256
INC = 5640
OQ, OK_, OV, OF_, OX, OG, OGA, OGR = 0, 512, 1024, 1536, 1544, 2568, 3592, 4616
EPS = 1e-6
BIG = 1.0e4
NCORES = 8
ARENA_SHIFT = [0]
ARENA_MAX = [0]


class Buf:
    __slots__ = ("w", "r", "name")

    def __init__(self, name=""):
        self.w = {}
        self.r = {}
        self.name = name


class Eng:
    def __init__(self, name, e, sem, key):
        self.name = name
        self.e = e
        self.sem = sem
        self.key = key
        self.n = 0
        self.seen = {}
        self.pending = False


class DQ:
    def __init__(self, eng, sems):
        self.eng = eng
        self.sems = sems
        self.cnt = [0] * len(sems)
        self.next = 0


def _merge(d, s):
    for k, v in s.items():
        if d.get(k, 0) < v:
            d[k] = v


class KB:
    def __init__(self, nc, stack):
        self.nc = nc
        self.semtab = {}
        self.engs = []
        for nm, e in (("pe", nc.tensor), ("act", nc.scalar), ("dve", nc.vector),
                      ("pool", nc.gpsimd), ("sp", nc.sync)):
            sem = stack.enter_context(nc.semaphore("s_" + nm))
            eng = Eng(nm, e, sem, "c_" + nm)
            self.semtab[eng.key] = sem
            setattr(self, nm, eng)
            self.engs.append(eng)
        self.queues = []
        for nm, eng, n in (("qsp", self.sp, 8), ("qpool", self.pool, 6)):
            sems = []
            for i in range(n):
                key = "d_%s%d" % (nm, i)
                sem = stack.enter_context(nc.semaphore(key))
                self.semtab[key] = sem
                sems.append((sem, key))
            q = DQ(eng, sems)
            setattr(self, nm, q)
            self.queues.append(q)

    def _wait(self, E_, deps):
        for k, v in deps.items():
            if E_.seen.get(k, 0) < v:
                E_.e.wait_ge(self.semtab[k], v)
                E_.seen[k] = v

    def op(self, E_, fn, reads=(), writes=(), inc=True):
        deps = {}
        for b in reads:
            _merge(deps, b.w)
        for b in writes:
            _merge(deps, b.w)
            _merge(deps, b.r)
        if E_.name == "pe":
            deps.pop(E_.key, None)
        self._wait(E_, deps)
        ins = fn()
        ev = E_.n + 1
        for b in reads:
            if b.r.get(E_.key, 0) < ev:
                b.r[E_.key] = ev
        for b in writes:
            b.w = {E_.key: ev}
            b.r = {}
        if inc:
            E_.n = ev
            ins.then_inc(E_.sem, 1)
            E_.pending = False
        else:
            E_.pending = True
        return ins

    def dma(self, Q, fn, reads=(), writes=(), swrites=()):
        E_ = Q.eng
        deps = {}
        for b in reads:
            _merge(deps, b.w)
        for b in writes:
            _merge(deps, b.w)
            _merge(deps, b.r)
        for b in swrites:
            _merge(deps, b.r)
        slot = Q.next
        Q.next = (Q.next + 1) % len(Q.sems)
        sem, key = Q.sems[slot]
        if Q.cnt[slot] > 0:
            if deps.get(key, 0) < 16 * Q.cnt[slot]:
                deps[key] = 16 * Q.cnt[slot]
        self._wait(E_, deps)
        ins = fn()
        Q.cnt[slot] += 1
        v = 16 * Q.cnt[slot]
        ins.then_inc(sem, 16)
        for b in reads:
            if b.r.get(key, 0) < v:
                b.r[key] = v
        for b in writes:
            b.w = {key: v}
            b.r = {}
        for b in swrites:
            if b.w.get(key, 0) < v:
                b.w[key] = v
        return ins

    def barrier(self):
        tot = {}
        for E_ in self.engs:
            assert not E_.pending
            if E_.n > 0:
                tot[E_.key] = E_.n
        for Q in self.queues:
            for i, (sem, key) in enumerate(Q.sems):
                if Q.cnt[i] > 0:
                    tot[key] = 16 * Q.cnt[i]
        for E_ in self.engs:
            self._wait(E_, dict(tot))


class Arena:
    def __init__(self, nc, limit):
        self.nc = nc
        self.off = 0
        self.limit = limit
        self.n = 0

    def alloc(self, name, shape, dtype):
        sz = 1
        for s in shape[1:]:
            sz *= s
        sz *= {F32: 4, BF16: 2, I32: 4, U32: 4}[dtype]
        sz = (sz + 63) // 64 * 64
        off = self.off
        assert off + sz <= self.limit, ("SBUF arena overflow", name, off, sz)
        self.off += sz
        self.n += 1
        ARENA_MAX[0] = max(ARENA_MAX[0], self.off)
        return self.nc.alloc_sbuf_tensor_at("%s_%d" % (name, self.n), list(shape), dtype, offset=off)

    def mark(self):
        return self.off

    def release(self, m):
        self.off = m


def build(NSEQ, S, skip_reload=True):
    NT_S = S // 128
    NTOK = NSEQ * S
    NT = NTOK // 128
    QB = min(512, S)
    TQ = QB // 128
    NQ = S // QB
    NB5 = S // QB
    NBLK = (NTOK * TOPK) // CB + E
    NSLOT = NBLK * CB

    nc = bass.Bass("TRN2", target_bir_lowering=False)
    dt = nc.dram_tensor

    def ein(name, shape, dtype=F32):
        return dt(name, list(shape), dtype, kind="ExternalInput")

    x_d = ein("x", [NTOK, D])
    cs_d = ein("csT", [128, 8, NSEQ])
    wada_d = ein("w_ada", [D, 6 * D])
    bada_d = ein("b_ada_rep", [NSEQ, 6 * D])
    fm_d = ein("fm", [128, 9, 8])
    bcv_d = ein("bcv", [128, 3, D])
    rb_d = ein("rbias", [128, E])
    bf_d = ein("b_forget", [NH, 1])
    win_d = ein("w_in", [D, INC])
    wrg_d = ein("wrg_bd", [8, 128, 128])
    wig_d = ein("wig_bd", [8, 128, 128])
    wba_d = ein("w_ba", [512, D])
    wbr_d = ein("w_br", [D, D])
    wout_d = ein("w_out", [D, D])
    wr_d = ein("w_router", [D, E])
    wsg_d = ein("w_sg", [D, FF])
    wsu_d = ein("w_su", [D, FF])
    wsd_d = ein("w_sd", [FF, D])
    wpg_d = ein("wpg", [E * 128, 2048])
    wpu_d = ein("wpu", [E * 128, 2048])
    wpd_d = ein("wpd", [E * 128, 2048])
    NCF = 128 + 128 + 64 + NBLK + 2
    cf_d = ein("cf", [128, NCF])
    cb_d = ein("cb", [128, 1792], BF16)
    out_d = dt("out", [NTOK, D], F32, kind="ExternalOutput")
    modd = dt("modd", [NSEQ, 6 * D], F32)
    h2_d = dt("h2s", [NTOK, D], BF16)
    x1_d = dt("x1s", [NTOK, D], F32)
    sh_d = dt("shs", [NTOK, D], BF16)
    xs_d = dt("xss", [NSLOT, D], BF16)
    ys_d = dt("yss", [NSLOT, D], BF16)
    wpb_d = [dt("wpb%d" % m_, [E * 128, 2048], BF16) for m_ in range(3)]

    stack = ExitStack()
    with stack:
        K = KB(nc, stack)
        al = Arena(nc, int(nc._sbuf_addr_for_side("right")) - 64)
        al.off = (int(nc._sbuf_addr_for_side("left")) + 63) // 64 * 64 + ARENA_SHIFT[0]
        ps = [stack.enter_context(nc.psum_tensor("ps%d" % i, [128, 512], F32)) for i in range(8)]
        pb = [Buf("pb%d" % i) for i in range(8)]
        rot = {"l": list(range(8)), "i": 0}

        def nb():
            i = rot["l"][rot["i"] % len(rot["l"])]
            rot["i"] += 1
            return i

        PE, ACT, DVE, POOL = K.pe, K.act, K.dve, K.pool
        reg_slot = nc.gpsimd.alloc_register("bc_slot")
        nc.gpsimd.reg_mov(reg_slot, NSLOT - 1)
        reg_w = nc.gpsimd.alloc_register("bc_w")
        nc.gpsimd.reg_mov(reg_w, E * 128 - 1)
        v_, a_, g_, t_ = nc.vector, nc.scalar, nc.gpsimd, nc.tensor

        def mm(out, lhsT, rhs, start, stop, r, w, inc):
            return K.op(PE, lambda: t_.matmul(out, lhsT, rhs, start=start, stop=stop), r, w, inc)

        def tr(out, in_, ident, r, w, inc):
            return K.op(PE, lambda: t_.transpose(out, in_, ident), r, w, inc)

        def sp_dma(out, in_, r=(), w=(), sw=()):
            return K.dma(K.qsp, lambda: nc.sync.dma_start(out=out, in_=in_), r, w, sw)

        def pl_dma(out, in_, r=(), w=(), sw=()):
            return K.dma(K.qpool, lambda: nc.gpsimd.dma_start(out=out, in_=in_), r, w, sw)

        cf = al.alloc("cf", [128, NCF], F32)
        cbt = al.alloc("cb", [128, 1792], BF16)
        b_const = Buf("const")
        ident_f = cf[:, 0:128]
        sel127 = cf[:, 128:256]
        iota64 = cf[:, 256:320]
        iotab = cf[:, 320:320 + NBLK]
        pidx = cf[:, 320 + NBLK:321 + NBLK]
        ones_c = cf[:, 321 + NBLK:322 + NBLK]
        ident_b = cbt[:, 0:128]
        triu_b = cbt[:, 128:256]
        trim_b = cbt[:, 256:384]
        ones_b = cbt[:, 384:512]
        negm_b = cbt[:, 1536:1664]
        swap_b = cbt[:, 1664:1792]
        fm = al.alloc("fm", [128, 9, 8], F32)
        sm = al.alloc("sm", [128, 6, 8], F32)
        rbias = al.alloc("rbias", [128, E], F32)
        nbf = al.alloc("nbf", [128, 2], F32)
        b_nbf = Buf()
        gsmT = al.alloc("gsmT", [128, 8, NSEQ], F32)
        shmT = al.alloc("shmT", [128, 8, NSEQ], F32)
        run = al.alloc("run", [128, E], F32)
        rankm = al.alloc("rankm", [128, NT, E], F32)
        eidx = al.alloc("eidx", [128, NT, 8], F32)
        wk = al.alloc("wk", [128, NT, 8], F32)
        wkn = al.alloc("wkn", [128, NT, 8], F32)
        desti = al.alloc("desti", [128, NT, 8], I32)
        widx = al.alloc("widx", [128, NBLK], I32)
        b_fm, b_sm, b_gs, b_run, b_route = Buf(), Buf(), Buf(), Buf(), Buf()
        b_widx = Buf()
        zt = al.alloc("zt", [128, 2, D], BF16)
        b_zt, b_xs0 = Buf(), Buf()
        K.op(POOL, lambda: g_.memset(zt[:], 0.0), [], [b_zt])
        zf = {"n": 0}
        ZF_PER = -(-NBLK // NT)

        b_wcast = Buf()
        pcast = {"n": 0}
        PC_PER = -(-(3 * E) // (NSEQ * 4 * NQ))

        def precast(cnt):
            for _ in range(cnt):
                if pcast["n"] < 3 * E:
                    e_, m_ = pcast["n"] // 3, pcast["n"] % 3
                    src = (wpg_d, wpu_d, wpd_d)[m_]
                    pl_dma(wpb_d[m_][e_ * 128:(e_ + 1) * 128, :], src[e_ * 128:(e_ + 1) * 128, :], sw=[b_wcast])
                    pcast["n"] += 1

        def zero_fill(cnt):
            for _ in range(cnt):
                if zf["n"] < NBLK:
                    r0_ = zf["n"] * CB
                    sp_dma(xs_d[r0_:r0_ + CB, :].rearrange("(s p) d -> p s d", p=128), zt[:], r=[b_zt], sw=[b_xs0])
                    zf["n"] += 1

        sp_dma(cf[:], cf_d.ap(), w=[b_const])
        sp_dma(cbt[:], cb_d.ap(), w=[b_const])
        sp_dma(fm[:], fm_d.ap(), w=[b_fm])
        sp_dma(rbias[:], rb_d.ap(), w=[b_const])
        K.op(DVE, lambda: v_.memset(nbf[:], 0.0), [], [b_nbf])
        for g3 in range(3):
            sp_dma(nbf[g3 * 32:g3 * 32 + NH, 0:1], bf_d.ap(), w=[b_nbf])
        K.op(DVE, lambda: v_.memset(run[:], 0.0), [], [b_run])

        m0 = al.mark()
        cs = al.alloc("cs", [128, 8, NSEQ], F32)
        th0 = al.alloc("th0", [128, 8, NSEQ], F32)
        siluT = al.alloc("siluT", [128, 8, NSEQ], BF16)
        modt = al.alloc("modt", [NSEQ, 6 * D], F32)
        bada = al.alloc("bada", [NSEQ, 6 * D], F32)
        wada = [al.alloc("wada", [128, 8, 512], BF16) for _ in range(2)]
        b_cs, b_th0, b_silu, b_modt, b_bada = Buf(), Buf(), Buf(), Buf(), Buf()
        b_wada = [Buf(), Buf()]

        K.op(DVE, lambda: v_.tensor_scalar(out=nbf[0:72, 1:2], in0=nbf[0:72, 0:1], scalar1=-1.0, scalar2=None,
                                           op0=ALU.mult), [b_nbf], [b_nbf])
        K.op(ACT, lambda: a_.activation(out=sm[:, 4, :], in_=fm[:, 8, :], func=AF.Exp, scale=-1.0), [b_fm], [b_sm])
        K.op(ACT, lambda: a_.activation(out=sm[:, 5, :], in_=sm[:, 4, :], func=AF.Ln, bias=1.0, scale=1.0), [b_sm], [b_sm])
        K.op(DVE, lambda: v_.tensor_scalar(out=sm[:, 0, :], in0=sm[:, 5, :], scalar1=-8.0, scalar2=None, op0=ALU.mult), [b_sm], [b_sm])
        K.op(DVE, lambda: v_.tensor_scalar(out=sm[:, 1, :], in0=sm[:, 5, :], scalar1=-4.0, scalar2=None, op0=ALU.mult), [b_sm], [b_sm])
        K.op(DVE, lambda: v_.tensor_scalar(out=sm[:, 2, :], in0=fm[:, 6, :], scalar1=0.5, scalar2=None, op0=ALU.mult), [b_fm, b_sm], [b_sm])
        K.op(DVE, lambda: v_.tensor_scalar(out=sm[:, 3, :], in0=fm[:, 7, :], scalar1=0.5, scalar2=None, op0=ALU.mult), [b_fm, b_sm], [b_sm])
        cneg = sm[:, 0, :]
        hcneg = sm[:, 1, :]
        hbrg = sm[:, 2, :]
        hbig = sm[:, 3, :]

        sp_dma(cs[:], cs_d.ap(), w=[b_cs])
        sp_dma(bada[:], bada_d.ap(), w=[b_bada])
        K.op(ACT, lambda: a_.activation(out=th0[:], in_=cs[:], func=AF.Tanh, scale=0.5), [b_cs], [b_th0])
        K.op(DVE, lambda: v_.scalar_tensor_tensor(out=th0[:], in0=th0[:], scalar=1.0, in1=cs[:], op0=ALU.add, op1=ALU.mult),
             [b_cs, b_th0], [b_th0])
        K.op(DVE, lambda: v_.tensor_scalar(out=siluT[:], in0=th0[:], scalar1=0.5, scalar2=None, op0=ALU.mult), [b_th0], [b_silu])
        for g in range(12):
            wb = wada[g % 2]
            bw = b_wada[g % 2]
            pl_dma(wb[:], wada_d[:, g * 512:(g + 1) * 512].rearrange("(kc p) n -> p kc n", p=128), w=[bw])
            i = nb()
            for kc in range(8):
                mm(ps[i][0:NSEQ, :], siluT[:, kc, :], wb[:, kc, :], kc == 0, kc == 7, [b_silu, bw], [pb[i]], kc == 7)
            K.op(DVE, lambda: v_.tensor_tensor(out=modt[:, g * 512:(g + 1) * 512], in0=ps[i][0:NSEQ, :],
                                               in1=bada[:, g * 512:(g + 1) * 512], op=ALU.add), [pb[i], b_bada], [b_modt])
        b_modd = Buf()
        sp_dma(modd.ap(), modt[:], r=[b_modt], w=[b_modd])
        i = nb()
        pT0 = ps[i][:, 0:16 * NSEQ].rearrange("p (a b) -> p a b", b=NSEQ)
        for kc in range(16):
            col = (D + kc * 128) if kc < 8 else ((kc - 8) * 128)
            tr(pT0[:, kc, :], modt[0:NSEQ, col:col + 128], ident_f[0:NSEQ, 0:NSEQ], [b_modt, b_const], [pb[i]], kc == 15)
        K.op(DVE, lambda: v_.scalar_tensor_tensor(out=gsmT[:], in0=pT0[:, 0:8, :], scalar=1.0,
                                                  in1=fm[:, 0, :].unsqueeze(2).to_broadcast([128, 8, NSEQ]),
                                                  op0=ALU.add, op1=ALU.mult), [pb[i], b_fm], [b_gs])
        K.op(DVE, lambda: v_.tensor_copy(out=shmT[:], in_=pT0[:, 8:16, :]), [pb[i]], [b_gs])
        K.barrier()
        al.release(m0)

        off_hT = al.off
        hT = al.alloc("hT", [128, 8, S], BF16)
        yaT = al.alloc("yaT", [128, 4, S], BF16)
        off_yrT = al.off
        yrT = al.alloc("yrT", [128, 8, S], BF16)
        LIMIT = al.limit
        b_hT = [Buf() for _ in range(NT_S)]
        b_yaT, b_yrT = Buf(), Buf()
        mU = al.mark()

        for b in range(NSEQ):
            tok0 = b * S
            al.release(mU)
            x_sb = [al.alloc("x_sb", [128, D], F32) for _ in range(4)]
            xn = [al.alloc("xn", [128, D], BF16) for _ in range(2)]
            jk = al.alloc("jk", [128, D], BF16)
            st = [al.alloc("st", [128, 4], F32) for _ in range(3)]
            b_x, b_xn, b_st, b_jk = [Buf() for _ in range(4)], [Buf(), Buf()], [Buf() for _ in range(3)], Buf()
            rot["l"] = list(range(8))
            def m1_L(t):
                sp_dma(x_sb[t % 4][:], x_d[tok0 + t * 128: tok0 + (t + 1) * 128, :], w=[b_x[t % 4]])
                zero_fill(ZF_PER)

            def m1_A1(t):
                xb, stb, bx, bst = x_sb[t % 4], st[t % 3], b_x[t % 4], b_st[t % 3]
                K.op(ACT, lambda: a_.activation(out=jk[:], in_=xb[:], func=AF.Square, accum_out=stb[:, 0:1]), [bx], [b_jk, bst])
                K.op(ACT, lambda: a_.activation(out=stb[:, 1:2], in_=stb[:, 0:1], func=AF.Sqrt, bias=EPS, scale=1.0 / D), [bst], [bst])
                K.op(DVE, lambda: v_.reciprocal(out=stb[:, 2:3], in_=stb[:, 1:2]), [bst], [bst])

            def m1_A2(t):
                xb, xnb, stb = x_sb[t % 4], xn[t % 2], st[t % 3]
                bx, bxn, bst = b_x[t % 4], b_xn[t % 2], b_st[t % 3]
                K.op(ACT, lambda: a_.activation(out=xnb[:], in_=xb[:], func=AF.Identity, scale=stb[:, 2:3]), [bx, bst], [bxn])

            def m1_B(t):
                xnb, bxn = xn[t % 2], b_xn[t % 2]
                i = nb()
                pT = ps[i][:, :].bitcast(BF16)
                for kc in range(8):
                    tr(pT[:, kc * 128:(kc + 1) * 128], xnb[:, kc * 128:(kc + 1) * 128], ident_b, [bxn, b_const], [pb[i]], kc == 7)
                for kc in range(8):
                    o_ap = hT[:, kc, t * 128:(t + 1) * 128]
                    i_ap = pT[:, kc * 128:(kc + 1) * 128]
                    if kc % 2 == 0:
                        K.op(ACT, lambda: a_.activation(out=o_ap, in_=i_ap, func=AF.Identity, bias=shmT[:, kc, b:b + 1],
                                                        scale=gsmT[:, kc, b:b + 1]), [pb[i], b_gs], [b_hT[t]])
                    else:
                        K.op(DVE, lambda: v_.tensor_scalar(out=o_ap, in0=i_ap, scalar1=gsmT[:, kc, b:b + 1],
                                                           scalar2=shmT[:, kc, b:b + 1], op0=ALU.mult, op1=ALU.add),
                             [pb[i], b_gs], [b_hT[t]])
            for t0_ in range(min(3, NT_S)):
                m1_L(t0_)
            m1_A1(0)
            if NT_S > 1:
                m1_A1(1)
            m1_A2(0)
            for t in range(NT_S):
                if t + 3 < NT_S:
                    m1_L(t + 3)
                if t + 2 < NT_S:
                    m1_A1(t + 2)
                if t + 1 < NT_S:
                    m1_A2(t + 1)
                m1_B(t)
            K.barrier()

            al.release(mU)
            qT = al.alloc("qT", [128, 4, 2, S], BF16)
            kT = al.alloc("kT", [128, 4, S], BF16)
            sv_ = al.off
            al.off = off_yrT
            vS = al.alloc("vS", [128, NT_S, 4, 192], BF16)
            ef = al.alloc("ef", [72, S], F32)
            assert al.off <= mU
            al.off = sv_
            off_wq = al.off
            wq = al.alloc("wq", [128, 8, 512], BF16)
            wkk = al.alloc("wk", [128, 8, 512], BF16)
            wv = al.alloc("wv", [128, 8, 512], BF16)
            wf = al.alloc("wf", [128, 8, 72], BF16)
            caug = al.alloc("caug", [128, S], BF16)
            tmpb = al.alloc("tmpb", [72, S], BF16)
            b_caug, b_tmpb = Buf(), Buf()
            Lf = al.alloc("Lf", [72, S], F32)
            cumLT = al.alloc("cumLT", [128, NT_S, NH], F32)
            b_Rb, b_Rs = Buf(), Buf()
            b_q, b_k, b_v, b_wq, b_wk, b_wv, b_wf = Buf(), Buf(), Buf(), Buf(), Buf(), Buf(), Buf()
            b_ef, b_L, b_cumLT = Buf(), Buf(), Buf()
            b_PT = [Buf() for _ in range(6)]

            def wview(c0, n):
                return win_d[:, c0:c0 + n].rearrange("(kc p) n -> p kc n", p=128)
            K.op(POOL, lambda: g_.memset(vS[:, :, :, 64:128], 1.0), [], [b_v])
            K.op(POOL, lambda: g_.memset(qT[:], 0.0), [], [b_q])
            pl_dma(wq[:], wview(OQ, 512), w=[b_wq])
            pl_dma(wkk[:], wview(OK_, 512), w=[b_wk])
            pl_dma(wv[:], wview(OV, 512), w=[b_wv])
            K.op(POOL, lambda: g_.memset(wf[:], 0.0), [], [b_wf])
            K.op(POOL, lambda: g_.memset(caug[:], 0.0), [], [b_caug])
            for g3 in range(3):
                pl_dma(wf[:, :, g3 * 32:g3 * 32 + NH], wview(OF_, 8), w=[b_wf])
            rot["l"] = list(range(8))
            cnt = 0
            for n in range(NQ):
                i = nb()
                for kc in range(8):
                    mm(ps[i][0:72, 0:QB], wf[:, kc, :], hT[:, kc, n * QB:(n + 1) * QB], kc == 0, kc == 7,
                       [b_wf] + b_hT[n * TQ:(n + 1) * TQ], [pb[i]], kc == 7)
                K.op(ACT, lambda: a_.activation(out=ef[:, n * QB:(n + 1) * QB], in_=ps[i][0:72, 0:QB], func=AF.Exp,
                                                bias=nbf[0:72, 1:2], scale=-1.0), [pb[i], b_nbf], [b_ef])
            K.op(ACT, lambda: a_.activation(out=Lf[:], in_=ef[:], func=AF.Ln, bias=1.0, scale=1.0), [b_ef], [b_L])
            K.op(DVE, lambda: v_.tensor_tensor_scan(out=ef[:], data0=ones_c[0:72, 0:1].to_broadcast([72, S]), data1=Lf[:],
                                                    initial=0.0, op0=ALU.mult, op1=ALU.add), [b_L, b_const, b_ef], [b_ef])
            K.op(DVE, lambda: v_.tensor_scalar(out=Lf[:], in0=ef[:], scalar1=-8.0, scalar2=None, op0=ALU.mult), [b_ef, b_L], [b_L])
            K.op(DVE, lambda: v_.tensor_copy(out=tmpb[:], in_=Lf[:]), [b_L], [b_tmpb])
            K.op(DVE, lambda: v_.tensor_copy(out=caug[0:NH, :], in_=tmpb[0:NH, :]), [b_tmpb], [b_caug])
            K.op(DVE, lambda: v_.tensor_tensor(out=Lf[:], in0=Lf[:], in1=tmpb[:], op=ALU.subtract), [b_L, b_tmpb], [b_L])
            K.op(DVE, lambda: v_.tensor_copy(out=tmpb[:], in_=Lf[:]), [b_L, b_caug], [b_tmpb])
            K.op(DVE, lambda: v_.tensor_copy(out=caug[32:32 + NH, :], in_=tmpb[32:32 + NH, :]), [b_tmpb], [b_caug])
            K.op(DVE, lambda: v_.tensor_tensor(out=Lf[:], in0=Lf[:], in1=tmpb[:], op=ALU.subtract), [b_L, b_tmpb], [b_L])
            K.op(DVE, lambda: v_.tensor_copy(out=caug[64:64 + NH, :], in_=Lf[64:64 + NH, :]), [b_L], [b_caug])
            for (wt, bw, dst, bd, isq) in ((wq, b_wq, qT, b_q, True), (wkk, b_wk, kT, b_k, False)):
                for j in range(4):
                    for n in range(NQ):
                        i = nb()
                        for kc in range(8):
                            mm(ps[i][:, 0:QB], wt[:, kc, j * 128:(j + 1) * 128], hT[:, kc, n * QB:(n + 1) * QB], kc == 0, kc == 7,
                               [bw] + b_hT[n * TQ:(n + 1) * TQ], [pb[i]], kc == 7)
                        cols = slice(n * QB, (n + 1) * QB)
                        if isq:
                            K.op(ACT, lambda: a_.copy(out=qT[0:64, j, 0, cols], in_=ps[i][0:64, 0:QB]), [pb[i]], [bd])
                            K.op(DVE, lambda: v_.tensor_copy(out=qT[64:128, j, 1, cols], in_=ps[i][64:128, 0:QB]), [pb[i]], [bd])
                        elif cnt % 2 == 0:
                            K.op(ACT, lambda: a_.copy(out=kT[:, j, cols], in_=ps[i][:, 0:QB]), [pb[i]], [bd])
                        else:
                            K.op(DVE, lambda: v_.tensor_copy(out=kT[:, j, cols], in_=ps[i][:, 0:QB]), [pb[i]], [bd])
                        cnt += 1
            for t in range(NT_S):
                i = nb()
                for kc in range(8):
                    mm(ps[i][:, :], hT[:, kc, t * 128:(t + 1) * 128], wv[:, kc, :], kc == 0, kc == 7, [b_wv, b_hT[t]], [pb[i]], kc == 7)
                o_v = vS[:, t, :, :].rearrange("p j (c w) -> p j c w", w=64)[:, :, 0::2, :]
                i_v = ps[i][:, :].rearrange("p (j c w) -> p j c w", c=2, w=64)
                if t % 2 == 0:
                    K.op(ACT, lambda: a_.copy(out=o_v, in_=i_v), [pb[i]], [b_v])
                else:
                    K.op(DVE, lambda: v_.tensor_copy(out=o_v, in_=i_v), [pb[i]], [b_v])
            i = nb()
            pT1 = ps[i][:, 0:NT_S * NH].rearrange("p (a b) -> p a b", b=NH)
            for t in range(NT_S):
                tr(pT1[:, t, :], ef[0:NH, t * 128:(t + 1) * 128], ident_f[0:NH, 0:NH], [b_ef, b_const], [pb[i]], t == NT_S - 1)
            K.op(DVE, lambda: v_.tensor_copy(out=cumLT[:], in_=pT1), [pb[i]], [b_cumLT])

            K.barrier()
            sv3 = al.off
            al.off = off_wq
            PT = [al.alloc("PT", [128, QB], BF16) for _ in range(6)]
            Rb = al.alloc("Rb", [128, QB], BF16)
            Rs = al.alloc("Rs", [128, QB], F32)
            al.off = sv3
            rot["l"] = [4, 5, 6, 7]
            pcount = 0
            LAG = 3
            grp = {"n": 0}
            pend_backs = []

            def att_front(j, q, t, half, nt, gpar):
                nonlocal pcount
                d = t - q * TQ
                q0 = max(d, 0) * 128
                h = 2 * j + half
                rows = slice(half * 64, half * 64 + 64)
                i = nb()
                mm(ps[i][:, q0:QB], kT[:, j, t * 128:(t + 1) * 128], qT[:, j, half, q * QB + q0:(q + 1) * QB],
                   True, False, [b_q, b_k], [pb[i]], False)
                mm(ps[i][:, q0:QB], cbt[:, 512 + h * 128:512 + (h + 1) * 128], caug[:, q * QB + q0:(q + 1) * QB],
                   False, d < 0, [b_const, b_caug], [pb[i]], d < 0)
                if d >= 0:
                    mm(ps[i][:, q0:q0 + 128], ident_b, negm_b, False, True, [b_const], [pb[i]], True)
                pt = PT[pcount % 6]
                bpt = b_PT[pcount % 6]
                pcount += 1
                K.op(ACT, lambda: a_.activation(out=pt[:, q0:QB], in_=ps[i][:, q0:QB], func=AF.Exp,
                                                bias=cumLT[:, t, h:h + 1], scale=0.125), [pb[i], b_cumLT], [bpt])

                def back():
                    yi = half + 2 * gpar
                    ya_, yb_ = 2 * gpar, 2 * gpar + 1
                    lo = 0 if half == 0 else 64
                    mm(ps[yi][:, q0:QB], vS[:, t, j, lo:lo + 128], pt[:, q0:QB], t == 0, t == nt - 1,
                       [b_v, bpt], [pb[yi]], True)
                    if t == nt - 1 and half == 1:
                        K.op(DVE, lambda: v_.reciprocal(out=Rs[64:128, :], in_=ps[ya_][64:128, 0:QB]), [pb[ya_], b_Rs], [b_Rs])
                        K.op(DVE, lambda: v_.reciprocal(out=Rs[0:64, :], in_=ps[yb_][0:64, 0:QB]), [pb[yb_], b_Rs], [b_Rs])
                        K.op(DVE, lambda: v_.tensor_copy(out=Rb[:], in_=Rs[:]), [b_Rs, b_Rb], [b_Rb])
                        isw = nb()
                        mm(ps[isw][:, 0:QB], swap_b, Rb[:, :], True, True, [b_const, b_Rb], [pb[isw]], True)
                        K.op(ACT, lambda: a_.copy(out=Rs[:], in_=ps[isw][:, 0:QB]), [pb[isw]], [b_Rs])
                        K.op(DVE, lambda: v_.tensor_tensor(out=yaT[0:64, j, q * QB:(q + 1) * QB], in0=ps[ya_][0:64, 0:QB],
                                                           in1=Rs[0:64, :], op=ALU.mult), [pb[ya_], b_Rs], [b_yaT])
                        K.op(DVE, lambda: v_.tensor_tensor(out=yaT[64:128, j, q * QB:(q + 1) * QB], in0=ps[yb_][64:128, 0:QB],
                                                           in1=Rs[64:128, :], op=ALU.mult), [pb[yb_], b_Rs], [b_yaT])
                return back

            for j in range(4):
                for q in range(NQ):
                    nt = q * TQ + TQ
                    gpar = grp["n"] % 2
                    grp["n"] += 1
                    precast(PC_PER)
                    for t in range(nt):
                        for half in range(2):
                            pend_backs.append(att_front(j, q, t, half, nt, gpar))
                            if len(pend_backs) > LAG:
                                pend_backs.pop(0)()
            while pend_backs:
                pend_backs.pop(0)()
            K.barrier()

            al.release(mU)
            xp = [al.alloc("xp", [128, S + 4], F32) for _ in range(2)]
            uu = [al.alloc("uu", [128, S], F32) for _ in range(2)]
            ub = [al.alloc("ub", [128, S], BF16) for _ in range(2)]
            gg = [al.alloc("gg", [128, S], BF16) for _ in range(2)]
            thr = al.alloc("thr", [128, S], F32)
            thi = al.alloc("thi", [128, S], F32)
            e2 = al.alloc("e2", [128, S], F32)
            wx = [al.alloc("wx", [128, 8, 128], BF16) for _ in range(2)]
            wg = [al.alloc("wg", [128, 8, 128], BF16) for _ in range(2)]
            wrg = [al.alloc("wrg", [128, 128], BF16) for _ in range(2)]
            wig = [al.alloc("wig", [128, 128], BF16) for _ in range(2)]
            gt = [[al.alloc("gt", [128, QB], F32) for _ in range(2)] for _ in range(2)]
            b_xp, b_uu, b_ub, b_gg = [Buf(), Buf()], [Buf(), Buf()], [Buf(), Buf()], [Buf(), Buf()]
            b_thr, b_thi, b_e2 = Buf(), Buf(), Buf()
            b_w4 = [[Buf() for _ in range(4)] for _ in range(2)]
            b_gt = [[Buf() for _ in range(2)] for _ in range(2)]
            rot["l"] = list(range(8))
            for s2_ in range(2):
                K.op(DVE, lambda: v_.memset(xp[s2_][:, 0:4], 0.0), [], [b_xp[s2_]])

            def load_w4(c):
                s_ = c % 2
                pl_dma(wx[s_][:], wview(OX + c * 128, 128), w=[b_w4[s_][0]])
                pl_dma(wg[s_][:], wview(OG + c * 128, 128), w=[b_w4[s_][1]])
                pl_dma(wrg[s_][:], wrg_d[c, :, :], w=[b_w4[s_][2]])
                pl_dma(wig[s_][:], wig_d[c, :, :], w=[b_w4[s_][3]])

            gcnt4 = {"n": 0}

            def m4_A(c):
                s_ = c % 2
                bwx, bwg_, bwrg, bwig = b_w4[s_]
                xp_, uu_, ub_, gg_ = xp[s_], uu[s_], ub[s_], gg[s_]
                bxp, buu, bub, bgg = b_xp[s_], b_uu[s_], b_ub[s_], b_gg[s_]
                for n in range(NQ):
                    i = nb()
                    for kc in range(8):
                        mm(ps[i][:, 0:QB], wx[s_][:, kc, :], hT[:, kc, n * QB:(n + 1) * QB], kc == 0, kc == 7,
                           [bwx] + b_hT[n * TQ:(n + 1) * TQ], [pb[i]], kc == 7)
                    K.op(ACT, lambda: a_.copy(out=xp_[:, 3 + n * QB:3 + (n + 1) * QB], in_=ps[i][:, 0:QB]), [pb[i]], [bxp])
                K.op(DVE, lambda: v_.tensor_scalar(out=uu_[:], in0=xp_[:, 3:3 + S], scalar1=fm[:, 4, c:c + 1], scalar2=fm[:, 5, c:c + 1],
                                                   op0=ALU.mult, op1=ALU.add), [bxp, b_fm], [buu])
                for jj in range(3):
                    K.op(DVE, lambda: v_.scalar_tensor_tensor(out=uu_[:], in0=xp_[:, jj:jj + S], scalar=fm[:, 1 + jj, c:c + 1], in1=uu_[:],
                                                              op0=ALU.mult, op1=ALU.add), [bxp, b_fm, buu], [buu])
                K.op(POOL, lambda: g_.tensor_copy(out=ub_[:], in_=uu_[:]), [buu], [bub])
                for n in range(NQ):
                    i = nb()
                    for kc in range(8):
                        mm(ps[i][:, 0:QB], wg[s_][:, kc, :], hT[:, kc, n * QB:(n + 1) * QB], kc == 0, kc == 7,
                           [bwg_] + b_hT[n * TQ:(n + 1) * TQ], [pb[i]], kc == 7)
                    g0, g1 = gt[gcnt4["n"] % 2]
                    bg0, bg1 = b_gt[gcnt4["n"] % 2]
                    gcnt4["n"] += 1
                    pg = ps[i][:, 0:QB]
                    K.op(ACT, lambda: a_.activation(out=g0[:], in_=pg, func=AF.Square), [pb[i]], [bg0])
                    K.op(DVE, lambda: v_.tensor_scalar(out=g0[:], in0=g0[:], scalar1=0.044715, scalar2=1.0, op0=ALU.mult, op1=ALU.add),
                         [bg0], [bg0])
                    K.op(DVE, lambda: v_.tensor_tensor(out=g0[:], in0=g0[:], in1=pg, op=ALU.mult), [bg0, pb[i]], [bg0])
                    K.op(ACT, lambda: a_.activation(out=g1[:], in_=g0[:], func=AF.Tanh, scale=0.7978845608028654), [bg0], [bg1])
                    K.op(DVE, lambda: v_.scalar_tensor_tensor(out=gg_[:, n * QB:(n + 1) * QB], in0=g1[:], scalar=1.0, in1=pg,
                                                              op0=ALU.add, op1=ALU.mult), [bg1, pb[i]], [bgg])

            def m4_B(c):
                s_ = c % 2
                bwx, bwg_, bwrg, bwig = b_w4[s_]
                uu_, ub_, gg_ = uu[s_], ub[s_], gg[s_]
                buu, bub, bgg = b_uu[s_], b_ub[s_], b_gg[s_]
                for n in range(NQ):
                    i = nb()
                    mm(ps[i][:, 0:QB], wrg[s_][:, :], ub_[:, n * QB:(n + 1) * QB], True, True, [bwrg, bub], [pb[i]], True)
                    K.op(ACT, lambda: a_.activation(out=thr[:, n * QB:(n + 1) * QB], in_=ps[i][:, 0:QB], func=AF.Tanh,
                                                    bias=hbrg[:, c:c + 1], scale=0.5), [pb[i], b_sm], [b_thr])
                    i2 = nb()
                    mm(ps[i2][:, 0:QB], wig[s_][:, :], ub_[:, n * QB:(n + 1) * QB], True, True, [bwig, bub], [pb[i2]], True)
                    K.op(ACT, lambda: a_.activation(out=thi[:, n * QB:(n + 1) * QB], in_=ps[i2][:, 0:QB], func=AF.Tanh,
                                                    bias=hbig[:, c:c + 1], scale=0.5), [pb[i2], b_sm], [b_thi])
                K.op(ACT, lambda: a_.activation(out=e2[:], in_=thr[:], func=AF.Exp, bias=cneg[:, c:c + 1], scale=cneg[:, c:c + 1]),
                     [b_thr, b_sm], [b_e2])
                K.op(ACT, lambda: a_.activation(out=thr[:], in_=thr[:], func=AF.Exp, bias=hcneg[:, c:c + 1], scale=hcneg[:, c:c + 1]),
                     [b_thr, b_sm], [b_thr])
                K.op(DVE, lambda: v_.tensor_scalar(out=e2[:], in0=e2[:], scalar1=1.0 - 1.0e-7, scalar2=None, op0=ALU.min), [b_e2], [b_e2])
                K.op(ACT, lambda: a_.activation(out=e2[:], in_=e2[:], func=AF.Sqrt, bias=1.0, scale=-1.0), [b_e2], [b_e2])
                K.op(DVE, lambda: v_.scalar_tensor_tensor(out=thi[:], in0=thi[:], scalar=1.0, in1=e2[:], op0=ALU.add, op1=ALU.mult),
                     [b_thi, b_e2], [b_thi])
                K.op(DVE, lambda: v_.scalar_tensor_tensor(out=thi[:], in0=thi[:], scalar=0.5, in1=uu_[:], op0=ALU.mult, op1=ALU.mult),
                     [b_thi, buu], [b_thi])
                K.op(DVE, lambda: v_.tensor_tensor_scan(out=e2[:], data0=thr[:], data1=thi[:], initial=0.0, op0=ALU.mult, op1=ALU.add),
                     [b_thr, b_thi, b_e2], [b_e2])
                K.op(DVE, lambda: v_.scalar_tensor_tensor(out=yrT[:, c, :], in0=gg_[:], scalar=0.5, in1=e2[:], op0=ALU.mult, op1=ALU.mult),
                     [bgg, b_e2], [b_yrT])

            load_w4(0)
            load_w4(1)
            m4_A(0)
            for c in range(8):
                if c + 1 < 8:
                    m4_A(c + 1)
                m4_B(c)
                if c + 2 < 8:
                    load_w4(c + 2)
            K.barrier()

            al.release(mU)
            mgT = al.alloc("mgT", [128, 8, S], BF16)
            b_mg = [Buf() for _ in range(NT_S)]
            m5 = al.mark()
            w5 = [al.alloc("w5", [128, 28, 128], BF16) for _ in range(2)]
            b_w5 = [[Buf() for _ in range(4)] for _ in range(2)]
            sa = [[al.alloc("sa", [128, QB], F32) for _ in range(4)] for _ in range(2)]
            b_sa = [[Buf() for _ in range(4)] for _ in range(2)]
            rot["l"] = list(range(8))

            def load_w5(m):
                s_ = m % 2
                cs_ = slice(m * 128, (m + 1) * 128)
                pl_dma(w5[s_][:, 0:4, :], wba_d[:, cs_].rearrange("(kc p) n -> p kc n", p=128), w=[b_w5[s_][0]])
                pl_dma(w5[s_][:, 4:12, :], wbr_d[:, cs_].rearrange("(kc p) n -> p kc n", p=128), w=[b_w5[s_][1]])
                pl_dma(w5[s_][:, 12:20, :], wview(OGA + m * 128, 128), w=[b_w5[s_][2]])
                pl_dma(w5[s_][:, 20:28, :], wview(OGR + m * 128, 128), w=[b_w5[s_][3]])
            load_w5(0)
            scount = 0
            for m in range(8):
                s_ = m % 2
                bwa, bwr_, bwga, bwgr = b_w5[s_]
                if m + 1 < 8:
                    load_w5(m + 1)
                for n in range(NQ):
                    cols = slice(n * QB, (n + 1) * QB)
                    hb = b_hT[n * TQ:(n + 1) * TQ]
                    iA, iR, iGA, iGR = nb(), nb(), nb(), nb()
                    for kc in range(4):
                        mm(ps[iA][:, 0:QB], w5[s_][:, kc, :], yaT[:, kc, cols], kc == 0, kc == 3, [bwa, b_yaT], [pb[iA]], kc == 3)
                    for kc in range(8):
                        mm(ps[iGA][:, 0:QB], w5[s_][:, 12 + kc, :], hT[:, kc, cols], kc == 0, kc == 7, [bwga] + hb, [pb[iGA]], kc == 7)
                    for kc in range(8):
                        mm(ps[iR][:, 0:QB], w5[s_][:, 4 + kc, :], yrT[:, kc, cols], kc == 0, kc == 7, [bwr_, b_yrT], [pb[iR]], kc == 7)
                    for kc in range(8):
                        mm(ps[iGR][:, 0:QB], w5[s_][:, 20 + kc, :], hT[:, kc, cols], kc == 0, kc == 7, [bwgr] + hb, [pb[iGR]], kc == 7)
                    s0, s1, s2, s3 = sa[scount % 2]
                    c0, c1, c2, c3 = b_sa[scount % 2]
                    scount += 1
                    K.op(ACT, lambda: a_.activation(out=s0[:], in_=ps[iGA][:, 0:QB], func=AF.Sigmoid), [pb[iGA]], [c0])
                    K.op(DVE, lambda: v_.tensor_tensor(out=s1[:], in0=s0[:], in1=ps[iA][:, 0:QB], op=ALU.mult), [c0, pb[iA]], [c1])
                    K.op(ACT, lambda: a_.activation(out=s2[:], in_=ps[iGR][:, 0:QB], func=AF.Sigmoid), [pb[iGR]], [c2])
                    K.op(DVE, lambda: v_.tensor_tensor(out=s3[:], in0=s2[:], in1=ps[iR][:, 0:QB], op=ALU.mult), [c2, pb[iR]], [c3])
                    K.op(POOL, lambda: g_.tensor_tensor(out=mgT[:, m, cols], in0=s1[:], in1=s3[:], op=ALU.add), [c1, c3],
                         b_mg[n * TQ:(n + 1) * TQ])
            K.barrier()

            al.release(m5)
            offB = al.off
            useA = (mU - off_hT) >= 80 * 1024
            if useA:
                al.off = off_hT
                al.limit = mU
            wo = al.alloc("wo", [128, 8, D], BF16)
            h2Tb = [al.alloc("h2Tb", [128, 8, QB], BF16) for _ in range(2)]
            xs2 = [al.alloc("xs2", [128, D], F32) for _ in range(2)]
            x1 = [al.alloc("x1", [128, D], F32) for _ in range(2)]
            h2 = [al.alloc("h2", [128, D], F32) for _ in range(2)]
            h2T = [al.alloc("h2T", [128, 8, 128], F32) for _ in range(2)]
            shs = [al.alloc("shs", [128, D], BF16) for _ in range(2)]
            h2b = [al.alloc("h2b", [128, D], BF16) for _ in range(2)]
            if useA:
                al.off = offB
                al.limit = LIMIT
            wsg = al.alloc("wsg", [128, 8, FF], BF16)
            wsu = al.alloc("wsu", [128, 8, FF], BF16)
            wsd = al.alloc("wsd", [128, 2, D], BF16)
            wr = al.alloc("wr", [128, 8, E], F32)
            bcv5 = al.alloc("bcv5", [128, 2, D], F32)
            b_bcv5 = Buf()
            gmb = al.alloc("gmb", [128, D], F32)
            gsfb = al.alloc("gsfb", [128, D], F32)
            shfb = al.alloc("shfb", [128, D], F32)
            sg = [al.alloc("sg", [128, QB], F32) for _ in range(2)]
            actT = al.alloc("actT", [128, 2, QB], BF16)
            rs = [al.alloc("rs", [128, 8], F32) for _ in range(2)]
            rt_ = [al.alloc("rt", [128, 6, E], F32) for _ in range(2)]
            g8 = [al.alloc("g8", [128, 8, 8], F32) for _ in range(2)]
            r8 = [al.alloc("r8", [128, 4, 8], F32) for _ in range(2)]
            i8 = [al.alloc("i8", [128, 8], U32) for _ in range(2)]
            mkb = [al.alloc("mkb", [128, E], BF16) for _ in range(2)]
            b_wo, b_ws, b_wr, b_bc5 = Buf(), Buf(), Buf(), Buf()
            b_xs2, b_x1, b_h2, b_h2b, b_h2T = [Buf(), Buf()], [Buf(), Buf()], [Buf(), Buf()], [Buf(), Buf()], [Buf(), Buf()]
            b_h2Tb, b_shs, b_sg, b_act, b_rs, b_rt = [Buf(), Buf()], [Buf(), Buf()], [Buf(), Buf()], Buf(), [Buf(), Buf()], [Buf(), Buf()]
            b_jk5 = Buf()
            jk5 = al.alloc("jk5", [128, D], BF16)
            pl_dma(wo[:], wout_d.ap().rearrange("(kc p) n -> p kc n", p=128), w=[b_wo])
            b_wsg, b_wsu, b_wsd = Buf(), Buf(), Buf()
            pl_dma(wsg[:], wsg_d.ap().rearrange("(kc p) n -> p kc n", p=128), w=[b_wsg])
            pl_dma(wsu[:], wsu_d.ap().rearrange("(kc p) n -> p kc n", p=128), w=[b_wsu])
            pl_dma(wsd[:], wsd_d.ap().rearrange("(kc p) n -> p kc n", p=128), w=[b_wsd])
            sp_dma(wr[:], wr_d.ap().rearrange("(kc p) n -> p kc n", p=128), w=[b_wr])
            sp_dma(bcv5[:], bcv_d[:, 0:2, :], w=[b_bcv5])
            sp_dma(gmb[:], modd[b:b + 1, 2 * D:3 * D].partition_broadcast(128), r=[b_modd], w=[b_bc5])
            sp_dma(gsfb[:], modd[b:b + 1, 4 * D:5 * D].partition_broadcast(128), r=[b_modd], w=[b_bc5])
            sp_dma(shfb[:], modd[b:b + 1, 3 * D:4 * D].partition_broadcast(128), r=[b_modd], w=[b_bc5])
            K.op(DVE, lambda: v_.tensor_tensor(out=gmb[:], in0=gmb[:], in1=bcv5[:, 0, :], op=ALU.mult), [b_bc5, b_bcv5], [b_bc5])
            K.op(DVE, lambda: v_.scalar_tensor_tensor(out=gsfb[:], in0=gsfb[:], scalar=1.0, in1=bcv5[:, 1, :], op0=ALU.add, op1=ALU.mult),
                 [b_bc5, b_bcv5], [b_bc5])
            rot["l"] = list(range(8))
            def m5_A(t):
                n, tt = t // TQ, t % TQ
                T = b * NT_S + t
                r0 = tok0 + t * 128
                p_ = t % 2
                xb, x1b, h2_, h2b_, h2T_, rs_ = xs2[p_], x1[p_], h2[p_], h2b[p_], h2T[p_], rs[p_]
                bxb, bx1, bh2, bh2b, bh2T, brs = b_xs2[p_], b_x1[p_], b_h2[p_], b_h2b[p_], b_h2T[p_], b_rs[p_]
                sp_dma(xb[:], x_d[r0:r0 + 128, :], w=[bxb])
                io = [nb(), nb()]
                for hf in range(2):
                    for kc in range(8):
                        mm(ps[io[hf]][:, :], mgT[:, kc, t * 128:(t + 1) * 128], wo[:, kc, hf * 512:(hf + 1) * 512], kc == 0, kc == 7,
                           [b_mg[t], b_wo], [pb[io[hf]]], kc == 7)
                for hf in range(2):
                    K.op(ACT, lambda: a_.activation(out=jk5[:, hf * 512:(hf + 1) * 512], in_=ps[io[hf]][:, :], func=AF.Square,
                                                    accum_out=rs_[:, hf:hf + 1]), [pb[io[hf]]], [b_jk5, brs])
                K.op(DVE, lambda: v_.tensor_tensor(out=rs_[:, 2:3], in0=rs_[:, 0:1], in1=rs_[:, 1:2], op=ALU.add), [brs], [brs])
                K.op(ACT, lambda: a_.activation(out=rs_[:, 3:4], in_=rs_[:, 2:3], func=AF.Sqrt, bias=EPS, scale=1.0 / D), [brs], [brs])
                K.op(DVE, lambda: v_.reciprocal(out=rs_[:, 4:5], in_=rs_[:, 3:4]), [brs], [brs])
                for hf in range(2):
                    cs_ = slice(hf * 512, (hf + 1) * 512)
                    K.op(DVE, lambda: v_.scalar_tensor_tensor(out=x1b[:, cs_], in0=ps[io[hf]][:, :], scalar=rs_[:, 4:5], in1=gmb[:, cs_],
                                                              op0=ALU.mult, op1=ALU.mult), [pb[io[hf]], brs, b_bc5], [bx1])
                K.op(POOL, lambda: g_.tensor_tensor(out=x1b[:], in0=x1b[:], in1=xb[:], op=ALU.add), [bx1, bxb], [bx1])
                sp_dma(x1_d[r0:r0 + 128, :], x1b[:], r=[bx1])
                K.op(ACT, lambda: a_.activation(out=jk5[:], in_=x1b[:], func=AF.Square, accum_out=rs_[:, 5:6]), [bx1], [b_jk5, brs])
                K.op(ACT, lambda: a_.activation(out=rs_[:, 6:7], in_=rs_[:, 5:6], func=AF.Sqrt, bias=EPS, scale=1.0 / D), [brs], [brs])
                K.op(DVE, lambda: v_.reciprocal(out=rs_[:, 7:8], in_=rs_[:, 6:7]), [brs], [brs])
                K.op(DVE, lambda: v_.scalar_tensor_tensor(out=h2_[:], in0=x1b[:], scalar=rs_[:, 7:8], in1=gsfb[:], op0=ALU.mult, op1=ALU.mult),
                     [bx1, brs, b_bc5], [bh2])
                K.op(POOL, lambda: g_.tensor_tensor(out=h2_[:], in0=h2_[:], in1=shfb[:], op=ALU.add), [bh2, b_bc5], [bh2])
                K.op(POOL, lambda: g_.tensor_copy(out=h2b_[:], in_=h2_[:]), [bh2], [bh2b])
                sp_dma(h2_d[r0:r0 + 128, :], h2b_[:], r=[bh2b])

            def m5_B(t):
                n, tt = t // TQ, t % TQ
                hb_ = h2Tb[n % 2]
                bhb = b_h2Tb[n % 2]
                T = b * NT_S + t
                p_ = t % 2
                h2_, h2T_ = h2[p_], h2T[p_]
                bh2, bh2T = b_h2[p_], b_h2T[p_]
                it = [nb(), nb()]
                for kc in range(8):
                    ib = it[kc // 4]
                    tr(ps[ib][:, (kc % 4) * 128:(kc % 4 + 1) * 128], h2_[:, kc * 128:(kc + 1) * 128], ident_f, [bh2, b_const], [pb[ib]],
                       kc % 4 == 3)
                for hf in range(2):
                    K.op(ACT, lambda: a_.copy(out=h2T_[:, hf * 4:(hf + 1) * 4, :], in_=ps[it[hf]][:, :].rearrange("p (a b) -> p a b", b=128)),
                         [pb[it[hf]]], [bh2T])
                K.op(POOL, lambda: g_.tensor_copy(out=hb_[:, :, tt * 128:(tt + 1) * 128], in_=h2T_[:]), [bh2T], [bhb])
                il = nb()
                for kc in range(8):
                    mm(ps[il][:, 0:E], h2T_[:, kc, :], wr[:, kc, :], kc == 0, kc == 7, [bh2T, b_wr], [pb[il]], kc == 7)
                R_, g8_, r8_, i8_, mk_ = rt_[p_], g8[p_], r8[p_], i8[p_], mkb[p_]
                brt = b_rt[p_]
                sc, sel, selm, mkf, wfull, tmp = (R_[:, k_, :] for k_ in range(6))
                K.op(ACT, lambda: a_.activation(out=sc, in_=ps[il][:, 0:E], func=AF.Sigmoid), [pb[il]], [brt])
                K.op(DVE, lambda: v_.tensor_tensor(out=sel, in0=sc, in1=rbias[:], op=ALU.add), [brt, b_const], [brt])
                for gg in range(8):
                    K.op(DVE, lambda: v_.max(out=g8_[:, gg, :], in_=sel[:, gg * 8:(gg + 1) * 8]), [brt], [brt])
                K.op(DVE, lambda: v_.tensor_tensor(out=r8_[:, 0, :], in0=g8_[:, :, 0], in1=g8_[:, :, 1], op=ALU.add), [brt], [brt])
                K.op(DVE, lambda: v_.max(out=r8_[:, 1, :], in_=r8_[:, 0, :]), [brt], [brt])
                K.op(DVE, lambda: v_.tensor_scalar(out=r8_[:, 2, :], in0=r8_[:, 0, :], scalar1=r8_[:, 1, 3:4], scalar2=-BIG,
                                                   op0=ALU.is_lt, op1=ALU.mult), [brt], [brt])
                K.op(DVE, lambda: v_.tensor_tensor(out=selm.rearrange("p (a b) -> p a b", b=8), in0=sel.rearrange("p (a b) -> p a b", b=8),
                                                   in1=r8_[:, 2, :].unsqueeze(2).to_broadcast([128, 8, 8]), op=ALU.add), [brt], [brt])
                K.op(DVE, lambda: v_.max(out=r8_[:, 3, :], in_=selm), [brt], [brt])
                K.op(DVE, lambda: v_.max_index(out=i8_[:], in_max=r8_[:, 3, :], in_values=selm), [brt], [brt])
                K.op(DVE, lambda: v_.tensor_scalar(out=mkf, in0=selm, scalar1=r8_[:, 3, 5:6], scalar2=None, op0=ALU.is_ge), [brt], [brt])
                K.op(DVE, lambda: v_.tensor_tensor(out=wfull, in0=sc, in1=mkf, op=ALU.mult), [brt], [brt])
                K.op(DVE, lambda: v_.tensor_reduce(out=r8_[:, 2, 0:1], in_=wfull, axis=AX.X, op=ALU.add), [brt], [brt])
                K.op(DVE, lambda: v_.reciprocal(out=r8_[:, 2, 1:2], in_=r8_[:, 2, 0:1]), [brt], [brt])
                K.op(POOL, lambda: g_.tensor_copy(out=mk_[:], in_=mkf), [brt], [brt])
                ik = nb()
                mm(ps[ik][:, 0:E], triu_b, mk_[:], True, True, [brt, b_const], [pb[ik]], False)
                mm(ps[ik][:, E:2 * E], ones_b, mk_[:], True, True, [brt, b_const], [pb[ik]], True)
                K.op(DVE, lambda: v_.tensor_tensor(out=tmp, in0=ps[ik][:, 0:E], in1=run[:], op=ALU.add), [pb[ik], b_run, brt], [brt])
                K.op(DVE, lambda: v_.tensor_tensor(out=rankm[:, T, :], in0=tmp, in1=mkf, op=ALU.mult), [brt], [b_route])
                K.op(DVE, lambda: v_.tensor_tensor(out=run[:], in0=run[:], in1=ps[ik][:, E:2 * E], op=ALU.add), [pb[ik], b_run], [b_run])
                K.op(DVE, lambda: v_.tensor_copy(out=eidx[:, T, :], in_=i8_[:]), [brt], [b_route])
                for k_ in range(TOPK):
                    K.op(DVE, lambda: v_.scalar_tensor_tensor(out=tmp, in0=iota64, scalar=eidx[:, T, k_:k_ + 1], in1=wfull,
                                                              op0=ALU.is_equal, op1=ALU.mult, accum_out=wk[:, T, k_:k_ + 1]),
                         [brt, b_route, b_const], [brt, b_route])
                K.op(DVE, lambda: v_.tensor_scalar(out=wkn[:, T, 0:TOPK], in0=wk[:, T, 0:TOPK], scalar1=r8_[:, 2, 1:2], scalar2=2.5,
                                                   op0=ALU.mult, op1=ALU.mult), [brt, b_route], [b_route])

            def m5_SH(n):
                hb_ = h2Tb[n % 2]
                bhb = b_h2Tb[n % 2]
                ig = [nb(), nb()]
                iu = [nb(), nb()]
                for c in range(2):
                    for kc in range(8):
                        mm(ps[ig[c]][:, 0:QB], wsg[:, kc, c * 128:(c + 1) * 128], hb_[:, kc, :], kc == 0, kc == 7, [b_wsg, bhb], [pb[ig[c]]], kc == 7)
                    for kc in range(8):
                        mm(ps[iu[c]][:, 0:QB], wsu[:, kc, c * 128:(c + 1) * 128], hb_[:, kc, :], kc == 0, kc == 7, [b_wsu, bhb], [pb[iu[c]]], kc == 7)
                    sg_ = sg[c]
                    K.op(ACT, lambda: a_.activation(out=sg_[:], in_=ps[ig[c]][:, 0:QB], func=AF.Sigmoid), [pb[ig[c]]], [b_sg[c]])
                    K.op(DVE, lambda: v_.tensor_tensor(out=sg_[:], in0=sg_[:], in1=ps[ig[c]][:, 0:QB], op=ALU.mult), [b_sg[c], pb[ig[c]]], [b_sg[c]])
                    K.op(DVE, lambda: v_.tensor_tensor(out=actT[:, c, :], in0=sg_[:], in1=ps[iu[c]][:, 0:QB], op=ALU.mult),
                         [b_sg[c], pb[iu[c]]], [b_act])
                for tt in range(TQ):
                    t = n * TQ + tt
                    r0 = tok0 + t * 128
                    iy = [nb(), nb()]
                    for hf in range(2):
                        for c in range(2):
                            mm(ps[iy[hf]][:, :], actT[:, c, tt * 128:(tt + 1) * 128], wsd[:, c, hf * 512:(hf + 1) * 512], c == 0, c == 1,
                               [b_act, b_wsd], [pb[iy[hf]]], c == 1)
                    sh_ = shs[t % 2]
                    K.op(ACT, lambda: a_.copy(out=sh_[:, 0:512], in_=ps[iy[0]][:, :]), [pb[iy[0]]], [b_shs[t % 2]])
                    K.op(DVE, lambda: v_.tensor_copy(out=sh_[:, 512:1024], in_=ps[iy[1]][:, :]), [pb[iy[1]]], [b_shs[t % 2]])
                    sp_dma(sh_d[r0:r0 + 128, :], sh_[:], r=[b_shs[t % 2]])

            m5_A(0)
            for t in range(NT_S):
                if t + 1 < NT_S:
                    m5_A(t + 1)
                m5_B(t)
                if t % TQ == TQ - 1:
                    m5_SH(t // TQ)
            K.barrier()

        al.release(mU)
        al.off = mU
        pe_ = al.alloc("pend", [128, 4, E], F32)
        pei = al.alloc("pei", [128, E], I32)
        ebf = al.alloc("ebf", [128, 2, NBLK], F32)
        djk = al.alloc("djk", [128, E], F32)
        rkp = al.alloc("rkp", [128, E], F32)
        destf = al.alloc("destf", [128, NT, 8], F32)
        b_pe, b_eb, b_dj, b_rkp, b_destf, b_desti = Buf(), Buf(), Buf(), Buf(), Buf(), Buf()
        b_desti_t = [Buf() for _ in range(NT)]
        K.op(DVE, lambda: v_.tensor_scalar(out=pe_[:, 3, :], in0=run[:], scalar1=float(CB - 1), scalar2=None, op0=ALU.add), [b_run], [b_pe])
        K.op(DVE, lambda: v_.tensor_copy(out=pei[:], in_=pe_[:, 3, :]), [b_pe], [b_pe])
        K.op(DVE, lambda: v_.tensor_single_scalar(out=pei[:], in_=pei[:], scalar=8, op=ALU.arith_shift_right), [b_pe], [b_pe])
        K.op(DVE, lambda: v_.tensor_copy(out=pe_[:, 0, :], in_=pei[:]), [b_pe], [b_pe])
        K.op(DVE, lambda: v_.tensor_tensor_scan(out=pe_[:, 1, :], data0=ones_c[:, 0:1].to_broadcast([128, E]), data1=pe_[:, 0, :],
                                                initial=0.0, op0=ALU.mult, op1=ALU.add), [b_pe, b_const], [b_pe])
        K.op(DVE, lambda: v_.tensor_tensor(out=pe_[:, 2, :], in0=pe_[:, 1, :], in1=pe_[:, 0, :], op=ALU.subtract), [b_pe], [b_pe])
        K.op(DVE, lambda: v_.tensor_scalar(out=pe_[:, 2, :], in0=pe_[:, 2, :], scalar1=float(CB), scalar2=None, op0=ALU.mult), [b_pe], [b_pe])
        for T in range(NT):
            K.op(DVE, lambda: v_.tensor_tensor(out=rkp[:], in0=rankm[:, T, :], in1=pe_[:, 2, :], op=ALU.add), [b_route, b_pe, b_rkp], [b_rkp])
            for k_ in range(TOPK):
                K.op(DVE, lambda: v_.scalar_tensor_tensor(out=djk[:], in0=iota64, scalar=eidx[:, T, k_:k_ + 1], in1=rkp[:],
                                                          op0=ALU.is_equal, op1=ALU.mult, accum_out=destf[:, T, k_:k_ + 1]),
                     [b_route, b_rkp, b_const], [b_dj, b_destf])
            K.op(DVE, lambda: v_.tensor_copy(out=desti[:, T, 0:TOPK], in_=destf[:, T, 0:TOPK]), [b_destf], [b_desti_t[T]])
        hg = [al.alloc("hg", [128, D], BF16) for _ in range(3)]
        b_hg = [Buf() for _ in range(3)]
        b_xs = Buf()
        for T in range(NT):
            hb_ = hg[T % 3]
            sp_dma(hb_[:], h2_d[T * 128:(T + 1) * 128, :], w=[b_hg[T % 3]])
            for k_ in range(TOPK):
                K.dma(K.qpool, lambda: g_.indirect_dma_start(out=xs_d[:, :], out_offset=bass.IndirectOffsetOnAxis(ap=desti[:, T, k_:k_ + 1], axis=0),
                                                             in_=hb_[:], in_offset=None, bounds_check=reg_slot, oob_is_err=False),
                      [b_hg[T % 3], b_desti_t[T], b_xs0], [], [b_xs])
        K.op(DVE, lambda: v_.memset(ebf[:, 0, :], 0.0), [], [b_eb])
        for e_ in range(E):
            K.op(DVE, lambda: v_.scalar_tensor_tensor(out=ebf[:, 0, :], in0=iotab, scalar=pe_[:, 1, e_:e_ + 1], in1=ebf[:, 0, :],
                                                      op0=ALU.is_ge, op1=ALU.add), [b_pe, b_const, b_eb], [b_eb])
        K.op(DVE, lambda: v_.tensor_scalar(out=ebf[:, 0, :], in0=ebf[:, 0, :], scalar1=float(E - 1), scalar2=128.0, op0=ALU.min, op1=ALU.mult),
             [b_eb], [b_eb])
        K.op(DVE, lambda: v_.tensor_scalar(out=ebf[:, 1, :], in0=ebf[:, 0, :], scalar1=pidx, scalar2=None, op0=ALU.add), [b_eb, b_const], [b_eb])
        if skip_reload and NBLK > 2:
            K.op(DVE, lambda: v_.tensor_tensor(out=ebf[:, 0, 2:NBLK], in0=ebf[:, 0, 2:NBLK], in1=ebf[:, 1, 0:NBLK - 2], op=ALU.subtract),
                 [b_eb], [b_eb])
            K.op(DVE, lambda: v_.tensor_scalar(out=ebf[:, 0, 2:NBLK], in0=ebf[:, 0, 2:NBLK], scalar1=pidx, scalar2=None, op0=ALU.add),
                 [b_eb, b_const], [b_eb])
            K.op(DVE, lambda: v_.tensor_scalar(out=ebf[:, 0, 2:NBLK], in0=ebf[:, 0, 2:NBLK], scalar1=0.0, scalar2=1.0e6,
                                               op0=ALU.is_equal, op1=ALU.mult), [b_eb], [b_eb])
            K.op(DVE, lambda: v_.tensor_tensor(out=ebf[:, 1, 2:NBLK], in0=ebf[:, 1, 2:NBLK], in1=ebf[:, 0, 2:NBLK], op=ALU.add), [b_eb], [b_eb])
        K.op(DVE, lambda: v_.tensor_copy(out=widx[:], in_=ebf[:, 1, :]), [b_eb], [b_widx])
        K.barrier()

        al.off = mU
        wE = [[al.alloc("wE", [128, 2048], BF16) for _ in range(3)] for _ in range(2)]
        b_wE = [[Buf() for _ in range(3)] for _ in range(2)]
        xsb = [al.alloc("xsb", [128, 2, D], BF16) for _ in range(3)]
        xTe = [al.alloc("xTe", [128, 8, CB], BF16) for _ in range(3)]
        sge = [al.alloc("sge", [128, 2 * CB], F32) for _ in range(2)]
        acte = [al.alloc("acte", [128, 2 * CB], BF16) for _ in range(2)]
        ysb = [al.alloc("ysb", [128, D], BF16) for _ in range(4)]
        b_xsb, b_xTe, b_sge, b_acte = [Buf(), Buf(), Buf()], [Buf(), Buf(), Buf()], [Buf(), Buf()], [Buf(), Buf()]
        b_ysb = [Buf() for _ in range(4)]
        b_ys = Buf()
        rot["l"] = list(range(8))
        wsrc = wpb_d

        def load_wE(blk, which):
            s_ = blk % 2
            for m in which:
                K.dma(K.qpool, lambda: g_.indirect_dma_start(out=wE[s_][m][:], out_offset=None, in_=wsrc[m][:, :],
                                                             in_offset=bass.IndirectOffsetOnAxis(ap=widx[:, blk:blk + 1], axis=0),
                                                             bounds_check=reg_w, oob_is_err=False),
                      [b_widx, b_wcast], [b_wE[s_][m]])

        def load_xs(blk):
            p_ = blk % 3
            sp_dma(xsb[p_][:], xs_d[blk * CB:(blk + 1) * CB, :].rearrange("(s p) d -> p s d", p=128), r=[b_xs], w=[b_xsb[p_]])

        def stage_T(blk):
            p_ = blk % 3
            for s2 in range(2):
                i = nb()
                pT = ps[i][:, :].bitcast(BF16)
                for kc in range(8):
                    tr(pT[:, kc * 128:(kc + 1) * 128], xsb[p_][:, s2, kc * 128:(kc + 1) * 128], ident_b, [b_xsb[p_], b_const], [pb[i]], kc == 7)
                o_ap = xTe[p_][:, :, s2 * 128:(s2 + 1) * 128]
                i_ap = pT.rearrange("p (a b) -> p a b", b=128)
                if s2 == 0:
                    K.op(ACT, lambda: a_.copy(out=o_ap, in_=i_ap), [pb[i]], [b_xTe[p_]])
                else:
                    K.op(DVE, lambda: v_.tensor_copy(out=o_ap, in_=i_ap), [pb[i]], [b_xTe[p_]])

        def stage_GU(blk):
            s_ = blk % 2
            p_ = blk % 2
            x_ = blk % 3
            wgE, wuE, wdE = wE[s_]
            bwg, bwu, bwd = b_wE[s_]
            ig_, iu_ = nb(), nb()
            for c in range(2):
                for kc in range(8):
                    mm(ps[ig_][:, c * CB:(c + 1) * CB], wgE[:, kc * FF + c * 128: kc * FF + (c + 1) * 128], xTe[x_][:, kc, :], kc == 0, kc == 7,
                       [bwg, b_xTe[x_]], [pb[ig_]], kc == 7)
            for c in range(2):
                for kc in range(8):
                    mm(ps[iu_][:, c * CB:(c + 1) * CB], wuE[:, kc * FF + c * 128: kc * FF + (c + 1) * 128], xTe[x_][:, kc, :], kc == 0, kc == 7,
                       [bwu, b_xTe[x_]], [pb[iu_]], kc == 7)
            K.op(ACT, lambda: a_.activation(out=sge[p_][:], in_=ps[ig_][:, :], func=AF.Sigmoid), [pb[ig_]], [b_sge[p_]])
            K.op(DVE, lambda: v_.tensor_tensor(out=sge[p_][:], in0=sge[p_][:], in1=ps[ig_][:, :], op=ALU.mult), [b_sge[p_], pb[ig_]], [b_sge[p_]])
            K.op(DVE, lambda: v_.tensor_tensor(out=acte[p_][:], in0=sge[p_][:], in1=ps[iu_][:, :], op=ALU.mult), [b_sge[p_], pb[iu_]], [b_acte[p_]])

        ycnt = {"n": 0}

        def stage_D(blk):
            s_ = blk % 2
            p_ = blk % 2
            wdE = wE[s_][2]
            bwd = b_wE[s_][2]
            for s2 in range(2):
                yb = ysb[ycnt["n"] % 4]
                byb = b_ysb[ycnt["n"] % 4]
                ycnt["n"] += 1
                for hf in range(2):
                    i = nb()
                    for c in range(2):
                        mm(ps[i][:, :], acte[p_][:, c * CB + s2 * 128: c * CB + (s2 + 1) * 128], wdE[:, c * D + hf * 512: c * D + (hf + 1) * 512],
                           c == 0, c == 1, [b_acte[p_], bwd], [pb[i]], c == 1)
                    if hf == 0:
                        K.op(ACT, lambda: a_.copy(out=yb[:, 0:512], in_=ps[i][:, :]), [pb[i]], [byb])
                    else:
                        K.op(DVE, lambda: v_.tensor_copy(out=yb[:, 512:1024], in_=ps[i][:, :]), [pb[i]], [byb])
                r0 = blk * CB + s2 * 128
                sp_dma(ys_d[r0:r0 + 128, :], yb[:], r=[byb], sw=[b_ys])

        precast(3 * E)
        load_wE(0, (0, 1, 2))
        if NBLK > 1:
            load_wE(1, (0, 1, 2))
        for b0 in range(min(3, NBLK)):
            load_xs(b0)
        stage_T(0)
        if NBLK > 1:
            stage_T(1)
        stage_GU(0)
        for blk in range(NBLK):
            if blk + 2 < NBLK:
                load_wE(blk + 2, (0, 1))
                stage_T(blk + 2)
                if blk + 3 < NBLK:
                    load_xs(blk + 3)
            if blk >= 1:
                stage_D(blk - 1)
                if blk + 1 < NBLK:
                    load_wE(blk + 1, (2,))
            if blk + 1 < NBLK:
                stage_GU(blk + 1)
        stage_D(NBLK - 1)
        K.barrier()

        al.off = mU
        gfb = al.alloc("gfb", [128, NSEQ, D], F32)
        bcvc = al.alloc("bcvc", [128, D], F32)
        b_bcvc = Buf()
        sp_dma(bcvc[:], bcv_d[:, 2, :], w=[b_bcvc])
        b_gfb = Buf()
        for b in range(NSEQ):
            sp_dma(gfb[:, b, :], modd[b:b + 1, 5 * D:6 * D].partition_broadcast(128), r=[b_modd], w=[b_gfb])
            K.op(DVE, lambda: v_.tensor_tensor(out=gfb[:, b, :], in0=gfb[:, b, :], in1=bcvc[:], op=ALU.mult), [b_gfb, b_bcvc], [b_gfb])
        x1c = [al.alloc("x1c", [128, D], F32) for _ in range(2)]
        zc = [al.alloc("zc", [128, D], F32) for _ in range(2)]
        yg = [al.alloc("yg", [128, D], BF16) for _ in range(12)]
        shc = [al.alloc("shc", [128, D], BF16) for _ in range(2)]
        b_shc = [Buf(), Buf()]
        jkc = al.alloc("jkc", [128, D], BF16)
        rc_ = [al.alloc("rc", [128, 4], F32) for _ in range(2)]
        b_x1c, b_zc, b_rc = [Buf(), Buf()], [Buf(), Buf()], [Buf(), Buf()]
        b_yg = [Buf() for _ in range(12)]
        b_jkc = Buf()
        b_out = Buf()
        z2 = [al.alloc("z2", [128, D], F32) for _ in range(2)]
        b_z2 = [Buf(), Buf()]
        z3 = [al.alloc("z3", [128, D], F32) for _ in range(2)]
        b_z3 = [Buf(), Buf()]
        gc = {"n": 0}

        dg = [al.alloc("dg", [128, 128], BF16) for _ in range(12)]
        b_dg = [Buf() for _ in range(12)]
        rot["l"] = list(range(8))
        cps = {}

        def c_A(T):
            p_ = T % 2
            r0 = T * 128
            sp_dma(x1c[p_][:], x1_d[r0:r0 + 128, :], w=[b_x1c[p_]])
            sp_dma(shc[p_][:], sh_d[r0:r0 + 128, :], w=[b_shc[p_]])
            ys_ = []
            for k_ in range(TOPK):
                y_ = yg[gc["n"] % 12]
                by = b_yg[gc["n"] % 12]
                d_ = dg[gc["n"] % 12]
                bd = b_dg[gc["n"] % 12]
                gc["n"] += 1
                K.dma(K.qpool, lambda: g_.indirect_dma_start(out=y_[:], out_offset=None, in_=ys_d[:, :],
                                                             in_offset=bass.IndirectOffsetOnAxis(ap=desti[:, T, k_:k_ + 1], axis=0),
                                                             bounds_check=reg_slot, oob_is_err=False),
                      [b_ys, b_desti_t[T]], [by])
                K.op(ACT, lambda: a_.activation(out=d_[:], in_=ident_f, func=AF.Identity, scale=wkn[:, T, k_:k_ + 1]),
                     [b_const, b_route], [bd])
                ys_.append((y_, by, d_, bd))
            banks = [nb(), nb()]
            cps[T] = banks
            for hf in range(2):
                i = banks[hf]
                cs_ = slice(hf * 512, (hf + 1) * 512)
                mm(ps[i][:, :], ident_b, shc[p_][:, cs_], True, False, [b_const, b_shc[p_]], [pb[i]], False)
                for k_ in range(TOPK):
                    y_, by, d_, bd = ys_[k_]
                    mm(ps[i][:, :], d_[:], y_[:, cs_], False, k_ == TOPK - 1, [bd, by], [pb[i]], k_ == TOPK - 1)

        def c_B(T):
            b = T // NT_S
            p_ = T % 2
            r0 = T * 128
            banks = cps.pop(T)
            for hf in range(2):
                K.op(ACT, lambda: a_.activation(out=jkc[:, hf * 512:(hf + 1) * 512], in_=ps[banks[hf]][:, :], func=AF.Square,
                                                accum_out=rc_[p_][:, hf:hf + 1]), [pb[banks[hf]]], [b_jkc, b_rc[p_]])
            K.op(DVE, lambda: v_.tensor_tensor(out=rc_[p_][:, 3:4], in0=rc_[p_][:, 0:1], in1=rc_[p_][:, 1:2], op=ALU.add), [b_rc[p_]], [b_rc[p_]])
            K.op(ACT, lambda: a_.activation(out=rc_[p_][:, 1:2], in_=rc_[p_][:, 3:4], func=AF.Sqrt, bias=EPS, scale=1.0 / D), [b_rc[p_]], [b_rc[p_]])
            K.op(DVE, lambda: v_.reciprocal(out=rc_[p_][:, 2:3], in_=rc_[p_][:, 1:2]), [b_rc[p_]], [b_rc[p_]])
            for hf in range(2):
                cs_ = slice(hf * 512, (hf + 1) * 512)
                K.op(DVE, lambda: v_.scalar_tensor_tensor(out=zc[p_][:, cs_], in0=ps[banks[hf]][:, :], scalar=rc_[p_][:, 2:3], in1=gfb[:, b, cs_],
                                                          op0=ALU.mult, op1=ALU.mult), [pb[banks[hf]], b_rc[p_], b_gfb], [b_zc[p_]])
            K.op(DVE, lambda: v_.tensor_tensor(out=zc[p_][:], in0=zc[p_][:], in1=x1c[p_][:], op=ALU.add), [b_zc[p_], b_x1c[p_]], [b_zc[p_]])
            sp_dma(out_d[r0:r0 + 128, :], zc[p_][:], r=[b_zc[p_]], sw=[b_out])

        c_A(0)
        for T in range(NT):
            if T + 1 < NT:
                c_A(T + 1)
            c_B(T)
        K.barrier()
    return nc


def _bf16():
    import ml_dtypes
    return ml_dtypes.bfloat16


def make_consts(NBLK):
    NCF = 128 + 128 + 64 + NBLK + 2
    cf = np.zeros((128, NCF), np.float32)
    cf[:, 0:128] = np.eye(128, dtype=np.float32)
    cf[127, 128:256] = 1.0
    cf[:, 256:320] = np.arange(64, dtype=np.float32)[None, :]
    cf[:, 320:320 + NBLK] = np.arange(NBLK, dtype=np.float32)[None, :]
    cf[:, 320 + NBLK] = np.arange(128, dtype=np.float32)
    cf[:, 321 + NBLK] = 1.0
    cb = np.zeros((128, 1792), np.float32)
    cb[:, 0:128] = np.eye(128)
    k = np.arange(128)[:, None]
    m = np.arange(128)[None, :]
    cb[:, 128:256] = (k < m)
    cb[:, 256:384] = (m >= k)
    cb[:, 384:512] = 1.0
    for h in range(NH):
        for g3 in range(3):
            cb[g3 * 32 + h, 512 + h * 128:512 + (h + 1) * 128] = 1.0
    cb[:, 1536:1664] = np.where(m < k, -30000.0, 0.0)
    cb[:, 1664:1792] = (m == (k + 64) % 128)
    return cf, cb.astype(_bf16())


def prep_shared(inp):
    f = np.float32
    sh = {}
    sh["w_ada"] = np.ascontiguousarray(inp["w_ada"][0], f)
    fm = np.zeros((128, 9, 8), f)

    def fmaj(v):
        return np.asarray(v, f).reshape(8, 128).T
    fm[:, 0, :] = fmaj(inp["g_pre_mix"][0])
    for j in range(4):
        fm[:, 1 + j, :] = fmaj(inp["w_conv"][0, j])
    fm[:, 5, :] = fmaj(inp["b_conv"][0])
    fm[:, 6, :] = fmaj(inp["b_rg"][0])
    fm[:, 7, :] = fmaj(inp["b_ig"][0])
    fm[:, 8, :] = fmaj(inp["rglru_lambda"][0])
    sh["fm"] = fm
    bcv = np.zeros((128, 3, D), f)
    bcv[:, 0, :] = np.asarray(inp["g_post_mix"][0], f)[None, :]
    bcv[:, 1, :] = np.asarray(inp["g_pre_ffn"][0], f)[None, :]
    bcv[:, 2, :] = np.asarray(inp["g_post_ffn"][0], f)[None, :]
    sh["bcv"] = bcv
    sh["rbias"] = np.ascontiguousarray(np.broadcast_to(np.asarray(inp["router_bias"][0], f)[None, :], (128, E)))
    sh["b_forget"] = np.asarray(inp["b_forget"][0], f).reshape(NH, 1)
    sh["w_in"] = np.ascontiguousarray(inp["w_in"][0], f)
    for nm, src in (("wrg_bd", inp["w_rg"][0]), ("wig_bd", inp["w_ig"][0])):
        bd = np.zeros((8, 128, 128), f)
        for c in range(8):
            bd[c, 0:64, 0:64] = src[2 * c]
            bd[c, 64:128, 64:128] = src[2 * c + 1]
        sh[nm] = bd
    sh["w_ba"] = np.ascontiguousarray(inp["w_branch_attn"][0], f)
    sh["w_br"] = np.ascontiguousarray(inp["w_branch_rnn"][0], f)
    sh["w_out"] = np.ascontiguousarray(inp["w_out"][0], f)
    sh["w_router"] = np.ascontiguousarray(inp["w_router"][0], f)
    sh["w_sg"] = np.ascontiguousarray(inp["w_sh_gate"][0], f)
    sh["w_su"] = np.ascontiguousarray(inp["w_sh_up"][0], f)
    sh["w_sd"] = np.ascontiguousarray(inp["w_sh_down"][0], f)
    wg = np.asarray(inp["w_exp_gate"][0], f).reshape(E, 8, 128, FF).transpose(0, 2, 1, 3)
    sh["wpg"] = np.ascontiguousarray(wg).reshape(E * 128, 2048)
    wu = np.asarray(inp["w_exp_up"][0], f).reshape(E, 8, 128, FF).transpose(0, 2, 1, 3)
    sh["wpu"] = np.ascontiguousarray(wu).reshape(E * 128, 2048)
    wd = np.asarray(inp["w_exp_down"][0], f).reshape(E, 2, 128, D).transpose(0, 2, 1, 3)
    sh["wpd"] = np.ascontiguousarray(wd).reshape(E * 128, 2048)
    return sh


def prep_core(inp, sh, core, NSEQ, S, NBLK):
    f = np.float32
    m = dict(sh)
    xs = np.asarray(inp["x"][core * NSEQ:(core + 1) * NSEQ], f).reshape(NSEQ * S, D)
    m["x"] = np.ascontiguousarray(xs)
    c = np.asarray(inp["c"][core * NSEQ:(core + 1) * NSEQ], f)
    m["csT"] = np.ascontiguousarray(c.T.reshape(8, 128, NSEQ).transpose(1, 0, 2))
    m["b_ada_rep"] = np.ascontiguousarray(np.broadcast_to(np.asarray(inp["b_ada"][0], f)[None, :], (NSEQ, 6 * D)))
    cf, cb = make_consts(NBLK)
    m["cf"] = cf
    m["cb"] = cb
    return m


def kernel(**inputs):
    B, S = inputs["x"].shape[0], inputs["x"].shape[1]
    NSEQ = B // NCORES
    NTOK = NSEQ * S
    NBLK = (NTOK * TOPK) // CB + E
    nc = build(NSEQ, S, skip_reload=True)
    sh = prep_shared(inputs)
    in_maps = [prep_core(inputs, sh, i, NSEQ, S, NBLK) for i in range(NCORES)]
    res = run_bass_kernel_spmd(nc, in_maps, core_ids=list(range(NCORES)))
    outs = [np.asarray(r["out"], np.float32).reshape(NSEQ, S, D) for r in res.results]
    return np.concatenate(outs, axis=0)
```

```python
import numpy as np
import concourse.bass as bass
import concourse.mybir as mybir
from concourse.bass_utils import run_bass_kernel_spmd
from contextlib import ExitStack

F32 = mybir.dt.float32
BF16 = mybir.dt.bfloat16
I32 = mybir.dt.int32
U32 = mybir.dt.uint32
AF = mybir.ActivationFunctionType
ALU = mybir.AluOpType
AX = mybir.AxisListType

D = 1024
NH = 8
E = 64
TOPK = 6
FF = 256
CB = 256
INC = 5640
OQ, OK_, OV, OF_, OX, OG, OGA, OGR = 0, 512, 1024, 1536, 1544, 2568, 3592, 4616
EPS = 1e-6
BIG = 1.0e4
NCORES = 8
ARENA_SHIFT = [0]
ARENA_MAX = [0]


class Buf:
    __slots__ = ("w", "r", "name")

    def __init__(self, name=""):
        self.w = {}
        self.r = {}
        self.name = name


class Eng:
    def __init__(self, name, e, sem, key):
        self.name = name
        self.e = e
        self.sem = sem
        self.key = key
        self.n = 0
        self.seen = {}
        self.pending = False


class DQ:
    def __init__(self, eng, sems):
        self.eng = eng
        self.sems = sems
        self.cnt = [0] * len(sems)
        self.next = 0


def _merge(d, s):
    for k, v in s.items():
        if d.get(k, 0) < v:
            d[k] = v


class KB:
    def __init__(self, nc, stack):
        self.nc = nc
        self.semtab = {}
        self.engs = []
        for nm, e in (("pe", nc.tensor), ("act", nc.scalar), ("dve", nc.vector),
                      ("pool", nc.gpsimd), ("sp", nc.sync)):
            sem = stack.enter_context(nc.semaphore("s_" + nm))
            eng = Eng(nm, e, sem, "c_" + nm)
            self.semtab[eng.key] = sem
            setattr(self, nm, eng)
            self.engs.append(eng)
        self.queues = []
        for nm, eng, n in (("qsp", self.sp, 8), ("qpool", self.pool, 6)):
            sems = []
            for i in range(n):
                key = "d_%s%d" % (nm, i)
                sem = stack.enter_context(nc.semaphore(key))
                self.semtab[key] = sem
                sems.append((sem, key))
            q = DQ(eng, sems)
            setattr(self, nm, q)
            self.queues.append(q)

    def _wait(self, E_, deps):
        for k, v in deps.items():
            if E_.seen.get(k, 0) < v:
                E_.e.wait_ge(self.semtab[k], v)
                E_.seen[k] = v

    def op(self, E_, fn, reads=(), writes=(), inc=True):
        deps = {}
        for b in reads:
            _merge(deps, b.w)
        for b in writes:
            _merge(deps, b.w)
            _merge(deps, b.r)
        if E_.name == "pe":
            deps.pop(E_.key, None)
        self._wait(E_, deps)
        ins = fn()
        ev = E_.n + 1
        for b in reads:
            if b.r.get(E_.key, 0) < ev:
                b.r[E_.key] = ev
        for b in writes:
            b.w = {E_.key: ev}
            b.r = {}
        if inc:
            E_.n = ev
            ins.then_inc(E_.sem, 1)
            E_.pending = False
        else:
            E_.pending = True
        return ins

    def dma(self, Q, fn, reads=(), writes=(), swrites=()):
        E_ = Q.eng
        deps = {}
        for b in reads:
            _merge(deps, b.w)
        for b in writes:
            _merge(deps, b.w)
            _merge(deps, b.r)
        for b in swrites:
            _merge(deps, b.r)
        slot = Q.next
        Q.next = (Q.next + 1) % len(Q.sems)
        sem, key = Q.sems[slot]
        if Q.cnt[slot] > 0:
            if deps.get(key, 0) < 16 * Q.cnt[slot]:
                deps[key] = 16 * Q.cnt[slot]
        self._wait(E_, deps)
        ins = fn()
        Q.cnt[slot] += 1
        v = 16 * Q.cnt[slot]
        ins.then_inc(sem, 16)
        for b in reads:
            if b.r.get(key, 0) < v:
                b.r[key] = v
        for b in writes:
            b.w = {key: v}
            b.r = {}
        for b in swrites:
            if b.w.get(key, 0) < v:
                b.w[key] = v
        return ins

    def barrier(self):
        tot = {}
        for E_ in self.engs:
            assert not E_.pending
            if E_.n > 0:
                tot[E_.key] = E_.n
        for Q in self.queues:
            for i, (sem, key) in enumerate(Q.sems):
                if Q.cnt[i] > 0:
                    tot[key] = 16 * Q.cnt[i]
        for E_ in self.engs:
            self._wait(E_, dict(tot))


class Arena:
    def __init__(self, nc, limit):
        self.nc = nc
        self.off = 0
        self.limit = limit
        self.n = 0

    def alloc(self, name, shape, dtype):
        sz = 1
        for s in shape[1:]:
            sz *= s
        sz *= {F32: 4, BF16: 2, I32: 4, U32: 4}[dtype]
        sz = (sz + 63) // 64 * 64
        off = self.off
        assert off + sz <= self.limit, ("SBUF arena overflow", name, off, sz)
        self.off += sz
        self.n += 1
        ARENA_MAX[0] = max(ARENA_MAX[0], self.off)
        return self.nc.alloc_sbuf_tensor_at("%s_%d" % (name, self.n), list(shape), dtype, offset=off)

    def mark(self):
        return self.off

    def release(self, m):
        self.off = m


def build(NSEQ, S, skip_reload=True):
    NT_S = S // 128
    NTOK = NSEQ * S
    NT = NTOK // 128
    QB = min(512, S)
    TQ = QB // 128
    NQ = S // QB
    NB5 = S // QB
    NBLK = (NTOK * TOPK) // CB + E
    NSLOT = NBLK * CB

    nc = bass.Bass("TRN2", target_bir_lowering=False)
    dt = nc.dram_tensor

    def ein(name, shape, dtype=F32):
        return dt(name, list(shape), dtype, kind="ExternalInput")

    x_d = ein("x", [NTOK, D])
    cs_d = ein("csT", [128, 8, NSEQ])
    wada_d = ein("w_ada", [D, 6 * D])
    bada_d = ein("b_ada_rep", [NSEQ, 6 * D])
    fm_d = ein("fm", [128, 9, 8])
    bcv_d = ein("bcv", [128, 3, D])
    rb_d = ein("rbias", [128, E])
    bf_d = ein("b_forget", [NH, 1])
    win_d = ein("w_in", [D, INC])
    wrg_d = ein("wrg_bd", [8, 128, 128])
    wig_d = ein("wig_bd", [8, 128, 128])
    wba_d = ein("w_ba", [512, D])
    wbr_d = ein("w_br", [D, D])
    wout_d = ein("w_out", [D, D])
    wr_d = ein("w_router", [D, E])
    wsg_d = ein("w_sg", [D, FF])
    wsu_d = ein("w_su", [D, FF])
    wsd_d = ein("w_sd", [FF, D])
    wpg_d = ein("wpg", [E * 128, 2048])
    wpu_d = ein("wpu", [E * 128, 2048])
    wpd_d = ein("wpd", [E * 128, 2048])
    NCF = 128 + 128 + 64 + NBLK + 2
    cf_d = ein("cf", [128, NCF])
    cb_d = ein("cb", [128, 1792], BF16)
    out_d = dt("out", [NTOK, D], F32, kind="ExternalOutput")
    modd = dt("modd", [NSEQ, 6 * D], F32)
    h2_d = dt("h2s", [NTOK, D], BF16)
    x1_d = dt("x1s", [NTOK, D], F32)
    sh_d = dt("shs", [NTOK, D], BF16)
    xs_d = dt("xss", [NSLOT, D], BF16)
    ys_d = dt("yss", [NSLOT, D], BF16)
    wpb_d = [dt("wpb%d" % m_, [E * 128, 2048], BF16) for m_ in range(3)]

    stack = ExitStack()
    with stack:
        K = KB(nc, stack)
        al = Arena(nc, int(nc._sbuf_addr_for_side("right")) - 64)
        al.off = (int(nc._sbuf_addr_for_side("left")) + 63) // 64 * 64 + ARENA_SHIFT[0]
        ps = [stack.enter_context(nc.psum_tensor("ps%d" % i, [128, 512], F32)) for i in range(8)]
        pb = [Buf("pb%d" % i) for i in range(8)]
        rot = {"l": list(range(8)), "i": 0}

        def nb():
            i = rot["l"][rot["i"] % len(rot["l"])]
            rot["i"] += 1
            return i

        PE, ACT, DVE, POOL = K.pe, K.act, K.dve, K.pool
        reg_slot = nc.gpsimd.alloc_register("bc_slot")
        nc.gpsimd.reg_mov(reg_slot, NSLOT - 1)
        reg_w = nc.gpsimd.alloc_register("bc_w")
        nc.gpsimd.reg_mov(reg_w, E * 128 - 1)
        v_, a_, g_, t_ = nc.vector, nc.scalar, nc.gpsimd, nc.tensor

        def mm(out, lhsT, rhs, start, stop, r, w, inc):
            return K.op(PE, lambda: t_.matmul(out, lhsT, rhs, start=start, stop=stop), r, w, inc)

        def tr(out, in_, ident, r, w, inc):
            return K.op(PE, lambda: t_.transpose(out, in_, ident), r, w, inc)

        def sp_dma(out, in_, r=(), w=(), sw=()):
            return K.dma(K.qsp, lambda: nc.sync.dma_start(out=out, in_=in_), r, w, sw)

        def pl_dma(out, in_, r=(), w=(), sw=()):
            return K.dma(K.qpool, lambda: nc.gpsimd.dma_start(out=out, in_=in_), r, w, sw)

        cf = al.alloc("cf", [128, NCF], F32)
        cbt = al.alloc("cb", [128, 1792], BF16)
        b_const = Buf("const")
        ident_f = cf[:, 0:128]
        sel127 = cf[:, 128:256]
        iota64 = cf[:, 256:320]
        iotab = cf[:, 320:320 + NBLK]
        pidx = cf[:, 320 + NBLK:321 + NBLK]
        ones_c = cf[:, 321 + NBLK:322 + NBLK]
        ident_b = cbt[:, 0:128]
        triu_b = cbt[:, 128:256]
        trim_b = cbt[:, 256:384]
        ones_b = cbt[:, 384:512]
        negm_b = cbt[:, 1536:1664]
        swap_b = cbt[:, 1664:1792]
        fm = al.alloc("fm", [128, 9, 8], F32)
        sm = al.alloc("sm", [128, 6, 8], F32)
        rbias = al.alloc("rbias", [128, E], F32)
        nbf = al.alloc("nbf", [128, 2], F32)
        b_nbf = Buf()
        gsmT = al.alloc("gsmT", [128, 8, NSEQ], F32)
        shmT = al.alloc("shmT", [128, 8, NSEQ], F32)
        run = al.alloc("run", [128, E], F32)
        rankm = al.alloc("rankm", [128, NT, E], F32)
        eidx = al.alloc("eidx", [128, NT, 8], F32)
        wk = al.alloc("wk", [128, NT, 8], F32)
        wkn = al.alloc("wkn", [128, NT, 8], F32)
        desti = al.alloc("desti", [128, NT, 8], I32)
        widx = al.alloc("widx", [128, NBLK], I32)
        b_fm, b_sm, b_gs, b_run, b_route = Buf(), Buf(), Buf(), Buf(), Buf()
        b_widx = Buf()
        zt = al.alloc("zt", [128, 2, D], BF16)
        b_zt, b_xs0 = Buf(), Buf()
        K.op(POOL, lambda: g_.memset(zt[:], 0.0), [], [b_zt])
        zf = {"n": 0}
        ZF_PER = -(-NBLK // NT)

        b_wcast = Buf()
        pcast = {"n": 0}
        PC_PER = -(-(3 * E) // (NSEQ * 4 * NQ))

        def precast(cnt):
            for _ in range(cnt):
                if pcast["n"] < 3 * E:
                    e_, m_ = pcast["n"] // 3, pcast["n"] % 3
                    src = (wpg_d, wpu_d, wpd_d)[m_]
                    pl_dma(wpb_d[m_][e_ * 128:(e_ + 1) * 128, :], src[e_ * 128:(e_ + 1) * 128, :], sw=[b_wcast])
                    pcast["n"] += 1

        def zero_fill(cnt):
            for _ in range(cnt):
                if zf["n"] < NBLK:
                    r0_ = zf["n"] * CB
                    sp_dma(xs_d[r0_:r0_ + CB, :].rearrange("(s p) d -> p s d", p=128), zt[:], r=[b_zt], sw=[b_xs0])
                    zf["n"] += 1

        sp_dma(cf[:], cf_d.ap(), w=[b_const])
        sp_dma(cbt[:], cb_d.ap(), w=[b_const])
        sp_dma(fm[:], fm_d.ap(), w=[b_fm])
        sp_dma(rbias[:], rb_d.ap(), w=[b_const])
        K.op(DVE, lambda: v_.memset(nbf[:], 0.0), [], [b_nbf])
        for g3 in range(3):
            sp_dma(nbf[g3 * 32:g3 * 32 + NH, 0:1], bf_d.ap(), w=[b_nbf])
        K.op(DVE, lambda: v_.memset(run[:], 0.0), [], [b_run])

        m0 = al.mark()
        cs = al.alloc("cs", [128, 8, NSEQ], F32)
        th0 = al.alloc("th0", [128, 8, NSEQ], F32)
        siluT = al.alloc("siluT", [128, 8, NSEQ], BF16)
        modt = al.alloc("modt", [NSEQ, 6 * D], F32)
        bada = al.alloc("bada", [NSEQ, 6 * D], F32)
        wada = [al.alloc("wada", [128, 8, 512], BF16) for _ in range(2)]
        b_cs, b_th0, b_silu, b_modt, b_bada = Buf(), Buf(), Buf(), Buf(), Buf()
        b_wada = [Buf(), Buf()]

        K.op(DVE, lambda: v_.tensor_scalar(out=nbf[0:72, 1:2], in0=nbf[0:72, 0:1], scalar1=-1.0, scalar2=None,
                                           op0=ALU.mult), [b_nbf], [b_nbf])
        K.op(ACT, lambda: a_.activation(out=sm[:, 4, :], in_=fm[:, 8, :], func=AF.Exp, scale=-1.0), [b_fm], [b_sm])
        K.op(ACT, lambda: a_.activation(out=sm[:, 5, :], in_=sm[:, 4, :], func=AF.Ln, bias=1.0, scale=1.0), [b_sm], [b_sm])
        K.op(DVE, lambda: v_.tensor_scalar(out=sm[:, 0, :], in0=sm[:, 5, :], scalar1=-8.0, scalar2=None, op0=ALU.mult), [b_sm], [b_sm])
        K.op(DVE, lambda: v_.tensor_scalar(out=sm[:, 1, :], in0=sm[:, 5, :], scalar1=-4.0, scalar2=None, op0=ALU.mult), [b_sm], [b_sm])
        K.op(DVE, lambda: v_.tensor_scalar(out=sm[:, 2, :], in0=fm[:, 6, :], scalar1=0.5, scalar2=None, op0=ALU.mult), [b_fm, b_sm], [b_sm])
        K.op(DVE, lambda: v_.tensor_scalar(out=sm[:, 3, :], in0=fm[:, 7, :], scalar1=0.5, scalar2=None, op0=ALU.mult), [b_fm, b_sm], [b_sm])
        cneg = sm[:, 0, :]
        hcneg = sm[:, 1, :]
        hbrg = sm[:, 2, :]
        hbig = sm[:, 3, :]

        sp_dma(cs[:], cs_d.ap(), w=[b_cs])
        sp_dma(bada[:], bada_d.ap(), w=[b_bada])
        K.op(ACT, lambda: a_.activation(out=th0[:], in_=cs[:], func=AF.Tanh, scale=0.5), [b_cs], [b_th0])
        K.op(DVE, lambda: v_.scalar_tensor_tensor(out=th0[:], in0=th0[:], scalar=1.0, in1=cs[:], op0=ALU.add, op1=ALU.mult),
             [b_cs, b_th0], [b_th0])
        K.op(DVE, lambda: v_.tensor_scalar(out=siluT[:], in0=th0[:], scalar1=0.5, scalar2=None, op0=ALU.mult), [b_th0], [b_silu])
        for g in range(12):
            wb = wada[g % 2]
            bw = b_wada[g % 2]
            pl_dma(wb[:], wada_d[:, g * 512:(g + 1) * 512].rearrange("(kc p) n -> p kc n", p=128), w=[bw])
            i = nb()
            for kc in range(8):
                mm(ps[i][0:NSEQ, :], siluT[:, kc, :], wb[:, kc, :], kc == 0, kc == 7, [b_silu, bw], [pb[i]], kc == 7)
            K.op(DVE, lambda: v_.tensor_tensor(out=modt[:, g * 512:(g + 1) * 512], in0=ps[i][0:NSEQ, :],
                                               in1=bada[:, g * 512:(g + 1) * 512], op=ALU.add), [pb[i], b_bada], [b_modt])
        b_modd = Buf()
        sp_dma(modd.ap(), modt[:], r=[b_modt], w=[b_modd])
        i = nb()
        pT0 = ps[i][:, 0:16 * NSEQ].rearrange("p (a b) -> p a b", b=NSEQ)
        for kc in range(16):
            col = (D + kc * 128) if kc < 8 else ((kc - 8) * 128)
            tr(pT0[:, kc, :], modt[0:NSEQ, col:col + 128], ident_f[0:NSEQ, 0:NSEQ], [b_modt, b_const], [pb[i]], kc == 15)
        K.op(DVE, lambda: v_.scalar_tensor_tensor(out=gsmT[:], in0=pT0[:, 0:8, :], scalar=1.0,
                                                  in1=fm[:, 0, :].unsqueeze(2).to_broadcast([128, 8, NSEQ]),
                                                  op0=ALU.add, op1=ALU.mult), [pb[i], b_fm], [b_gs])
        K.op(DVE, lambda: v_.tensor_copy(out=shmT[:], in_=pT0[:, 8:16, :]), [pb[i]], [b_gs])
        K.barrier()
        al.release(m0)

        off_hT = al.off
        hT = al.alloc("hT", [128, 8, S], BF16)
        yaT = al.alloc("yaT", [128, 4, S], BF16)
        off_yrT = al.off
        yrT = al.alloc("yrT", [128, 8, S], BF16)
        LIMIT = al.limit
        b_hT = [Buf() for _ in range(NT_S)]
        b_yaT, b_yrT = Buf(), Buf()
        mU = al.mark()

        for b in range(NSEQ):
            tok0 = b * S
            al.release(mU)
            x_sb = [al.alloc("x_sb", [128, D], F32) for _ in range(4)]
            xn = [al.alloc("xn", [128, D], BF16) for _ in range(2)]
            jk = al.alloc("jk", [128, D], BF16)
            st = [al.alloc("st", [128, 4], F32) for _ in range(3)]
            b_x, b_xn, b_st, b_jk = [Buf() for _ in range(4)], [Buf(), Buf()], [Buf() for _ in range(3)], Buf()
            rot["l"] = list(range(8))
            def m1_L(t):
                sp_dma(x_sb[t % 4][:], x_d[tok0 + t * 128: tok0 + (t + 1) * 128, :], w=[b_x[t % 4]])

            def m1_A1(t):
                xb, stb, bx, bst = x_sb[t % 4], st[t % 3], b_x[t % 4], b_st[t % 3]
                K.op(ACT, lambda: a_.activation(out=jk[:], in_=xb[:], func=AF.Square, accum_out=stb[:, 0:1]), [bx], [b_jk, bst])
                K.op(ACT, lambda: a_.activation(out=stb[:, 1:2], in_=stb[:, 0:1], func=AF.Sqrt, bias=EPS, scale=1.0 / D), [bst], [bst])
                K.op(DVE, lambda: v_.reciprocal(out=stb[:, 2:3], in_=stb[:, 1:2]), [bst], [bst])

            def m1_A2(t):
                xb, xnb, stb = x_sb[t % 4], xn[t % 2], st[t % 3]
                bx, bxn, bst = b_x[t % 4], b_xn[t % 2], b_st[t % 3]
                K.op(ACT, lambda: a_.activation(out=xnb[:], in_=xb[:], func=AF.Identity, scale=stb[:, 2:3]), [bx, bst], [bxn])

            def m1_B(t):
                xnb, bxn = xn[t % 2], b_xn[t % 2]
                i = nb()
                pT = ps[i][:, :].bitcast(BF16)
                for kc in range(8):
                    tr(pT[:, kc * 128:(kc + 1) * 128], xnb[:, kc * 128:(kc + 1) * 128], ident_b, [bxn, b_const], [pb[i]], kc == 7)
                for kc in range(8):
                    o_ap = hT[:, kc, t * 128:(t + 1) * 128]
                    i_ap = pT[:, kc * 128:(kc + 1) * 128]
                    if kc % 2 == 0:
                        K.op(ACT, lambda: a_.activation(out=o_ap, in_=i_ap, func=AF.Identity, bias=shmT[:, kc, b:b + 1],
                                                        scale=gsmT[:, kc, b:b + 1]), [pb[i], b_gs], [b_hT[t]])
                    else:
                        K.op(DVE, lambda: v_.tensor_scalar(out=o_ap, in0=i_ap, scalar1=gsmT[:, kc, b:b + 1],
                                                           scalar2=shmT[:, kc, b:b + 1], op0=ALU.mult, op1=ALU.add),
                             [pb[i], b_gs], [b_hT[t]])
            for t0_ in range(min(3, NT_S)):
                m1_L(t0_)
            m1_A1(0)
            if NT_S > 1:
                m1_A1(1)
            m1_A2(0)
            for t in range(NT_S):
                if t + 3 < NT_S:
                    m1_L(t + 3)
                if t + 2 < NT_S:
                    m1_A1(t + 2)
                if t + 1 < NT_S:
                    m1_A2(t + 1)
                m1_B(t)
            K.barrier()

            al.release(mU)
            qT = al.alloc("qT", [128, 4, 2, S], BF16)
            kT = al.alloc("kT", [128, 4, S], BF16)
            sv_ = al.off
            al.off = off_yrT
            vS = al.alloc("vS", [128, NT_S, 4, 192], BF16)
            ef = al.alloc("ef", [72, S], F32)
            assert al.off <= mU
            al.off = sv_
            off_wq = al.off
            wq = al.alloc("wq", [128, 8, 512], BF16)
            wkk = al.alloc("wk", [128, 8, 512], BF16)
            wv = al.alloc("wv", [128, 8, 512], BF16)
            wf = al.alloc("wf", [128, 8, 72], BF16)
            caug = al.alloc("caug", [128, S], BF16)
            tmpb = al.alloc("tmpb", [72, S], BF16)
            b_caug, b_tmpb = Buf(), Buf()
            Lf = al.alloc("Lf", [72, S], F32)
            cumLT = al.alloc("cumLT", [128, NT_S, NH], F32)
            b_Rb, b_Rs = Buf(), Buf()
            b_q, b_k, b_v, b_wq, b_wk, b_wv, b_wf = Buf(), Buf(), Buf(), Buf(), Buf(), Buf(), Buf()
            b_ef, b_L, b_cumLT = Buf(), Buf(), Buf()
            b_PT = [Buf() for _ in range(6)]

            def wview(c0, n):
                return win_d[:, c0:c0 + n].rearrange("(kc p) n -> p kc n", p=128)
            K.op(POOL, lambda: g_.memset(vS[:, :, :, 64:128], 1.0), [], [b_v])
            K.op(POOL, lambda: g_.memset(qT[:], 0.0), [], [b_q])
            pl_dma(wq[:], wview(OQ, 512), w=[b_wq])
            pl_dma(wkk[:], wview(OK_, 512), w=[b_wk])
            pl_dma(wv[:], wview(OV, 512), w=[b_wv])
            K.op(POOL, lambda: g_.memset(wf[:], 0.0), [], [b_wf])
            K.op(POOL, lambda: g_.memset(caug[:], 0.0), [], [b_caug])
            for g3 in range(3):
                pl_dma(wf[:, :, g3 * 32:g3 * 32 + NH], wview(OF_, 8), w=[b_wf])
            rot["l"] = list(range(8))
            cnt = 0
            for n in range(NQ):
                i = nb()
                for kc in range(8):
                    mm(ps[i][0:72, 0:QB], wf[:, kc, :], hT[:, kc, n * QB:(n + 1) * QB], kc == 0, kc == 7,
                       [b_wf] + b_hT[n * TQ:(n + 1) * TQ], [pb[i]], kc == 7)
                K.op(ACT, lambda: a_.activation(out=ef[:, n * QB:(n + 1) * QB], in_=ps[i][0:72, 0:QB], func=AF.Exp,
                                                bias=nbf[0:72, 1:2], scale=-1.0), [pb[i], b_nbf], [b_ef])
            K.op(ACT, lambda: a_.activation(out=Lf[:], in_=ef[:], func=AF.Ln, bias=1.0, scale=1.0), [b_ef], [b_L])
            K.op(DVE, lambda: v_.tensor_tensor_scan(out=ef[:], data0=ones_c[0:72, 0:1].to_broadcast([72, S]), data1=Lf[:],
                                                    initial=0.0, op0=ALU.mult, op1=ALU.add), [b_L, b_const, b_ef], [b_ef])
            K.op(DVE, lambda: v_.tensor_scalar(out=Lf[:], in0=ef[:], scalar1=-8.0, scalar2=None, op0=ALU.mult), [b_ef, b_L], [b_L])
            K.op(DVE, lambda: v_.tensor_copy(out=tmpb[:], in_=Lf[:]), [b_L], [b_tmpb])
            K.op(DVE, lambda: v_.tensor_copy(out=caug[0:NH, :], in_=tmpb[0:NH, :]), [b_tmpb], [b_caug])
            K.op(DVE, lambda: v_.tensor_tensor(out=Lf[:], in0=Lf[:], in1=tmpb[:], op=ALU.subtract), [b_L, b_tmpb], [b_L])
            K.op(DVE, lambda: v_.tensor_copy(out=tmpb[:], in_=Lf[:]), [b_L, b_caug], [b_tmpb])
            K.op(DVE, lambda: v_.tensor_copy(out=caug[32:32 + NH, :], in_=tmpb[32:32 + NH, :]), [b_tmpb], [b_caug])
            K.op(DVE, lambda: v_.tensor_tensor(out=Lf[:], in0=Lf[:], in1=tmpb[:], op=ALU.subtract), [b_L, b_tmpb], [b_L])
            K.op(DVE, lambda: v_.tensor_copy(out=caug[64:64 + NH, :], in_=Lf[64:64 + NH, :]), [b_L], [b_caug])
            for (wt, bw, dst, bd, isq) in ((wq, b_wq, qT, b_q, True), (wkk, b_wk, kT, b_k, False)):
                for j in range(4):
                    for n in range(NQ):
                        i = nb()
                        for kc in range(8):
                            mm(ps[i][:, 0:QB], wt[:, kc, j * 128:(j + 1) * 128], hT[:, kc, n * QB:(n + 1) * QB], kc == 0, kc == 7,
                               [bw] + b_hT[n * TQ:(n + 1) * TQ], [pb[i]], kc == 7)
                        cols = slice(n * QB, (n + 1) * QB)
                        if isq:
                            K.op(ACT, lambda: a_.copy(out=qT[0:64, j, 0, cols], in_=ps[i][0:64, 0:QB]), [pb[i]], [bd])
                            K.op(DVE, lambda: v_.tensor_copy(out=qT[64:128, j, 1, cols], in_=ps[i][64:128, 0:QB]), [pb[i]], [bd])
                        elif cnt % 2 == 0:
                            K.op(ACT, lambda: a_.copy(out=kT[:, j, cols], in_=ps[i][:, 0:QB]), [pb[i]], [bd])
                        else:
                            K.op(DVE, lambda: v_.tensor_copy(out=kT[:, j, cols], in_=ps[i][:, 0:QB]), [pb[i]], [bd])
                        cnt += 1
            for t in range(NT_S):
                i = nb()
                for kc in range(8):
                    mm(ps[i][:, :], hT[:, kc, t * 128:(t + 1) * 128], wv[:, kc, :], kc == 0, kc == 7, [b_wv, b_hT[t]], [pb[i]], kc == 7)
                o_v = vS[:, t, :, :].rearrange("p j (c w) -> p j c w", w=64)[:, :, 0::2, :]
                i_v = ps[i][:, :].rearrange("p (j c w) -> p j c w", c=2, w=64)
                if t % 2 == 0:
                    K.op(ACT, lambda: a_.copy(out=o_v, in_=i_v), [pb[i]], [b_v])
                else:
                    K.op(DVE, lambda: v_.tensor_copy(out=o_v, in_=i_v), [pb[i]], [b_v])
            i = nb()
            pT1 = ps[i][:, 0:NT_S * NH].rearrange("p (a b) -> p a b", b=NH)
            for t in range(NT_S):
                tr(pT1[:, t, :], ef[0:NH, t * 128:(t + 1) * 128], ident_f[0:NH, 0:NH], [b_ef, b_const], [pb[i]], t == NT_S - 1)
            K.op(DVE, lambda: v_.tensor_copy(out=cumLT[:], in_=pT1), [pb[i]], [b_cumLT])

            K.barrier()
            sv3 = al.off
            al.off = off_wq
            PT = [al.alloc("PT", [128, QB], BF16) for _ in range(6)]
            Rb = al.alloc("Rb", [128, QB], BF16)
            Rs = al.alloc("Rs", [128, QB], F32)
            al.off = sv3
            rot["l"] = [4, 5, 6, 7]
            pcount = 0
            LAG = 3
            grp = {"n": 0}
            pend_backs = []

            def att_front(j, q, t, half, nt, gpar):
                nonlocal pcount
                d = t - q * TQ
                q0 = max(d, 0) * 128
                h = 2 * j + half
                rows = slice(half * 64, half * 64 + 64)
                i = nb()
                mm(ps[i][:, q0:QB], kT[:, j, t * 128:(t + 1) * 128], qT[:, j, half, q * QB + q0:(q + 1) * QB],
                   True, False, [b_q, b_k], [pb[i]], False)
                mm(ps[i][:, q0:QB], cbt[:, 512 + h * 128:512 + (h + 1) * 128], caug[:, q * QB + q0:(q + 1) * QB],
                   False, d < 0, [b_const, b_caug], [pb[i]], d < 0)
                if d >= 0:
                    mm(ps[i][:, q0:q0 + 128], ident_b, negm_b, False, True, [b_const], [pb[i]], True)
                pt = PT[pcount % 6]
                bpt = b_PT[pcount % 6]
                pcount += 1
                K.op(ACT, lambda: a_.activation(out=pt[:, q0:QB], in_=ps[i][:, q0:QB], func=AF.Exp,
                                                bias=cumLT[:, t, h:h + 1], scale=0.125), [pb[i], b_cumLT], [bpt])

                def back():
                    yi = half + 2 * gpar
                    ya_, yb_ = 2 * gpar, 2 * gpar + 1
                    lo = 0 if half == 0 else 64
                    mm(ps[yi][:, q0:QB], vS[:, t, j, lo:lo + 128], pt[:, q0:QB], t == 0, t == nt - 1,
                       [b_v, bpt], [pb[yi]], True)
                    if t == nt - 1 and half == 1:
                        K.op(DVE, lambda: v_.reciprocal(out=Rs[64:128, :], in_=ps[ya_][64:128, 0:QB]), [pb[ya_], b_Rs], [b_Rs])
                        K.op(DVE, lambda: v_.reciprocal(out=Rs[0:64, :], in_=ps[yb_][0:64, 0:QB]), [pb[yb_], b_Rs], [b_Rs])
                        K.op(DVE, lambda: v_.tensor_copy(out=Rb[:], in_=Rs[:]), [b_Rs, b_Rb], [b_Rb])
                        isw = nb()
                        mm(ps[isw][:, 0:QB], swap_b, Rb[:, :], True, True, [b_const, b_Rb], [pb[isw]], True)
                        K.op(ACT, lambda: a_.copy(out=Rs[:], in_=ps[isw][:, 0:QB]), [pb[isw]], [b_Rs])
                        K.op(DVE, lambda: v_.tensor_tensor(out=yaT[0:64, j, q * QB:(q + 1) * QB], in0=ps[ya_][0:64, 0:QB],
                                                           in1=Rs[0:64, :], op=ALU.mult), [pb[ya_], b_Rs], [b_yaT])
                        K.op(DVE, lambda: v_.tensor_tensor(out=yaT[64:128, j, q * QB:(q + 1) * QB], in0=ps[yb_][64:128, 0:QB],
                                                           in1=Rs[64:128, :], op=ALU.mult), [pb[yb_], b_Rs], [b_yaT])
                return back

            for j in range(4):
                for q in range(NQ):
                    nt = q * TQ + TQ
                    gpar = grp["n"] % 2
                    grp["n"] += 1
                    precast(PC_PER)
                    for t in range(nt):
                        for half in range(2):
                            pend_backs.append(att_front(j, q, t, half, nt, gpar))
                            if len(pend_backs) > LAG:
                                pend_backs.pop(0)()
            while pend_backs:
                pend_backs.pop(0)()
            K.barrier()

            al.release(mU)
            xp = [al.alloc("xp", [128, S + 4], F32) for _ in range(2)]
            uu = [al.alloc("uu", [128, S], F32) for _ in range(2)]
            ub = [al.alloc("ub", [128, S], BF16) for _ in range(2)]
            gg = [al.alloc("gg", [128, S], BF16) for _ in range(2)]
            thr = al.alloc("thr", [128, S], F32)
            thi = al.alloc("thi", [128, S], F32)
            e2 = al.alloc("e2", [128, S], F32)
            wx = [al.alloc("wx", [128, 8, 128], BF16) for _ in range(2)]
            wg = [al.alloc("wg", [128, 8, 128], BF16) for _ in range(2)]
            wrg = [al.alloc("wrg", [128, 128], BF16) for _ in range(2)]
            wig = [al.alloc("wig", [128, 128], BF16) for _ in range(2)]
            gt = [[al.alloc("gt", [128, QB], F32) for _ in range(2)] for _ in range(2)]
            b_xp, b_uu, b_ub, b_gg = [Buf(), Buf()], [Buf(), Buf()], [Buf(), Buf()], [Buf(), Buf()]
            b_thr, b_thi, b_e2 = Buf(), Buf(), Buf()
            b_w4 = [[Buf() for _ in range(4)] for _ in range(2)]
            b_gt = [[Buf() for _ in range(2)] for _ in range(2)]
            rot["l"] = list(range(8))
            for s2_ in range(2):
                K.op(DVE, lambda: v_.memset(xp[s2_][:, 0:4], 0.0), [], [b_xp[s2_]])

            def load_w4(c):
                s_ = c % 2
                pl_dma(wx[s_][:], wview(OX + c * 128, 128), w=[b_w4[s_][0]])
                pl_dma(wg[s_][:], wview(OG + c * 128, 128), w=[b_w4[s_][1]])
                pl_dma(wrg[s_][:], wrg_d[c, :, :], w=[b_w4[s_][2]])
                pl_dma(wig[s_][:], wig_d[c, :, :], w=[b_w4[s_][3]])

            gcnt4 = {"n": 0}

            def m4_A(c):
                zero_fill(-(-NBLK // (NSEQ * 8)))
                s_ = c % 2
                bwx, bwg_, bwrg, bwig = b_w4[s_]
                xp_, uu_, ub_, gg_ = xp[s_], uu[s_], ub[s_], gg[s_]
                bxp, buu, bub, bgg = b_xp[s_], b_uu[s_], b_ub[s_], b_gg[s_]
                for n in range(NQ):
                    i = nb()
                    for kc in range(8):
                        mm(ps[i][:, 0:QB], wx[s_][:, kc, :], hT[:, kc, n * QB:(n + 1) * QB], kc == 0, kc == 7,
                           [bwx] + b_hT[n * TQ:(n + 1) * TQ], [pb[i]], kc == 7)
                    K.op(ACT, lambda: a_.copy(out=xp_[:, 3 + n * QB:3 + (n + 1) * QB], in_=ps[i][:, 0:QB]), [pb[i]], [bxp])
                K.op(DVE, lambda: v_.tensor_scalar(out=uu_[:], in0=xp_[:, 3:3 + S], scalar1=fm[:, 4, c:c + 1], scalar2=fm[:, 5, c:c + 1],
                                                   op0=ALU.mult, op1=ALU.add), [bxp, b_fm], [buu])
                for jj in range(3):
                    K.op(DVE, lambda: v_.scalar_tensor_tensor(out=uu_[:], in0=xp_[:, jj:jj + S], scalar=fm[:, 1 + jj, c:c + 1], in1=uu_[:],
                                                              op0=ALU.mult, op1=ALU.add), [bxp, b_fm, buu], [buu])
                K.op(POOL, lambda: g_.tensor_copy(out=ub_[:], in_=uu_[:]), [buu], [bub])
                for n in range(NQ):
                    i = nb()
                    for kc in range(8):
                        mm(ps[i][:, 0:QB], wg[s_][:, kc, :], hT[:, kc, n * QB:(n + 1) * QB], kc == 0, kc == 7,
                           [bwg_] + b_hT[n * TQ:(n + 1) * TQ], [pb[i]], kc == 7)
                    g0, g1 = gt[gcnt4["n"] % 2]
                    bg0, bg1 = b_gt[gcnt4["n"] % 2]
                    gcnt4["n"] += 1
                    pg = ps[i][:, 0:QB]
                    K.op(ACT, lambda: a_.activation(out=g0[:], in_=pg, func=AF.Square), [pb[i]], [bg0])
                    K.op(DVE, lambda: v_.tensor_scalar(out=g0[:], in0=g0[:], scalar1=0.044715, scalar2=1.0, op0=ALU.mult, op1=ALU.add),
                         [bg0], [bg0])
                    K.op(DVE, lambda: v_.tensor_tensor(out=g0[:], in0=g0[:], in1=pg, op=ALU.mult), [bg0, pb[i]], [bg0])
                    K.op(ACT, lambda: a_.activation(out=g1[:], in_=g0[:], func=AF.Tanh, scale=0.7978845608028654), [bg0], [bg1])
                    K.op(DVE, lambda: v_.scalar_tensor_tensor(out=gg_[:, n * QB:(n + 1) * QB], in0=g1[:], scalar=1.0, in1=pg,
                                                              op0=ALU.add, op1=ALU.mult), [bg1, pb[i]], [bgg])

            def m4_B(c):
                s_ = c % 2
                bwx, bwg_, bwrg, bwig = b_w4[s_]
                uu_, ub_, gg_ = uu[s_], ub[s_], gg[s_]
                buu, bub, bgg = b_uu[s_], b_ub[s_], b_gg[s_]
                for n in range(NQ):
                    i = nb()
                    mm(ps[i][:, 0:QB], wrg[s_][:, :], ub_[:, n * QB:(n + 1) * QB], True, True, [bwrg, bub], [pb[i]], True)
                    K.op(ACT, lambda: a_.activation(out=thr[:, n * QB:(n + 1) * QB], in_=ps[i][:, 0:QB], func=AF.Tanh,
                                                    bias=hbrg[:, c:c + 1], scale=0.5), [pb[i], b_sm], [b_thr])
                    i2 = nb()
                    mm(ps[i2][:, 0:QB], wig[s_][:, :], ub_[:, n * QB:(n + 1) * QB], True, True, [bwig, bub], [pb[i2]], True)
                    K.op(ACT, lambda: a_.activation(out=thi[:, n * QB:(n + 1) * QB], in_=ps[i2][:, 0:QB], func=AF.Tanh,
                                                    bias=hbig[:, c:c + 1], scale=0.5), [pb[i2], b_sm], [b_thi])
                K.op(ACT, lambda: a_.activation(out=e2[:], in_=thr[:], func=AF.Exp, bias=cneg[:, c:c + 1], scale=cneg[:, c:c + 1]),
                     [b_thr, b_sm], [b_e2])
                K.op(ACT, lambda: a_.activation(out=thr[:], in_=thr[:], func=AF.Exp, bias=hcneg[:, c:c + 1], scale=hcneg[:, c:c + 1]),
                     [b_thr, b_sm], [b_thr])
                K.op(DVE, lambda: v_.tensor_scalar(out=e2[:], in0=e2[:], scalar1=1.0 - 1.0e-7, scalar2=None, op0=ALU.min), [b_e2], [b_e2])
                K.op(ACT, lambda: a_.activation(out=e2[:], in_=e2[:], func=AF.Sqrt, bias=1.0, scale=-1.0), [b_e2], [b_e2])
                K.op(DVE, lambda: v_.scalar_tensor_tensor(out=thi[:], in0=thi[:], scalar=1.0, in1=e2[:], op0=ALU.add, op1=ALU.mult),
                     [b_thi, b_e2], [b_thi])
                K.op(DVE, lambda: v_.scalar_tensor_tensor(out=thi[:], in0=thi[:], scalar=0.5, in1=uu_[:], op0=ALU.mult, op1=ALU.mult),
                     [b_thi, buu], [b_thi])
                K.op(DVE, lambda: v_.tensor_tensor_scan(out=e2[:], data0=thr[:], data1=thi[:], initial=0.0, op0=ALU.mult, op1=ALU.add),
                     [b_thr, b_thi, b_e2], [b_e2])
                K.op(DVE, lambda: v_.scalar_tensor_tensor(out=yrT[:, c, :], in0=gg_[:], scalar=0.5, in1=e2[:], op0=ALU.mult, op1=ALU.mult),
                     [bgg, b_e2], [b_yrT])

            load_w4(0)
            load_w4(1)
            m4_A(0)
            for c in range(8):
                if c + 1 < 8:
                    m4_A(c + 1)
                m4_B(c)
                if c + 2 < 8:
                    load_w4(c + 2)
            K.barrier()

            al.release(mU)
            mgT = al.alloc("mgT", [128, 8, S], BF16)
            b_mg = [Buf() for _ in range(NT_S)]
            m5 = al.mark()
            w5 = [al.alloc("w5", [128, 28, 128], BF16) for _ in range(2)]
            b_w5 = [[Buf() for _ in range(4)] for _ in range(2)]
            sa = [[al.alloc("sa", [128, QB], F32) for _ in range(4)] for _ in range(2)]
            b_sa = [[Buf() for _ in range(4)] for _ in range(2)]
            rot["l"] = list(range(8))

            def load_w5(m):
                s_ = m % 2
                cs_ = slice(m * 128, (m + 1) * 128)
                pl_dma(w5[s_][:, 0:4, :], wba_d[:, cs_].rearrange("(kc p) n -> p kc n", p=128), w=[b_w5[s_][0]])
                pl_dma(w5[s_][:, 4:12, :], wbr_d[:, cs_].rearrange("(kc p) n -> p kc n", p=128), w=[b_w5[s_][1]])
                pl_dma(w5[s_][:, 12:20, :], wview(OGA + m * 128, 128), w=[b_w5[s_][2]])
                pl_dma(w5[s_][:, 20:28, :], wview(OGR + m * 128, 128), w=[b_w5[s_][3]])
            load_w5(0)
            scount = 0
            for m in range(8):
                s_ = m % 2
                bwa, bwr_, bwga, bwgr = b_w5[s_]
                if m + 1 < 8:
                    load_w5(m + 1)
                for n in range(NQ):
                    cols = slice(n * QB, (n + 1) * QB)
                    hb = b_hT[n * TQ:(n + 1) * TQ]
                    iA, iR, iGA, iGR = nb(), nb(), nb(), nb()
                    for kc in range(4):
                        mm(ps[iA][:, 0:QB], w5[s_][:, kc, :], yaT[:, kc, cols], kc == 0, kc == 3, [bwa, b_yaT], [pb[iA]], kc == 3)
                    for kc in range(8):
                        mm(ps[iGA][:, 0:QB], w5[s_][:, 12 + kc, :], hT[:, kc, cols], kc == 0, kc == 7, [bwga] + hb, [pb[iGA]], kc == 7)
                    for kc in range(8):
                        mm(ps[iR][:, 0:QB], w5[s_][:, 4 + kc, :], yrT[:, kc, cols], kc == 0, kc == 7, [bwr_, b_yrT], [pb[iR]], kc == 7)
                    for kc in range(8):
                        mm(ps[iGR][:, 0:QB], w5[s_][:, 20 + kc, :], hT[:, kc, cols], kc == 0, kc == 7, [bwgr] + hb, [pb[iGR]], kc == 7)
                    s0, s1, s2, s3 = sa[scount % 2]
                    c0, c1, c2, c3 = b_sa[scount % 2]
                    scount += 1
                    K.op(ACT, lambda: a_.activation(out=s0[:], in_=ps[iGA][:, 0:QB], func=AF.Sigmoid), [pb[iGA]], [c0])
                    K.op(DVE, lambda: v_.tensor_tensor(out=s1[:], in0=s0[:], in1=ps[iA][:, 0:QB], op=ALU.mult), [c0, pb[iA]], [c1])
                    K.op(ACT, lambda: a_.activation(out=s2[:], in_=ps[iGR][:, 0:QB], func=AF.Sigmoid), [pb[iGR]], [c2])
                    K.op(DVE, lambda: v_.tensor_tensor(out=s3[:], in0=s2[:], in1=ps[iR][:, 0:QB], op=ALU.mult), [c2, pb[iR]], [c3])
                    K.op(POOL, lambda: g_.tensor_tensor(out=mgT[:, m, cols], in0=s1[:], in1=s3[:], op=ALU.add), [c1, c3],
                         b_mg[n * TQ:(n + 1) * TQ])
            K.barrier()

            al.release(m5)
            offB = al.off
            useA = (mU - off_hT) >= 80 * 1024
            if useA:
                al.off = off_hT
                al.limit = mU
            wo = al.alloc("wo", [128, 8, D], BF16)
            h2Tb = [al.alloc("h2Tb", [128, 8, QB], BF16) for _ in range(2)]
            xs2 = [al.alloc("xs2", [128, D], F32) for _ in range(2)]
            x1 = [al.alloc("x1", [128, D], F32) for _ in range(2)]
            h2 = [al.alloc("h2", [128, D], F32) for _ in range(2)]
            h2T = [al.alloc("h2T", [128, 8, 128], F32) for _ in range(2)]
            shs = [al.alloc("shs", [128, D], BF16) for _ in range(2)]
            h2b = [al.alloc("h2b", [128, D], BF16) for _ in range(2)]
            if useA:
                al.off = offB
                al.limit = LIMIT
            wsg = al.alloc("wsg", [128, 8, FF], BF16)
            wsu = al.alloc("wsu", [128, 8, FF], BF16)
            wsd = al.alloc("wsd", [128, 2, D], BF16)
            wr = al.alloc("wr", [128, 8, E], F32)
            bcv5 = al.alloc("bcv5", [128, 2, D], F32)
            b_bcv5 = Buf()
            gmb = al.alloc("gmb", [128, D], F32)
            gsfb = al.alloc("gsfb", [128, D], F32)
            shfb = al.alloc("shfb", [128, D], F32)
            sg = [al.alloc("sg", [128, QB], F32) for _ in range(2)]
            actT = al.alloc("actT", [128, 2, QB], BF16)
            rs = [al.alloc("rs", [128, 8], F32) for _ in range(2)]
            rt_ = [al.alloc("rt", [128, 6, E], F32) for _ in range(2)]
            g8 = [al.alloc("g8", [128, 8, 8], F32) for _ in range(2)]
            r8 = [al.alloc("r8", [128, 4, 8], F32) for _ in range(2)]
            i8 = [al.alloc("i8", [128, 8], U32) for _ in range(2)]
            mkb = [al.alloc("mkb", [128, E], BF16) for _ in range(2)]
            b_wo, b_ws, b_wr, b_bc5 = Buf(), Buf(), Buf(), Buf()
            b_xs2, b_x1, b_h2, b_h2b, b_h2T = [Buf(), Buf()], [Buf(), Buf()], [Buf(), Buf()], [Buf(), Buf()], [Buf(), Buf()]
            b_h2Tb, b_shs, b_sg, b_act, b_rs, b_rt = [Buf(), Buf()], [Buf(), Buf()], [Buf(), Buf()], Buf(), [Buf(), Buf()], [Buf(), Buf()]
            b_jk5 = Buf()
            jk5 = al.alloc("jk5", [128, D], BF16)
            pl_dma(wo[:], wout_d.ap().rearrange("(kc p) n -> p kc n", p=128), w=[b_wo])
            b_wsg, b_wsu, b_wsd = Buf(), Buf(), Buf()
            pl_dma(wsg[:], wsg_d.ap().rearrange("(kc p) n -> p kc n", p=128), w=[b_wsg])
            pl_dma(wsu[:], wsu_d.ap().rearrange("(kc p) n -> p kc n", p=128), w=[b_wsu])
            pl_dma(wsd[:], wsd_d.ap().rearrange("(kc p) n -> p kc n", p=128), w=[b_wsd])
            sp_dma(wr[:], wr_d.ap().rearrange("(kc p) n -> p kc n", p=128), w=[b_wr])
            sp_dma(bcv5[:], bcv_d[:, 0:2, :], w=[b_bcv5])
            sp_dma(gmb[:], modd[b:b + 1, 2 * D:3 * D].partition_broadcast(128), r=[b_modd], w=[b_bc5])
            sp_dma(gsfb[:], modd[b:b + 1, 4 * D:5 * D].partition_broadcast(128), r=[b_modd], w=[b_bc5])
            sp_dma(shfb[:], modd[b:b + 1, 3 * D:4 * D].partition_broadcast(128), r=[b_modd], w=[b_bc5])
            K.op(DVE, lambda: v_.tensor_tensor(out=gmb[:], in0=gmb[:], in1=bcv5[:, 0, :], op=ALU.mult), [b_bc5, b_bcv5], [b_bc5])
            K.op(DVE, lambda: v_.scalar_tensor_tensor(out=gsfb[:], in0=gsfb[:], scalar=1.0, in1=bcv5[:, 1, :], op0=ALU.add, op1=ALU.mult),
                 [b_bc5, b_bcv5], [b_bc5])
            rot["l"] = list(range(8))
            def m5_A(t):
                n, tt = t // TQ, t % TQ
                T = b * NT_S + t
                r0 = tok0 + t * 128
                p_ = t % 2
                xb, x1b, h2_, h2b_, h2T_, rs_ = xs2[p_], x1[p_], h2[p_], h2b[p_], h2T[p_], rs[p_]
                bxb, bx1, bh2, bh2b, bh2T, brs = b_xs2[p_], b_x1[p_], b_h2[p_], b_h2b[p_], b_h2T[p_], b_rs[p_]
                sp_dma(xb[:], x_d[r0:r0 + 128, :], w=[bxb])
                io = [nb(), nb()]
                for hf in range(2):
                    for kc in range(8):
                        mm(ps[io[hf]][:, :], mgT[:, kc, t * 128:(t + 1) * 128], wo[:, kc, hf * 512:(hf + 1) * 512], kc == 0, kc == 7,
                           [b_mg[t], b_wo], [pb[io[hf]]], kc == 7)
                for hf in range(2):
                    K.op(ACT, lambda: a_.activation(out=jk5[:, hf * 512:(hf + 1) * 512], in_=ps[io[hf]][:, :], func=AF.Square,
                                                    accum_out=rs_[:, hf:hf + 1]), [pb[io[hf]]], [b_jk5, brs])
                K.op(DVE, lambda: v_.tensor_tensor(out=rs_[:, 2:3], in0=rs_[:, 0:1], in1=rs_[:, 1:2], op=ALU.add), [brs], [brs])
                K.op(ACT, lambda: a_.activation(out=rs_[:, 3:4], in_=rs_[:, 2:3], func=AF.Sqrt, bias=EPS, scale=1.0 / D), [brs], [brs])
                K.op(DVE, lambda: v_.reciprocal(out=rs_[:, 4:5], in_=rs_[:, 3:4]), [brs], [brs])
                for hf in range(2):
                    cs_ = slice(hf * 512, (hf + 1) * 512)
                    K.op(DVE, lambda: v_.scalar_tensor_tensor(out=x1b[:, cs_], in0=ps[io[hf]][:, :], scalar=rs_[:, 4:5], in1=gmb[:, cs_],
                                                              op0=ALU.mult, op1=ALU.mult), [pb[io[hf]], brs, b_bc5], [bx1])
                K.op(POOL, lambda: g_.tensor_tensor(out=x1b[:], in0=x1b[:], in1=xb[:], op=ALU.add), [bx1, bxb], [bx1])
                sp_dma(x1_d[r0:r0 + 128, :], x1b[:], r=[bx1])
                K.op(ACT, lambda: a_.activation(out=jk5[:], in_=x1b[:], func=AF.Square, accum_out=rs_[:, 5:6]), [bx1], [b_jk5, brs])
                K.op(ACT, lambda: a_.activation(out=rs_[:, 6:7], in_=rs_[:, 5:6], func=AF.Sqrt, bias=EPS, scale=1.0 / D), [brs], [brs])
                K.op(DVE, lambda: v_.reciprocal(out=rs_[:, 7:8], in_=rs_[:, 6:7]), [brs], [brs])
                K.op(DVE, lambda: v_.scalar_tensor_tensor(out=h2_[:], in0=x1b[:], scalar=rs_[:, 7:8], in1=gsfb[:], op0=ALU.mult, op1=ALU.mult),
                     [bx1, brs, b_bc5], [bh2])
                K.op(POOL, lambda: g_.tensor_tensor(out=h2_[:], in0=h2_[:], in1=shfb[:], op=ALU.add), [bh2, b_bc5], [bh2])
                K.op(POOL, lambda: g_.tensor_copy(out=h2b_[:], in_=h2_[:]), [bh2], [bh2b])
                sp_dma(h2_d[r0:r0 + 128, :], h2b_[:], r=[bh2b])

            def m5_B(t):
                n, tt = t // TQ, t % TQ
                hb_ = h2Tb[n % 2]
                bhb = b_h2Tb[n % 2]
                T = b * NT_S + t
                p_ = t % 2
                h2_, h2T_ = h2[p_], h2T[p_]
                bh2, bh2T = b_h2[p_], b_h2T[p_]
                it = [nb(), nb()]
                for kc in range(8):
                    ib = it[kc // 4]
                    tr(ps[ib][:, (kc % 4) * 128:(kc % 4 + 1) * 128], h2_[:, kc * 128:(kc + 1) * 128], ident_f, [bh2, b_const], [pb[ib]],
                       kc % 4 == 3)
                for hf in range(2):
                    K.op(ACT, lambda: a_.copy(out=h2T_[:, hf * 4:(hf + 1) * 4, :], in_=ps[it[hf]][:, :].rearrange("p (a b) -> p a b", b=128)),
                         [pb[it[hf]]], [bh2T])
                K.op(POOL, lambda: g_.tensor_copy(out=hb_[:, :, tt * 128:(tt + 1) * 128], in_=h2T_[:]), [bh2T], [bhb])
                il = nb()
                for kc in range(8):
                    mm(ps[il][:, 0:E], h2T_[:, kc, :], wr[:, kc, :], kc == 0, kc == 7, [bh2T, b_wr], [pb[il]], kc == 7)
                R_, g8_, r8_, i8_, mk_ = rt_[p_], g8[p_], r8[p_], i8[p_], mkb[p_]
                brt = b_rt[p_]
                sc, sel, selm, mkf, wfull, tmp = (R_[:, k_, :] for k_ in range(6))
                K.op(ACT, lambda: a_.activation(out=sc, in_=ps[il][:, 0:E], func=AF.Sigmoid), [pb[il]], [brt])
                K.op(DVE, lambda: v_.tensor_tensor(out=sel, in0=sc, in1=rbias[:], op=ALU.add), [brt, b_const], [brt])
                for gg in range(8):
                    K.op(DVE, lambda: v_.max(out=g8_[:, gg, :], in_=sel[:, gg * 8:(gg + 1) * 8]), [brt], [brt])
                K.op(DVE, lambda: v_.tensor_tensor(out=r8_[:, 0, :], in0=g8_[:, :, 0], in1=g8_[:, :, 1], op=ALU.add), [brt], [brt])
                K.op(DVE, lambda: v_.max(out=r8_[:, 1, :], in_=r8_[:, 0, :]), [brt], [brt])
                K.op(DVE, lambda: v_.tensor_scalar(out=r8_[:, 2, :], in0=r8_[:, 0, :], scalar1=r8_[:, 1, 3:4], scalar2=-BIG,
                                                   op0=ALU.is_lt, op1=ALU.mult), [brt], [brt])
                K.op(DVE, lambda: v_.tensor_tensor(out=selm.rearrange("p (a b) -> p a b", b=8), in0=sel.rearrange("p (a b) -> p a b", b=8),
                                                   in1=r8_[:, 2, :].unsqueeze(2).to_broadcast([128, 8, 8]), op=ALU.add), [brt], [brt])
                K.op(DVE, lambda: v_.max(out=r8_[:, 3, :], in_=selm), [brt], [brt])
                K.op(DVE, lambda: v_.max_index(out=i8_[:], in_max=r8_[:, 3, :], in_values=selm), [brt], [brt])
                K.op(DVE, lambda: v_.tensor_scalar(out=mkf, in0=selm, scalar1=r8_[:, 3, 5:6], scalar2=None, op0=ALU.is_ge), [brt], [brt])
                K.op(DVE, lambda: v_.tensor_tensor(out=wfull, in0=sc, in1=mkf, op=ALU.mult), [brt], [brt])
                K.op(DVE, lambda: v_.tensor_reduce(out=r8_[:, 2, 0:1], in_=wfull, axis=AX.X, op=ALU.add), [brt], [brt])
                K.op(DVE, lambda: v_.reciprocal(out=r8_[:, 2, 1:2], in_=r8_[:, 2, 0:1]), [brt], [brt])
                K.op(POOL, lambda: g_.tensor_copy(out=mk_[:], in_=mkf), [brt], [brt])
                ik = nb()
                mm(ps[ik][:, 0:E], triu_b, mk_[:], True, True, [brt, b_const], [pb[ik]], False)
                mm(ps[ik][:, E:2 * E], ones_b, mk_[:], True, True, [brt, b_const], [pb[ik]], True)
                K.op(DVE, lambda: v_.tensor_tensor(out=tmp, in0=ps[ik][:, 0:E], in1=run[:], op=ALU.add), [pb[ik], b_run, brt], [brt])
                K.op(DVE, lambda: v_.tensor_tensor(out=rankm[:, T, :], in0=tmp, in1=mkf, op=ALU.mult), [brt], [b_route])
                K.op(DVE, lambda: v_.tensor_tensor(out=run[:], in0=run[:], in1=ps[ik][:, E:2 * E], op=ALU.add), [pb[ik], b_run], [b_run])
                K.op(DVE, lambda: v_.tensor_copy(out=eidx[:, T, :], in_=i8_[:]), [brt], [b_route])
                for k_ in range(TOPK):
                    K.op(DVE, lambda: v_.scalar_tensor_tensor(out=tmp, in0=iota64, scalar=eidx[:, T, k_:k_ + 1], in1=wfull,
                                                              op0=ALU.is_equal, op1=ALU.mult, accum_out=wk[:, T, k_:k_ + 1]),
                         [brt, b_route, b_const], [brt, b_route])
                K.op(DVE, lambda: v_.tensor_scalar(out=wkn[:, T, 0:TOPK], in0=wk[:, T, 0:TOPK], scalar1=r8_[:, 2, 1:2], scalar2=2.5,
                                                   op0=ALU.mult, op1=ALU.mult), [brt, b_route], [b_route])

            def m5_SH(n):
                hb_ = h2Tb[n % 2]
                bhb = b_h2Tb[n % 2]
                ig = [nb(), nb()]
                iu = [nb(), nb()]
                for c in range(2):
                    for kc in range(8):
                        mm(ps[ig[c]][:, 0:QB], wsg[:, kc, c * 128:(c + 1) * 128], hb_[:, kc, :], kc == 0, kc == 7, [b_wsg, bhb], [pb[ig[c]]], kc == 7)
                    for kc in range(8):
                        mm(ps[iu[c]][:, 0:QB], wsu[:, kc, c * 128:(c + 1) * 128], hb_[:, kc, :], kc == 0, kc == 7, [b_wsu, bhb], [pb[iu[c]]], kc == 7)
                    sg_ = sg[c]
                    K.op(ACT, lambda: a_.activation(out=sg_[:], in_=ps[ig[c]][:, 0:QB], func=AF.Sigmoid), [pb[ig[c]]], [b_sg[c]])
                    K.op(DVE, lambda: v_.tensor_tensor(out=sg_[:], in0=sg_[:], in1=ps[ig[c]][:, 0:QB], op=ALU.mult), [b_sg[c], pb[ig[c]]], [b_sg[c]])
                    K.op(DVE, lambda: v_.tensor_tensor(out=actT[:, c, :], in0=sg_[:], in1=ps[iu[c]][:, 0:QB], op=ALU.mult),
                         [b_sg[c], pb[iu[c]]], [b_act])
                for tt in range(TQ):
                    t = n * TQ + tt
                    r0 = tok0 + t * 128
                    iy = [nb(), nb()]
                    for hf in range(2):
                        for c in range(2):
                            mm(ps[iy[hf]][:, :], actT[:, c, tt * 128:(tt + 1) * 128], wsd[:, c, hf * 512:(hf + 1) * 512], c == 0, c == 1,
                               [b_act, b_wsd], [pb[iy[hf]]], c == 1)
                    sh_ = shs[t % 2]
                    K.op(ACT, lambda: a_.copy(out=sh_[:, 0:512], in_=ps[iy[0]][:, :]), [pb[iy[0]]], [b_shs[t % 2]])
                    K.op(DVE, lambda: v_.tensor_copy(out=sh_[:, 512:1024], in_=ps[iy[1]][:, :]), [pb[iy[1]]], [b_shs[t % 2]])
                    sp_dma(sh_d[r0:r0 + 128, :], sh_[:], r=[b_shs[t % 2]])

            m5_A(0)
            for t in range(NT_S):
                if t + 1 < NT_S:
                    m5_A(t + 1)
                m5_B(t)
                if t % TQ == TQ - 1:
                    m5_SH(t // TQ)
            K.barrier()

        al.release(mU)
        al.off = mU
        pe_ = al.alloc("pend", [128, 4, E], F32)
        pei = al.alloc("pei", [128, E], I32)
        ebf = al.alloc("ebf", [128, 2, NBLK], F32)
        djk = al.alloc("djk", [128, E], F32)
        rkp = al.alloc("rkp", [128, E], F32)
        destf = al.alloc("destf", [128, NT, 8], F32)
        b_pe, b_eb, b_dj, b_rkp, b_destf, b_desti = Buf(), Buf(), Buf(), Buf(), Buf(), Buf()
        b_desti_t = [Buf() for _ in range(NT)]
        K.op(DVE, lambda: v_.tensor_scalar(out=pe_[:, 3, :], in0=run[:], scalar1=float(CB - 1), scalar2=None, op0=ALU.add), [b_run], [b_pe])
        K.op(DVE, lambda: v_.tensor_copy(out=pei[:], in_=pe_[:, 3, :]), [b_pe], [b_pe])
        K.op(DVE, lambda: v_.tensor_single_scalar(out=pei[:], in_=pei[:], scalar=8, op=ALU.arith_shift_right), [b_pe], [b_pe])
        K.op(DVE, lambda: v_.tensor_copy(out=pe_[:, 0, :], in_=pei[:]), [b_pe], [b_pe])
        K.op(DVE, lambda: v_.tensor_tensor_scan(out=pe_[:, 1, :], data0=ones_c[:, 0:1].to_broadcast([128, E]), data1=pe_[:, 0, :],
                                                initial=0.0, op0=ALU.mult, op1=ALU.add), [b_pe, b_const], [b_pe])
        K.op(DVE, lambda: v_.tensor_tensor(out=pe_[:, 2, :], in0=pe_[:, 1, :], in1=pe_[:, 0, :], op=ALU.subtract), [b_pe], [b_pe])
        K.op(DVE, lambda: v_.tensor_scalar(out=pe_[:, 2, :], in0=pe_[:, 2, :], scalar1=float(CB), scalar2=None, op0=ALU.mult), [b_pe], [b_pe])
        for T in range(NT):
            K.op(DVE, lambda: v_.tensor_tensor(out=rkp[:], in0=rankm[:, T, :], in1=pe_[:, 2, :], op=ALU.add), [b_route, b_pe, b_rkp], [b_rkp])
            for k_ in range(TOPK):
                K.op(DVE, lambda: v_.scalar_tensor_tensor(out=djk[:], in0=iota64, scalar=eidx[:, T, k_:k_ + 1], in1=rkp[:],
                                                          op0=ALU.is_equal, op1=ALU.mult, accum_out=destf[:, T, k_:k_ + 1]),
                     [b_route, b_rkp, b_const], [b_dj, b_destf])
            K.op(DVE, lambda: v_.tensor_copy(out=desti[:, T, 0:TOPK], in_=destf[:, T, 0:TOPK]), [b_destf], [b_desti_t[T]])
        hg = [al.alloc("hg", [128, D], BF16) for _ in range(3)]
        b_hg = [Buf() for _ in range(3)]
        b_xs = Buf()
        for T in range(NT):
            hb_ = hg[T % 3]
            sp_dma(hb_[:], h2_d[T * 128:(T + 1) * 128, :], w=[b_hg[T % 3]])
            for k_ in range(TOPK):
                K.dma(K.qpool, lambda: g_.indirect_dma_start(out=xs_d[:, :], out_offset=bass.IndirectOffsetOnAxis(ap=desti[:, T, k_:k_ + 1], axis=0),
                                                             in_=hb_[:], in_offset=None, bounds_check=reg_slot, oob_is_err=False),
                      [b_hg[T % 3], b_desti_t[T], b_xs0], [], [b_xs])
        K.op(DVE, lambda: v_.memset(ebf[:, 0, :], 0.0), [], [b_eb])
        for e_ in range(E):
            K.op(DVE, lambda: v_.scalar_tensor_tensor(out=ebf[:, 0, :], in0=iotab, scalar=pe_[:, 1, e_:e_ + 1], in1=ebf[:, 0, :],
                                                      op0=ALU.is_ge, op1=ALU.add), [b_pe, b_const, b_eb], [b_eb])
        K.op(DVE, lambda: v_.tensor_scalar(out=ebf[:, 0, :], in0=ebf[:, 0, :], scalar1=float(E - 1), scalar2=128.0, op0=ALU.min, op1=ALU.mult),
             [b_eb], [b_eb])
        K.op(DVE, lambda: v_.tensor_scalar(out=ebf[:, 1, :], in0=ebf[:, 0, :], scalar1=pidx, scalar2=None, op0=ALU.add), [b_eb, b_const], [b_eb])
        if skip_reload and NBLK > 2:
            K.op(DVE, lambda: v_.tensor_tensor(out=ebf[:, 0, 2:NBLK], in0=ebf[:, 0, 2:NBLK], in1=ebf[:, 1, 0:NBLK - 2], op=ALU.subtract),
                 [b_eb], [b_eb])
            K.op(DVE, lambda: v_.tensor_scalar(out=ebf[:, 0, 2:NBLK], in0=ebf[:, 0, 2:NBLK], scalar1=pidx, scalar2=None, op0=ALU.add),
                 [b_eb, b_const], [b_eb])
            K.op(DVE, lambda: v_.tensor_scalar(out=ebf[:, 0, 2:NBLK], in0=ebf[:, 0, 2:NBLK], scalar1=0.0, scalar2=1.0e6,
                                               op0=ALU.is_equal, op1=ALU.mult), [b_eb], [b_eb])
            K.op(DVE, lambda: v_.tensor_tensor(out=ebf[:, 1, 2:NBLK], in0=ebf[:, 1, 2:NBLK], in1=ebf[:, 0, 2:NBLK], op=ALU.add), [b_eb], [b_eb])
        K.op(DVE, lambda: v_.tensor_copy(out=widx[:], in_=ebf[:, 1, :]), [b_eb], [b_widx])
        K.barrier()

        al.off = mU
        wE = [[al.alloc("wE", [128, 2048], BF16) for _ in range(3)] for _ in range(2)]
        b_wE = [[Buf() for _ in range(3)] for _ in range(2)]
        xsb = [al.alloc("xsb", [128, 2, D], BF16) for _ in range(3)]
        xTe = [al.alloc("xTe", [128, 8, CB], BF16) for _ in range(3)]
        sge = [al.alloc("sge", [128, 2 * CB], F32) for _ in range(2)]
        acte = [al.alloc("acte", [128, 2 * CB], BF16) for _ in range(2)]
        ysb = [al.alloc("ysb", [128, D], BF16) for _ in range(4)]
        b_xsb, b_xTe, b_sge, b_acte = [Buf(), Buf(), Buf()], [Buf(), Buf(), Buf()], [Buf(), Buf()], [Buf(), Buf()]
        b_ysb = [Buf() for _ in range(4)]
        b_ys = Buf()
        rot["l"] = list(range(8))
        wsrc = wpb_d

        def load_wE(blk, which):
            s_ = blk % 2
            for m in which:
                K.dma(K.qpool, lambda: g_.indirect_dma_start(out=wE[s_][m][:], out_offset=None, in_=wsrc[m][:, :],
                                                             in_offset=bass.IndirectOffsetOnAxis(ap=widx[:, blk:blk + 1], axis=0),
                                                             bounds_check=reg_w, oob_is_err=False),
                      [b_widx, b_wcast], [b_wE[s_][m]])

        def load_xs(blk):
            p_ = blk % 3
            sp_dma(xsb[p_][:], xs_d[blk * CB:(blk + 1) * CB, :].rearrange("(s p) d -> p s d", p=128), r=[b_xs], w=[b_xsb[p_]])

        def stage_T(blk):
            p_ = blk % 3
            for s2 in range(2):
                i = nb()
                pT = ps[i][:, :].bitcast(BF16)
                for kc in range(8):
                    tr(pT[:, kc * 128:(kc + 1) * 128], xsb[p_][:, s2, kc * 128:(kc + 1) * 128], ident_b, [b_xsb[p_], b_const], [pb[i]], kc == 7)
                o_ap = xTe[p_][:, :, s2 * 128:(s2 + 1) * 128]
                i_ap = pT.rearrange("p (a b) -> p a b", b=128)
                if s2 == 0:
                    K.op(ACT, lambda: a_.copy(out=o_ap, in_=i_ap), [pb[i]], [b_xTe[p_]])
                else:
                    K.op(DVE, lambda: v_.tensor_copy(out=o_ap, in_=i_ap), [pb[i]], [b_xTe[p_]])

        def stage_GU(blk):
            s_ = blk % 2
            p_ = blk % 2
            x_ = blk % 3
            wgE, wuE, wdE = wE[s_]
            bwg, bwu, bwd = b_wE[s_]
            ig_, iu_ = nb(), nb()
            for c in range(2):
                for kc in range(8):
                    mm(ps[ig_][:, c * CB:(c + 1) * CB], wgE[:, kc * FF + c * 128: kc * FF + (c + 1) * 128], xTe[x_][:, kc, :], kc == 0, kc == 7,
                       [bwg, b_xTe[x_]], [pb[ig_]], kc == 7)
            for c in range(2):
                for kc in range(8):
                    mm(ps[iu_][:, c * CB:(c + 1) * CB], wuE[:, kc * FF + c * 128: kc * FF + (c + 1) * 128], xTe[x_][:, kc, :], kc == 0, kc == 7,
                       [bwu, b_xTe[x_]], [pb[iu_]], kc == 7)
            K.op(ACT, lambda: a_.activation(out=sge[p_][:], in_=ps[ig_][:, :], func=AF.Sigmoid), [pb[ig_]], [b_sge[p_]])
            K.op(DVE, lambda: v_.tensor_tensor(out=sge[p_][:], in0=sge[p_][:], in1=ps[ig_][:, :], op=ALU.mult), [b_sge[p_], pb[ig_]], [b_sge[p_]])
            K.op(DVE, lambda: v_.tensor_tensor(out=acte[p_][:], in0=sge[p_][:], in1=ps[iu_][:, :], op=ALU.mult), [b_sge[p_], pb[iu_]], [b_acte[p_]])

        ycnt = {"n": 0}

        def stage_D(blk):
            s_ = blk % 2
            p_ = blk % 2
            wdE = wE[s_][2]
            bwd = b_wE[s_][2]
            for s2 in range(2):
                yb = ysb[ycnt["n"] % 4]
                byb = b_ysb[ycnt["n"] % 4]
                ycnt["n"] += 1
                for hf in range(2):
                    i = nb()
                    for c in range(2):
                        mm(ps[i][:, :], acte[p_][:, c * CB + s2 * 128: c * CB + (s2 + 1) * 128], wdE[:, c * D + hf * 512: c * D + (hf + 1) * 512],
                           c == 0, c == 1, [b_acte[p_], bwd], [pb[i]], c == 1)
                    if hf == 0:
                        K.op(ACT, lambda: a_.copy(out=yb[:, 0:512], in_=ps[i][:, :]), [pb[i]], [byb])
                    else:
                        K.op(DVE, lambda: v_.tensor_copy(out=yb[:, 512:1024], in_=ps[i][:, :]), [pb[i]], [byb])
                r0 = blk * CB + s2 * 128
                sp_dma(ys_d[r0:r0 + 128, :], yb[:], r=[byb], sw=[b_ys])

        precast(3 * E)
        load_wE(0, (0, 1, 2))
        if NBLK > 1:
            load_wE(1, (0, 1, 2))
        for b0 in range(min(3, NBLK)):
            load_xs(b0)
        stage_T(0)
        if NBLK > 1:
            stage_T(1)
        stage_GU(0)
        for blk in range(NBLK):
            if blk + 2 < NBLK:
                load_wE(blk + 2, (0, 1))
                stage_T(blk + 2)
                if blk + 3 < NBLK:
                    load_xs(blk + 3)
            if blk >= 1:
                stage_D(blk - 1)
                if blk + 1 < NBLK:
                    load_wE(blk + 1, (2,))
            if blk + 1 < NBLK:
                stage_GU(blk + 1)
        stage_D(NBLK - 1)
        K.barrier()

        al.off = mU
        gfb = al.alloc("gfb", [128, NSEQ, D], F32)
        bcvc = al.alloc("bcvc", [128, D], F32)
        b_bcvc = Buf()
        sp_dma(bcvc[:], bcv_d[:, 2, :], w=[b_bcvc])
        b_gfb = Buf()
        for b in range(NSEQ):
            sp_dma(gfb[:, b, :], modd[b:b + 1, 5 * D:6 * D].partition_broadcast(128), r=[b_modd], w=[b_gfb])
            K.op(DVE, lambda: v_.tensor_tensor(out=gfb[:, b, :], in0=gfb[:, b, :], in1=bcvc[:], op=ALU.mult), [b_gfb, b_bcvc], [b_gfb])
        x1c = [al.alloc("x1c", [128, D], F32) for _ in range(2)]
        zc = [al.alloc("zc", [128, D], F32) for _ in range(2)]
        yg = [al.alloc("yg", [128, D], BF16) for _ in range(12)]
        shc = [al.alloc("shc", [128, D], BF16) for _ in range(2)]
        b_shc = [Buf(), Buf()]
        jkc = al.alloc("jkc", [128, D], BF16)
        rc_ = [al.alloc("rc", [128, 4], F32) for _ in range(2)]
        b_x1c, b_zc, b_rc = [Buf(), Buf()], [Buf(), Buf()], [Buf(), Buf()]
        b_yg = [Buf() for _ in range(12)]
        b_jkc = Buf()
        b_out = Buf()
        z2 = [al.alloc("z2", [128, D], F32) for _ in range(2)]
        b_z2 = [Buf(), Buf()]
        z3 = [al.alloc("z3", [128, D], F32) for _ in range(2)]
        b_z3 = [Buf(), Buf()]
        gc = {"n": 0}

        dg = [al.alloc("dg", [128, 128], BF16) for _ in range(12)]
        b_dg = [Buf() for _ in range(12)]
        rot["l"] = list(range(8))
        cps = {}

        def c_A(T):
            p_ = T % 2
            r0 = T * 128
            sp_dma(x1c[p_][:], x1_d[r0:r0 + 128, :], w=[b_x1c[p_]])
            sp_dma(shc[p_][:], sh_d[r0:r0 + 128, :], w=[b_shc[p_]])
            ys_ = []
            for k_ in range(TOPK):
                y_ = yg[gc["n"] % 12]
                by = b_yg[gc["n"] % 12]
                d_ = dg[gc["n"] % 12]
                bd = b_dg[gc["n"] % 12]
                gc["n"] += 1
                K.dma(K.qpool, lambda: g_.indirect_dma_start(out=y_[:], out_offset=None, in_=ys_d[:, :],
                                                             in_offset=bass.IndirectOffsetOnAxis(ap=desti[:, T, k_:k_ + 1], axis=0),
                                                             bounds_check=reg_slot, oob_is_err=False),
                      [b_ys, b_desti_t[T]], [by])
                K.op(ACT, lambda: a_.activation(out=d_[:], in_=ident_f, func=AF.Identity, scale=wkn[:, T, k_:k_ + 1]),
                     [b_const, b_route], [bd])
                ys_.append((y_, by, d_, bd))
            banks = [nb(), nb()]
            cps[T] = banks
            for hf in range(2):
                i = banks[hf]
                cs_ = slice(hf * 512, (hf + 1) * 512)
                mm(ps[i][:, :], ident_b, shc[p_][:, cs_], True, False, [b_const, b_shc[p_]], [pb[i]], False)
                for k_ in range(TOPK):
                    y_, by, d_, bd = ys_[k_]
                    mm(ps[i][:, :], d_[:], y_[:, cs_], False, k_ == TOPK - 1, [bd, by], [pb[i]], k_ == TOPK - 1)

        def c_B(T):
            b = T // NT_S
            p_ = T % 2
            r0 = T * 128
            banks = cps.pop(T)
            for hf in range(2):
                K.op(ACT, lambda: a_.activation(out=jkc[:, hf * 512:(hf + 1) * 512], in_=ps[banks[hf]][:, :], func=AF.Square,
                                                accum_out=rc_[p_][:, hf:hf + 1]), [pb[banks[hf]]], [b_jkc, b_rc[p_]])
            K.op(DVE, lambda: v_.tensor_tensor(out=rc_[p_][:, 3:4], in0=rc_[p_][:, 0:1], in1=rc_[p_][:, 1:2], op=ALU.add), [b_rc[p_]], [b_rc[p_]])
            K.op(ACT, lambda: a_.activation(out=rc_[p_][:, 1:2], in_=rc_[p_][:, 3:4], func=AF.Sqrt, bias=EPS, scale=1.0 / D), [b_rc[p_]], [b_rc[p_]])
            K.op(DVE, lambda: v_.reciprocal(out=rc_[p_][:, 2:3], in_=rc_[p_][:, 1:2]), [b_rc[p_]], [b_rc[p_]])
            for hf in range(2):
                cs_ = slice(hf * 512, (hf + 1) * 512)
                K.op(DVE, lambda: v_.scalar_tensor_tensor(out=zc[p_][:, cs_], in0=ps[banks[hf]][:, :], scalar=rc_[p_][:, 2:3], in1=gfb[:, b, cs_],
                                                          op0=ALU.mult, op1=ALU.mult), [pb[banks[hf]], b_rc[p_], b_gfb], [b_zc[p_]])
            K.op(DVE, lambda: v_.tensor_tensor(out=zc[p_][:], in0=zc[p_][:], in1=x1c[p_][:], op=ALU.add), [b_zc[p_], b_x1c[p_]], [b_zc[p_]])
            sp_dma(out_d[r0:r0 + 128, :], zc[p_][:], r=[b_zc[p_]], sw=[b_out])

        c_A(0)
        for T in range(NT):
            if T + 1 < NT:
                c_A(T + 1)
            c_B(T)
        K.barrier()
    return nc


def _bf16():
    import ml_dtypes
    return ml_dtypes.bfloat16


def make_consts(NBLK):
    NCF = 128 + 128 + 64 + NBLK + 2
    cf = np.zeros((128, NCF), np.float32)
    cf[:, 0:128] = np.eye(128, dtype=np.float32)
    cf[127, 128:256] = 1.0
    cf[:, 256:320] = np.arange(64, dtype=np.float32)[None, :]
    cf[:, 320:320 + NBLK] = np.arange(NBLK, dtype=np.float32)[None, :]
    cf[:, 320 + NBLK] = np.arange(128, dtype=np.float32)
    cf[:, 321 + NBLK] = 1.0
    cb = np.zeros((128, 1792), np.float32)
    cb[:, 0:128] = np.eye(128)
    k = np.arange(128)[:, None]
    m = np.arange(128)[None, :]
    cb[:, 128:256] = (k < m)
    cb[:, 256:384] = (m >= k)
    cb[:, 384:512] = 1.0
    for h in range(NH):
        for g3 in range(3):
            cb[g3 * 32 + h, 512 + h * 128:512 + (h + 1) * 128] = 1.0
    cb[:, 1536:1664] = np.where(m < k, -30000.0, 0.0)
    cb[:, 1664:1792] = (m == (k + 64) % 128)
    return cf, cb.astype(_bf16())


def prep_shared(inp):
    f = np.float32
    sh = {}
    sh["w_ada"] = np.ascontiguousarray(inp["w_ada"][0], f)
    fm = np.zeros((128, 9, 8), f)

    def fmaj(v):
        return np.asarray(v, f).reshape(8, 128).T
    fm[:, 0, :] = fmaj(inp["g_pre_mix"][0])
    for j in range(4):
        fm[:, 1 + j, :] = fmaj(inp["w_conv"][0, j])
    fm[:, 5, :] = fmaj(inp["b_conv"][0])
    fm[:, 6, :] = fmaj(inp["b_rg"][0])
    fm[:, 7, :] = fmaj(inp["b_ig"][0])
    fm[:, 8, :] = fmaj(inp["rglru_lambda"][0])
    sh["fm"] = fm
    bcv = np.zeros((128, 3, D), f)
    bcv[:, 0, :] = np.asarray(inp["g_post_mix"][0], f)[None, :]
    bcv[:, 1, :] = np.asarray(inp["g_pre_ffn"][0], f)[None, :]
    bcv[:, 2, :] = np.asarray(inp["g_post_ffn"][0], f)[None, :]
    sh["bcv"] = bcv
    sh["rbias"] = np.ascontiguousarray(np.broadcast_to(np.asarray(inp["router_bias"][0], f)[None, :], (128, E)))
    sh["b_forget"] = np.asarray(inp["b_forget"][0], f).reshape(NH, 1)
    sh["w_in"] = np.ascontiguousarray(inp["w_in"][0], f)
    for nm, src in (("wrg_bd", inp["w_rg"][0]), ("wig_bd", inp["w_ig"][0])):
        bd = np.zeros((8, 128, 128), f)
        for c in range(8):
            bd[c, 0:64, 0:64] = src[2 * c]
            bd[c, 64:128, 64:128] = src[2 * c + 1]
        sh[nm] = bd
    sh["w_ba"] = np.ascontiguousarray(inp["w_branch_attn"][0], f)
    sh["w_br"] = np.ascontiguousarray(inp["w_branch_rnn"][0], f)
    sh["w_out"] = np.ascontiguousarray(inp["w_out"][0], f)
    sh["w_router"] = np.ascontiguousarray(inp["w_router"][0], f)
    sh["w_sg"] = np.ascontiguousarray(inp["w_sh_gate"][0], f)
    sh["w_su"] = np.ascontiguousarray(inp["w_sh_up"][0], f)
    sh["w_sd"] = np.ascontiguousarray(inp["w_sh_down"][0], f)
    wg = np.asarray(inp["w_exp_gate"][0], f).reshape(E, 8, 128, FF).transpose(0, 2, 1, 3)
    sh["wpg"] = np.ascontiguousarray(wg).reshape(E * 128, 2048)
    wu = np.asarray(inp["w_exp_up"][0], f).reshape(E, 8, 128, FF).transpose(0, 2, 1, 3)
    sh["wpu"] = np.ascontiguousarray(wu).reshape(E * 128, 2048)
    wd = np.asarray(inp["w_exp_down"][0], f).reshape(E, 2, 128, D).transpose(0, 2, 1, 3)
    sh["wpd"] = np.ascontiguousarray(wd).reshape(E * 128, 2048)
    return sh


def prep_core(inp, sh, core, NSEQ, S, NBLK):
    f = np.float32
    m = dict(sh)
    xs = np.asarray(inp["x"][core * NSEQ:(core + 1) * NSEQ], f).reshape(NSEQ * S, D)
    m["x"] = np.ascontiguousarray(xs)
    c = np.asarray(inp["c"][core * NSEQ:(core + 1) * NSEQ], f)
    m["csT"] = np.ascontiguousarray(c.T.reshape(8, 128, NSEQ).transpose(1, 0, 2))
    m["b_ada_rep"] = np.ascontiguousarray(np.broadcast_to(np.asarray(inp["b_ada"][0], f)[None, :], (NSEQ, 6 * D)))
    cf, cb = make_consts(NBLK)
    m["cf"] = cf
    m["cb"] = cb
    return m


def kernel(**inputs):
    B, S = inputs["x"].shape[0], inputs["x"].shape[1]
    NSEQ = B // NCORES
    NTOK = NSEQ * S
    NBLK = (NTOK * TOPK) // CB + E
    nc = build(NSEQ, S, skip_reload=True)
    sh = prep_shared(inputs)
    in_maps = [prep_core(inputs, sh, i, NSEQ, S, NBLK) for i in range(NCORES)]
    res = run_bass_kernel_spmd(nc, in_maps, core_ids=list(range(NCORES)))
    outs = [np.asarray(r["out"], np.float32).reshape(NSEQ, S, D) for r in res.results]
    return np.concatenate(outs, axis=0)
```

```python
import numpy as np
import concourse.bass as bass
import concourse.mybir as mybir
from concourse.bass_utils import run_bass_kernel_spmd
from contextlib import ExitStack

F32 = mybir.dt.float32
BF16 = mybir.dt.bfloat16
I32 = mybir.dt.int32
U32 = mybir.dt.uint32
AF = mybir.ActivationFunctionType
ALU = mybir.AluOpType
AX = mybir.AxisListType

D = 1024
NH = 8
E = 64
TOPK = 6
FF = 256
CB = 256
INC = 5640
OQ, OK_, OV, OF_, OX, OG, OGA, OGR = 0, 512, 1024, 1536, 1544, 2568, 3592, 4616
EPS = 1e-6
BIG = 1.0e4
NCORES = 8
ARENA_SHIFT = [0]
ARENA_MAX = [0]


class Buf:
    __slots__ = ("w", "r", "name")

    def __init__(self, name=""):
        self.w = {}
        self.r = {}
        self.name = name


class Eng:
    def __init__(self, name, e, sem, key):
        self.name = name
        self.e = e
        self.sem = sem
        self.key = key
        self.n = 0
        self.seen = {}
        self.pending = False


class DQ:
    def __init__(self, eng, sems):
        self.eng = eng
        self.sems = sems
        self.cnt = [0] * len(sems)
        self.next = 0


def _merge(d, s):
    for k, v in s.items():
        if d.get(k, 0) < v:
            d[k] = v


class KB:
    def __init__(self, nc, stack):
        self.nc = nc
        self.semtab = {}
        self.engs = []
        for nm, e in (("pe", nc.tensor), ("act", nc.scalar), ("dve", nc.vector),
                      ("pool", nc.gpsimd), ("sp", nc.sync)):
            sem = stack.enter_context(nc.semaphore("s_" + nm))
            eng = Eng(nm, e, sem, "c_" + nm)
            self.semtab[eng.key] = sem
            setattr(self, nm, eng)
            self.engs.append(eng)
        self.queues = []
        for nm, eng, n in (("qsp", self.sp, 8), ("qpool", self.pool, 6)):
            sems = []
            for i in range(n):
                key = "d_%s%d" % (nm, i)
                sem = stack.enter_context(nc.semaphore(key))
                self.semtab[key] = sem
                sems.append((sem, key))
            q = DQ(eng, sems)
            setattr(self, nm, q)
            self.queues.append(q)

    def _wait(self, E_, deps):
        for k, v in deps.items():
            if E_.seen.get(k, 0) < v:
                E_.e.wait_ge(self.semtab[k], v)
                E_.seen[k] = v

    def op(self, E_, fn, reads=(), writes=(), inc=True):
        deps = {}
        for b in reads:
            _merge(deps, b.w)
        for b in writes:
            _merge(deps, b.w)
            _merge(deps, b.r)
        if E_.name == "pe":
            deps.pop(E_.key, None)
        self._wait(E_, deps)
        ins = fn()
        ev = E_.n + 1
        for b in reads:
            if b.r.get(E_.key, 0) < ev:
                b.r[E_.key] = ev
        for b in writes:
            b.w = {E_.key: ev}
            b.r = {}
        if inc:
            E_.n = ev
            ins.then_inc(E_.sem, 1)
            E_.pending = False
        else:
            E_.pending = True
        return ins

    def dma(self, Q, fn, reads=(), writes=(), swrites=()):
        E_ = Q.eng
        deps = {}
        for b in reads:
            _merge(deps, b.w)
        for b in writes:
            _merge(deps, b.w)
            _merge(deps, b.r)
        for b in swrites:
            _merge(deps, b.r)
        slot = Q.next
        Q.next = (Q.next + 1) % len(Q.sems)
        sem, key = Q.sems[slot]
        if Q.cnt[slot] > 0:
            if deps.get(key, 0) < 16 * Q.cnt[slot]:
                deps[key] = 16 * Q.cnt[slot]
        self._wait(E_, deps)
        ins = fn()
        Q.cnt[slot] += 1
        v = 16 * Q.cnt[slot]
        ins.then_inc(sem, 16)
        for b in reads:
            if b.r.get(key, 0) < v:
                b.r[key] = v
        for b in writes:
            b.w = {key: v}
            b.r = {}
        for b in swrites:
            if b.w.get(key, 0) < v:
                b.w[key] = v
        return ins

    def barrier(self):
        tot = {}
        for E_ in self.engs:
            assert not E_.pending
            if E_.n > 0:
                tot[E_.key] = E_.n
        for Q in self.queues:
            for i, (sem, key) in enumerate(Q.sems):
                if Q.cnt[i] > 0:
                    tot[key] = 16 * Q.cnt[i]
        for E_ in self.engs:
            self._wait(E_, dict(tot))


class Arena:
    def __init__(self, nc, limit):
        self.nc = nc
        self.off = 0
        self.limit = limit
        self.n = 0

    def alloc(self, name, shape, dtype):
        sz = 1
        for s in shape[1:]:
            sz *= s
        sz *= {F32: 4, BF16: 2, I32: 4, U32: 4}[dtype]
        sz = (sz + 63) // 64 * 64
        off = self.off
        assert off + sz <= self.limit, ("SBUF arena overflow", name, off, sz)
        self.off += sz
        self.n += 1
        ARENA_MAX[0] = max(ARENA_MAX[0], self.off)
        return self.nc.alloc_sbuf_tensor_at("%s_%d" % (name, self.n), list(shape), dtype, offset=off)

    def mark(self):
        return self.off

    def release(self, m):
        self.off = m


def build(NSEQ, S, skip_reload=True):
    NT_S = S // 128
    NTOK = NSEQ * S
    NT = NTOK // 128
    QB = min(512, S)
    TQ = QB // 128
    NQ = S // QB
    NB5 = S // QB
    NBLK = (NTOK * TOPK) // CB + E
    NSLOT = NBLK * CB

    nc = bass.Bass("TRN2", target_bir_lowering=False)
    dt = nc.dram_tensor

    def ein(name, shape, dtype=F32):
        return dt(name, list(shape), dtype, kind="ExternalInput")

    x_d = ein("x", [NTOK, D])
    cs_d = ein("csT", [128, 8, NSEQ])
    wada_d = ein("w_ada", [D, 6 * D])
    bada_d = ein("b_ada_rep", [NSEQ, 6 * D])
    fm_d = ein("fm", [128, 9, 8])
    bcv_d = ein("bcv", [128, 3, D])
    rb_d = ein("rbias", [128, E])
    bf_d = ein("b_forget", [NH, 1])
    win_d = ein("w_in", [D, INC])
    wrg_d = ein("wrg_bd", [8, 128, 128])
    wig_d = ein("wig_bd", [8, 128, 128])
    wba_d = ein("w_ba", [512, D])
    wbr_d = ein("w_br", [D, D])
    wout_d = ein("w_out", [D, D])
    wr_d = ein("w_router", [D, E])
    wsg_d = ein("w_sg", [D, FF])
    wsu_d = ein("w_su", [D, FF])
    wsd_d = ein("w_sd", [FF, D])
    wpg_d = ein("wpg", [E * 128, 2048])
    wpu_d = ein("wpu", [E * 128, 2048])
    wpd_d = ein("wpd", [E * 128, 2048])
    NCF = 128 + 128 + 64 + NBLK + 2
    cf_d = ein("cf", [128, NCF])
    cb_d = ein("cb", [128, 1792], BF16)
    out_d = dt("out", [NTOK, D], F32, kind="ExternalOutput")
    modd = dt("modd", [NSEQ, 6 * D], F32)
    h2_d = dt("h2s", [NTOK, D], BF16)
    x1_d = dt("x1s", [NTOK, D], F32)
    sh_d = dt("shs", [NTOK, D], BF16)
    xs_d = dt("xss", [NSLOT, D], BF16)
    ys_d = dt("yss", [NSLOT, D], BF16)
    wpb_d = [dt("wpb%d" % m_, [E * 128, 2048], BF16) for m_ in range(3)]

    stack = ExitStack()
    with stack:
        K = KB(nc, stack)
        al = Arena(nc, int(nc._sbuf_addr_for_side("right")) - 64)
        al.off = (int(nc._sbuf_addr_for_side("left")) + 63) // 64 * 64 + ARENA_SHIFT[0]
        ps = [stack.enter_context(nc.psum_tensor("ps%d" % i, [128, 512], F32)) for i in range(8)]
        pb = [Buf("pb%d" % i) for i in range(8)]
        rot = {"l": list(range(8)), "i": 0}

        def nb():
            i = rot["l"][rot["i"] % len(rot["l"])]
            rot["i"] += 1
            return i

        PE, ACT, DVE, POOL = K.pe, K.act, K.dve, K.pool
        reg_slot = nc.gpsimd.alloc_register("bc_slot")
        nc.gpsimd.reg_mov(reg_slot, NSLOT - 1)
        reg_w = nc.gpsimd.alloc_register("bc_w")
        nc.gpsimd.reg_mov(reg_w, E * 128 - 1)
        v_, a_, g_, t_ = nc.vector, nc.scalar, nc.gpsimd, nc.tensor

        def mm(out, lhsT, rhs, start, stop, r, w, inc):
            return K.op(PE, lambda: t_.matmul(out, lhsT, rhs, start=start, stop=stop), r, w, inc)

        def tr(out, in_, ident, r, w, inc):
            return K.op(PE, lambda: t_.transpose(out, in_, ident), r, w, inc)

        def sp_dma(out, in_, r=(), w=(), sw=()):
            return K.dma(K.qsp, lambda: nc.sync.dma_start(out=out, in_=in_), r, w, sw)

        def pl_dma(out, in_, r=(), w=(), sw=()):
            return K.dma(K.qpool, lambda: nc.gpsimd.dma_start(out=out, in_=in_), r, w, sw)

        cf = al.alloc("cf", [128, NCF], F32)
        cbt = al.alloc("cb", [128, 1792], BF16)
        b_const = Buf("const")
        ident_f = cf[:, 0:128]
        sel127 = cf[:, 128:256]
        iota64 = cf[:, 256:320]
        iotab = cf[:, 320:320 + NBLK]
        pidx = cf[:, 320 + NBLK:321 + NBLK]
        ones_c = cf[:, 321 + NBLK:322 + NBLK]
        ident_b = cbt[:, 0:128]
        triu_b = cbt[:, 128:256]
        trim_b = cbt[:, 256:384]
        ones_b = cbt[:, 384:512]
        negm_b = cbt[:, 1536:1664]
        swap_b = cbt[:, 1664:1792]
        fm = al.alloc("fm", [128, 9, 8], F32)
        sm = al.alloc("sm", [128, 6, 8], F32)
        rbias = al.alloc("rbias", [128, E], F32)
        nbf = al.alloc("nbf", [128, 2], F32)
        b_nbf = Buf()
        gsmT = al.alloc("gsmT", [128, 8, NSEQ], F32)
        shmT = al.alloc("shmT", [128, 8, NSEQ], F32)
        run = al.alloc("run", [128, E], F32)
        rankm = al.alloc("rankm", [128, NT, E], F32)
        eidx = al.alloc("eidx", [128, NT, 8], F32)
        wk = al.alloc("wk", [128, NT, 8], F32)
        wkn = al.alloc("wkn", [128, NT, 8], F32)
        desti = al.alloc("desti", [128, NT, 8], I32)
        widx = al.alloc("widx", [128, NBLK], I32)
        b_fm, b_sm, b_gs, b_run, b_route = Buf(), Buf(), Buf(), Buf(), Buf()
        b_widx = Buf()
        zt = al.alloc("zt", [128, 2, D], BF16)
        b_zt, b_xs0 = Buf(), Buf()
        K.op(POOL, lambda: g_.memset(zt[:], 0.0), [], [b_zt])
        zf = {"n": 0}
        ZF_PER = -(-NBLK // NT)

        b_wcast = Buf()
        pcast = {"n": 0}
        PC_PER = -(-(3 * E) // (NSEQ * 4 * NQ))

        def precast(cnt):
            for _ in range(cnt):
                if pcast["n"] < 3 * E:
                    e_, m_ = pcast["n"] // 3, pcast["n"] % 3
                    src = (wpg_d, wpu_d, wpd_d)[m_]
                    pl_dma(wpb_d[m_][e_ * 128:(e_ + 1) * 128, :], src[e_ * 128:(e_ + 1) * 128, :], sw=[b_wcast])
                    pcast["n"] += 1

        def zero_fill(cnt):
            for _ in range(cnt):
                if zf["n"] < NBLK:
                    r0_ = zf["n"] * CB
                    sp_dma(xs_d[r0_:r0_ + CB, :].rearrange("(s p) d -> p s d", p=128), zt[:], r=[b_zt], sw=[b_xs0])
                    zf["n"] += 1

        sp_dma(cf[:], cf_d.ap(), w=[b_const])
        sp_dma(cbt[:], cb_d.ap(), w=[b_const])
        sp_dma(fm[:], fm_d.ap(), w=[b_fm])
        sp_dma(rbias[:], rb_d.ap(), w=[b_const])
        K.op(DVE, lambda: v_.memset(nbf[:], 0.0), [], [b_nbf])
        for g3 in range(3):
            sp_dma(nbf[g3 * 32:g3 * 32 + NH, 0:1], bf_d.ap(), w=[b_nbf])
        K.op(DVE, lambda: v_.memset(run[:], 0.0), [], [b_run])

        m0 = al.mark()
        cs = al.alloc("cs", [128, 8, NSEQ], F32)
        th0 = al.alloc("th0", [128, 8, NSEQ], F32)
        siluT = al.alloc("siluT", [128, 8, NSEQ], BF16)
        modt = al.alloc("modt", [NSEQ, 6 * D], F32)
        bada = al.alloc("bada", [NSEQ, 6 * D], F32)
        wada = [al.alloc("wada", [128, 8, 512], BF16) for _ in range(2)]
        b_cs, b_th0, b_silu, b_modt, b_bada = Buf(), Buf(), Buf(), Buf(), Buf()
        b_wada = [Buf(), Buf()]

        K.op(DVE, lambda: v_.tensor_scalar(out=nbf[0:72, 1:2], in0=nbf[0:72, 0:1], scalar1=-1.0, scalar2=None,
                                           op0=ALU.mult), [b_nbf], [b_nbf])
        K.op(ACT, lambda: a_.activation(out=sm[:, 4, :], in_=fm[:, 8, :], func=AF.Exp, scale=-1.0), [b_fm], [b_sm])
        K.op(ACT, lambda: a_.activation(out=sm[:, 5, :], in_=sm[:, 4, :], func=AF.Ln, bias=1.0, scale=1.0), [b_sm], [b_sm])
        K.op(DVE, lambda: v_.tensor_scalar(out=sm[:, 0, :], in0=sm[:, 5, :], scalar1=-8.0, scalar2=None, op0=ALU.mult), [b_sm], [b_sm])
        K.op(DVE, lambda: v_.tensor_scalar(out=sm[:, 1, :], in0=sm[:, 5, :], scalar1=-4.0, scalar2=None, op0=ALU.mult), [b_sm], [b_sm])
        K.op(DVE, lambda: v_.tensor_scalar(out=sm[:, 2, :], in0=fm[:, 6, :], scalar1=0.5, scalar2=None, op0=ALU.mult), [b_fm, b_sm], [b_sm])
        K.op(DVE, lambda: v_.tensor_scalar(out=sm[:, 3, :], in0=fm[:, 7, :], scalar1=0.5, scalar2=None, op0=ALU.mult), [b_fm, b_sm], [b_sm])
        cneg = sm[:, 0, :]
        hcneg = sm[:, 1, :]
        hbrg = sm[:, 2, :]
        hbig = sm[:, 3, :]

        sp_dma(cs[:], cs_d.ap(), w=[b_cs])
        sp_dma(bada[:], bada_d.ap(), w=[b_bada])
        K.op(ACT, lambda: a_.activation(out=th0[:], in_=cs[:], func=AF.Tanh, scale=0.5), [b_cs], [b_th0])
        K.op(DVE, lambda: v_.scalar_tensor_tensor(out=th0[:], in0=th0[:], scalar=1.0, in1=cs[:], op0=ALU.add, op1=ALU.mult),
             [b_cs, b_th0], [b_th0])
        K.op(DVE, lambda: v_.tensor_scalar(out=siluT[:], in0=th0[:], scalar1=0.5, scalar2=None, op0=ALU.mult), [b_th0], [b_silu])
        for g in range(12):
            wb = wada[g % 2]
            bw = b_wada[g % 2]
            pl_dma(wb[:], wada_d[:, g * 512:(g + 1) * 512].rearrange("(kc p) n -> p kc n", p=128), w=[bw])
            i = nb()
            for kc in range(8):
                mm(ps[i][0:NSEQ, :], siluT[:, kc, :], wb[:, kc, :], kc == 0, kc == 7, [b_silu, bw], [pb[i]], kc == 7)
            K.op(DVE, lambda: v_.tensor_tensor(out=modt[:, g * 512:(g + 1) * 512], in0=ps[i][0:NSEQ, :],
                                               in1=bada[:, g * 512:(g + 1) * 512], op=ALU.add), [pb[i], b_bada], [b_modt])
        b_modd = Buf()
        sp_dma(modd.ap(), modt[:], r=[b_modt], w=[b_modd])
        i = nb()
        pT0 = ps[i][:, 0:16 * NSEQ].rearrange("p (a b) -> p a b", b=NSEQ)
        for kc in range(16):
            col = (D + kc * 128) if kc < 8 else ((kc - 8) * 128)
            tr(pT0[:, kc, :], modt[0:NSEQ, col:col + 128], ident_f[0:NSEQ, 0:NSEQ], [b_modt, b_const], [pb[i]], kc == 15)
        K.op(DVE, lambda: v_.scalar_tensor_tensor(out=gsmT[:], in0=pT0[:, 0:8, :], scalar=1.0,
                                                  in1=fm[:, 0, :].unsqueeze(2).to_broadcast([128, 8, NSEQ]),
                                                  op0=ALU.add, op1=ALU.mult), [pb[i], b_fm], [b_gs])
        K.op(DVE, lambda: v_.tensor_copy(out=shmT[:], in_=pT0[:, 8:16, :]), [pb[i]], [b_gs])
        K.barrier()
        al.release(m0)

        off_hT = al.off
        hT = al.alloc("hT", [128, 8, S], BF16)
        yaT = al.alloc("yaT", [128, 4, S], BF16)
        off_yrT = al.off
        yrT = al.alloc("yrT", [128, 8, S], BF16)
        LIMIT = al.limit
        b_hT = [Buf() for _ in range(NT_S)]
        b_hT2 = [Buf() for _ in range(NT_S)]
        b_yaT, b_yrT = Buf(), Buf()
        mU = al.mark()

        for b in range(NSEQ):
            tok0 = b * S
            al.release(mU)
            x_sb = [al.alloc("x_sb", [128, D], F32) for _ in range(4)]
            xn = [al.alloc("xn", [128, D], BF16) for _ in range(2)]
            jk = al.alloc("jk", [128, D], BF16)
            st = [al.alloc("st", [128, 4], F32) for _ in range(3)]
            b_x, b_xn, b_st, b_jk = [Buf() for _ in range(4)], [Buf(), Buf()], [Buf() for _ in range(3)], Buf()
            rot["l"] = list(range(8))
            def m1_L(t):
                sp_dma(x_sb[t % 4][:], x_d[tok0 + t * 128: tok0 + (t + 1) * 128, :], w=[b_x[t % 4]])

            def m1_A1(t):
                xb, stb, bx, bst = x_sb[t % 4], st[t % 3], b_x[t % 4], b_st[t % 3]
                K.op(ACT, lambda: a_.activation(out=jk[:], in_=xb[:], func=AF.Square, accum_out=stb[:, 0:1]), [bx], [b_jk, bst])
                K.op(ACT, lambda: a_.activation(out=stb[:, 1:2], in_=stb[:, 0:1], func=AF.Sqrt, bias=EPS, scale=1.0 / D), [bst], [bst])
                K.op(DVE, lambda: v_.reciprocal(out=stb[:, 2:3], in_=stb[:, 1:2]), [bst], [bst])

            def m1_A2(t):
                xb, xnb, stb = x_sb[t % 4], xn[t % 2], st[t % 3]
                bx, bxn, bst = b_x[t % 4], b_xn[t % 2], b_st[t % 3]
                K.op(ACT, lambda: a_.activation(out=xnb[:], in_=xb[:], func=AF.Identity, scale=stb[:, 2:3]), [bx, bst], [bxn])

            def m1_B(t):
                xnb, bxn = xn[t % 2], b_xn[t % 2]
                ia, ib = nb(), nb()
                pTa = ps[ia][:, :].bitcast(BF16)
                pTb = ps[ib][:, :].bitcast(BF16)
                for kc in range(0, 8, 2):
                    tr(pTa[:, (kc // 2) * 128:(kc // 2 + 1) * 128], xnb[:, kc * 128:(kc + 1) * 128], ident_b, [bxn, b_const], [pb[ia]], kc == 6)
                for kc in range(1, 8, 2):
                    tr(pTb[:, (kc // 2) * 128:(kc // 2 + 1) * 128], xnb[:, kc * 128:(kc + 1) * 128], ident_b, [bxn, b_const], [pb[ib]], kc == 7)
                for kc in range(8):
                    o_ap = hT[:, kc, t * 128:(t + 1) * 128]
                    if kc % 2 == 0:
                        i_ap = pTa[:, (kc // 2) * 128:(kc // 2 + 1) * 128]
                        K.op(ACT, lambda: a_.activation(out=o_ap, in_=i_ap, func=AF.Identity, bias=shmT[:, kc, b:b + 1],
                                                        scale=gsmT[:, kc, b:b + 1]), [pb[ia], b_gs], [b_hT[t]])
                    else:
                        i_ap = pTb[:, (kc // 2) * 128:(kc // 2 + 1) * 128]
                        K.op(DVE, lambda: v_.tensor_scalar(out=o_ap, in0=i_ap, scalar1=gsmT[:, kc, b:b + 1],
                                                           scalar2=shmT[:, kc, b:b + 1], op0=ALU.mult, op1=ALU.add),
                             [pb[ib], b_gs], [b_hT2[t]])
            for t0_ in range(min(3, NT_S)):
                m1_L(t0_)
            m1_A1(0)
            if NT_S > 1:
                m1_A1(1)
            m1_A2(0)
            for t in range(NT_S):
                if t + 3 < NT_S:
                    m1_L(t + 3)
                if t + 2 < NT_S:
                    m1_A1(t + 2)
                if t + 1 < NT_S:
                    m1_A2(t + 1)
                m1_B(t)
            K.barrier()

            al.release(mU)
            qT = al.alloc("qT", [128, 4, 2, S], BF16)
            kT = al.alloc("kT", [128, 4, S], BF16)
            sv_ = al.off
            al.off = off_yrT
            vS = al.alloc("vS", [128, NT_S, 4, 192], BF16)
            ef = al.alloc("ef", [72, S], F32)
            assert al.off <= mU
            al.off = sv_
            off_wq = al.off
            wq = al.alloc("wq", [128, 8, 512], BF16)
            wkk = al.alloc("wk", [128, 8, 512], BF16)
            wv = al.alloc("wv", [128, 8, 512], BF16)
            wf = al.alloc("wf", [128, 8, 72], BF16)
            caug = al.alloc("caug", [128, S], BF16)
            tmpb = al.alloc("tmpb", [72, S], BF16)
            b_caug, b_tmpb = Buf(), Buf()
            Lf = al.alloc("Lf", [72, S], F32)
            cumLT = al.alloc("cumLT", [128, NT_S, NH], F32)
            b_Rb, b_Rs = Buf(), Buf()
            b_q, b_k, b_v, b_wq, b_wk, b_wv, b_wf = Buf(), Buf(), Buf(), Buf(), Buf(), Buf(), Buf()
            b_ef, b_L, b_cumLT = Buf(), Buf(), Buf()
            b_PT = [Buf() for _ in range(6)]

            def wview(c0, n):
                return win_d[:, c0:c0 + n].rearrange("(kc p) n -> p kc n", p=128)
            K.op(POOL, lambda: g_.memset(vS[:, :, :, 64:128], 1.0), [], [b_v])
            K.op(POOL, lambda: g_.memset(qT[:], 0.0), [], [b_q])
            pl_dma(wq[:], wview(OQ, 512), w=[b_wq])
            pl_dma(wkk[:], wview(OK_, 512), w=[b_wk])
            pl_dma(wv[:], wview(OV, 512), w=[b_wv])
            K.op(POOL, lambda: g_.memset(wf[:], 0.0), [], [b_wf])
            K.op(POOL, lambda: g_.memset(caug[:], 0.0), [], [b_caug])
            for g3 in range(3):
                pl_dma(wf[:, :, g3 * 32:g3 * 32 + NH], wview(OF_, 8), w=[b_wf])
            rot["l"] = list(range(8))
            cnt = 0
            for n in range(NQ):
                i = nb()
                for kc in range(8):
                    mm(ps[i][0:72, 0:QB], wf[:, kc, :], hT[:, kc, n * QB:(n + 1) * QB], kc == 0, kc == 7,
                       [b_wf] + (b_hT[n * TQ:(n + 1) * TQ] + b_hT2[n * TQ:(n + 1) * TQ]), [pb[i]], kc == 7)
                K.op(ACT, lambda: a_.activation(out=ef[:, n * QB:(n + 1) * QB], in_=ps[i][0:72, 0:QB], func=AF.Exp,
                                                bias=nbf[0:72, 1:2], scale=-1.0), [pb[i], b_nbf], [b_ef])
            K.op(ACT, lambda: a_.activation(out=Lf[:], in_=ef[:], func=AF.Ln, bias=1.0, scale=1.0), [b_ef], [b_L])
            K.op(DVE, lambda: v_.tensor_tensor_scan(out=ef[:], data0=ones_c[0:72, 0:1].to_broadcast([72, S]), data1=Lf[:],
                                                    initial=0.0, op0=ALU.mult, op1=ALU.add), [b_L, b_const, b_ef], [b_ef])
            K.op(DVE, lambda: v_.tensor_scalar(out=Lf[:], in0=ef[:], scalar1=-8.0, scalar2=None, op0=ALU.mult), [b_ef, b_L], [b_L])
            K.op(DVE, lambda: v_.tensor_copy(out=tmpb[:], in_=Lf[:]), [b_L], [b_tmpb])
            K.op(DVE, lambda: v_.tensor_copy(out=caug[0:NH, :], in_=tmpb[0:NH, :]), [b_tmpb], [b_caug])
            K.op(DVE, lambda: v_.tensor_tensor(out=Lf[:], in0=Lf[:], in1=tmpb[:], op=ALU.subtract), [b_L, b_tmpb], [b_L])
            K.op(DVE, lambda: v_.tensor_copy(out=tmpb[:], in_=Lf[:]), [b_L, b_caug], [b_tmpb])
            K.op(DVE, lambda: v_.tensor_copy(out=caug[32:32 + NH, :], in_=tmpb[32:32 + NH, :]), [b_tmpb], [b_caug])
            K.op(DVE, lambda: v_.tensor_tensor(out=Lf[:], in0=Lf[:], in1=tmpb[:], op=ALU.subtract), [b_L, b_tmpb], [b_L])
            K.op(DVE, lambda: v_.tensor_copy(out=caug[64:64 + NH, :], in_=Lf[64:64 + NH, :]), [b_L], [b_caug])
            for (wt, bw, dst, bd, isq) in ((wq, b_wq, qT, b_q, True), (wkk, b_wk, kT, b_k, False)):
                for j in range(4):
                    for n in range(NQ):
                        i = nb()
                        for kc in range(8):
                            mm(ps[i][:, 0:QB], wt[:, kc, j * 128:(j + 1) * 128], hT[:, kc, n * QB:(n + 1) * QB], kc == 0, kc == 7,
                               [bw] + (b_hT[n * TQ:(n + 1) * TQ] + b_hT2[n * TQ:(n + 1) * TQ]), [pb[i]], kc == 7)
                        cols = slice(n * QB, (n + 1) * QB)
                        if isq:
                            K.op(ACT, lambda: a_.copy(out=qT[0:64, j, 0, cols], in_=ps[i][0:64, 0:QB]), [pb[i]], [bd])
                            K.op(DVE, lambda: v_.tensor_copy(out=qT[64:128, j, 1, cols], in_=ps[i][64:128, 0:QB]), [pb[i]], [bd])
                        elif cnt % 2 == 0:
                            K.op(ACT, lambda: a_.copy(out=kT[:, j, cols], in_=ps[i][:, 0:QB]), [pb[i]], [bd])
                        else:
                            K.op(DVE, lambda: v_.tensor_copy(out=kT[:, j, cols], in_=ps[i][:, 0:QB]), [pb[i]], [bd])
                        cnt += 1
            for t in range(NT_S):
                i = nb()
                for kc in range(8):
                    mm(ps[i][:, :], hT[:, kc, t * 128:(t + 1) * 128], wv[:, kc, :], kc == 0, kc == 7, [b_wv, b_hT[t], b_hT2[t]], [pb[i]], kc == 7)
                o_v = vS[:, t, :, :].rearrange("p j (c w) -> p j c w", w=64)[:, :, 0::2, :]
                i_v = ps[i][:, :].rearrange("p (j c w) -> p j c w", c=2, w=64)
                if t % 2 == 0:
                    K.op(ACT, lambda: a_.copy(out=o_v, in_=i_v), [pb[i]], [b_v])
                else:
                    K.op(DVE, lambda: v_.tensor_copy(out=o_v, in_=i_v), [pb[i]], [b_v])
            i = nb()
            pT1 = ps[i][:, 0:NT_S * NH].rearrange("p (a b) -> p a b", b=NH)
            for t in range(NT_S):
                tr(pT1[:, t, :], ef[0:NH, t * 128:(t + 1) * 128], ident_f[0:NH, 0:NH], [b_ef, b_const], [pb[i]], t == NT_S - 1)
            K.op(DVE, lambda: v_.tensor_copy(out=cumLT[:], in_=pT1), [pb[i]], [b_cumLT])

            K.barrier()
            sv3 = al.off
            al.off = off_wq
            PT = [al.alloc("PT", [128, QB], BF16) for _ in range(6)]
            Rb = al.alloc("Rb", [128, QB], BF16)
            Rs = al.alloc("Rs", [128, QB], F32)
            al.off = sv3
            rot["l"] = [4, 5, 6, 7]
            pcount = 0
            LAG = 3
            grp = {"n": 0}
            pend_backs = []

            def att_front(j, q, t, half, nt, gpar):
                nonlocal pcount
                d = t - q * TQ
                q0 = max(d, 0) * 128
                h = 2 * j + half
                rows = slice(half * 64, half * 64 + 64)
                i = nb()
                mm(ps[i][:, q0:QB], kT[:, j, t * 128:(t + 1) * 128], qT[:, j, half, q * QB + q0:(q + 1) * QB],
                   True, False, [b_q, b_k], [pb[i]], False)
                mm(ps[i][:, q0:QB], cbt[:, 512 + h * 128:512 + (h + 1) * 128], caug[:, q * QB + q0:(q + 1) * QB],
                   False, d < 0, [b_const, b_caug], [pb[i]], d < 0)
                if d >= 0:
                    mm(ps[i][:, q0:q0 + 128], ident_b, negm_b, False, True, [b_const], [pb[i]], True)
                pt = PT[pcount % 6]
                bpt = b_PT[pcount % 6]
                pcount += 1
                K.op(ACT, lambda: a_.activation(out=pt[:, q0:QB], in_=ps[i][:, q0:QB], func=AF.Exp,
                                                bias=cumLT[:, t, h:h + 1], scale=0.125), [pb[i], b_cumLT], [bpt])

                def back():
                    yi = half + 2 * gpar
                    ya_, yb_ = 2 * gpar, 2 * gpar + 1
                    lo = 0 if half == 0 else 64
                    mm(ps[yi][:, q0:QB], vS[:, t, j, lo:lo + 128], pt[:, q0:QB], t == 0, t == nt - 1,
                       [b_v, bpt], [pb[yi]], True)
                    if t == nt - 1 and half == 1:
                        K.op(DVE, lambda: v_.reciprocal(out=Rs[64:128, :], in_=ps[ya_][64:128, 0:QB]), [pb[ya_], b_Rs], [b_Rs])
                        K.op(DVE, lambda: v_.reciprocal(out=Rs[0:64, :], in_=ps[yb_][0:64, 0:QB]), [pb[yb_], b_Rs], [b_Rs])
                        K.op(DVE, lambda: v_.tensor_copy(out=Rb[:], in_=Rs[:]), [b_Rs, b_Rb], [b_Rb])
                        isw = nb()
                        mm(ps[isw][:, 0:QB], swap_b, Rb[:, :], True, True, [b_const, b_Rb], [pb[isw]], True)
                        K.op(ACT, lambda: a_.copy(out=Rs[:], in_=ps[isw][:, 0:QB]), [pb[isw]], [b_Rs])
                        K.op(DVE, lambda: v_.tensor_tensor(out=yaT[0:64, j, q * QB:(q + 1) * QB], in0=ps[ya_][0:64, 0:QB],
                                                           in1=Rs[0:64, :], op=ALU.mult), [pb[ya_], b_Rs], [b_yaT])
                        K.op(DVE, lambda: v_.tensor_tensor(out=yaT[64:128, j, q * QB:(q + 1) * QB], in0=ps[yb_][64:128, 0:QB],
                                                           in1=Rs[64:128, :], op=ALU.mult), [pb[yb_], b_Rs], [b_yaT])
                return back

            for j in range(4):
                for q in range(NQ):
                    nt = q * TQ + TQ
                    gpar = grp["n"] % 2
                    grp["n"] += 1
                    precast(PC_PER)
                    for t in range(nt):
                        for half in range(2):
                            pend_backs.append(att_front(j, q, t, half, nt, gpar))
                            if len(pend_backs) > LAG:
                                pend_backs.pop(0)()
            while pend_backs:
                pend_backs.pop(0)()
            K.barrier()

            al.release(mU)
            xp = [al.alloc("xp", [128, S + 4], F32) for _ in range(2)]
            uu = [al.alloc("uu", [128, S], F32) for _ in range(2)]
            ub = [al.alloc("ub", [128, S], BF16) for _ in range(2)]
            gg = [al.alloc("gg", [128, S], BF16) for _ in range(2)]
            thr = al.alloc("thr", [128, S], F32)
            thi = al.alloc("thi", [128, S], F32)
            e2 = al.alloc("e2", [128, S], F32)
            wx = [al.alloc("wx", [128, 8, 128], BF16) for _ in range(2)]
            wg = [al.alloc("wg", [128, 8, 128], BF16) for _ in range(2)]
            wrg = [al.alloc("wrg", [128, 128], BF16) for _ in range(2)]
            wig = [al.alloc("wig", [128, 128], BF16) for _ in range(2)]
            gt = [[al.alloc("gt", [128, QB], F32) for _ in range(2)] for _ in range(2)]
            b_xp, b_uu, b_ub, b_gg = [Buf(), Buf()], [Buf(), Buf()], [Buf(), Buf()], [Buf(), Buf()]
            b_thr, b_thi, b_e2 = Buf(), Buf(), Buf()
            b_w4 = [[Buf() for _ in range(4)] for _ in range(2)]
            b_gt = [[Buf() for _ in range(2)] for _ in range(2)]
            rot["l"] = list(range(8))
            for s2_ in range(2):
                K.op(DVE, lambda: v_.memset(xp[s2_][:, 0:4], 0.0), [], [b_xp[s2_]])

            def load_w4(c):
                s_ = c % 2
                pl_dma(wx[s_][:], wview(OX + c * 128, 128), w=[b_w4[s_][0]])
                pl_dma(wg[s_][:], wview(OG + c * 128, 128), w=[b_w4[s_][1]])
                pl_dma(wrg[s_][:], wrg_d[c, :, :], w=[b_w4[s_][2]])
                pl_dma(wig[s_][:], wig_d[c, :, :], w=[b_w4[s_][3]])

            gcnt4 = {"n": 0}

            def m4_A(c):
                zero_fill(-(-NBLK // (NSEQ * 8)))
                s_ = c % 2
                bwx, bwg_, bwrg, bwig = b_w4[s_]
                xp_, uu_, ub_, gg_ = xp[s_], uu[s_], ub[s_], gg[s_]
                bxp, buu, bub, bgg = b_xp[s_], b_uu[s_], b_ub[s_], b_gg[s_]
                for n in range(NQ):
                    i = nb()
                    for kc in range(8):
                        mm(ps[i][:, 0:QB], wx[s_][:, kc, :], hT[:, kc, n * QB:(n + 1) * QB], kc == 0, kc == 7,
                           [bwx] + (b_hT[n * TQ:(n + 1) * TQ] + b_hT2[n * TQ:(n + 1) * TQ]), [pb[i]], kc == 7)
                    K.op(ACT, lambda: a_.copy(out=xp_[:, 3 + n * QB:3 + (n + 1) * QB], in_=ps[i][:, 0:QB]), [pb[i]], [bxp])
                K.op(DVE, lambda: v_.tensor_scalar(out=uu_[:], in0=xp_[:, 3:3 + S], scalar1=fm[:, 4, c:c + 1], scalar2=fm[:, 5, c:c + 1],
                                                   op0=ALU.mult, op1=ALU.add), [bxp, b_fm], [buu])
                for jj in range(3):
                    K.op(DVE, lambda: v_.scalar_tensor_tensor(out=uu_[:], in0=xp_[:, jj:jj + S], scalar=fm[:, 1 + jj, c:c + 1], in1=uu_[:],
                                                              op0=ALU.mult, op1=ALU.add), [bxp, b_fm, buu], [buu])
                K.op(POOL, lambda: g_.tensor_copy(out=ub_[:], in_=uu_[:]), [buu], [bub])
                for n in range(NQ):
                    i = nb()
                    for kc in range(8):
                        mm(ps[i][:, 0:QB], wg[s_][:, kc, :], hT[:, kc, n * QB:(n + 1) * QB], kc == 0, kc == 7,
                           [bwg_] + (b_hT[n * TQ:(n + 1) * TQ] + b_hT2[n * TQ:(n + 1) * TQ]), [pb[i]], kc == 7)
                    g0, g1 = gt[gcnt4["n"] % 2]
                    bg0, bg1 = b_gt[gcnt4["n"] % 2]
                    gcnt4["n"] += 1
                    pg = ps[i][:, 0:QB]
                    K.op(ACT, lambda: a_.activation(out=g0[:], in_=pg, func=AF.Square), [pb[i]], [bg0])
                    K.op(DVE, lambda: v_.tensor_scalar(out=g0[:], in0=g0[:], scalar1=0.044715, scalar2=1.0, op0=ALU.mult, op1=ALU.add),
                         [bg0], [bg0])
                    K.op(DVE, lambda: v_.tensor_tensor(out=g0[:], in0=g0[:], in1=pg, op=ALU.mult), [bg0, pb[i]], [bg0])
                    K.op(ACT, lambda: a_.activation(out=g1[:], in_=g0[:], func=AF.Tanh, scale=0.7978845608028654), [bg0], [bg1])
                    K.op(DVE, lambda: v_.scalar_tensor_tensor(out=gg_[:, n * QB:(n + 1) * QB], in0=g1[:], scalar=1.0, in1=pg,
                                                              op0=ALU.add, op1=ALU.mult), [bg1, pb[i]], [bgg])

            def m4_B(c):
                s_ = c % 2
                bwx, bwg_, bwrg, bwig = b_w4[s_]
                uu_, ub_, gg_ = uu[s_], ub[s_], gg[s_]
                buu, bub, bgg = b_uu[s_], b_ub[s_], b_gg[s_]
                for n in range(NQ):
                    i = nb()
                    mm(ps[i][:, 0:QB], wrg[s_][:, :], ub_[:, n * QB:(n + 1) * QB], True, True, [bwrg, bub], [pb[i]], True)
                    K.op(ACT, lambda: a_.activation(out=thr[:, n * QB:(n + 1) * QB], in_=ps[i][:, 0:QB], func=AF.Tanh,
                                                    bias=hbrg[:, c:c + 1], scale=0.5), [pb[i], b_sm], [b_thr])
                    i2 = nb()
                    mm(ps[i2][:, 0:QB], wig[s_][:, :], ub_[:, n * QB:(n + 1) * QB], True, True, [bwig, bub], [pb[i2]], True)
                    K.op(ACT, lambda: a_.activation(out=thi[:, n * QB:(n + 1) * QB], in_=ps[i2][:, 0:QB], func=AF.Tanh,
                                                    bias=hbig[:, c:c + 1], scale=0.5), [pb[i2], b_sm], [b_thi])
                K.op(ACT, lambda: a_.activation(out=e2[:], in_=thr[:], func=AF.Exp, bias=cneg[:, c:c + 1], scale=cneg[:, c:c + 1]),
                     [b_thr, b_sm], [b_e2])
                K.op(ACT, lambda: a_.activation(out=thr[:], in_=thr[:], func=AF.Exp, bias=hcneg[:, c:c + 1], scale=hcneg[:, c:c + 1]),
                     [b_thr, b_sm], [b_thr])
                K.op(DVE, lambda: v_.tensor_scalar(out=e2[:], in0=e2[:], scalar1=1.0 - 1.0e-7, scalar2=None, op0=ALU.min), [b_e2], [b_e2])
                K.op(ACT, lambda: a_.activation(out=e2[:], in_=e2[:], func=AF.Sqrt, bias=1.0, scale=-1.0), [b_e2], [b_e2])
                K.op(DVE, lambda: v_.scalar_tensor_tensor(out=thi[:], in0=thi[:], scalar=1.0, in1=e2[:], op0=ALU.add, op1=ALU.mult),
                     [b_thi, b_e2], [b_thi])
                K.op(DVE, lambda: v_.scalar_tensor_tensor(out=thi[:], in0=thi[:], scalar=0.5, in1=uu_[:], op0=ALU.mult, op1=ALU.mult),
                     [b_thi, buu], [b_thi])
                K.op(DVE, lambda: v_.tensor_tensor_scan(out=e2[:], data0=thr[:], data1=thi[:], initial=0.0, op0=ALU.mult, op1=ALU.add),
                     [b_thr, b_thi, b_e2], [b_e2])
                K.op(DVE, lambda: v_.scalar_tensor_tensor(out=yrT[:, c, :], in0=gg_[:], scalar=0.5, in1=e2[:], op0=ALU.mult, op1=ALU.mult),
                     [bgg, b_e2], [b_yrT])

            load_w4(0)
            load_w4(1)
            m4_A(0)
            for c in range(8):
                if c + 1 < 8:
                    m4_A(c + 1)
                m4_B(c)
                if c + 2 < 8:
                    load_w4(c + 2)
            K.barrier()

            al.release(mU)
            mgT = al.alloc("mgT", [128, 8, S], BF16)
            b_mg = [Buf() for _ in range(NT_S)]
            m5 = al.mark()
            w5 = [al.alloc("w5", [128, 28, 128], BF16) for _ in range(2)]
            b_w5 = [[Buf() for _ in range(4)] for _ in range(2)]
            sa = [[al.alloc("sa", [128, QB], F32) for _ in range(4)] for _ in range(2)]
            b_sa = [[Buf() for _ in range(4)] for _ in range(2)]
            rot["l"] = list(range(8))

            def load_w5(m):
                s_ = m % 2
                cs_ = slice(m * 128, (m + 1) * 128)
                pl_dma(w5[s_][:, 0:4, :], wba_d[:, cs_].rearrange("(kc p) n -> p kc n", p=128), w=[b_w5[s_][0]])
                pl_dma(w5[s_][:, 4:12, :], wbr_d[:, cs_].rearrange("(kc p) n -> p kc n", p=128), w=[b_w5[s_][1]])
                pl_dma(w5[s_][:, 12:20, :], wview(OGA + m * 128, 128), w=[b_w5[s_][2]])
                pl_dma(w5[s_][:, 20:28, :], wview(OGR + m * 128, 128), w=[b_w5[s_][3]])
            load_w5(0)
            scount = 0
            for m in range(8):
                s_ = m % 2
                bwa, bwr_, bwga, bwgr = b_w5[s_]
                if m + 1 < 8:
                    load_w5(m + 1)
                for n in range(NQ):
                    cols = slice(n * QB, (n + 1) * QB)
                    hb = (b_hT[n * TQ:(n + 1) * TQ] + b_hT2[n * TQ:(n + 1) * TQ])
                    iA, iR, iGA, iGR = nb(), nb(), nb(), nb()
                    for kc in range(4):
                        mm(ps[iA][:, 0:QB], w5[s_][:, kc, :], yaT[:, kc, cols], kc == 0, kc == 3, [bwa, b_yaT], [pb[iA]], kc == 3)
                    for kc in range(8):
                        mm(ps[iGA][:, 0:QB], w5[s_][:, 12 + kc, :], hT[:, kc, cols], kc == 0, kc == 7, [bwga] + hb, [pb[iGA]], kc == 7)
                    for kc in range(8):
                        mm(ps[iR][:, 0:QB], w5[s_][:, 4 + kc, :], yrT[:, kc, cols], kc == 0, kc == 7, [bwr_, b_yrT], [pb[iR]], kc == 7)
                    for kc in range(8):
                        mm(ps[iGR][:, 0:QB], w5[s_][:, 20 + kc, :], hT[:, kc, cols], kc == 0, kc == 7, [bwgr] + hb, [pb[iGR]], kc == 7)
                    s0, s1, s2, s3 = sa[scount % 2]
                    c0, c1, c2, c3 = b_sa[scount % 2]
                    scount += 1
                    K.op(ACT, lambda: a_.activation(out=s0[:], in_=ps[iGA][:, 0:QB], func=AF.Sigmoid), [pb[iGA]], [c0])
                    K.op(DVE, lambda: v_.tensor_tensor(out=s1[:], in0=s0[:], in1=ps[iA][:, 0:QB], op=ALU.mult), [c0, pb[iA]], [c1])
                    K.op(ACT, lambda: a_.activation(out=s2[:], in_=ps[iGR][:, 0:QB], func=AF.Sigmoid), [pb[iGR]], [c2])
                    K.op(DVE, lambda: v_.tensor_tensor(out=s3[:], in0=s2[:], in1=ps[iR][:, 0:QB], op=ALU.mult), [c2, pb[iR]], [c3])
                    K.op(POOL, lambda: g_.tensor_tensor(out=mgT[:, m, cols], in0=s1[:], in1=s3[:], op=ALU.add), [c1, c3],
                         b_mg[n * TQ:(n + 1) * TQ])
            K.barrier()

            al.release(m5)
            offB = al.off
            useA = (mU - off_hT) >= 80 * 1024
            if useA:
                al.off = off_hT
                al.limit = mU
            wo = al.alloc("wo", [128, 8, D], BF16)
            h2Tb = [al.alloc("h2Tb", [128, 8, QB], BF16) for _ in range(2)]
            xs2 = [al.alloc("xs2", [128, D], F32) for _ in range(2)]
            x1 = [al.alloc("x1", [128, D], F32) for _ in range(2)]
            h2 = [al.alloc("h2", [128, D], F32) for _ in range(2)]
            h2T = [al.alloc("h2T", [128, 8, 128], F32) for _ in range(2)]
            shs = [al.alloc("shs", [128, D], BF16) for _ in range(2)]
            h2b = [al.alloc("h2b", [128, D], BF16) for _ in range(2)]
            if useA:
                al.off = offB
                al.limit = LIMIT
            wsg = al.alloc("wsg", [128, 8, FF], BF16)
            wsu = al.alloc("wsu", [128, 8, FF], BF16)
            wsd = al.alloc("wsd", [128, 2, D], BF16)
            wr = al.alloc("wr", [128, 8, E], F32)
            bcv5 = al.alloc("bcv5", [128, 2, D], F32)
            b_bcv5 = Buf()
            gmb = al.alloc("gmb", [128, D], F32)
            gsfb = al.alloc("gsfb", [128, D], F32)
            shfb = al.alloc("shfb", [128, D], F32)
            sg = [al.alloc("sg", [128, QB], F32) for _ in range(2)]
            actT = al.alloc("actT", [128, 2, QB], BF16)
            rs = [al.alloc("rs", [128, 8], F32) for _ in range(2)]
            rt_ = [al.alloc("rt", [128, 6, E], F32) for _ in range(2)]
            g8 = [al.alloc("g8", [128, 8, 8], F32) for _ in range(2)]
            r8 = [al.alloc("r8", [128, 4, 8], F32) for _ in range(2)]
            i8 = [al.alloc("i8", [128, 8], U32) for _ in range(2)]
            mkb = [al.alloc("mkb", [128, E], BF16) for _ in range(2)]
            b_wo, b_ws, b_wr, b_bc5 = Buf(), Buf(), Buf(), Buf()
            b_xs2, b_x1, b_h2, b_h2b, b_h2T = [Buf(), Buf()], [Buf(), Buf()], [Buf(), Buf()], [Buf(), Buf()], [Buf(), Buf()]
            b_h2Tb, b_shs, b_sg, b_act, b_rs, b_rt = [Buf(), Buf()], [Buf(), Buf()], [Buf(), Buf()], Buf(), [Buf(), Buf()], [Buf(), Buf()]
            b_jk5 = Buf()
            jk5 = al.alloc("jk5", [128, D], BF16)
            pl_dma(wo[:], wout_d.ap().rearrange("(kc p) n -> p kc n", p=128), w=[b_wo])
            b_wsg, b_wsu, b_wsd = Buf(), Buf(), Buf()
            pl_dma(wsg[:], wsg_d.ap().rearrange("(kc p) n -> p kc n", p=128), w=[b_wsg])
            pl_dma(wsu[:], wsu_d.ap().rearrange("(kc p) n -> p kc n", p=128), w=[b_wsu])
            pl_dma(wsd[:], wsd_d.ap().rearrange("(kc p) n -> p kc n", p=128), w=[b_wsd])
            sp_dma(wr[:], wr_d.ap().rearrange("(kc p) n -> p kc n", p=128), w=[b_wr])
            sp_dma(bcv5[:], bcv_d[:, 0:2, :], w=[b_bcv5])
            sp_dma(gmb[:], modd[b:b + 1, 2 * D:3 * D].partition_broadcast(128), r=[b_modd], w=[b_bc5])
            sp_dma(gsfb[:], modd[b:b + 1, 4 * D:5 * D].partition_broadcast(128), r=[b_modd], w=[b_bc5])
            sp_dma(shfb[:], modd[b:b + 1, 3 * D:4 * D].partition_broadcast(128), r=[b_modd], w=[b_bc5])
            K.op(DVE, lambda: v_.tensor_tensor(out=gmb[:], in0=gmb[:], in1=bcv5[:, 0, :], op=ALU.mult), [b_bc5, b_bcv5], [b_bc5])
            K.op(DVE, lambda: v_.scalar_tensor_tensor(out=gsfb[:], in0=gsfb[:], scalar=1.0, in1=bcv5[:, 1, :], op0=ALU.add, op1=ALU.mult),
                 [b_bc5, b_bcv5], [b_bc5])
            rot["l"] = list(range(8))
            def m5_A(t):
                n, tt = t // TQ, t % TQ
                T = b * NT_S + t
                r0 = tok0 + t * 128
                p_ = t % 2
                xb, x1b, h2_, h2b_, h2T_, rs_ = xs2[p_], x1[p_], h2[p_], h2b[p_], h2T[p_], rs[p_]
                bxb, bx1, bh2, bh2b, bh2T, brs = b_xs2[p_], b_x1[p_], b_h2[p_], b_h2b[p_], b_h2T[p_], b_rs[p_]
                sp_dma(xb[:], x_d[r0:r0 + 128, :], w=[bxb])
                io = [nb(), nb()]
                for hf in range(2):
                    for kc in range(8):
                        mm(ps[io[hf]][:, :], mgT[:, kc, t * 128:(t + 1) * 128], wo[:, kc, hf * 512:(hf + 1) * 512], kc == 0, kc == 7,
                           [b_mg[t], b_wo], [pb[io[hf]]], kc == 7)
                for hf in range(2):
                    K.op(ACT, lambda: a_.activation(out=jk5[:, hf * 512:(hf + 1) * 512], in_=ps[io[hf]][:, :], func=AF.Square,
                                                    accum_out=rs_[:, hf:hf + 1]), [pb[io[hf]]], [b_jk5, brs])
                K.op(DVE, lambda: v_.tensor_tensor(out=rs_[:, 2:3], in0=rs_[:, 0:1], in1=rs_[:, 1:2], op=ALU.add), [brs], [brs])
                K.op(ACT, lambda: a_.activation(out=rs_[:, 3:4], in_=rs_[:, 2:3], func=AF.Sqrt, bias=EPS, scale=1.0 / D), [brs], [brs])
                K.op(DVE, lambda: v_.reciprocal(out=rs_[:, 4:5], in_=rs_[:, 3:4]), [brs], [brs])
                for hf in range(2):
                    cs_ = slice(hf * 512, (hf + 1) * 512)
                    K.op(DVE, lambda: v_.scalar_tensor_tensor(out=x1b[:, cs_], in0=ps[io[hf]][:, :], scalar=rs_[:, 4:5], in1=gmb[:, cs_],
                                                              op0=ALU.mult, op1=ALU.mult), [pb[io[hf]], brs, b_bc5], [bx1])
                K.op(POOL, lambda: g_.tensor_tensor(out=x1b[:], in0=x1b[:], in1=xb[:], op=ALU.add), [bx1, bxb], [bx1])
                sp_dma(x1_d[r0:r0 + 128, :], x1b[:], r=[bx1])
                K.op(ACT, lambda: a_.activation(out=jk5[:], in_=x1b[:], func=AF.Square, accum_out=rs_[:, 5:6]), [bx1], [b_jk5, brs])
                K.op(ACT, lambda: a_.activation(out=rs_[:, 6:7], in_=rs_[:, 5:6], func=AF.Sqrt, bias=EPS, scale=1.0 / D), [brs], [brs])
                K.op(DVE, lambda: v_.reciprocal(out=rs_[:, 7:8], in_=rs_[:, 6:7]), [brs], [brs])
                K.op(DVE, lambda: v_.scalar_tensor_tensor(out=h2_[:], in0=x1b[:], scalar=rs_[:, 7:8], in1=gsfb[:], op0=ALU.mult, op1=ALU.mult),
                     [bx1, brs, b_bc5], [bh2])
                K.op(POOL, lambda: g_.tensor_tensor(out=h2_[:], in0=h2_[:], in1=shfb[:], op=ALU.add), [bh2, b_bc5], [bh2])
                K.op(POOL, lambda: g_.tensor_copy(out=h2b_[:], in_=h2_[:]), [bh2], [bh2b])
                sp_dma(h2_d[r0:r0 + 128, :], h2b_[:], r=[bh2b])

            def m5_B(t):
                n, tt = t // TQ, t % TQ
                hb_ = h2Tb[n % 2]
                bhb = b_h2Tb[n % 2]
                T = b * NT_S + t
                p_ = t % 2
                h2_, h2T_ = h2[p_], h2T[p_]
                bh2, bh2T = b_h2[p_], b_h2T[p_]
                it = [nb(), nb()]
                for kc in range(8):
                    ib = it[kc // 4]
                    tr(ps[ib][:, (kc % 4) * 128:(kc % 4 + 1) * 128], h2_[:, kc * 128:(kc + 1) * 128], ident_f, [bh2, b_const], [pb[ib]],
                       kc % 4 == 3)
                for hf in range(2):
                    K.op(ACT, lambda: a_.copy(out=h2T_[:, hf * 4:(hf + 1) * 4, :], in_=ps[it[hf]][:, :].rearrange("p (a b) -> p a b", b=128)),
                         [pb[it[hf]]], [bh2T])
                K.op(POOL, lambda: g_.tensor_copy(out=hb_[:, :, tt * 128:(tt + 1) * 128], in_=h2T_[:]), [bh2T], [bhb])
                il = nb()
                for kc in range(8):
                    mm(ps[il][:, 0:E], h2T_[:, kc, :], wr[:, kc, :], kc == 0, kc == 7, [bh2T, b_wr], [pb[il]], kc == 7)
                R_, g8_, r8_, i8_, mk_ = rt_[p_], g8[p_], r8[p_], i8[p_], mkb[p_]
                brt = b_rt[p_]
                sc, sel, selm, mkf, wfull, tmp = (R_[:, k_, :] for k_ in range(6))
                K.op(ACT, lambda: a_.activation(out=sc, in_=ps[il][:, 0:E], func=AF.Sigmoid), [pb[il]], [brt])
                K.op(DVE, lambda: v_.tensor_tensor(out=sel, in0=sc, in1=rbias[:], op=ALU.add), [brt, b_const], [brt])
                for gg in range(8):
                    K.op(DVE, lambda: v_.max(out=g8_[:, gg, :], in_=sel[:, gg * 8:(gg + 1) * 8]), [brt], [brt])
                K.op(DVE, lambda: v_.tensor_tensor(out=r8_[:, 0, :], in0=g8_[:, :, 0], in1=g8_[:, :, 1], op=ALU.add), [brt], [brt])
                K.op(DVE, lambda: v_.max(out=r8_[:, 1, :], in_=r8_[:, 0, :]), [brt], [brt])
                K.op(DVE, lambda: v_.tensor_scalar(out=r8_[:, 2, :], in0=r8_[:, 0, :], scalar1=r8_[:, 1, 3:4], scalar2=-BIG,
                                                   op0=ALU.is_lt, op1=ALU.mult), [brt], [brt])
                K.op(DVE, lambda: v_.tensor_tensor(out=selm.rearrange("p (a b) -> p a b", b=8), in0=sel.rearrange("p (a b) -> p a b", b=8),
                                                   in1=r8_[:, 2, :].unsqueeze(2).to_broadcast([128, 8, 8]), op=ALU.add), [brt], [brt])
                K.op(DVE, lambda: v_.max(out=r8_[:, 3, :], in_=selm), [brt], [brt])
                K.op(DVE, lambda: v_.max_index(out=i8_[:], in_max=r8_[:, 3, :], in_values=selm), [brt], [brt])
                K.op(DVE, lambda: v_.tensor_scalar(out=mkf, in0=selm, scalar1=r8_[:, 3, 5:6], scalar2=None, op0=ALU.is_ge), [brt], [brt])
                K.op(DVE, lambda: v_.tensor_tensor(out=wfull, in0=sc, in1=mkf, op=ALU.mult), [brt], [brt])
                K.op(DVE, lambda: v_.tensor_reduce(out=r8_[:, 2, 0:1], in_=wfull, axis=AX.X, op=ALU.add), [brt], [brt])
                K.op(DVE, lambda: v_.reciprocal(out=r8_[:, 2, 1:2], in_=r8_[:, 2, 0:1]), [brt], [brt])
                K.op(POOL, lambda: g_.tensor_copy(out=mk_[:], in_=mkf), [brt], [brt])
                ik = nb()
                mm(ps[ik][:, 0:E], triu_b, mk_[:], True, True, [brt, b_const], [pb[ik]], False)
                mm(ps[ik][:, E:2 * E], ones_b, mk_[:], True, True, [brt, b_const], [pb[ik]], True)
                K.op(DVE, lambda: v_.tensor_tensor(out=tmp, in0=ps[ik][:, 0:E], in1=run[:], op=ALU.add), [pb[ik], b_run, brt], [brt])
                K.op(DVE, lambda: v_.tensor_tensor(out=rankm[:, T, :], in0=tmp, in1=mkf, op=ALU.mult), [brt], [b_route])
                K.op(DVE, lambda: v_.tensor_tensor(out=run[:], in0=run[:], in1=ps[ik][:, E:2 * E], op=ALU.add), [pb[ik], b_run], [b_run])
                K.op(DVE, lambda: v_.tensor_copy(out=eidx[:, T, :], in_=i8_[:]), [brt], [b_route])
                for k_ in range(TOPK):
                    K.op(DVE, lambda: v_.scalar_tensor_tensor(out=tmp, in0=iota64, scalar=eidx[:, T, k_:k_ + 1], in1=wfull,
                                                              op0=ALU.is_equal, op1=ALU.mult, accum_out=wk[:, T, k_:k_ + 1]),
                         [brt, b_route, b_const], [brt, b_route])
                K.op(DVE, lambda: v_.tensor_scalar(out=wkn[:, T, 0:TOPK], in0=wk[:, T, 0:TOPK], scalar1=r8_[:, 2, 1:2], scalar2=2.5,
                                                   op0=ALU.mult, op1=ALU.mult), [brt, b_route], [b_route])

            def m5_SH(n):
                hb_ = h2Tb[n % 2]
                bhb = b_h2Tb[n % 2]
                ig = [nb(), nb()]
                iu = [nb(), nb()]
                for c in range(2):
                    for kc in range(8):
                        mm(ps[ig[c]][:, 0:QB], wsg[:, kc, c * 128:(c + 1) * 128], hb_[:, kc, :], kc == 0, kc == 7, [b_wsg, bhb], [pb[ig[c]]], kc == 7)
                    for kc in range(8):
                        mm(ps[iu[c]][:, 0:QB], wsu[:, kc, c * 128:(c + 1) * 128], hb_[:, kc, :], kc == 0, kc == 7, [b_wsu, bhb], [pb[iu[c]]], kc == 7)
                    sg_ = sg[c]
                    K.op(ACT, lambda: a_.activation(out=sg_[:], in_=ps[ig[c]][:, 0:QB], func=AF.Sigmoid), [pb[ig[c]]], [b_sg[c]])
                    K.op(DVE, lambda: v_.tensor_tensor(out=sg_[:], in0=sg_[:], in1=ps[ig[c]][:, 0:QB], op=ALU.mult), [b_sg[c], pb[ig[c]]], [b_sg[c]])
                    K.op(DVE, lambda: v_.tensor_tensor(out=actT[:, c, :], in0=sg_[:], in1=ps[iu[c]][:, 0:QB], op=ALU.mult),
                         [b_sg[c], pb[iu[c]]], [b_act])
                for tt in range(TQ):
                    t = n * TQ + tt
                    r0 = tok0 + t * 128
                    iy = [nb(), nb()]
                    for hf in range(2):
                        for c in range(2):
                            mm(ps[iy[hf]][:, :], actT[:, c, tt * 128:(tt + 1) * 128], wsd[:, c, hf * 512:(hf + 1) * 512], c == 0, c == 1,
                               [b_act, b_wsd], [pb[iy[hf]]], c == 1)
                    sh_ = shs[t % 2]
                    K.op(ACT, lambda: a_.copy(out=sh_[:, 0:512], in_=ps[iy[0]][:, :]), [pb[iy[0]]], [b_shs[t % 2]])
                    K.op(DVE, lambda: v_.tensor_copy(out=sh_[:, 512:1024], in_=ps[iy[1]][:, :]), [pb[iy[1]]], [b_shs[t % 2]])
                    sp_dma(sh_d[r0:r0 + 128, :], sh_[:], r=[b_shs[t % 2]])

            m5_A(0)
            for t in range(NT_S):
                if t + 1 < NT_S:
                    m5_A(t + 1)
                m5_B(t)
                if t % TQ == TQ - 1:
                    m5_SH(t // TQ)
            K.barrier()

        al.release(mU)
        al.off = mU
        pe_ = al.alloc("pend", [128, 4, E], F32)
        pei = al.alloc("pei", [128, E], I32)
        ebf = al.alloc("ebf", [128, 2, NBLK], F32)
        djk = al.alloc("djk", [128, E], F32)
        rkp = al.alloc("rkp", [128, E], F32)
        destf = al.alloc("destf", [128, NT, 8], F32)
        b_pe, b_eb, b_dj, b_rkp, b_destf, b_desti = Buf(), Buf(), Buf(), Buf(), Buf(), Buf()
        b_desti_t = [Buf() for _ in range(NT)]
        K.op(DVE, lambda: v_.tensor_scalar(out=pe_[:, 3, :], in0=run[:], scalar1=float(CB - 1), scalar2=None, op0=ALU.add), [b_run], [b_pe])
        K.op(DVE, lambda: v_.tensor_copy(out=pei[:], in_=pe_[:, 3, :]), [b_pe], [b_pe])
        K.op(DVE, lambda: v_.tensor_single_scalar(out=pei[:], in_=pei[:], scalar=8, op=ALU.arith_shift_right), [b_pe], [b_pe])
        K.op(DVE, lambda: v_.tensor_copy(out=pe_[:, 0, :], in_=pei[:]), [b_pe], [b_pe])
        K.op(DVE, lambda: v_.tensor_tensor_scan(out=pe_[:, 1, :], data0=ones_c[:, 0:1].to_broadcast([128, E]), data1=pe_[:, 0, :],
                                                initial=0.0, op0=ALU.mult, op1=ALU.add), [b_pe, b_const], [b_pe])
        K.op(DVE, lambda: v_.tensor_tensor(out=pe_[:, 2, :], in0=pe_[:, 1, :], in1=pe_[:, 0, :], op=ALU.subtract), [b_pe], [b_pe])
        K.op(DVE, lambda: v_.tensor_scalar(out=pe_[:, 2, :], in0=pe_[:, 2, :], scalar1=float(CB), scalar2=None, op0=ALU.mult), [b_pe], [b_pe])
        for T in range(NT):
            K.op(DVE, lambda: v_.tensor_tensor(out=rkp[:], in0=rankm[:, T, :], in1=pe_[:, 2, :], op=ALU.add), [b_route, b_pe, b_rkp], [b_rkp])
            for k_ in range(TOPK):
                K.op(DVE, lambda: v_.scalar_tensor_tensor(out=djk[:], in0=iota64, scalar=eidx[:, T, k_:k_ + 1], in1=rkp[:],
                                                          op0=ALU.is_equal, op1=ALU.mult, accum_out=destf[:, T, k_:k_ + 1]),
                     [b_route, b_rkp, b_const], [b_dj, b_destf])
            K.op(DVE, lambda: v_.tensor_copy(out=desti[:, T, 0:TOPK], in_=destf[:, T, 0:TOPK]), [b_destf], [b_desti_t[T]])
        hg = [al.alloc("hg", [128, D], BF16) for _ in range(3)]
        b_hg = [Buf() for _ in range(3)]
        b_xs = Buf()
        for T in range(NT):
            hb_ = hg[T % 3]
            sp_dma(hb_[:], h2_d[T * 128:(T + 1) * 128, :], w=[b_hg[T % 3]])
            for k_ in range(TOPK):
                K.dma(K.qpool, lambda: g_.indirect_dma_start(out=xs_d[:, :], out_offset=bass.IndirectOffsetOnAxis(ap=desti[:, T, k_:k_ + 1], axis=0),
                                                             in_=hb_[:], in_offset=None, bounds_check=reg_slot, oob_is_err=False),
                      [b_hg[T % 3], b_desti_t[T], b_xs0], [], [b_xs])
        K.op(DVE, lambda: v_.memset(ebf[:, 0, :], 0.0), [], [b_eb])
        for e_ in range(E):
            K.op(DVE, lambda: v_.scalar_tensor_tensor(out=ebf[:, 0, :], in0=iotab, scalar=pe_[:, 1, e_:e_ + 1], in1=ebf[:, 0, :],
                                                      op0=ALU.is_ge, op1=ALU.add), [b_pe, b_const, b_eb], [b_eb])
        K.op(DVE, lambda: v_.tensor_scalar(out=ebf[:, 0, :], in0=ebf[:, 0, :], scalar1=float(E - 1), scalar2=128.0, op0=ALU.min, op1=ALU.mult),
             [b_eb], [b_eb])
        K.op(DVE, lambda: v_.tensor_scalar(out=ebf[:, 1, :], in0=ebf[:, 0, :], scalar1=pidx, scalar2=None, op0=ALU.add), [b_eb, b_const], [b_eb])
        if skip_reload and NBLK > 2:
            K.op(DVE, lambda: v_.tensor_tensor(out=ebf[:, 0, 2:NBLK], in0=ebf[:, 0, 2:NBLK], in1=ebf[:, 1, 0:NBLK - 2], op=ALU.subtract),
                 [b_eb], [b_eb])
            K.op(DVE, lambda: v_.tensor_scalar(out=ebf[:, 0, 2:NBLK], in0=ebf[:, 0, 2:NBLK], scalar1=pidx, scalar2=None, op0=ALU.add),
                 [b_eb, b_const], [b_eb])
            K.op(DVE, lambda: v_.tensor_scalar(out=ebf[:, 0, 2:NBLK], in0=ebf[:, 0, 2:NBLK], scalar1=0.0, scalar2=1.0e6,
                                               op0=ALU.is_equal, op1=ALU.mult), [b_eb], [b_eb])
            K.op(DVE, lambda: v_.tensor_tensor(out=ebf[:, 1, 2:NBLK], in0=ebf[:, 1, 2:NBLK], in1=ebf[:, 0, 2:NBLK], op=ALU.add), [b_eb], [b_eb])
        K.op(DVE, lambda: v_.tensor_copy(out=widx[:], in_=ebf[:, 1, :]), [b_eb], [b_widx])
        K.barrier()

        al.off = mU
        wE = [[al.alloc("wE", [128, 2048], BF16) for _ in range(3)] for _ in range(2)]
        b_wE = [[Buf() for _ in range(3)] for _ in range(2)]
        xsb = [al.alloc("xsb", [128, 2, D], BF16) for _ in range(3)]
        xTe = [al.alloc("xTe", [128, 8, CB], BF16) for _ in range(3)]
        sge = [al.alloc("sge", [128, 2 * CB], F32) for _ in range(2)]
        acte = [al.alloc("acte", [128, 2 * CB], BF16) for _ in range(2)]
        ysb = [al.alloc("ysb", [128, D], BF16) for _ in range(4)]
        b_xsb, b_xTe, b_sge, b_acte = [Buf(), Buf(), Buf()], [Buf(), Buf(), Buf()], [Buf(), Buf()], [Buf(), Buf()]
        b_ysb = [Buf() for _ in range(4)]
        b_ys = Buf()
        rot["l"] = list(range(8))
        wsrc = wpb_d

        def load_wE(blk, which):
            s_ = blk % 2
            for m in which:
                K.dma(K.qpool, lambda: g_.indirect_dma_start(out=wE[s_][m][:], out_offset=None, in_=wsrc[m][:, :],
                                                             in_offset=bass.IndirectOffsetOnAxis(ap=widx[:, blk:blk + 1], axis=0),
                                                             bounds_check=reg_w, oob_is_err=False),
                      [b_widx, b_wcast], [b_wE[s_][m]])

        def load_xs(blk):
            p_ = blk % 3
            sp_dma(xsb[p_][:], xs_d[blk * CB:(blk + 1) * CB, :].rearrange("(s p) d -> p s d", p=128), r=[b_xs], w=[b_xsb[p_]])

        def stage_T(blk):
            p_ = blk % 3
            for s2 in range(2):
                i = nb()
                pT = ps[i][:, :].bitcast(BF16)
                for kc in range(8):
                    tr(pT[:, kc * 128:(kc + 1) * 128], xsb[p_][:, s2, kc * 128:(kc + 1) * 128], ident_b, [b_xsb[p_], b_const], [pb[i]], kc == 7)
                o_ap = xTe[p_][:, :, s2 * 128:(s2 + 1) * 128]
                i_ap = pT.rearrange("p (a b) -> p a b", b=128)
                if s2 == 0:
                    K.op(ACT, lambda: a_.copy(out=o_ap, in_=i_ap), [pb[i]], [b_xTe[p_]])
                else:
                    K.op(DVE, lambda: v_.tensor_copy(out=o_ap, in_=i_ap), [pb[i]], [b_xTe[p_]])

        def stage_GU(blk):
            s_ = blk % 2
            p_ = blk % 2
            x_ = blk % 3
            wgE, wuE, wdE = wE[s_]
            bwg, bwu, bwd = b_wE[s_]
            ig_, iu_ = nb(), nb()
            for c in range(2):
                for kc in range(8):
                    mm(ps[ig_][:, c * CB:(c + 1) * CB], wgE[:, kc * FF + c * 128: kc * FF + (c + 1) * 128], xTe[x_][:, kc, :], kc == 0, kc == 7,
                       [bwg, b_xTe[x_]], [pb[ig_]], kc == 7)
            for c in range(2):
                for kc in range(8):
                    mm(ps[iu_][:, c * CB:(c + 1) * CB], wuE[:, kc * FF + c * 128: kc * FF + (c + 1) * 128], xTe[x_][:, kc, :], kc == 0, kc == 7,
                       [bwu, b_xTe[x_]], [pb[iu_]], kc == 7)
            K.op(ACT, lambda: a_.activation(out=sge[p_][:], in_=ps[ig_][:, :], func=AF.Sigmoid), [pb[ig_]], [b_sge[p_]])
            K.op(DVE, lambda: v_.tensor_tensor(out=sge[p_][:], in0=sge[p_][:], in1=ps[ig_][:, :], op=ALU.mult), [b_sge[p_], pb[ig_]], [b_sge[p_]])
            K.op(DVE, lambda: v_.tensor_tensor(out=acte[p_][:], in0=sge[p_][:], in1=ps[iu_][:, :], op=ALU.mult), [b_sge[p_], pb[iu_]], [b_acte[p_]])

        ycnt = {"n": 0}

        def stage_D(blk):
            s_ = blk % 2
            p_ = blk % 2
            wdE = wE[s_][2]
            bwd = b_wE[s_][2]
            for s2 in range(2):
                yb = ysb[ycnt["n"] % 4]
                byb = b_ysb[ycnt["n"] % 4]
                ycnt["n"] += 1
                for hf in range(2):
                    i = nb()
                    for c in range(2):
                        mm(ps[i][:, :], acte[p_][:, c * CB + s2 * 128: c * CB + (s2 + 1) * 128], wdE[:, c * D + hf * 512: c * D + (hf + 1) * 512],
                           c == 0, c == 1, [b_acte[p_], bwd], [pb[i]], c == 1)
                    if hf == 0:
                        K.op(ACT, lambda: a_.copy(out=yb[:, 0:512], in_=ps[i][:, :]), [pb[i]], [byb])
                    else:
                        K.op(DVE, lambda: v_.tensor_copy(out=yb[:, 512:1024], in_=ps[i][:, :]), [pb[i]], [byb])
                r0 = blk * CB + s2 * 128
                sp_dma(ys_d[r0:r0 + 128, :], yb[:], r=[byb], sw=[b_ys])

        precast(3 * E)
        load_wE(0, (0, 1, 2))
        if NBLK > 1:
            load_wE(1, (0, 1, 2))
        for b0 in range(min(3, NBLK)):
            load_xs(b0)
        stage_T(0)
        if NBLK > 1:
            stage_T(1)
        stage_GU(0)
        for blk in range(NBLK):
            if blk + 2 < NBLK:
                load_wE(blk + 2, (0, 1))
                stage_T(blk + 2)
                if blk + 3 < NBLK:
                    load_xs(blk + 3)
            if blk >= 1:
                stage_D(blk - 1)
                if blk + 1 < NBLK:
                    load_wE(blk + 1, (2,))
            if blk + 1 < NBLK:
                stage_GU(blk + 1)
        stage_D(NBLK - 1)
        K.barrier()

        al.off = mU
        gfb = al.alloc("gfb", [128, NSEQ, D], F32)
        bcvc = al.alloc("bcvc", [128, D], F32)
        b_bcvc = Buf()
        sp_dma(bcvc[:], bcv_d[:, 2, :], w=[b_bcvc])
        b_gfb = Buf()
        for b in range(NSEQ):
            sp_dma(gfb[:, b, :], modd[b:b + 1, 5 * D:6 * D].partition_broadcast(128), r=[b_modd], w=[b_gfb])
            K.op(DVE, lambda: v_.tensor_tensor(out=gfb[:, b, :], in0=gfb[:, b, :], in1=bcvc[:], op=ALU.mult), [b_gfb, b_bcvc], [b_gfb])
        x1c = [al.alloc("x1c", [128, D], F32) for _ in range(2)]
        zc = [al.alloc("zc", [128, D], F32) for _ in range(2)]
        yg = [al.alloc("yg", [128, D], BF16) for _ in range(12)]
        shc = [al.alloc("shc", [128, D], BF16) for _ in range(2)]
        b_shc = [Buf(), Buf()]
        jkc = al.alloc("jkc", [128, D], BF16)
        rc_ = [al.alloc("rc", [128, 4], F32) for _ in range(2)]
        b_x1c, b_zc, b_rc = [Buf(), Buf()], [Buf(), Buf()], [Buf(), Buf()]
        b_yg = [Buf() for _ in range(12)]
        b_jkc = Buf()
        b_out = Buf()
        z2 = [al.alloc("z2", [128, D], F32) for _ in range(2)]
        b_z2 = [Buf(), Buf()]
        z3 = [al.alloc("z3", [128, D], F32) for _ in range(2)]
        b_z3 = [Buf(), Buf()]
        gc = {"n": 0}

        dg = [al.alloc("dg", [128, 128], BF16) for _ in range(12)]
        b_dg = [Buf() for _ in range(12)]
        rot["l"] = list(range(8))
        cps = {}

        def c_A(T):
            p_ = T % 2
            r0 = T * 128
            sp_dma(x1c[p_][:], x1_d[r0:r0 + 128, :], w=[b_x1c[p_]])
            sp_dma(shc[p_][:], sh_d[r0:r0 + 128, :], w=[b_shc[p_]])
            ys_ = []
            for k_ in range(TOPK):
                y_ = yg[gc["n"] % 12]
                by = b_yg[gc["n"] % 12]
                d_ = dg[gc["n"] % 12]
                bd = b_dg[gc["n"] % 12]
                gc["n"] += 1
                K.dma(K.qpool, lambda: g_.indirect_dma_start(out=y_[:], out_offset=None, in_=ys_d[:, :],
                                                             in_offset=bass.IndirectOffsetOnAxis(ap=desti[:, T, k_:k_ + 1], axis=0),
                                                             bounds_check=reg_slot, oob_is_err=False),
                      [b_ys, b_desti_t[T]], [by])
                K.op(ACT, lambda: a_.activation(out=d_[:], in_=ident_f, func=AF.Identity, scale=wkn[:, T, k_:k_ + 1]),
                     [b_const, b_route], [bd])
                ys_.append((y_, by, d_, bd))
            banks = [nb(), nb()]
            cps[T] = banks
            for hf in range(2):
                i = banks[hf]
                cs_ = slice(hf * 512, (hf + 1) * 512)
                mm(ps[i][:, :], ident_b, shc[p_][:, cs_], True, False, [b_const, b_shc[p_]], [pb[i]], False)
                for k_ in range(TOPK):
                    y_, by, d_, bd = ys_[k_]
                    mm(ps[i][:, :], d_[:], y_[:, cs_], False, k_ == TOPK - 1, [bd, by], [pb[i]], k_ == TOPK - 1)

        def c_B(T):
            b = T // NT_S
            p_ = T % 2
            r0 = T * 128
            banks = cps.pop(T)
            for hf in range(2):
                K.op(ACT, lambda: a_.activation(out=jkc[:, hf * 512:(hf + 1) * 512], in_=ps[banks[hf]][:, :], func=AF.Square,
                                                accum_out=rc_[p_][:, hf:hf + 1]), [pb[banks[hf]]], [b_jkc, b_rc[p_]])
            K.op(DVE, lambda: v_.tensor_tensor(out=rc_[p_][:, 3:4], in0=rc_[p_][:, 0:1], in1=rc_[p_][:, 1:2], op=ALU.add), [b_rc[p_]], [b_rc[p_]])
            K.op(ACT, lambda: a_.activation(out=rc_[p_][:, 1:2], in_=rc_[p_][:, 3:4], func=AF.Sqrt, bias=EPS, scale=1.0 / D), [b_rc[p_]], [b_rc[p_]])
            K.op(DVE, lambda: v_.reciprocal(out=rc_[p_][:, 2:3], in_=rc_[p_][:, 1:2]), [b_rc[p_]], [b_rc[p_]])
            for hf in range(2):
                cs_ = slice(hf * 512, (hf + 1) * 512)
                K.op(DVE, lambda: v_.scalar_tensor_tensor(out=zc[p_][:, cs_], in0=ps[banks[hf]][:, :], scalar=rc_[p_][:, 2:3], in1=gfb[:, b, cs_],
                                                          op0=ALU.mult, op1=ALU.mult), [pb[banks[hf]], b_rc[p_], b_gfb], [b_zc[p_]])
            K.op(DVE, lambda: v_.tensor_tensor(out=zc[p_][:], in0=zc[p_][:], in1=x1c[p_][:], op=ALU.add), [b_zc[p_], b_x1c[p_]], [b_zc[p_]])
            sp_dma(out_d[r0:r0 + 128, :], zc[p_][:], r=[b_zc[p_]], sw=[b_out])

        c_A(0)
        for T in range(NT):
            if T + 1 < NT:
                c_A(T + 1)
            c_B(T)
        K.barrier()
    return nc


def _bf16():
    import ml_dtypes
    return ml_dtypes.bfloat16


def make_consts(NBLK):
    NCF = 128 + 128 + 64 + NBLK + 2
    cf = np.zeros((128, NCF), np.float32)
    cf[:, 0:128] = np.eye(128, dtype=np.float32)
    cf[127, 128:256] = 1.0
    cf[:, 256:320] = np.arange(64, dtype=np.float32)[None, :]
    cf[:, 320:320 + NBLK] = np.arange(NBLK, dtype=np.float32)[None, :]
    cf[:, 320 + NBLK] = np.arange(128, dtype=np.float32)
    cf[:, 321 + NBLK] = 1.0
    cb = np.zeros((128, 1792), np.float32)
    cb[:, 0:128] = np.eye(128)
    k = np.arange(128)[:, None]
    m = np.arange(128)[None, :]
    cb[:, 128:256] = (k < m)
    cb[:, 256:384] = (m >= k)
    cb[:, 384:512] = 1.0
    for h in range(NH):
        for g3 in range(3):
            cb[g3 * 32 + h, 512 + h * 128:512 + (h + 1) * 128] = 1.0
    cb[:, 1536:1664] = np.where(m < k, -30000.0, 0.0)
    cb[:, 1664:1792] = (m == (k + 64) % 128)
    return cf, cb.astype(_bf16())


def prep_shared(inp):
    f = np.float32
    sh = {}
    sh["w_ada"] = np.ascontiguousarray(inp["w_ada"][0], f)
    fm = np.zeros((128, 9, 8), f)

    def fmaj(v):
        return np.asarray(v, f).reshape(8, 128).T
    fm[:, 0, :] = fmaj(inp["g_pre_mix"][0])
    for j in range(4):
        fm[:, 1 + j, :] = fmaj(inp["w_conv"][0, j])
    fm[:, 5, :] = fmaj(inp["b_conv"][0])
    fm[:, 6, :] = fmaj(inp["b_rg"][0])
    fm[:, 7, :] = fmaj(inp["b_ig"][0])
    fm[:, 8, :] = fmaj(inp["rglru_lambda"][0])
    sh["fm"] = fm
    bcv = np.zeros((128, 3, D), f)
    bcv[:, 0, :] = np.asarray(inp["g_post_mix"][0], f)[None, :]
    bcv[:, 1, :] = np.asarray(inp["g_pre_ffn"][0], f)[None, :]
    bcv[:, 2, :] = np.asarray(inp["g_post_ffn"][0], f)[None, :]
    sh["bcv"] = bcv
    sh["rbias"] = np.ascontiguousarray(np.broadcast_to(np.asarray(inp["router_bias"][0], f)[None, :], (128, E)))
    sh["b_forget"] = np.asarray(inp["b_forget"][0], f).reshape(NH, 1)
    sh["w_in"] = np.ascontiguousarray(inp["w_in"][0], f)
    for nm, src in (("wrg_bd", inp["w_rg"][0]), ("wig_bd", inp["w_ig"][0])):
        bd = np.zeros((8, 128, 128), f)
        for c in range(8):
            bd[c, 0:64, 0:64] = src[2 * c]
            bd[c, 64:128, 64:128] = src[2 * c + 1]
        sh[nm] = bd
    sh["w_ba"] = np.ascontiguousarray(inp["w_branch_attn"][0], f)
    sh["w_br"] = np.ascontiguousarray(inp["w_branch_rnn"][0], f)
    sh["w_out"] = np.ascontiguousarray(inp["w_out"][0], f)
    sh["w_router"] = np.ascontiguousarray(inp["w_router"][0], f)
    sh["w_sg"] = np.ascontiguousarray(inp["w_sh_gate"][0], f)
    sh["w_su"] = np.ascontiguousarray(inp["w_sh_up"][0], f)
    sh["w_sd"] = np.ascontiguousarray(inp["w_sh_down"][0], f)
    wg = np.asarray(inp["w_exp_gate"][0], f).reshape(E, 8, 128, FF).transpose(0, 2, 1, 3)
    sh["wpg"] = np.ascontiguousarray(wg).reshape(E * 128, 2048)
    wu = np.asarray(inp["w_exp_up"][0], f).reshape(E, 8, 128, FF).transpose(0, 2, 1, 3)
    sh["wpu"] = np.ascontiguousarray(wu).reshape(E * 128, 2048)
    wd = np.asarray(inp["w_exp_down"][0], f).reshape(E, 2, 128, D).transpose(0, 2, 1, 3)
    sh["wpd"] = np.ascontiguousarray(wd).reshape(E * 128, 2048)
    return sh


def prep_core(inp, sh, core, NSEQ, S, NBLK):
    f = np.float32
    m = dict(sh)
    xs = np.asarray(inp["x"][core * NSEQ:(core + 1) * NSEQ], f).reshape(NSEQ * S, D)
    m["x"] = np.ascontiguousarray(xs)
    c = np.asarray(inp["c"][core * NSEQ:(core + 1) * NSEQ], f)
    m["csT"] = np.ascontiguousarray(c.T.reshape(8, 128, NSEQ).transpose(1, 0, 2))
    m["b_ada_rep"] = np.ascontiguousarray(np.broadcast_to(np.asarray(inp["b_ada"][0], f)[None, :], (NSEQ, 6 * D)))
    cf, cb = make_consts(NBLK)
    m["cf"] = cf
    m["cb"] = cb
    return m


def kernel(**inputs):
    B, S = inputs["x"].shape[0], inputs["x"].shape[1]
    NSEQ = B // NCORES
    NTOK = NSEQ * S
    NBLK = (NTOK * TOPK) // CB + E
    nc = build(NSEQ, S, skip_reload=True)
    sh = prep_shared(inputs)
    in_maps = [prep_core(inputs, sh, i, NSEQ, S, NBLK) for i in range(NCORES)]
    res = run_bass_kernel_spmd(nc, in_maps, core_ids=list(range(NCORES)))
    outs = [np.asarray(r["out"], np.float32).reshape(NSEQ, S, D) for r in res.results]
    return np.concatenate(outs, axis=0)
```

```python
import numpy as np
import concourse.bass as bass
import concourse.mybir as mybir
from concourse.bass_utils import run_bass_kernel_spmd
from contextlib import ExitStack

F32 = mybir.dt.float32
BF16 = mybir.dt.bfloat16
I32 = mybir.dt.int32
U32 = mybir.dt.uint32
AF = mybir.ActivationFunctionType
ALU = mybir.AluOpType
AX = mybir.AxisListType

D = 1024
NH = 8
E = 64
TOPK = 6
FF = 256
CB = 256
INC = 5640
OQ, OK_, OV, OF_, OX, OG, OGA, OGR = 0, 512, 1024, 1536, 1544, 2568, 3592, 4616
EPS = 1e-6
BIG = 1.0e4
NCORES = 8
ARENA_SHIFT = [0]
ARENA_MAX = [0]


class Buf:
    __slots__ = ("w", "r", "name")

    def __init__(self, name=""):
        self.w = {}
        self.r = {}
        self.name = name


class Eng:
    def __init__(self, name, e, sem, key):
        self.name = name
        self.e = e
        self.sem = sem
        self.key = key
        self.n = 0
        self.seen = {}
        self.pending = False


class DQ:
    def __init__(self, eng, sems):
        self.eng = eng
        self.sems = sems
        self.cnt = [0] * len(sems)
        self.next = 0


def _merge(d, s):
    for k, v in s.items():
        if d.get(k, 0) < v:
            d[k] = v


class KB:
    def __init__(self, nc, stack):
        self.nc = nc
        self.semtab = {}
        self.engs = []
        for nm, e in (("pe", nc.tensor), ("act", nc.scalar), ("dve", nc.vector),
                      ("pool", nc.gpsimd), ("sp", nc.sync)):
            sem = stack.enter_context(nc.semaphore("s_" + nm))
            eng = Eng(nm, e, sem, "c_" + nm)
            self.semtab[eng.key] = sem
            setattr(self, nm, eng)
            self.engs.append(eng)
        self.queues = []
        for nm, eng, n in (("qsp", self.sp, 8), ("qpool", self.pool, 6)):
            sems = []
            for i in range(n):
                key = "d_%s%d" % (nm, i)
                sem = stack.enter_context(nc.semaphore(key))
                self.semtab[key] = sem
                sems.append((sem, key))
            q = DQ(eng, sems)
            setattr(self, nm, q)
            self.queues.append(q)

    def _wait(self, E_, deps):
        for k, v in deps.items():
            if E_.seen.get(k, 0) < v:
                E_.e.wait_ge(self.semtab[k], v)
                E_.seen[k] = v

    def op(self, E_, fn, reads=(), writes=(), inc=True):
        deps = {}
        for b in reads:
            _merge(deps, b.w)
        for b in writes:
            _merge(deps, b.w)
            _merge(deps, b.r)
        if E_.name == "pe":
            deps.pop(E_.key, None)
        self._wait(E_, deps)
        ins = fn()
        ev = E_.n + 1
        for b in reads:
            if b.r.get(E_.key, 0) < ev:
                b.r[E_.key] = ev
        for b in writes:
            b.w = {E_.key: ev}
            b.r = {}
        if inc:
            E_.n = ev
            ins.then_inc(E_.sem, 1)
            E_.pending = False
        else:
            E_.pending = True
        return ins

    def dma(self, Q, fn, reads=(), writes=(), swrites=()):
        E_ = Q.eng
        deps = {}
        for b in reads:
            _merge(deps, b.w)
        for b in writes:
            _merge(deps, b.w)
            _merge(deps, b.r)
        for b in swrites:
            _merge(deps, b.r)
        slot = Q.next
        Q.next = (Q.next + 1) % len(Q.sems)
        sem, key = Q.sems[slot]
        if Q.cnt[slot] > 0:
            if deps.get(key, 0) < 16 * Q.cnt[slot]:
                deps[key] = 16 * Q.cnt[slot]
        self._wait(E_, deps)
        ins = fn()
        Q.cnt[slot] += 1
        v = 16 * Q.cnt[slot]
        ins.then_inc(sem, 16)
        for b in reads:
            if b.r.get(key, 0) < v:
                b.r[key] = v
        for b in writes:
            b.w = {key: v}
            b.r = {}
        for b in swrites:
            if b.w.get(key, 0) < v:
                b.w[key] = v
        return ins

    def barrier(self):
        tot = {}
        for E_ in self.engs:
            assert not E_.pending
            if E_.n > 0:
                tot[E_.key] = E_.n
        for Q in self.queues:
            for i, (sem, key) in enumerate(Q.sems):
                if Q.cnt[i] > 0:
                    tot[key] = 16 * Q.cnt[i]
        for E_ in self.engs:
            self._wait(E_, dict(tot))


class Arena:
    def __init__(self, nc, limit):
        self.nc = nc
        self.off = 0
        self.limit = limit
        self.n = 0

    def alloc(self, name, shape, dtype):
        sz = 1
        for s in shape[1:]:
            sz *= s
        sz *= {F32: 4, BF16: 2, I32: 4, U32: 4}[dtype]
        sz = (sz + 63) // 64 * 64
        off = self.off
        assert off + sz <= self.limit, ("SBUF arena overflow", name, off, sz)
        self.off += sz
        self.n += 1
        ARENA_MAX[0] = max(ARENA_MAX[0], self.off)
        return self.nc.alloc_sbuf_tensor_at("%s_%d" % (name, self.n), list(shape), dtype, offset=off)

    def mark(self):
        return self.off

    def release(self, m):
        self.off = m


def build(NSEQ, S, skip_reload=True):
    NT_S = S // 128
    NTOK = NSEQ * S
    NT = NTOK // 128
    QB = min(512, S)
    TQ = QB // 128
    NQ = S // QB
    NB5 = S // QB
    NBLK = (NTOK * TOPK) // CB + E
    NSLOT = NBLK * CB

    nc = bass.Bass("TRN2", target_bir_lowering=False)
    dt = nc.dram_tensor

    def ein(name, shape, dtype=F32):
        return dt(name, list(shape), dtype, kind="ExternalInput")

    x_d = ein("x", [NTOK, D])
    cs_d = ein("csT", [128, 8, NSEQ])
    wada_d = ein("w_ada", [D, 6 * D])
    bada_d = ein("b_ada_rep", [NSEQ, 6 * D])
    fm_d = ein("fm", [128, 9, 8])
    bcv_d = ein("bcv", [128, 3, D])
    rb_d = ein("rbias", [128, E])
    bf_d = ein("b_forget", [NH, 1])
    win_d = ein("w_in", [D, INC])
    wrg_d = ein("wrg_bd", [8, 128, 128])
    wig_d = ein("wig_bd", [8, 128, 128])
    wba_d = ein("w_ba", [512, D])
    wbr_d = ein("w_br", [D, D])
    wout_d = ein("w_out", [D, D])
    wr_d = ein("w_router", [D, E])
    wsg_d = ein("w_sg", [D, FF])
    wsu_d = ein("w_su", [D, FF])
    wsd_d = ein("w_sd", [FF, D])
    wpg_d = ein("wpg", [E * 128, 2048])
    wpu_d = ein("wpu", [E * 128, 2048])
    wpd_d = ein("wpd", [E * 128, 2048])
    NCF = 128 + 128 + 64 + NBLK + 2
    cf_d = ein("cf", [128, NCF])
    cb_d = ein("cb", [128, 1792], BF16)
    out_d = dt("out", [NTOK, D], F32, kind="ExternalOutput")
    modd = dt("modd", [NSEQ, 6 * D], F32)
    h2_d = dt("h2s", [NTOK, D], BF16)
    x1_d = dt("x1s", [NTOK, D], F32)
    sh_d = dt("shs", [NTOK, D], BF16)
    xs_d = dt("xss", [NSLOT, D], BF16)
    ys_d = dt("yss", [NSLOT, D], BF16)
    wpb_d = [dt("wpb%d" % m_, [E * 128, 2048], BF16) for m_ in range(3)]

    stack = ExitStack()
    with stack:
        K = KB(nc, stack)
        al = Arena(nc, int(nc._sbuf_addr_for_side("right")) - 64)
        al.off = (int(nc._sbuf_addr_for_side("left")) + 63) // 64 * 64 + ARENA_SHIFT[0]
        ps = [stack.enter_context(nc.psum_tensor("ps%d" % i, [128, 512], F32)) for i in range(8)]
        pb = [Buf("pb%d" % i) for i in range(8)]
        rot = {"l": list(range(8)), "i": 0}

        def nb():
            i = rot["l"][rot["i"] % len(rot["l"])]
            rot["i"] += 1
            return i

        PE, ACT, DVE, POOL = K.pe, K.act, K.dve, K.pool
        reg_slot = nc.gpsimd.alloc_register("bc_slot")
        nc.gpsimd.reg_mov(reg_slot, NSLOT - 1)
        reg_w = nc.gpsimd.alloc_register("bc_w")
        nc.gpsimd.reg_mov(reg_w, E * 128 - 1)
        v_, a_, g_, t_ = nc.vector, nc.scalar, nc.gpsimd, nc.tensor

        def mm(out, lhsT, rhs, start, stop, r, w, inc):
            return K.op(PE, lambda: t_.matmul(out, lhsT, rhs, start=start, stop=stop), r, w, inc)

        def tr(out, in_, ident, r, w, inc):
            return K.op(PE, lambda: t_.transpose(out, in_, ident), r, w, inc)

        def sp_dma(out, in_, r=(), w=(), sw=()):
            return K.dma(K.qsp, lambda: nc.sync.dma_start(out=out, in_=in_), r, w, sw)

        def pl_dma(out, in_, r=(), w=(), sw=()):
            return K.dma(K.qpool, lambda: nc.gpsimd.dma_start(out=out, in_=in_), r, w, sw)

        cf = al.alloc("cf", [128, NCF], F32)
        cbt = al.alloc("cb", [128, 1792], BF16)
        b_const = Buf("const")
        ident_f = cf[:, 0:128]
        sel127 = cf[:, 128:256]
        iota64 = cf[:, 256:320]
        iotab = cf[:, 320:320 + NBLK]
        pidx = cf[:, 320 + NBLK:321 + NBLK]
        ones_c = cf[:, 321 + NBLK:322 + NBLK]
        ident_b = cbt[:, 0:128]
        triu_b = cbt[:, 128:256]
        trim_b = cbt[:, 256:384]
        ones_b = cbt[:, 384:512]
        negm_b = cbt[:, 1536:1664]
        swap_b = cbt[:, 1664:1792]
        fm = al.alloc("fm", [128, 9, 8], F32)
        sm = al.alloc("sm", [128, 6, 8], F32)
        rbias = al.alloc("rbias", [128, E], F32)
        nbf = al.alloc("nbf", [128, 2], F32)
        b_nbf = Buf()
        gsmT = al.alloc("gsmT", [128, 8, NSEQ], F32)
        shmT = al.alloc("shmT", [128, 8, NSEQ], F32)
        run = al.alloc("run", [128, E], F32)
        rankm = al.alloc("rankm", [128, NT, E], F32)
        eidx = al.alloc("eidx", [128, NT, 8], F32)
        wk = al.alloc("wk", [128, NT, 8], F32)
        wkn = al.alloc("wkn", [128, NT, 8], F32)
        desti = al.alloc("desti", [128, NT, 8], I32)
        widx = al.alloc("widx", [128, NBLK], I32)
        b_fm, b_sm, b_gs, b_run, b_route = Buf(), Buf(), Buf(), Buf(), Buf()
        b_widx = Buf()
        zt = al.alloc("zt", [128, 2, D], BF16)
        b_zt, b_xs0 = Buf(), Buf()
        K.op(POOL, lambda: g_.memset(zt[:], 0.0), [], [b_zt])
        zf = {"n": 0}
        ZF_PER = -(-NBLK // NT)

        b_wcast = Buf()
        pcast = {"n": 0}
        PC_PER = -(-(3 * E) // (NSEQ * 4 * NQ))

        def precast(cnt):
            for _ in range(cnt):
                if pcast["n"] < 3 * E:
                    e_, m_ = pcast["n"] // 3, pcast["n"] % 3
                    src = (wpg_d, wpu_d, wpd_d)[m_]
                    pl_dma(wpb_d[m_][e_ * 128:(e_ + 1) * 128, :], src[e_ * 128:(e_ + 1) * 128, :], sw=[b_wcast])
                    pcast["n"] += 1

        def zero_fill(cnt):
            for _ in range(cnt):
                if zf["n"] < NBLK:
                    r0_ = zf["n"] * CB
                    sp_dma(xs_d[r0_:r0_ + CB, :].rearrange("(s p) d -> p s d", p=128), zt[:], r=[b_zt], sw=[b_xs0])
                    zf["n"] += 1

        sp_dma(cf[:], cf_d.ap(), w=[b_const])
        sp_dma(cbt[:], cb_d.ap(), w=[b_const])
        sp_dma(fm[:], fm_d.ap(), w=[b_fm])
        sp_dma(rbias[:], rb_d.ap(), w=[b_const])
        K.op(DVE, lambda: v_.memset(nbf[:], 0.0), [], [b_nbf])
        for g3 in range(3):
            sp_dma(nbf[g3 * 32:g3 * 32 + NH, 0:1], bf_d.ap(), w=[b_nbf])
        K.op(DVE, lambda: v_.memset(run[:], 0.0), [], [b_run])

        m0 = al.mark()
        cs = al.alloc("cs", [128, 8, NSEQ], F32)
        th0 = al.alloc("th0", [128, 8, NSEQ], F32)
        siluT = al.alloc("siluT", [128, 8, NSEQ], BF16)
        modt = al.alloc("modt", [NSEQ, 6 * D], F32)
        bada = al.alloc("bada", [NSEQ, 6 * D], F32)
        wada = [al.alloc("wada", [128, 8, 512], BF16) for _ in range(2)]
        b_cs, b_th0, b_silu, b_modt, b_bada = Buf(), Buf(), Buf(), Buf(), Buf()
        b_wada = [Buf(), Buf()]

        K.op(DVE, lambda: v_.tensor_scalar(out=nbf[0:72, 1:2], in0=nbf[0:72, 0:1], scalar1=-1.0, scalar2=None,
                                           op0=ALU.mult), [b_nbf], [b_nbf])
        K.op(ACT, lambda: a_.activation(out=sm[:, 4, :], in_=fm[:, 8, :], func=AF.Exp, scale=-1.0), [b_fm], [b_sm])
        K.op(ACT, lambda: a_.activation(out=sm[:, 5, :], in_=sm[:, 4, :], func=AF.Ln, bias=1.0, scale=1.0), [b_sm], [b_sm])
        K.op(DVE, lambda: v_.tensor_scalar(out=sm[:, 0, :], in0=sm[:, 5, :], scalar1=-8.0, scalar2=None, op0=ALU.mult), [b_sm], [b_sm])
        K.op(DVE, lambda: v_.tensor_scalar(out=sm[:, 1, :], in0=sm[:, 5, :], scalar1=-4.0, scalar2=None, op0=ALU.mult), [b_sm], [b_sm])
        K.op(DVE, lambda: v_.tensor_scalar(out=sm[:, 2, :], in0=fm[:, 6, :], scalar1=0.5, scalar2=None, op0=ALU.mult), [b_fm, b_sm], [b_sm])
        K.op(DVE, lambda: v_.tensor_scalar(out=sm[:, 3, :], in0=fm[:, 7, :], scalar1=0.5, scalar2=None, op0=ALU.mult), [b_fm, b_sm], [b_sm])
        cneg = sm[:, 0, :]
        hcneg = sm[:, 1, :]
        hbrg = sm[:, 2, :]
        hbig = sm[:, 3, :]

        sp_dma(cs[:], cs_d.ap(), w=[b_cs])
        sp_dma(bada[:], bada_d.ap(), w=[b_bada])
        K.op(ACT, lambda: a_.activation(out=th0[:], in_=cs[:], func=AF.Tanh, scale=0.5), [b_cs], [b_th0])
        K.op(DVE, lambda: v_.scalar_tensor_tensor(out=th0[:], in0=th0[:], scalar=1.0, in1=cs[:], op0=ALU.add, op1=ALU.mult),
             [b_cs, b_th0], [b_th0])
        K.op(DVE, lambda: v_.tensor_scalar(out=siluT[:], in0=th0[:], scalar1=0.5, scalar2=None, op0=ALU.mult), [b_th0], [b_silu])
        for g in range(12):
            wb = wada[g % 2]
            bw = b_wada[g % 2]
            pl_dma(wb[:], wada_d[:, g * 512:(g + 1) * 512].rearrange("(kc p) n -> p kc n", p=128), w=[bw])
            i = nb()
            for kc in range(8):
                mm(ps[i][0:NSEQ, :], siluT[:, kc, :], wb[:, kc, :], kc == 0, kc == 7, [b_silu, bw], [pb[i]], kc == 7)
            K.op(DVE, lambda: v_.tensor_tensor(out=modt[:, g * 512:(g + 1) * 512], in0=ps[i][0:NSEQ, :],
                                               in1=bada[:, g * 512:(g + 1) * 512], op=ALU.add), [pb[i], b_bada], [b_modt])
        b_modd = Buf()
        sp_dma(modd.ap(), modt[:], r=[b_modt], w=[b_modd])
        i = nb()
        pT0 = ps[i][:, 0:16 * NSEQ].rearrange("p (a b) -> p a b", b=NSEQ)
        for kc in range(16):
            col = (D + kc * 128) if kc < 8 else ((kc - 8) * 128)
            tr(pT0[:, kc, :], modt[0:NSEQ, col:col + 128], ident_f[0:NSEQ, 0:NSEQ], [b_modt, b_const], [pb[i]], kc == 15)
        K.op(DVE, lambda: v_.scalar_tensor_tensor(out=gsmT[:], in0=pT0[:, 0:8, :], scalar=1.0,
                                                  in1=fm[:, 0, :].unsqueeze(2).to_broadcast([128, 8, NSEQ]),
                                                  op0=ALU.add, op1=ALU.mult), [pb[i], b_fm], [b_gs])
        K.op(DVE, lambda: v_.tensor_copy(out=shmT[:], in_=pT0[:, 8:16, :]), [pb[i]], [b_gs])
        K.barrier()
        al.release(m0)

        off_hT = al.off
        hT = al.alloc("hT", [128, 8, S], BF16)
        yaT = al.alloc("yaT", [128, 4, S], BF16)
        off_yrT = al.off
        yrT = al.alloc("yrT", [128, 8, S], BF16)
        LIMIT = al.limit
        b_hT = [Buf() for _ in range(NT_S)]
        b_hT2 = [Buf() for _ in range(NT_S)]
        b_yaT, b_yrT = Buf(), Buf()
        mU = al.mark()

        for b in range(NSEQ):
            tok0 = b * S
            al.release(mU)
            x_sb = [al.alloc("x_sb", [128, D], F32) for _ in range(4)]
            xn = [al.alloc("xn", [128, D], BF16) for _ in range(2)]
            jk = al.alloc("jk", [128, D], BF16)
            st = [al.alloc("st", [128, 4], F32) for _ in range(3)]
            b_x, b_xn, b_st, b_jk = [Buf() for _ in range(4)], [Buf(), Buf()], [Buf() for _ in range(3)], Buf()
            rot["l"] = list(range(8))
            def m1_L(t):
                sp_dma(x_sb[t % 4][:], x_d[tok0 + t * 128: tok0 + (t + 1) * 128, :], w=[b_x[t % 4]])

            def m1_A1(t):
                xb, stb, bx, bst = x_sb[t % 4], st[t % 3], b_x[t % 4], b_st[t % 3]
                K.op(ACT, lambda: a_.activation(out=jk[:], in_=xb[:], func=AF.Square, accum_out=stb[:, 0:1]), [bx], [b_jk, bst])
                K.op(ACT, lambda: a_.activation(out=stb[:, 1:2], in_=stb[:, 0:1], func=AF.Sqrt, bias=EPS, scale=1.0 / D), [bst], [bst])
                K.op(DVE, lambda: v_.reciprocal(out=stb[:, 2:3], in_=stb[:, 1:2]), [bst], [bst])

            def m1_A2(t):
                xb, xnb, stb = x_sb[t % 4], xn[t % 2], st[t % 3]
                bx, bxn, bst = b_x[t % 4], b_xn[t % 2], b_st[t % 3]
                K.op(ACT, lambda: a_.activation(out=xnb[:], in_=xb[:], func=AF.Identity, scale=stb[:, 2:3]), [bx, bst], [bxn])

            def m1_B(t):
                xnb, bxn = xn[t % 2], b_xn[t % 2]
                ia, ib = nb(), nb()
                pTa = ps[ia][:, :].bitcast(BF16)
                pTb = ps[ib][:, :].bitcast(BF16)
                for kc in range(0, 8, 2):
                    tr(pTa[:, (kc // 2) * 128:(kc // 2 + 1) * 128], xnb[:, kc * 128:(kc + 1) * 128], ident_b, [bxn, b_const], [pb[ia]], kc == 6)
                for kc in range(1, 8, 2):
                    tr(pTb[:, (kc // 2) * 128:(kc // 2 + 1) * 128], xnb[:, kc * 128:(kc + 1) * 128], ident_b, [bxn, b_const], [pb[ib]], kc == 7)
                for kc in range(8):
                    o_ap = hT[:, kc, t * 128:(t + 1) * 128]
                    if kc % 2 == 0:
                        i_ap = pTa[:, (kc // 2) * 128:(kc // 2 + 1) * 128]
                        K.op(ACT, lambda: a_.activation(out=o_ap, in_=i_ap, func=AF.Identity, bias=shmT[:, kc, b:b + 1],
                                                        scale=gsmT[:, kc, b:b + 1]), [pb[ia], b_gs], [b_hT[t]])
                    else:
                        i_ap = pTb[:, (kc // 2) * 128:(kc // 2 + 1) * 128]
                        K.op(DVE, lambda: v_.tensor_scalar(out=o_ap, in0=i_ap, scalar1=gsmT[:, kc, b:b + 1],
                                                           scalar2=shmT[:, kc, b:b + 1], op0=ALU.mult, op1=ALU.add),
                             [pb[ib], b_gs], [b_hT2[t]])
            for t0_ in range(min(3, NT_S)):
                m1_L(t0_)
            m1_A1(0)
            if NT_S > 1:
                m1_A1(1)
            m1_A2(0)
            for t in range(NT_S):
                if t + 3 < NT_S:
                    m1_L(t + 3)
                if t + 2 < NT_S:
                    m1_A1(t + 2)
                if t + 1 < NT_S:
                    m1_A2(t + 1)
                m1_B(t)
            K.barrier()

            al.release(mU)
            qT = al.alloc("qT", [128, 4, 2, S], BF16)
            kT = al.alloc("kT", [128, 4, S], BF16)
            sv_ = al.off
            al.off = off_yrT
            vS = al.alloc("vS", [128, NT_S, 4, 192], BF16)
            ef = al.alloc("ef", [72, S], F32)
            assert al.off <= mU
            al.off = sv_
            off_wq = al.off
            wq = al.alloc("wq", [128, 8, 512], BF16)
            wkk = al.alloc("wk", [128, 8, 512], BF16)
            wv = al.alloc("wv", [128, 8, 512], BF16)
            wf = al.alloc("wf", [128, 8, 72], BF16)
            caug = al.alloc("caug", [128, S], BF16)
            tmpb = al.alloc("tmpb", [72, S], BF16)
            b_caug, b_tmpb = Buf(), Buf()
            Lf = al.alloc("Lf", [72, S], F32)
            cumLT = al.alloc("cumLT", [128, NT_S, NH], F32)
            b_Rb, b_Rs = Buf(), Buf()
            b_q, b_k, b_v, b_wq, b_wk, b_wv, b_wf = Buf(), Buf(), Buf(), Buf(), Buf(), Buf(), Buf()
            b_ef, b_L, b_cumLT = Buf(), Buf(), Buf()
            b_PT = [Buf() for _ in range(6)]

            def wview(c0, n):
                return win_d[:, c0:c0 + n].rearrange("(kc p) n -> p kc n", p=128)
            K.op(POOL, lambda: g_.memset(vS[:, :, :, 64:128], 1.0), [], [b_v])
            K.op(POOL, lambda: g_.memset(qT[:], 0.0), [], [b_q])
            pl_dma(wq[:], wview(OQ, 512), w=[b_wq])
            pl_dma(wkk[:], wview(OK_, 512), w=[b_wk])
            pl_dma(wv[:], wview(OV, 512), w=[b_wv])
            K.op(POOL, lambda: g_.memset(wf[:], 0.0), [], [b_wf])
            K.op(POOL, lambda: g_.memset(caug[:], 0.0), [], [b_caug])
            for g3 in range(3):
                pl_dma(wf[:, :, g3 * 32:g3 * 32 + NH], wview(OF_, 8), w=[b_wf])
            rot["l"] = list(range(8))
            cnt = 0
            for n in range(NQ):
                i = nb()
                for kc in range(8):
                    mm(ps[i][0:72, 0:QB], wf[:, kc, :], hT[:, kc, n * QB:(n + 1) * QB], kc == 0, kc == 7,
                       [b_wf] + (b_hT[n * TQ:(n + 1) * TQ] + b_hT2[n * TQ:(n + 1) * TQ]), [pb[i]], kc == 7)
                K.op(ACT, lambda: a_.activation(out=ef[:, n * QB:(n + 1) * QB], in_=ps[i][0:72, 0:QB], func=AF.Exp,
                                                bias=nbf[0:72, 1:2], scale=-1.0), [pb[i], b_nbf], [b_ef])
            K.op(ACT, lambda: a_.activation(out=Lf[:], in_=ef[:], func=AF.Ln, bias=1.0, scale=1.0), [b_ef], [b_L])
            K.op(DVE, lambda: v_.tensor_tensor_scan(out=ef[:], data0=ones_c[0:72, 0:1].to_broadcast([72, S]), data1=Lf[:],
                                                    initial=0.0, op0=ALU.mult, op1=ALU.add), [b_L, b_const, b_ef], [b_ef])
            K.op(DVE, lambda: v_.tensor_scalar(out=Lf[:], in0=ef[:], scalar1=-8.0, scalar2=None, op0=ALU.mult), [b_ef, b_L], [b_L])
            K.op(DVE, lambda: v_.tensor_copy(out=tmpb[:], in_=Lf[:]), [b_L], [b_tmpb])
            K.op(DVE, lambda: v_.tensor_copy(out=caug[0:NH, :], in_=tmpb[0:NH, :]), [b_tmpb], [b_caug])
            K.op(DVE, lambda: v_.tensor_tensor(out=Lf[:], in0=Lf[:], in1=tmpb[:], op=ALU.subtract), [b_L, b_tmpb], [b_L])
            K.op(DVE, lambda: v_.tensor_copy(out=tmpb[:], in_=Lf[:]), [b_L, b_caug], [b_tmpb])
            K.op(DVE, lambda: v_.tensor_copy(out=caug[32:32 + NH, :], in_=tmpb[32:32 + NH, :]), [b_tmpb], [b_caug])
            K.op(DVE, lambda: v_.tensor_tensor(out=Lf[:], in0=Lf[:], in1=tmpb[:], op=ALU.subtract), [b_L, b_tmpb], [b_L])
            K.op(DVE, lambda: v_.tensor_copy(out=caug[64:64 + NH, :], in_=Lf[64:64 + NH, :]), [b_L], [b_caug])
            for (wt, bw, dst, bd, isq) in ((wq, b_wq, qT, b_q, True), (wkk, b_wk, kT, b_k, False)):
                for j in range(4):
                    for n in range(NQ):
                        i = nb()
                        for kc in range(8):
                            mm(ps[i][:, 0:QB], wt[:, kc, j * 128:(j + 1) * 128], hT[:, kc, n * QB:(n + 1) * QB], kc == 0, kc == 7,
                               [bw] + (b_hT[n * TQ:(n + 1) * TQ] + b_hT2[n * TQ:(n + 1) * TQ]), [pb[i]], kc == 7)
                        cols = slice(n * QB, (n + 1) * QB)
                        if isq:
                            K.op(ACT, lambda: a_.copy(out=qT[0:64, j, 0, cols], in_=ps[i][0:64, 0:QB]), [pb[i]], [bd])
                            K.op(DVE, lambda: v_.tensor_copy(out=qT[64:128, j, 1, cols], in_=ps[i][64:128, 0:QB]), [pb[i]], [bd])
                        elif cnt % 2 == 0:
                            K.op(ACT, lambda: a_.copy(out=kT[:, j, cols], in_=ps[i][:, 0:QB]), [pb[i]], [bd])
                        else:
                            K.op(DVE, lambda: v_.tensor_copy(out=kT[:, j, cols], in_=ps[i][:, 0:QB]), [pb[i]], [bd])
                        cnt += 1
            for t in range(NT_S):
                i = nb()
                for kc in range(8):
                    mm(ps[i][:, :], hT[:, kc, t * 128:(t + 1) * 128], wv[:, kc, :], kc == 0, kc == 7, [b_wv, b_hT[t], b_hT2[t]], [pb[i]], kc == 7)
                o_v = vS[:, t, :, :].rearrange("p j (c w) -> p j c w", w=64)[:, :, 0::2, :]
                i_v = ps[i][:, :].rearrange("p (j c w) -> p j c w", c=2, w=64)
                if t % 2 == 0:
                    K.op(ACT, lambda: a_.copy(out=o_v, in_=i_v), [pb[i]], [b_v])
                else:
                    K.op(DVE, lambda: v_.tensor_copy(out=o_v, in_=i_v), [pb[i]], [b_v])
            i = nb()
            pT1 = ps[i][:, 0:NT_S * NH].rearrange("p (a b) -> p a b", b=NH)
            for t in range(NT_S):
                tr(pT1[:, t, :], ef[0:NH, t * 128:(t + 1) * 128], ident_f[0:NH, 0:NH], [b_ef, b_const], [pb[i]], t == NT_S - 1)
            K.op(DVE, lambda: v_.tensor_copy(out=cumLT[:], in_=pT1), [pb[i]], [b_cumLT])

            K.barrier()
            sv3 = al.off
            al.off = off_wq
            PT = [al.alloc("PT", [128, QB], BF16) for _ in range(6)]
            Rb = al.alloc("Rb", [128, QB], BF16)
            Rs = al.alloc("Rs", [128, QB], F32)
            al.off = sv3
            rot["l"] = [4, 5, 6, 7]
            pcount = 0
            LAG = 3
            grp = {"n": 0}
            pend_backs = []

            def att_front(j, q, t, half, nt, gpar):
                nonlocal pcount
                d = t - q * TQ
                q0 = max(d, 0) * 128
                h = 2 * j + half
                rows = slice(half * 64, half * 64 + 64)
                i = nb()
                mm(ps[i][:, q0:QB], kT[:, j, t * 128:(t + 1) * 128], qT[:, j, half, q * QB + q0:(q + 1) * QB],
                   True, False, [b_q, b_k], [pb[i]], False)
                mm(ps[i][:, q0:QB], cbt[:, 512 + h * 128:512 + (h + 1) * 128], caug[:, q * QB + q0:(q + 1) * QB],
                   False, d < 0, [b_const, b_caug], [pb[i]], d < 0)
                if d >= 0:
                    mm(ps[i][:, q0:q0 + 128], ident_b, negm_b, False, True, [b_const], [pb[i]], True)
                pt = PT[pcount % 6]
                bpt = b_PT[pcount % 6]
                pcount += 1
                K.op(ACT, lambda: a_.activation(out=pt[:, q0:QB], in_=ps[i][:, q0:QB], func=AF.Exp,
                                                bias=cumLT[:, t, h:h + 1], scale=0.125), [pb[i], b_cumLT], [bpt])

                def back():
                    yi = half + 2 * gpar
                    ya_, yb_ = 2 * gpar, 2 * gpar + 1
                    lo = 0 if half == 0 else 64
                    mm(ps[yi][:, q0:QB], vS[:, t, j, lo:lo + 128], pt[:, q0:QB], t == 0, t == nt - 1,
                       [b_v, bpt], [pb[yi]], True)
                    if t == nt - 1 and half == 1:
                        K.op(DVE, lambda: v_.reciprocal(out=Rs[64:128, :], in_=ps[ya_][64:128, 0:QB]), [pb[ya_], b_Rs], [b_Rs])
                        K.op(DVE, lambda: v_.reciprocal(out=Rs[0:64, :], in_=ps[yb_][0:64, 0:QB]), [pb[yb_], b_Rs], [b_Rs])
                        K.op(DVE, lambda: v_.tensor_copy(out=Rb[:], in_=Rs[:]), [b_Rs, b_Rb], [b_Rb])
                        isw = nb()
                        mm(ps[isw][:, 0:QB], swap_b, Rb[:, :], True, True, [b_const, b_Rb], [pb[isw]], True)
                        K.op(ACT, lambda: a_.copy(out=Rs[:], in_=ps[isw][:, 0:QB]), [pb[isw]], [b_Rs])
                        K.op(DVE, lambda: v_.tensor_tensor(out=yaT[0:64, j, q * QB:(q + 1) * QB], in0=ps[ya_][0:64, 0:QB],
                                                           in1=Rs[0:64, :], op=ALU.mult), [pb[ya_], b_Rs], [b_yaT])
                        K.op(DVE, lambda: v_.tensor_tensor(out=yaT[64:128, j, q * QB:(q + 1) * QB], in0=ps[yb_][64:128, 0:QB],
                                                           in1=Rs[64:128, :], op=ALU.mult), [pb[yb_], b_Rs], [b_yaT])
                return back

            for j in range(4):
                for q in range(NQ):
                    nt = q * TQ + TQ
                    gpar = grp["n"] % 2
                    grp["n"] += 1
                    precast(PC_PER)
                    for t in range(nt):
                        for half in range(2):
                            pend_backs.append(att_front(j, q, t, half, nt, gpar))
                            if len(pend_backs) > LAG:
                                pend_backs.pop(0)()
            while pend_backs:
                pend_backs.pop(0)()
            K.barrier()

            al.release(mU)
            xp = [al.alloc("xp", [128, S + 4], F32) for _ in range(2)]
            uu = [al.alloc("uu", [128, S], F32) for _ in range(2)]
            ub = [al.alloc("ub", [128, S], BF16) for _ in range(2)]
            gg = [al.alloc("gg", [128, S], BF16) for _ in range(2)]
            thr = al.alloc("thr", [128, S], F32)
            thi = al.alloc("thi", [128, S], F32)
            e2 = al.alloc("e2", [128, S], F32)
            wx = [al.alloc("wx", [128, 8, 128], BF16) for _ in range(2)]
            wg = [al.alloc("wg", [128, 8, 128], BF16) for _ in range(2)]
            wrg = [al.alloc("wrg", [128, 128], BF16) for _ in range(2)]
            wig = [al.alloc("wig", [128, 128], BF16) for _ in range(2)]
            gt = [[al.alloc("gt", [128, QB], F32) for _ in range(2)] for _ in range(2)]
            b_xp, b_uu, b_ub, b_gg = [Buf(), Buf()], [Buf(), Buf()], [Buf(), Buf()], [Buf(), Buf()]
            b_thr, b_thi, b_e2 = Buf(), Buf(), Buf()
            b_w4 = [[Buf() for _ in range(4)] for _ in range(2)]
            b_gt = [[Buf() for _ in range(2)] for _ in range(2)]
            rot["l"] = list(range(8))
            for s2_ in range(2):
                K.op(DVE, lambda: v_.memset(xp[s2_][:, 0:4], 0.0), [], [b_xp[s2_]])

            def load_w4(c):
                s_ = c % 2
                pl_dma(wx[s_][:], wview(OX + c * 128, 128), w=[b_w4[s_][0]])
                pl_dma(wg[s_][:], wview(OG + c * 128, 128), w=[b_w4[s_][1]])
                pl_dma(wrg[s_][:], wrg_d[c, :, :], w=[b_w4[s_][2]])
                pl_dma(wig[s_][:], wig_d[c, :, :], w=[b_w4[s_][3]])

            gcnt4 = {"n": 0}

            def m4_A(c):
                zero_fill(-(-NBLK // (NSEQ * 8)))
                s_ = c % 2
                bwx, bwg_, bwrg, bwig = b_w4[s_]
                xp_, uu_, ub_, gg_ = xp[s_], uu[s_], ub[s_], gg[s_]
                bxp, buu, bub, bgg = b_xp[s_], b_uu[s_], b_ub[s_], b_gg[s_]
                for n in range(NQ):
                    i = nb()
                    for kc in range(8):
                        mm(ps[i][:, 0:QB], wx[s_][:, kc, :], hT[:, kc, n * QB:(n + 1) * QB], kc == 0, kc == 7,
                           [bwx] + (b_hT[n * TQ:(n + 1) * TQ] + b_hT2[n * TQ:(n + 1) * TQ]), [pb[i]], kc == 7)
                    K.op(ACT, lambda: a_.copy(out=xp_[:, 3 + n * QB:3 + (n + 1) * QB], in_=ps[i][:, 0:QB]), [pb[i]], [bxp])
                K.op(DVE, lambda: v_.tensor_scalar(out=uu_[:], in0=xp_[:, 3:3 + S], scalar1=fm[:, 4, c:c + 1], scalar2=fm[:, 5, c:c + 1],
                                                   op0=ALU.mult, op1=ALU.add), [bxp, b_fm], [buu])
                for jj in range(3):
                    K.op(DVE, lambda: v_.scalar_tensor_tensor(out=uu_[:], in0=xp_[:, jj:jj + S], scalar=fm[:, 1 + jj, c:c + 1], in1=uu_[:],
                                                              op0=ALU.mult, op1=ALU.add), [bxp, b_fm, buu], [buu])
                K.op(POOL, lambda: g_.tensor_copy(out=ub_[:], in_=uu_[:]), [buu], [bub])
                for n in range(NQ):
                    i = nb()
                    for kc in range(8):
                        mm(ps[i][:, 0:QB], wg[s_][:, kc, :], hT[:, kc, n * QB:(n + 1) * QB], kc == 0, kc == 7,
                           [bwg_] + (b_hT[n * TQ:(n + 1) * TQ] + b_hT2[n * TQ:(n + 1) * TQ]), [pb[i]], kc == 7)
                    g0, g1 = gt[gcnt4["n"] % 2]
                    bg0, bg1 = b_gt[gcnt4["n"] % 2]
                    gcnt4["n"] += 1
                    pg = ps[i][:, 0:QB]
                    K.op(ACT, lambda: a_.activation(out=g0[:], in_=pg, func=AF.Square), [pb[i]], [bg0])
                    K.op(DVE, lambda: v_.tensor_scalar(out=g0[:], in0=g0[:], scalar1=0.044715, scalar2=1.0, op0=ALU.mult, op1=ALU.add),
                         [bg0], [bg0])
                    K.op(DVE, lambda: v_.tensor_tensor(out=g0[:], in0=g0[:], in1=pg, op=ALU.mult), [bg0, pb[i]], [bg0])
                    K.op(ACT, lambda: a_.activation(out=g1[:], in_=g0[:], func=AF.Tanh, scale=0.7978845608028654), [bg0], [bg1])
                    K.op(DVE, lambda: v_.scalar_tensor_tensor(out=gg_[:, n * QB:(n + 1) * QB], in0=g1[:], scalar=1.0, in1=pg,
                                                              op0=ALU.add, op1=ALU.mult), [bg1, pb[i]], [bgg])

            def m4_B(c):
                s_ = c % 2
                bwx, bwg_, bwrg, bwig = b_w4[s_]
                uu_, ub_, gg_ = uu[s_], ub[s_], gg[s_]
                buu, bub, bgg = b_uu[s_], b_ub[s_], b_gg[s_]
                for n in range(NQ):
                    i = nb()
                    mm(ps[i][:, 0:QB], wrg[s_][:, :], ub_[:, n * QB:(n + 1) * QB], True, True, [bwrg, bub], [pb[i]], True)
                    K.op(ACT, lambda: a_.activation(out=thr[:, n * QB:(n + 1) * QB], in_=ps[i][:, 0:QB], func=AF.Tanh,
                                                    bias=hbrg[:, c:c + 1], scale=0.5), [pb[i], b_sm], [b_thr])
                    i2 = nb()
                    mm(ps[i2][:, 0:QB], wig[s_][:, :], ub_[:, n * QB:(n + 1) * QB], True, True, [bwig, bub], [pb[i2]], True)
                    K.op(ACT, lambda: a_.activation(out=thi[:, n * QB:(n + 1) * QB], in_=ps[i2][:, 0:QB], func=AF.Tanh,
                                                    bias=hbig[:, c:c + 1], scale=0.5), [pb[i2], b_sm], [b_thi])
                K.op(ACT, lambda: a_.activation(out=e2[:], in_=thr[:], func=AF.Exp, bias=cneg[:, c:c + 1], scale=cneg[:, c:c + 1]),
                     [b_thr, b_sm], [b_e2])
                K.op(ACT, lambda: a_.activation(out=thr[:], in_=thr[:], func=AF.Exp, bias=hcneg[:, c:c + 1], scale=hcneg[:, c:c + 1]),
                     [b_thr, b_sm], [b_thr])
                K.op(DVE, lambda: v_.tensor_scalar(out=e2[:], in0=e2[:], scalar1=1.0 - 1.0e-7, scalar2=None, op0=ALU.min), [b_e2], [b_e2])
                K.op(ACT, lambda: a_.activation(out=e2[:], in_=e2[:], func=AF.Sqrt, bias=1.0, scale=-1.0), [b_e2], [b_e2])
                K.op(DVE, lambda: v_.scalar_tensor_tensor(out=thi[:], in0=thi[:], scalar=1.0, in1=e2[:], op0=ALU.add, op1=ALU.mult),
                     [b_thi, b_e2], [b_thi])
                K.op(DVE, lambda: v_.scalar_tensor_tensor(out=thi[:], in0=thi[:], scalar=0.5, in1=uu_[:], op0=ALU.mult, op1=ALU.mult),
                     [b_thi, buu], [b_thi])
                K.op(DVE, lambda: v_.tensor_tensor_scan(out=e2[:], data0=thr[:], data1=thi[:], initial=0.0, op0=ALU.mult, op1=ALU.add),
                     [b_thr, b_thi, b_e2], [b_e2])
                K.op(DVE, lambda: v_.scalar_tensor_tensor(out=yrT[:, c, :], in0=gg_[:], scalar=0.5, in1=e2[:], op0=ALU.mult, op1=ALU.mult),
                     [bgg, b_e2], [b_yrT])

            load_w4(0)
            load_w4(1)
            m4_A(0)
            for c in range(8):
                if c + 1 < 8:
                    m4_A(c + 1)
                m4_B(c)
                if c + 2 < 8:
                    load_w4(c + 2)
            K.barrier()

            al.release(mU)
            mgT = al.alloc("mgT", [128, 8, S], BF16)
            b_mg = [Buf() for _ in range(NT_S)]
            m5 = al.mark()
            w5 = [al.alloc("w5", [128, 28, 128], BF16) for _ in range(2)]
            b_w5 = [[Buf() for _ in range(4)] for _ in range(2)]
            sa = [[al.alloc("sa", [128, QB], F32) for _ in range(4)] for _ in range(2)]
            b_sa = [[Buf() for _ in range(4)] for _ in range(2)]
            rot["l"] = list(range(8))

            def load_w5(m):
                s_ = m % 2
                cs_ = slice(m * 128, (m + 1) * 128)
                pl_dma(w5[s_][:, 0:4, :], wba_d[:, cs_].rearrange("(kc p) n -> p kc n", p=128), w=[b_w5[s_][0]])
                pl_dma(w5[s_][:, 4:12, :], wbr_d[:, cs_].rearrange("(kc p) n -> p kc n", p=128), w=[b_w5[s_][1]])
                pl_dma(w5[s_][:, 12:20, :], wview(OGA + m * 128, 128), w=[b_w5[s_][2]])
                pl_dma(w5[s_][:, 20:28, :], wview(OGR + m * 128, 128), w=[b_w5[s_][3]])
            load_w5(0)
            scount = 0
            for m in range(8):
                s_ = m % 2
                bwa, bwr_, bwga, bwgr = b_w5[s_]
                if m + 1 < 8:
                    load_w5(m + 1)
                for n in range(NQ):
                    cols = slice(n * QB, (n + 1) * QB)
                    hb = (b_hT[n * TQ:(n + 1) * TQ] + b_hT2[n * TQ:(n + 1) * TQ])
                    iA, iR, iGA, iGR = nb(), nb(), nb(), nb()
                    for kc in range(4):
                        mm(ps[iA][:, 0:QB], w5[s_][:, kc, :], yaT[:, kc, cols], kc == 0, kc == 3, [bwa, b_yaT], [pb[iA]], kc == 3)
                    for kc in range(8):
                        mm(ps[iGA][:, 0:QB], w5[s_][:, 12 + kc, :], hT[:, kc, cols], kc == 0, kc == 7, [bwga] + hb, [pb[iGA]], kc == 7)
                    for kc in range(8):
                        mm(ps[iR][:, 0:QB], w5[s_][:, 4 + kc, :], yrT[:, kc, cols], kc == 0, kc == 7, [bwr_, b_yrT], [pb[iR]], kc == 7)
                    for kc in range(8):
                        mm(ps[iGR][:, 0:QB], w5[s_][:, 20 + kc, :], hT[:, kc, cols], kc == 0, kc == 7, [bwgr] + hb, [pb[iGR]], kc == 7)
                    s0, s1, s2, s3 = sa[scount % 2]
                    c0, c1, c2, c3 = b_sa[scount % 2]
                    scount += 1
                    K.op(ACT, lambda: a_.activation(out=s0[:], in_=ps[iGA][:, 0:QB], func=AF.Sigmoid), [pb[iGA]], [c0])
                    K.op(DVE, lambda: v_.tensor_tensor(out=s1[:], in0=s0[:], in1=ps[iA][:, 0:QB], op=ALU.mult), [c0, pb[iA]], [c1])
                    K.op(ACT, lambda: a_.activation(out=s2[:], in_=ps[iGR][:, 0:QB], func=AF.Sigmoid), [pb[iGR]], [c2])
                    K.op(DVE, lambda: v_.tensor_tensor(out=s3[:], in0=s2[:], in1=ps[iR][:, 0:QB], op=ALU.mult), [c2, pb[iR]], [c3])
                    K.op(POOL, lambda: g_.tensor_tensor(out=mgT[:, m, cols], in0=s1[:], in1=s3[:], op=ALU.add), [c1, c3],
                         b_mg[n * TQ:(n + 1) * TQ])
            K.barrier()

            al.release(m5)
            offB = al.off
            useA = (mU - off_hT) >= 80 * 1024
            if useA:
                al.off = off_hT
                al.limit = mU
            wo = al.alloc("wo", [128, 8, D], BF16)
            h2Tb = [al.alloc("h2Tb", [128, 8, QB], BF16) for _ in range(2)]
            xs2 = [al.alloc("xs2", [128, D], F32) for _ in range(2)]
            x1 = [al.alloc("x1", [128, D], F32) for _ in range(2)]
            h2 = [al.alloc("h2", [128, D], F32) for _ in range(2)]
            h2T = [al.alloc("h2T", [128, 8, 128], F32) for _ in range(2)]
            shs = [al.alloc("shs", [128, D], BF16) for _ in range(2)]
            h2b = [al.alloc("h2b", [128, D], BF16) for _ in range(2)]
            if useA:
                al.off = offB
                al.limit = LIMIT
            wsg = al.alloc("wsg", [128, 8, FF], BF16)
            wsu = al.alloc("wsu", [128, 8, FF], BF16)
            wsd = al.alloc("wsd", [128, 2, D], BF16)
            wr = al.alloc("wr", [128, 8, E], F32)
            bcv5 = al.alloc("bcv5", [128, 2, D], F32)
            b_bcv5 = Buf()
            gmb = al.alloc("gmb", [128, D], F32)
            gsfb = al.alloc("gsfb", [128, D], F32)
            shfb = al.alloc("shfb", [128, D], F32)
            sg = [al.alloc("sg", [128, QB], F32) for _ in range(2)]
            actT = al.alloc("actT", [128, 2, QB], BF16)
            rs = [al.alloc("rs", [128, 8], F32) for _ in range(2)]
            rt_ = [al.alloc("rt", [128, 6, E], F32) for _ in range(2)]
            g8 = [al.alloc("g8", [128, 8, 8], F32) for _ in range(2)]
            r8 = [al.alloc("r8", [128, 4, 8], F32) for _ in range(2)]
            i8 = [al.alloc("i8", [128, 8], U32) for _ in range(2)]
            mkb = [al.alloc("mkb", [128, E], BF16) for _ in range(2)]
            b_wo, b_ws, b_wr, b_bc5 = Buf(), Buf(), Buf(), Buf()
            b_xs2, b_x1, b_h2, b_h2b, b_h2T = [Buf(), Buf()], [Buf(), Buf()], [Buf(), Buf()], [Buf(), Buf()], [Buf(), Buf()]
            b_h2Tb, b_shs, b_sg, b_act, b_rs, b_rt = [Buf(), Buf()], [Buf(), Buf()], [Buf(), Buf()], Buf(), [Buf(), Buf()], [Buf(), Buf()]
            b_jk5 = Buf()
            jk5 = al.alloc("jk5", [128, D], BF16)
            pl_dma(wo[:], wout_d.ap().rearrange("(kc p) n -> p kc n", p=128), w=[b_wo])
            b_wsg, b_wsu, b_wsd = Buf(), Buf(), Buf()
            pl_dma(wsg[:], wsg_d.ap().rearrange("(kc p) n -> p kc n", p=128), w=[b_wsg])
            pl_dma(wsu[:], wsu_d.ap().rearrange("(kc p) n -> p kc n", p=128), w=[b_wsu])
            pl_dma(wsd[:], wsd_d.ap().rearrange("(kc p) n -> p kc n", p=128), w=[b_wsd])
            sp_dma(wr[:], wr_d.ap().rearrange("(kc p) n -> p kc n", p=128), w=[b_wr])
            sp_dma(bcv5[:], bcv_d[:, 0:2, :], w=[b_bcv5])
            sp_dma(gmb[:], modd[b:b + 1, 2 * D:3 * D].partition_broadcast(128), r=[b_modd], w=[b_bc5])
            sp_dma(gsfb[:], modd[b:b + 1, 4 * D:5 * D].partition_broadcast(128), r=[b_modd], w=[b_bc5])
            sp_dma(shfb[:], modd[b:b + 1, 3 * D:4 * D].partition_broadcast(128), r=[b_modd], w=[b_bc5])
            K.op(DVE, lambda: v_.tensor_tensor(out=gmb[:], in0=gmb[:], in1=bcv5[:, 0, :], op=ALU.mult), [b_bc5, b_bcv5], [b_bc5])
            K.op(DVE, lambda: v_.scalar_tensor_tensor(out=gsfb[:], in0=gsfb[:], scalar=1.0, in1=bcv5[:, 1, :], op0=ALU.add, op1=ALU.mult),
                 [b_bc5, b_bcv5], [b_bc5])
            rot["l"] = list(range(8))
            def m5_A(t):
                n, tt = t // TQ, t % TQ
                T = b * NT_S + t
                r0 = tok0 + t * 128
                p_ = t % 2
                xb, x1b, h2_, h2b_, h2T_, rs_ = xs2[p_], x1[p_], h2[p_], h2b[p_], h2T[p_], rs[p_]
                bxb, bx1, bh2, bh2b, bh2T, brs = b_xs2[p_], b_x1[p_], b_h2[p_], b_h2b[p_], b_h2T[p_], b_rs[p_]
                sp_dma(xb[:], x_d[r0:r0 + 128, :], w=[bxb])
                io = [nb(), nb()]
                for hf in range(2):
                    for kc in range(8):
                        mm(ps[io[hf]][:, :], mgT[:, kc, t * 128:(t + 1) * 128], wo[:, kc, hf * 512:(hf + 1) * 512], kc == 0, kc == 7,
                           [b_mg[t], b_wo], [pb[io[hf]]], kc == 7)
                for hf in range(2):
                    K.op(ACT, lambda: a_.activation(out=jk5[:, hf * 512:(hf + 1) * 512], in_=ps[io[hf]][:, :], func=AF.Square,
                                                    accum_out=rs_[:, hf:hf + 1]), [pb[io[hf]]], [b_jk5, brs])
                K.op(DVE, lambda: v_.tensor_tensor(out=rs_[:, 2:3], in0=rs_[:, 0:1], in1=rs_[:, 1:2], op=ALU.add), [brs], [brs])
                K.op(ACT, lambda: a_.activation(out=rs_[:, 3:4], in_=rs_[:, 2:3], func=AF.Sqrt, bias=EPS, scale=1.0 / D), [brs], [brs])
                K.op(DVE, lambda: v_.reciprocal(out=rs_[:, 4:5], in_=rs_[:, 3:4]), [brs], [brs])
                for hf in range(2):
                    cs_ = slice(hf * 512, (hf + 1) * 512)
                    K.op(DVE, lambda: v_.scalar_tensor_tensor(out=x1b[:, cs_], in0=ps[io[hf]][:, :], scalar=rs_[:, 4:5], in1=gmb[:, cs_],
                                                              op0=ALU.mult, op1=ALU.mult), [pb[io[hf]], brs, b_bc5], [bx1])
                K.op(POOL, lambda: g_.tensor_tensor(out=x1b[:], in0=x1b[:], in1=xb[:], op=ALU.add), [bx1, bxb], [bx1])
                sp_dma(x1_d[r0:r0 + 128, :], x1b[:], r=[bx1])
                K.op(ACT, lambda: a_.activation(out=jk5[:], in_=x1b[:], func=AF.Square, accum_out=rs_[:, 5:6]), [bx1], [b_jk5, brs])
                K.op(ACT, lambda: a_.activation(out=rs_[:, 6:7], in_=rs_[:, 5:6], func=AF.Sqrt, bias=EPS, scale=1.0 / D), [brs], [brs])
                K.op(DVE, lambda: v_.reciprocal(out=rs_[:, 7:8], in_=rs_[:, 6:7]), [brs], [brs])
                K.op(DVE, lambda: v_.scalar_tensor_tensor(out=h2_[:], in0=x1b[:], scalar=rs_[:, 7:8], in1=gsfb[:], op0=ALU.mult, op1=ALU.mult),
                     [bx1, brs, b_bc5], [bh2])
                K.op(POOL, lambda: g_.tensor_tensor(out=h2_[:], in0=h2_[:], in1=shfb[:], op=ALU.add), [bh2, b_bc5], [bh2])
                K.op(POOL, lambda: g_.tensor_copy(out=h2b_[:], in_=h2_[:]), [bh2], [bh2b])
                sp_dma(h2_d[r0:r0 + 128, :], h2b_[:], r=[bh2b])

            def m5_B(t):
                n, tt = t // TQ, t % TQ
                hb_ = h2Tb[n % 2]
                bhb = b_h2Tb[n % 2]
                T = b * NT_S + t
                p_ = t % 2
                h2_, h2T_ = h2[p_], h2T[p_]
                bh2, bh2T = b_h2[p_], b_h2T[p_]
                it = [nb(), nb()]
                for kc in range(8):
                    ib = it[kc // 4]
                    tr(ps[ib][:, (kc % 4) * 128:(kc % 4 + 1) * 128], h2_[:, kc * 128:(kc + 1) * 128], ident_f, [bh2, b_const], [pb[ib]],
                       kc % 4 == 3)
                for hf in range(2):
                    K.op(ACT, lambda: a_.copy(out=h2T_[:, hf * 4:(hf + 1) * 4, :], in_=ps[it[hf]][:, :].rearrange("p (a b) -> p a b", b=128)),
                         [pb[it[hf]]], [bh2T])
                K.op(POOL, lambda: g_.tensor_copy(out=hb_[:, :, tt * 128:(tt + 1) * 128], in_=h2T_[:]), [bh2T], [bhb])
                il = nb()
                for kc in range(8):
                    mm(ps[il][:, 0:E], h2T_[:, kc, :], wr[:, kc, :], kc == 0, kc == 7, [bh2T, b_wr], [pb[il]], kc == 7)
                R_, g8_, r8_, i8_, mk_ = rt_[p_], g8[p_], r8[p_], i8[p_], mkb[p_]
                brt = b_rt[p_]
                sc, sel, selm, mkf, wfull, tmp = (R_[:, k_, :] for k_ in range(6))
                K.op(ACT, lambda: a_.activation(out=sc, in_=ps[il][:, 0:E], func=AF.Sigmoid), [pb[il]], [brt])
                K.op(DVE, lambda: v_.tensor_tensor(out=sel, in0=sc, in1=rbias[:], op=ALU.add), [brt, b_const], [brt])
                for gg in range(8):
                    K.op(DVE, lambda: v_.max(out=g8_[:, gg, :], in_=sel[:, gg * 8:(gg + 1) * 8]), [brt], [brt])
                K.op(DVE, lambda: v_.tensor_tensor(out=r8_[:, 0, :], in0=g8_[:, :, 0], in1=g8_[:, :, 1], op=ALU.add), [brt], [brt])
                K.op(DVE, lambda: v_.max(out=r8_[:, 1, :], in_=r8_[:, 0, :]), [brt], [brt])
                K.op(DVE, lambda: v_.tensor_scalar(out=r8_[:, 2, :], in0=r8_[:, 0, :], scalar1=r8_[:, 1, 3:4], scalar2=-BIG,
                                                   op0=ALU.is_lt, op1=ALU.mult), [brt], [brt])
                K.op(DVE, lambda: v_.tensor_tensor(out=selm.rearrange("p (a b) -> p a b", b=8), in0=sel.rearrange("p (a b) -> p a b", b=8),
                                                   in1=r8_[:, 2, :].unsqueeze(2).to_broadcast([128, 8, 8]), op=ALU.add), [brt], [brt])
                K.op(DVE, lambda: v_.max(out=r8_[:, 3, :], in_=selm), [brt], [brt])
                K.op(DVE, lambda: v_.max_index(out=i8_[:], in_max=r8_[:, 3, :], in_values=selm), [brt], [brt])
                K.op(DVE, lambda: v_.tensor_scalar(out=mkf, in0=selm, scalar1=r8_[:, 3, 5:6], scalar2=None, op0=ALU.is_ge), [brt], [brt])
                K.op(DVE, lambda: v_.tensor_tensor(out=wfull, in0=sc, in1=mkf, op=ALU.mult), [brt], [brt])
                K.op(DVE, lambda: v_.tensor_reduce(out=r8_[:, 2, 0:1], in_=wfull, axis=AX.X, op=ALU.add), [brt], [brt])
                K.op(DVE, lambda: v_.reciprocal(out=r8_[:, 2, 1:2], in_=r8_[:, 2, 0:1]), [brt], [brt])
                K.op(POOL, lambda: g_.tensor_copy(out=mk_[:], in_=mkf), [brt], [brt])
                ik = nb()
                mm(ps[ik][:, 0:E], triu_b, mk_[:], True, True, [brt, b_const], [pb[ik]], False)
                mm(ps[ik][:, E:2 * E], ones_b, mk_[:], True, True, [brt, b_const], [pb[ik]], True)
                K.op(DVE, lambda: v_.tensor_tensor(out=tmp, in0=ps[ik][:, 0:E], in1=run[:], op=ALU.add), [pb[ik], b_run, brt], [brt])
                K.op(DVE, lambda: v_.tensor_tensor(out=rankm[:, T, :], in0=tmp, in1=mkf, op=ALU.mult), [brt], [b_route])
                K.op(DVE, lambda: v_.tensor_tensor(out=run[:], in0=run[:], in1=ps[ik][:, E:2 * E], op=ALU.add), [pb[ik], b_run], [b_run])
                K.op(DVE, lambda: v_.tensor_copy(out=eidx[:, T, :], in_=i8_[:]), [brt], [b_route])
                for k_ in range(TOPK):
                    K.op(DVE, lambda: v_.scalar_tensor_tensor(out=tmp, in0=iota64, scalar=eidx[:, T, k_:k_ + 1], in1=wfull,
                                                              op0=ALU.is_equal, op1=ALU.mult, accum_out=wk[:, T, k_:k_ + 1]),
                         [brt, b_route, b_const], [brt, b_route])
                K.op(DVE, lambda: v_.tensor_scalar(out=wkn[:, T, 0:TOPK], in0=wk[:, T, 0:TOPK], scalar1=r8_[:, 2, 1:2], scalar2=2.5,
                                                   op0=ALU.mult, op1=ALU.mult), [brt, b_route], [b_route])

            def m5_SH(n):
                hb_ = h2Tb[n % 2]
                bhb = b_h2Tb[n % 2]
                ig = [nb(), nb()]
                iu = [nb(), nb()]
                for c in range(2):
                    for kc in range(8):
                        mm(ps[ig[c]][:, 0:QB], wsg[:, kc, c * 128:(c + 1) * 128], hb_[:, kc, :], kc == 0, kc == 7, [b_wsg, bhb], [pb[ig[c]]], kc == 7)
                    for kc in range(8):
                        mm(ps[iu[c]][:, 0:QB], wsu[:, kc, c * 128:(c + 1) * 128], hb_[:, kc, :], kc == 0, kc == 7, [b_wsu, bhb], [pb[iu[c]]], kc == 7)
                    sg_ = sg[c]
                    K.op(ACT, lambda: a_.activation(out=sg_[:], in_=ps[ig[c]][:, 0:QB], func=AF.Sigmoid), [pb[ig[c]]], [b_sg[c]])
                    K.op(DVE, lambda: v_.tensor_tensor(out=sg_[:], in0=sg_[:], in1=ps[ig[c]][:, 0:QB], op=ALU.mult), [b_sg[c], pb[ig[c]]], [b_sg[c]])
                    K.op(DVE, lambda: v_.tensor_tensor(out=actT[:, c, :], in0=sg_[:], in1=ps[iu[c]][:, 0:QB], op=ALU.mult),
                         [b_sg[c], pb[iu[c]]], [b_act])
                for tt in range(TQ):
                    t = n * TQ + tt
                    r0 = tok0 + t * 128
                    iy = [nb(), nb()]
                    for hf in range(2):
                        for c in range(2):
                            mm(ps[iy[hf]][:, :], actT[:, c, tt * 128:(tt + 1) * 128], wsd[:, c, hf * 512:(hf + 1) * 512], c == 0, c == 1,
                               [b_act, b_wsd], [pb[iy[hf]]], c == 1)
                    sh_ = shs[t % 2]
                    K.op(ACT, lambda: a_.copy(out=sh_[:, 0:512], in_=ps[iy[0]][:, :]), [pb[iy[0]]], [b_shs[t % 2]])
                    K.op(DVE, lambda: v_.tensor_copy(out=sh_[:, 512:1024], in_=ps[iy[1]][:, :]), [pb[iy[1]]], [b_shs[t % 2]])
                    sp_dma(sh_d[r0:r0 + 128, :], sh_[:], r=[b_shs[t % 2]])

            m5_A(0)
            for t in range(NT_S):
                if t + 1 < NT_S:
                    m5_A(t + 1)
                m5_B(t)
                if t % TQ == TQ - 1:
                    m5_SH(t // TQ)
            K.barrier()

        al.release(mU)
        al.off = mU
        pe_ = al.alloc("pend", [128, 4, E], F32)
        pei = al.alloc("pei", [128, E], I32)
        ebf = al.alloc("ebf", [128, 2, NBLK], F32)
        djk = al.alloc("djk", [128, E], F32)
        rkp = al.alloc("rkp", [128, E], F32)
        destf = al.alloc("destf", [128, NT, 8], F32)
        b_pe, b_eb, b_dj, b_rkp, b_destf, b_desti = Buf(), Buf(), Buf(), Buf(), Buf(), Buf()
        b_desti_t = [Buf() for _ in range(NT)]
        K.op(DVE, lambda: v_.tensor_scalar(out=pe_[:, 3, :], in0=run[:], scalar1=float(CB - 1), scalar2=None, op0=ALU.add), [b_run], [b_pe])
        K.op(DVE, lambda: v_.tensor_copy(out=pei[:], in_=pe_[:, 3, :]), [b_pe], [b_pe])
        K.op(DVE, lambda: v_.tensor_single_scalar(out=pei[:], in_=pei[:], scalar=8, op=ALU.arith_shift_right), [b_pe], [b_pe])
        K.op(DVE, lambda: v_.tensor_copy(out=pe_[:, 0, :], in_=pei[:]), [b_pe], [b_pe])
        K.op(DVE, lambda: v_.tensor_tensor_scan(out=pe_[:, 1, :], data0=ones_c[:, 0:1].to_broadcast([128, E]), data1=pe_[:, 0, :],
                                                initial=0.0, op0=ALU.mult, op1=ALU.add), [b_pe, b_const], [b_pe])
        K.op(DVE, lambda: v_.tensor_tensor(out=pe_[:, 2, :], in0=pe_[:, 1, :], in1=pe_[:, 0, :], op=ALU.subtract), [b_pe], [b_pe])
        K.op(DVE, lambda: v_.tensor_scalar(out=pe_[:, 2, :], in0=pe_[:, 2, :], scalar1=float(CB), scalar2=None, op0=ALU.mult), [b_pe], [b_pe])
        for T in range(NT):
            K.op(DVE, lambda: v_.tensor_tensor(out=rkp[:], in0=rankm[:, T, :], in1=pe_[:, 2, :], op=ALU.add), [b_route, b_pe, b_rkp], [b_rkp])
            for k_ in range(TOPK):
                K.op(DVE, lambda: v_.scalar_tensor_tensor(out=djk[:], in0=iota64, scalar=eidx[:, T, k_:k_ + 1], in1=rkp[:],
                                                          op0=ALU.is_equal, op1=ALU.mult, accum_out=destf[:, T, k_:k_ + 1]),
                     [b_route, b_rkp, b_const], [b_dj, b_destf])
            K.op(DVE, lambda: v_.tensor_copy(out=desti[:, T, 0:TOPK], in_=destf[:, T, 0:TOPK]), [b_destf], [b_desti_t[T]])
        hg = [al.alloc("hg", [128, D], BF16) for _ in range(3)]
        b_hg = [Buf() for _ in range(3)]
        b_xs = Buf()
        for T in range(NT):
            hb_ = hg[T % 3]
            sp_dma(hb_[:], h2_d[T * 128:(T + 1) * 128, :], w=[b_hg[T % 3]])
            for k_ in range(TOPK):
                K.dma(K.qpool, lambda: g_.indirect_dma_start(out=xs_d[:, :], out_offset=bass.IndirectOffsetOnAxis(ap=desti[:, T, k_:k_ + 1], axis=0),
                                                             in_=hb_[:], in_offset=None, bounds_check=reg_slot, oob_is_err=False),
                      [b_hg[T % 3], b_desti_t[T], b_xs0], [], [b_xs])
        K.op(DVE, lambda: v_.memset(ebf[:, 0, :], 0.0), [], [b_eb])
        for e_ in range(E):
            K.op(DVE, lambda: v_.scalar_tensor_tensor(out=ebf[:, 0, :], in0=iotab, scalar=pe_[:, 1, e_:e_ + 1], in1=ebf[:, 0, :],
                                                      op0=ALU.is_ge, op1=ALU.add), [b_pe, b_const, b_eb], [b_eb])
        K.op(DVE, lambda: v_.tensor_scalar(out=ebf[:, 0, :], in0=ebf[:, 0, :], scalar1=float(E - 1), scalar2=128.0, op0=ALU.min, op1=ALU.mult),
             [b_eb], [b_eb])
        K.op(DVE, lambda: v_.tensor_scalar(out=ebf[:, 1, :], in0=ebf[:, 0, :], scalar1=pidx, scalar2=None, op0=ALU.add), [b_eb, b_const], [b_eb])
        if skip_reload and NBLK > 2:
            K.op(DVE, lambda: v_.tensor_tensor(out=ebf[:, 0, 2:NBLK], in0=ebf[:, 0, 2:NBLK], in1=ebf[:, 1, 0:NBLK - 2], op=ALU.subtract),
                 [b_eb], [b_eb])
            K.op(DVE, lambda: v_.tensor_scalar(out=ebf[:, 0, 2:NBLK], in0=ebf[:, 0, 2:NBLK], scalar1=pidx, scalar2=None, op0=ALU.add),
                 [b_eb, b_const], [b_eb])
            K.op(DVE, lambda: v_.tensor_scalar(out=ebf[:, 0, 2:NBLK], in0=ebf[:, 0, 2:NBLK], scalar1=0.0, scalar2=1.0e6,
                                               op0=ALU.is_equal, op1=ALU.mult), [b_eb], [b_eb])
            K.op(DVE, lambda: v_.tensor_tensor(out=ebf[:, 1, 2:NBLK], in0=ebf[:, 1, 2:NBLK], in1=ebf[:, 0, 2:NBLK], op=ALU.add), [b_eb], [b_eb])
        K.op(DVE, lambda: v_.tensor_copy(out=widx[:], in_=ebf[:, 1, :]), [b_eb], [b_widx])
        K.barrier()

        al.off = mU
        wE = [[al.alloc("wE", [128, 2048], BF16) for _ in range(3)] for _ in range(2)]
        b_wE = [[Buf() for _ in range(3)] for _ in range(2)]
        xsb = [al.alloc("xsb", [128, 2, D], BF16) for _ in range(3)]
        xTe = [al.alloc("xTe", [128, 8, CB], BF16) for _ in range(3)]
        sge = [al.alloc("sge", [128, 2 * CB], F32) for _ in range(2)]
        acte = [al.alloc("acte", [128, 2 * CB], BF16) for _ in range(2)]
        ysb = [al.alloc("ysb", [128, D], BF16) for _ in range(4)]
        b_xsb, b_xTe, b_sge, b_acte = [Buf(), Buf(), Buf()], [Buf(), Buf(), Buf()], [Buf(), Buf()], [Buf(), Buf()]
        b_ysb = [Buf() for _ in range(4)]
        b_ysb2 = [Buf() for _ in range(4)]
        b_xTe2 = [Buf(), Buf(), Buf()]
        b_ys = Buf()
        rot["l"] = list(range(8))
        wsrc = wpb_d

        def load_wE(blk, which):
            s_ = blk % 2
            for m in which:
                K.dma(K.qpool, lambda: g_.indirect_dma_start(out=wE[s_][m][:], out_offset=None, in_=wsrc[m][:, :],
                                                             in_offset=bass.IndirectOffsetOnAxis(ap=widx[:, blk:blk + 1], axis=0),
                                                             bounds_check=reg_w, oob_is_err=False),
                      [b_widx, b_wcast], [b_wE[s_][m]])

        def load_xs(blk):
            p_ = blk % 3
            sp_dma(xsb[p_][:], xs_d[blk * CB:(blk + 1) * CB, :].rearrange("(s p) d -> p s d", p=128), r=[b_xs], w=[b_xsb[p_]])

        def stage_T(blk):
            p_ = blk % 3
            for s2 in range(2):
                i = nb()
                pT = ps[i][:, :].bitcast(BF16)
                for kc in range(8):
                    tr(pT[:, kc * 128:(kc + 1) * 128], xsb[p_][:, s2, kc * 128:(kc + 1) * 128], ident_b, [b_xsb[p_], b_const], [pb[i]], kc == 7)
                o_ap = xTe[p_][:, :, s2 * 128:(s2 + 1) * 128]
                i_ap = pT.rearrange("p (a b) -> p a b", b=128)
                if s2 == 0:
                    K.op(ACT, lambda: a_.copy(out=o_ap, in_=i_ap), [pb[i]], [b_xTe[p_]])
                else:
                    K.op(DVE, lambda: v_.tensor_copy(out=o_ap, in_=i_ap), [pb[i]], [b_xTe2[p_]])

        def stage_GU(blk):
            s_ = blk % 2
            p_ = blk % 2
            x_ = blk % 3
            wgE, wuE, wdE = wE[s_]
            bwg, bwu, bwd = b_wE[s_]
            ig_, iu_ = nb(), nb()
            for c in range(2):
                for kc in range(8):
                    mm(ps[ig_][:, c * CB:(c + 1) * CB], wgE[:, kc * FF + c * 128: kc * FF + (c + 1) * 128], xTe[x_][:, kc, :], kc == 0, kc == 7,
                       [bwg, b_xTe[x_], b_xTe2[x_]], [pb[ig_]], kc == 7)
            for c in range(2):
                for kc in range(8):
                    mm(ps[iu_][:, c * CB:(c + 1) * CB], wuE[:, kc * FF + c * 128: kc * FF + (c + 1) * 128], xTe[x_][:, kc, :], kc == 0, kc == 7,
                       [bwu, b_xTe[x_], b_xTe2[x_]], [pb[iu_]], kc == 7)
            K.op(ACT, lambda: a_.activation(out=sge[p_][:], in_=ps[ig_][:, :], func=AF.Sigmoid), [pb[ig_]], [b_sge[p_]])
            K.op(DVE, lambda: v_.tensor_tensor(out=sge[p_][:], in0=sge[p_][:], in1=ps[ig_][:, :], op=ALU.mult), [b_sge[p_], pb[ig_]], [b_sge[p_]])
            K.op(DVE, lambda: v_.tensor_tensor(out=acte[p_][:], in0=sge[p_][:], in1=ps[iu_][:, :], op=ALU.mult), [b_sge[p_], pb[iu_]], [b_acte[p_]])

        ycnt = {"n": 0}

        def stage_D(blk):
            s_ = blk % 2
            p_ = blk % 2
            wdE = wE[s_][2]
            bwd = b_wE[s_][2]
            for s2 in range(2):
                yb = ysb[ycnt["n"] % 4]
                byb = b_ysb[ycnt["n"] % 4]
                byb2 = b_ysb2[ycnt["n"] % 4]
                ycnt["n"] += 1
                for hf in range(2):
                    i = nb()
                    for c in range(2):
                        mm(ps[i][:, :], acte[p_][:, c * CB + s2 * 128: c * CB + (s2 + 1) * 128], wdE[:, c * D + hf * 512: c * D + (hf + 1) * 512],
                           c == 0, c == 1, [b_acte[p_], bwd], [pb[i]], c == 1)
                    if hf == 0:
                        K.op(ACT, lambda: a_.copy(out=yb[:, 0:512], in_=ps[i][:, :]), [pb[i]], [byb])
                    else:
                        K.op(DVE, lambda: v_.tensor_copy(out=yb[:, 512:1024], in_=ps[i][:, :]), [pb[i]], [byb2])
                r0 = blk * CB + s2 * 128
                sp_dma(ys_d[r0:r0 + 128, :], yb[:], r=[byb, byb2], sw=[b_ys])

        precast(3 * E)
        load_wE(0, (0, 1, 2))
        if NBLK > 1:
            load_wE(1, (0, 1, 2))
        for b0 in range(min(3, NBLK)):
            load_xs(b0)
        stage_T(0)
        if NBLK > 1:
            stage_T(1)
        stage_GU(0)
        for blk in range(NBLK):
            if blk + 2 < NBLK:
                load_wE(blk + 2, (0, 1))
                stage_T(blk + 2)
                if blk + 3 < NBLK:
                    load_xs(blk + 3)
            if blk >= 1:
                stage_D(blk - 1)
                if blk + 1 < NBLK:
                    load_wE(blk + 1, (2,))
            if blk + 1 < NBLK:
                stage_GU(blk + 1)
        stage_D(NBLK - 1)
        K.barrier()

        al.off = mU
        gfb = al.alloc("gfb", [128, NSEQ, D], F32)
        bcvc = al.alloc("bcvc", [128, D], F32)
        b_bcvc = Buf()
        sp_dma(bcvc[:], bcv_d[:, 2, :], w=[b_bcvc])
        b_gfb = Buf()
        for b in range(NSEQ):
            sp_dma(gfb[:, b, :], modd[b:b + 1, 5 * D:6 * D].partition_broadcast(128), r=[b_modd], w=[b_gfb])
            K.op(DVE, lambda: v_.tensor_tensor(out=gfb[:, b, :], in0=gfb[:, b, :], in1=bcvc[:], op=ALU.mult), [b_gfb, b_bcvc], [b_gfb])
        x1c = [al.alloc("x1c", [128, D], F32) for _ in range(2)]
        zc = [al.alloc("zc", [128, D], F32) for _ in range(2)]
        yg = [al.alloc("yg", [128, D], BF16) for _ in range(12)]
        shc = [al.alloc("shc", [128, D], BF16) for _ in range(2)]
        b_shc = [Buf(), Buf()]
        jkc = al.alloc("jkc", [128, D], BF16)
        rc_ = [al.alloc("rc", [128, 4], F32) for _ in range(2)]
        b_x1c, b_zc, b_rc = [Buf(), Buf()], [Buf(), Buf()], [Buf(), Buf()]
        b_yg = [Buf() for _ in range(12)]
        b_jkc = Buf()
        b_out = Buf()
        z2 = [al.alloc("z2", [128, D], F32) for _ in range(2)]
        b_z2 = [Buf(), Buf()]
        z3 = [al.alloc("z3", [128, D], F32) for _ in range(2)]
        b_z3 = [Buf(), Buf()]
        gc = {"n": 0}

        dg = [al.alloc("dg", [128, 128], BF16) for _ in range(12)]
        b_dg = [Buf() for _ in range(12)]
        rot["l"] = list(range(8))
        cps = {}

        def c_A(T):
            p_ = T % 2
            r0 = T * 128
            sp_dma(x1c[p_][:], x1_d[r0:r0 + 128, :], w=[b_x1c[p_]])
            sp_dma(shc[p_][:], sh_d[r0:r0 + 128, :], w=[b_shc[p_]])
            ys_ = []
            for k_ in range(TOPK):
                y_ = yg[gc["n"] % 12]
                by = b_yg[gc["n"] % 12]
                d_ = dg[gc["n"] % 12]
                bd = b_dg[gc["n"] % 12]
                gc["n"] += 1
                K.dma(K.qpool, lambda: g_.indirect_dma_start(out=y_[:], out_offset=None, in_=ys_d[:, :],
                                                             in_offset=bass.IndirectOffsetOnAxis(ap=desti[:, T, k_:k_ + 1], axis=0),
                                                             bounds_check=reg_slot, oob_is_err=False),
                      [b_ys, b_desti_t[T]], [by])
                K.op(ACT, lambda: a_.activation(out=d_[:], in_=ident_f, func=AF.Identity, scale=wkn[:, T, k_:k_ + 1]),
                     [b_const, b_route], [bd])
                ys_.append((y_, by, d_, bd))
            banks = [nb(), nb()]
            cps[T] = banks
            for hf in range(2):
                i = banks[hf]
                cs_ = slice(hf * 512, (hf + 1) * 512)
                mm(ps[i][:, :], ident_b, shc[p_][:, cs_], True, False, [b_const, b_shc[p_]], [pb[i]], False)
                for k_ in range(TOPK):
                    y_, by, d_, bd = ys_[k_]
                    mm(ps[i][:, :], d_[:], y_[:, cs_], False, k_ == TOPK - 1, [bd, by], [pb[i]], k_ == TOPK - 1)

        def c_B(T):
            b = T // NT_S
            p_ = T % 2
            r0 = T * 128
            banks = cps.pop(T)
            for hf in range(2):
                K.op(ACT, lambda: a_.activation(out=jkc[:, hf * 512:(hf + 1) * 512], in_=ps[banks[hf]][:, :], func=AF.Square,
                                                accum_out=rc_[p_][:, hf:hf + 1]), [pb[banks[hf]]], [b_jkc, b_rc[p_]])
            K.op(DVE, lambda: v_.tensor_tensor(out=rc_[p_][:, 3:4], in0=rc_[p_][:, 0:1], in1=rc_[p_][:, 1:2], op=ALU.add), [b_rc[p_]], [b_rc[p_]])
            K.op(ACT, lambda: a_.activation(out=rc_[p_][:, 1:2], in_=rc_[p_][:, 3:4], func=AF.Sqrt, bias=EPS, scale=1.0 / D), [b_rc[p_]], [b_rc[p_]])
            K.op(DVE, lambda: v_.reciprocal(out=rc_[p_][:, 2:3], in_=rc_[p_][:, 1:2]), [b_rc[p_]], [b_rc[p_]])
            for hf in range(2):
                cs_ = slice(hf * 512, (hf + 1) * 512)
                K.op(DVE, lambda: v_.scalar_tensor_tensor(out=zc[p_][:, cs_], in0=ps[banks[hf]][:, :], scalar=rc_[p_][:, 2:3], in1=gfb[:, b, cs_],
                                                          op0=ALU.mult, op1=ALU.mult), [pb[banks[hf]], b_rc[p_], b_gfb], [b_zc[p_]])
            K.op(DVE, lambda: v_.tensor_tensor(out=zc[p_][:], in0=zc[p_][:], in1=x1c[p_][:], op=ALU.add), [b_zc[p_], b_x1c[p_]], [b_zc[p_]])
            sp_dma(out_d[r0:r0 + 128, :], zc[p_][:], r=[b_zc[p_]], sw=[b_out])

        c_A(0)
        for T in range(NT):
            if T + 1 < NT:
                c_A(T + 1)
            c_B(T)
        K.barrier()
    return nc


def _bf16():
    import ml_dtypes
    return ml_dtypes.bfloat16


def make_consts(NBLK):
    NCF = 128 + 128 + 64 + NBLK + 2
    cf = np.zeros((128, NCF), np.float32)
    cf[:, 0:128] = np.eye(128, dtype=np.float32)
    cf[127, 128:256] = 1.0
    cf[:, 256:320] = np.arange(64, dtype=np.float32)[None, :]
    cf[:, 320:320 + NBLK] = np.arange(NBLK, dtype=np.float32)[None, :]
    cf[:, 320 + NBLK] = np.arange(128, dtype=np.float32)
    cf[:, 321 + NBLK] = 1.0
    cb = np.zeros((128, 1792), np.float32)
    cb[:, 0:128] = np.eye(128)
    k = np.arange(128)[:, None]
    m = np.arange(128)[None, :]
    cb[:, 128:256] = (k < m)
    cb[:, 256:384] = (m >= k)
    cb[:, 384:512] = 1.0
    for h in range(NH):
        for g3 in range(3):
            cb[g3 * 32 + h, 512 + h * 128:512 + (h + 1) * 128] = 1.0
    cb[:, 1536:1664] = np.where(m < k, -30000.0, 0.0)
    cb[:, 1664:1792] = (m == (k + 64) % 128)
    return cf, cb.astype(_bf16())


def prep_shared(inp):
    f = np.float32
    sh = {}
    sh["w_ada"] = np.ascontiguousarray(inp["w_ada"][0], f)
    fm = np.zeros((128, 9, 8), f)

    def fmaj(v):
        return np.asarray(v, f).reshape(8, 128).T
    fm[:, 0, :] = fmaj(inp["g_pre_mix"][0])
    for j in range(4):
        fm[:, 1 + j, :] = fmaj(inp["w_conv"][0, j])
    fm[:, 5, :] = fmaj(inp["b_conv"][0])
    fm[:, 6, :] = fmaj(inp["b_rg"][0])
    fm[:, 7, :] = fmaj(inp["b_ig"][0])
    fm[:, 8, :] = fmaj(inp["rglru_lambda"][0])
    sh["fm"] = fm
    bcv = np.zeros((128, 3, D), f)
    bcv[:, 0, :] = np.asarray(inp["g_post_mix"][0], f)[None, :]
    bcv[:, 1, :] = np.asarray(inp["g_pre_ffn"][0], f)[None, :]
    bcv[:, 2, :] = np.asarray(inp["g_post_ffn"][0], f)[None, :]
    sh["bcv"] = bcv
    sh["rbias"] = np.ascontiguousarray(np.broadcast_to(np.asarray(inp["router_bias"][0], f)[None, :], (128, E)))
    sh["b_forget"] = np.asarray(inp["b_forget"][0], f).reshape(NH, 1)
    sh["w_in"] = np.ascontiguousarray(inp["w_in"][0], f)
    for nm, src in (("wrg_bd", inp["w_rg"][0]), ("wig_bd", inp["w_ig"][0])):
        bd = np.zeros((8, 128, 128), f)
        for c in range(8):
            bd[c, 0:64, 0:64] = src[2 * c]
            bd[c, 64:128, 64:128] = src[2 * c + 1]
        sh[nm] = bd
    sh["w_ba"] = np.ascontiguousarray(inp["w_branch_attn"][0], f)
    sh["w_br"] = np.ascontiguousarray(inp["w_branch_rnn"][0], f)
    sh["w_out"] = np.ascontiguousarray(inp["w_out"][0], f)
    sh["w_router"] = np.ascontiguousarray(inp["w_router"][0], f)
    sh["w_sg"] = np.ascontiguousarray(inp["w_sh_gate"][0], f)
    sh["w_su"] = np.ascontiguousarray(inp["w_sh_up"][0], f)
    sh["w_sd"] = np.ascontiguousarray(inp["w_sh_down"][0], f)
    wg = np.asarray(inp["w_exp_gate"][0], f).reshape(E, 8, 128, FF).transpose(0, 2, 1, 3)
    sh["wpg"] = np.ascontiguousarray(wg).reshape(E * 128, 2048)
    wu = np.asarray(inp["w_exp_up"][0], f).reshape(E, 8, 128, FF).transpose(0, 2, 1, 3)
    sh["wpu"] = np.ascontiguousarray(wu).reshape(E * 128, 2048)
    wd = np.asarray(inp["w_exp_down"][0], f).reshape(E, 2, 128, D).transpose(0, 2, 1, 3)
    sh["wpd"] = np.ascontiguousarray(wd).reshape(E * 128, 2048)
    return sh


def prep_core(inp, sh, core, NSEQ, S, NBLK):
    f = np.float32
    m = dict(sh)
    xs = np.asarray(inp["x"][core * NSEQ:(core + 1) * NSEQ], f).reshape(NSEQ * S, D)
    m["x"] = np.ascontiguousarray(xs)
    c = np.asarray(inp["c"][core * NSEQ:(core + 1) * NSEQ], f)
    m["csT"] = np.ascontiguousarray(c.T.reshape(8, 128, NSEQ).transpose(1, 0, 2))
    m["b_ada_rep"] = np.ascontiguousarray(np.broadcast_to(np.asarray(inp["b_ada"][0], f)[None, :], (NSEQ, 6 * D)))
    cf, cb = make_consts(NBLK)
    m["cf"] = cf
    m["cb"] = cb
    return m


def kernel(**inputs):
    B, S = inputs["x"].shape[0], inputs["x"].shape[1]
    NSEQ = B // NCORES
    NTOK = NSEQ * S
    NBLK = (NTOK * TOPK) // CB + E
    nc = build(NSEQ, S, skip_reload=True)
    sh = prep_shared(inputs)
    in_maps = [prep_core(inputs, sh, i, NSEQ, S, NBLK) for i in range(NCORES)]
    res = run_bass_kernel_spmd(nc, in_maps, core_ids=list(range(NCORES)))
    outs = [np.asarray(r["out"], np.float32).reshape(NSEQ, S, D) for r in res.results]
    return np.concatenate(outs, axis=0)
```

```python
import numpy as np
import concourse.bass as bass
import concourse.mybir as mybir
from concourse.bass_utils import run_bass_kernel_spmd
from contextlib import ExitStack

F32 = mybir.dt.float32
BF16 = mybir.dt.bfloat16
I32 = mybir.dt.int32
U32 = mybir.dt.uint32
AF = mybir.ActivationFunctionType
ALU = mybir.AluOpType
AX = mybir.AxisListType

D = 1024
NH = 8
E = 64
TOPK = 6
FF = 256
CB = 256
INC = 5640
OQ, OK_, OV, OF_, OX, OG, OGA, OGR = 0, 512, 1024, 1536, 1544, 2568, 3592, 4616
EPS = 1e-6
BIG = 1.0e4
NCORES = 8
ARENA_SHIFT = [0]
ARENA_MAX = [0]


class Buf:
    __slots__ = ("w", "r", "name")

    def __init__(self, name=""):
        self.w = {}
        self.r = {}
        self.name = name


class Eng:
    def __init__(self, name, e, sem, key):
        self.name = name
        self.e = e
        self.sem = sem
        self.key = key
        self.n = 0
        self.seen = {}
        self.pending = False


class DQ:
    def __init__(self, eng, sems):
        self.eng = eng
        self.sems = sems
        self.cnt = [0] * len(sems)
        self.next = 0


def _merge(d, s):
    for k, v in s.items():
        if d.get(k, 0) < v:
            d[k] = v


class KB:
    def __init__(self, nc, stack):
        self.nc = nc
        self.semtab = {}
        self.engs = []
        for nm, e in (("pe", nc.tensor), ("act", nc.scalar), ("dve", nc.vector),
                      ("pool", nc.gpsimd), ("sp", nc.sync)):
            sem = stack.enter_context(nc.semaphore("s_" + nm))
            eng = Eng(nm, e, sem, "c_" + nm)
            self.semtab[eng.key] = sem
            setattr(self, nm, eng)
            self.engs.append(eng)
        self.queues = []
        for nm, eng, n in (("qsp", self.sp, 8), ("qpool", self.pool, 6)):
            sems = []
            for i in range(n):
                key = "d_%s%d" % (nm, i)
                sem = stack.enter_context(nc.semaphore(key))
                self.semtab[key] = sem
                sems.append((sem, key))
            q = DQ(eng, sems)
            setattr(self, nm, q)
            self.queues.append(q)

    def _wait(self, E_, deps):
        for k, v in deps.items():
            if E_.seen.get(k, 0) < v:
                E_.e.wait_ge(self.semtab[k], v)
                E_.seen[k] = v

    def op(self, E_, fn, reads=(), writes=(), inc=True):
        deps = {}
        for b in reads:
            _merge(deps, b.w)
        for b in writes:
            _merge(deps, b.w)
            _merge(deps, b.r)
        if E_.name == "pe":
            deps.pop(E_.key, None)
        self._wait(E_, deps)
        ins = fn()
        ev = E_.n + 1
        for b in reads:
            if b.r.get(E_.key, 0) < ev:
                b.r[E_.key] = ev
        for b in writes:
            b.w = {E_.key: ev}
            b.r = {}
        if inc:
            E_.n = ev
            ins.then_inc(E_.sem, 1)
            E_.pending = False
        else:
            E_.pending = True
        return ins

    def dma(self, Q, fn, reads=(), writes=(), swrites=()):
        E_ = Q.eng
        deps = {}
        for b in reads:
            _merge(deps, b.w)
        for b in writes:
            _merge(deps, b.w)
            _merge(deps, b.r)
        for b in swrites:
            _merge(deps, b.r)
        slot = Q.next
        Q.next = (Q.next + 1) % len(Q.sems)
        sem, key = Q.sems[slot]
        if Q.cnt[slot] > 0:
            if deps.get(key, 0) < 16 * Q.cnt[slot]:
                deps[key] = 16 * Q.cnt[slot]
        self._wait(E_, deps)
        ins = fn()
        Q.cnt[slot] += 1
        v = 16 * Q.cnt[slot]
        ins.then_inc(sem, 16)
        for b in reads:
            if b.r.get(key, 0) < v:
                b.r[key] = v
        for b in writes:
            b.w = {key: v}
            b.r = {}
        for b in swrites:
            if b.w.get(key, 0) < v:
                b.w[key] = v
        return ins

    def barrier(self):
        tot = {}
        for E_ in self.engs:
            assert not E_.pending
            if E_.n > 0:
                tot[E_.key] = E_.n
        for Q in self.queues:
            for i, (sem, key) in enumerate(Q.sems):
                if Q.cnt[i] > 0:
                    tot[key] = 16 * Q.cnt[i]
        for E_ in self.engs:
            self._wait(E_, dict(tot))


class Arena:
    def __init__(self, nc, limit):
        self.nc = nc
        self.off = 0
        self.limit = limit
        self.n = 0

    def alloc(self, name, shape, dtype):
        sz = 1
        for s in shape[1:]:
            sz *= s
        sz *= {F32: 4, BF16: 2, I32: 4, U32: 4}[dtype]
        sz = (sz + 63) // 64 * 64
        off = self.off
        assert off + sz <= self.limit, ("SBUF arena overflow", name, off, sz)
        self.off += sz
        self.n += 1
        ARENA_MAX[0] = max(ARENA_MAX[0], self.off)
        return self.nc.alloc_sbuf_tensor_at("%s_%d" % (name, self.n), list(shape), dtype, offset=off)

    def mark(self):
        return self.off

    def release(self, m):
        self.off = m


def build(NSEQ, S, skip_reload=True):
    NT_S = S // 128
    NTOK = NSEQ * S
    NT = NTOK // 128
    QB = min(512, S)
    TQ = QB // 128
    NQ = S // QB
    NB5 = S // QB
    NBLK = (NTOK * TOPK) // CB + E
    NSLOT = NBLK * CB

    nc = bass.Bass("TRN2", target_bir_lowering=False)
    dt = nc.dram_tensor

    def ein(name, shape, dtype=F32):
        return dt(name, list(shape), dtype, kind="ExternalInput")

    x_d = ein("x", [NTOK, D])
    cs_d = ein("csT", [128, 8, NSEQ])
    wada_d = ein("w_ada", [D, 6 * D])
    bada_d = ein("b_ada_rep", [NSEQ, 6 * D])
    fm_d = ein("fm", [128, 9, 8])
    bcv_d = ein("bcv", [128, 3, D])
    rb_d = ein("rbias", [128, E])
    bf_d = ein("b_forget", [NH, 1])
    win_d = ein("w_in", [D, INC])
    wrg_d = ein("wrg_bd", [8, 128, 128])
    wig_d = ein("wig_bd", [8, 128, 128])
    wba_d = ein("w_ba", [512, D])
    wbr_d = ein("w_br", [D, D])
    wout_d = ein("w_out", [D, D])
    wr_d = ein("w_router", [D, E])
    wsg_d = ein("w_sg", [D, FF])
    wsu_d = ein("w_su", [D, FF])
    wsd_d = ein("w_sd", [FF, D])
    wpg_d = ein("wpg", [E * 128, 2048])
    wpu_d = ein("wpu", [E * 128, 2048])
    wpd_d = ein("wpd", [E * 128, 2048])
    NCF = 128 + 128 + 64 + NBLK + 2
    cf_d = ein("cf", [128, NCF])
    cb_d = ein("cb", [128, 1792], BF16)
    out_d = dt("out", [NTOK, D], F32, kind="ExternalOutput")
    modd = dt("modd", [NSEQ, 6 * D], F32)
    h2_d = dt("h2s", [NTOK, D], BF16)
    x1_d = dt("x1s", [NTOK, D], F32)
    sh_d = dt("shs", [NTOK, D], BF16)
    xs_d = dt("xss", [NSLOT, D], BF16)
    ys_d = dt("yss", [NSLOT, D], BF16)
    wpb_d = [dt("wpb%d" % m_, [E * 128, 2048], BF16) for m_ in range(3)]

    stack = ExitStack()
    with stack:
        K = KB(nc, stack)
        al = Arena(nc, int(nc._sbuf_addr_for_side("right")) - 64)
        al.off = (int(nc._sbuf_addr_for_side("left")) + 63) // 64 * 64 + ARENA_SHIFT[0]
        ps = [stack.enter_context(nc.psum_tensor("ps%d" % i, [128, 512], F32)) for i in range(8)]
        pb = [Buf("pb%d" % i) for i in range(8)]
        rot = {"l": list(range(8)), "i": 0}

        def nb():
            i = rot["l"][rot["i"] % len(rot["l"])]
            rot["i"] += 1
            return i

        PE, ACT, DVE, POOL = K.pe, K.act, K.dve, K.pool
        reg_slot = nc.gpsimd.alloc_register("bc_slot")
        nc.gpsimd.reg_mov(reg_slot, NSLOT - 1)
        reg_w = nc.gpsimd.alloc_register("bc_w")
        nc.gpsimd.reg_mov(reg_w, E * 128 - 1)
        v_, a_, g_, t_ = nc.vector, nc.scalar, nc.gpsimd, nc.tensor

        def mm(out, lhsT, rhs, start, stop, r, w, inc):
            return K.op(PE, lambda: t_.matmul(out, lhsT, rhs, start=start, stop=stop), r, w, inc)

        def tr(out, in_, ident, r, w, inc):
            return K.op(PE, lambda: t_.transpose(out, in_, ident), r, w, inc)

        def sp_dma(out, in_, r=(), w=(), sw=()):
            return K.dma(K.qsp, lambda: nc.sync.dma_start(out=out, in_=in_), r, w, sw)

        def pl_dma(out, in_, r=(), w=(), sw=()):
            return K.dma(K.qpool, lambda: nc.gpsimd.dma_start(out=out, in_=in_), r, w, sw)

        cf = al.alloc("cf", [128, NCF], F32)
        cbt = al.alloc("cb", [128, 1792], BF16)
        b_const = Buf("const")
        ident_f = cf[:, 0:128]
        sel127 = cf[:, 128:256]
        iota64 = cf[:, 256:320]
        iotab = cf[:, 320:320 + NBLK]
        pidx = cf[:, 320 + NBLK:321 + NBLK]
        ones_c = cf[:, 321 + NBLK:322 + NBLK]
        ident_b = cbt[:, 0:128]
        triu_b = cbt[:, 128:256]
        trim_b = cbt[:, 256:384]
        ones_b = cbt[:, 384:512]
        negm_b = cbt[:, 1536:1664]
        swap_b = cbt[:, 1664:1792]
        fm = al.alloc("fm", [128, 9, 8], F32)
        sm = al.alloc("sm", [128, 6, 8], F32)
        rbias = al.alloc("rbias", [128, E], F32)
        nbf = al.alloc("nbf", [128, 2], F32)
        b_nbf = Buf()
        gsmT = al.alloc("gsmT", [128, 8, NSEQ], F32)
        shmT = al.alloc("shmT", [128, 8, NSEQ], F32)
        run = al.alloc("run", [128, E], F32)
        rankm = al.alloc("rankm", [128, NT, E], F32)
        eidx = al.alloc("eidx", [128, NT, 8], F32)
        wk = al.alloc("wk", [128, NT, 8], F32)
        wkn = al.alloc("wkn", [128, NT, 8], F32)
        desti = al.alloc("desti", [128, NT, 8], I32)
        widx = al.alloc("widx", [128, NBLK], I32)
        b_fm, b_sm, b_gs, b_run, b_route = Buf(), Buf(), Buf(), Buf(), Buf()
        b_widx = Buf()
        zt = al.alloc("zt", [128, 2, D], BF16)
        b_zt, b_xs0 = Buf(), Buf()
        K.op(POOL, lambda: g_.memset(zt[:], 0.0), [], [b_zt])
        zf = {"n": 0}
        ZF_PER = -(-NBLK // NT)

        b_wcast = Buf()
        pcast = {"n": 0}
        PC_PER = -(-(3 * E) // (NSEQ * 4 * NQ))

        def precast(cnt):
            for _ in range(cnt):
                if pcast["n"] < 3 * E:
                    e_, m_ = pcast["n"] // 3, pcast["n"] % 3
                    src = (wpg_d, wpu_d, wpd_d)[m_]
                    pl_dma(wpb_d[m_][e_ * 128:(e_ + 1) * 128, :], src[e_ * 128:(e_ + 1) * 128, :], sw=[b_wcast])
                    pcast["n"] += 1

        def zero_fill(cnt):
            for _ in range(cnt):
                if zf["n"] < NBLK:
                    r0_ = zf["n"] * CB
                    sp_dma(xs_d[r0_:r0_ + CB, :].rearrange("(s p) d -> p s d", p=128), zt[:], r=[b_zt], sw=[b_xs0])
                    zf["n"] += 1

        sp_dma(cf[:], cf_d.ap(), w=[b_const])
        sp_dma(cbt[:], cb_d.ap(), w=[b_const])
        sp_dma(fm[:], fm_d.ap(), w=[b_fm])
        sp_dma(rbias[:], rb_d.ap(), w=[b_const])
        K.op(DVE, lambda: v_.memset(nbf[:], 0.0), [], [b_nbf])
        for g3 in range(3):
            sp_dma(nbf[g3 * 32:g3 * 32 + NH, 0:1], bf_d.ap(), w=[b_nbf])
        K.op(DVE, lambda: v_.memset(run[:], 0.0), [], [b_run])

        m0 = al.mark()
        cs = al.alloc("cs", [128, 8, NSEQ], F32)
        th0 = al.alloc("th0", [128, 8, NSEQ], F32)
        siluT = al.alloc("siluT", [128, 8, NSEQ], BF16)
        modt = al.alloc("modt", [NSEQ, 6 * D], F32)
        bada = al.alloc("bada", [NSEQ, 6 * D], F32)
        wada = [al.alloc("wada", [128, 8, 512], BF16) for _ in range(2)]
        b_cs, b_th0, b_silu, b_modt, b_bada = Buf(), Buf(), Buf(), Buf(), Buf()
        b_wada = [Buf(), Buf()]

        K.op(DVE, lambda: v_.tensor_scalar(out=nbf[0:72, 1:2], in0=nbf[0:72, 0:1], scalar1=-1.0, scalar2=None,
                                           op0=ALU.mult), [b_nbf], [b_nbf])
        K.op(ACT, lambda: a_.activation(out=sm[:, 4, :], in_=fm[:, 8, :], func=AF.Exp, scale=-1.0), [b_fm], [b_sm])
        K.op(ACT, lambda: a_.activation(out=sm[:, 5, :], in_=sm[:, 4, :], func=AF.Ln, bias=1.0, scale=1.0), [b_sm], [b_sm])
        K.op(DVE, lambda: v_.tensor_scalar(out=sm[:, 0, :], in0=sm[:, 5, :], scalar1=-8.0, scalar2=None, op0=ALU.mult), [b_sm], [b_sm])
        K.op(DVE, lambda: v_.tensor_scalar(out=sm[:, 1, :], in0=sm[:, 5, :], scalar1=-4.0, scalar2=None, op0=ALU.mult), [b_sm], [b_sm])
        K.op(DVE, lambda: v_.tensor_scalar(out=sm[:, 2, :], in0=fm[:, 6, :], scalar1=0.5, scalar2=None, op0=ALU.mult), [b_fm, b_sm], [b_sm])
        K.op(DVE, lambda: v_.tensor_scalar(out=sm[:, 3, :], in0=fm[:, 7, :], scalar1=0.5, scalar2=None, op0=ALU.mult), [b_fm, b_sm], [b_sm])
        cneg = sm[:, 0, :]
        hcneg = sm[:, 1, :]
        hbrg = sm[:, 2, :]
        hbig = sm[:, 3, :]

        sp_dma(cs[:], cs_d.ap(), w=[b_cs])
        sp_dma(bada[:], bada_d.ap(), w=[b_bada])
        K.op(ACT, lambda: a_.activation(out=th0[:], in_=cs[:], func=AF.Tanh, scale=0.5), [b_cs], [b_th0])
        K.op(DVE, lambda: v_.scalar_tensor_tensor(out=th0[:], in0=th0[:], scalar=1.0, in1=cs[:], op0=ALU.add, op1=ALU.mult),
             [b_cs, b_th0], [b_th0])
        K.op(DVE, lambda: v_.tensor_scalar(out=siluT[:], in0=th0[:], scalar1=0.5, scalar2=None, op0=ALU.mult), [b_th0], [b_silu])
        for g in range(12):
            wb = wada[g % 2]
            bw = b_wada[g % 2]
            pl_dma(wb[:], wada_d[:, g * 512:(g + 1) * 512].rearrange("(kc p) n -> p kc n", p=128), w=[bw])
            i = nb()
            for kc in range(8):
                mm(ps[i][0:NSEQ, :], siluT[:, kc, :], wb[:, kc, :], kc == 0, kc == 7, [b_silu, bw], [pb[i]], kc == 7)
            K.op(DVE, lambda: v_.tensor_tensor(out=modt[:, g * 512:(g + 1) * 512], in0=ps[i][0:NSEQ, :],
                                               in1=bada[:, g * 512:(g + 1) * 512], op=ALU.add), [pb[i], b_bada], [b_modt])
        b_modd = Buf()
        sp_dma(modd.ap(), modt[:], r=[b_modt], w=[b_modd])
        i = nb()
        pT0 = ps[i][:, 0:16 * NSEQ].rearrange("p (a b) -> p a b", b=NSEQ)
        for kc in range(16):
            col = (D + kc * 128) if kc < 8 else ((kc - 8) * 128)
            tr(pT0[:, kc, :], modt[0:NSEQ, col:col + 128], ident_f[0:NSEQ, 0:NSEQ], [b_modt, b_const], [pb[i]], kc == 15)
        K.op(DVE, lambda: v_.scalar_tensor_tensor(out=gsmT[:], in0=pT0[:, 0:8, :], scalar=1.0,
                                                  in1=fm[:, 0, :].unsqueeze(2).to_broadcast([128, 8, NSEQ]),
                                                  op0=ALU.add, op1=ALU.mult), [pb[i], b_fm], [b_gs])
        K.op(DVE, lambda: v_.tensor_copy(out=shmT[:], in_=pT0[:, 8:16, :]), [pb[i]], [b_gs])
        K.barrier()
        al.release(m0)

        off_hT = al.off
        hT = al.alloc("hT", [128, 8, S], BF16)
        yaT = al.alloc("yaT", [128, 4, S], BF16)
        off_yrT = al.off
        yrT = al.alloc("yrT", [128, 8, S], BF16)
        LIMIT = al.limit
        b_hT = [Buf() for _ in range(NT_S)]
        b_hT2 = [Buf() for _ in range(NT_S)]
        b_yaT, b_yrT = Buf(), Buf()
        mU = al.mark()

        for b in range(NSEQ):
            tok0 = b * S
            al.release(mU)
            x_sb = [al.alloc("x_sb", [128, D], F32) for _ in range(4)]
            xn = [al.alloc("xn", [128, D], BF16) for _ in range(2)]
            jk = al.alloc("jk", [128, D], BF16)
            st = [al.alloc("st", [128, 4], F32) for _ in range(3)]
            b_x, b_xn, b_st, b_jk = [Buf() for _ in range(4)], [Buf(), Buf()], [Buf() for _ in range(3)], Buf()
            rot["l"] = list(range(8))
            def m1_L(t):
                sp_dma(x_sb[t % 4][:], x_d[tok0 + t * 128: tok0 + (t + 1) * 128, :], w=[b_x[t % 4]])

            def m1_A1(t):
                xb, stb, bx, bst = x_sb[t % 4], st[t % 3], b_x[t % 4], b_st[t % 3]
                K.op(ACT, lambda: a_.activation(out=jk[:], in_=xb[:], func=AF.Square, accum_out=stb[:, 0:1]), [bx], [b_jk, bst])
                K.op(ACT, lambda: a_.activation(out=stb[:, 1:2], in_=stb[:, 0:1], func=AF.Sqrt, bias=EPS, scale=1.0 / D), [bst], [bst])
                K.op(DVE, lambda: v_.reciprocal(out=stb[:, 2:3], in_=stb[:, 1:2]), [bst], [bst])

            def m1_A2(t):
                xb, xnb, stb = x_sb[t % 4], xn[t % 2], st[t % 3]
                bx, bxn, bst = b_x[t % 4], b_xn[t % 2], b_st[t % 3]
                K.op(ACT, lambda: a_.activation(out=xnb[:], in_=xb[:], func=AF.Identity, scale=stb[:, 2:3]), [bx, bst], [bxn])

            def m1_B(t):
                xnb, bxn = xn[t % 2], b_xn[t % 2]
                ia, ib = nb(), nb()
                pTa = ps[ia][:, :].bitcast(BF16)
                pTb = ps[ib][:, :].bitcast(BF16)
                for kc in range(0, 8, 2):
                    tr(pTa[:, (kc // 2) * 128:(kc // 2 + 1) * 128], xnb[:, kc * 128:(kc + 1) * 128], ident_b, [bxn, b_const], [pb[ia]], kc == 6)
                for kc in range(1, 8, 2):
                    tr(pTb[:, (kc // 2) * 128:(kc // 2 + 1) * 128], xnb[:, kc * 128:(kc + 1) * 128], ident_b, [bxn, b_const], [pb[ib]], kc == 7)
                for kc in range(8):
                    o_ap = hT[:, kc, t * 128:(t + 1) * 128]
                    if kc % 2 == 0:
                        i_ap = pTa[:, (kc // 2) * 128:(kc // 2 + 1) * 128]
                        K.op(ACT, lambda: a_.activation(out=o_ap, in_=i_ap, func=AF.Identity, bias=shmT[:, kc, b:b + 1],
                                                        scale=gsmT[:, kc, b:b + 1]), [pb[ia], b_gs], [b_hT[t]])
                    else:
                        i_ap = pTb[:, (kc // 2) * 128:(kc // 2 + 1) * 128]
                        K.op(DVE, lambda: v_.tensor_scalar(out=o_ap, in0=i_ap, scalar1=gsmT[:, kc, b:b + 1],
                                                           scalar2=shmT[:, kc, b:b + 1], op0=ALU.mult, op1=ALU.add),
                             [pb[ib], b_gs], [b_hT2[t]])
            for t0_ in range(min(3, NT_S)):
                m1_L(t0_)
            m1_A1(0)
            if NT_S > 1:
                m1_A1(1)
            m1_A2(0)
            for t in range(NT_S):
                if t + 3 < NT_S:
                    m1_L(t + 3)
                if t + 2 < NT_S:
                    m1_A1(t + 2)
                if t + 1 < NT_S:
                    m1_A2(t + 1)
                m1_B(t)
            K.barrier()

            al.release(mU)
            qT = al.alloc("qT", [128, 4, 2, S], BF16)
            kT = al.alloc("kT", [128, 4, S], BF16)
            sv_ = al.off
            al.off = off_yrT
            vS = al.alloc("vS", [128, NT_S, 4, 192], BF16)
            ef = al.alloc("ef", [72, S], F32)
            assert al.off <= mU
            al.off = sv_
            off_wq = al.off
            wq = al.alloc("wq", [128, 8, 512], BF16)
            wkk = al.alloc("wk", [128, 8, 512], BF16)
            wv = al.alloc("wv", [128, 8, 512], BF16)
            wf = al.alloc("wf", [128, 8, 72], BF16)
            caug = al.alloc("caug", [128, S], BF16)
            tmpb = al.alloc("tmpb", [72, S], BF16)
            b_caug, b_tmpb = Buf(), Buf()
            Lf = al.alloc("Lf", [72, S], F32)
            cumLT = al.alloc("cumLT", [128, NT_S, NH], F32)
            b_Rb, b_Rs = Buf(), Buf()
            b_q, b_k, b_v, b_wq, b_wk, b_wv, b_wf = Buf(), Buf(), Buf(), Buf(), Buf(), Buf(), Buf()
            b_k2, b_v2 = Buf(), Buf()
            b_ef, b_L, b_cumLT = Buf(), Buf(), Buf()
            b_PT = [Buf() for _ in range(6)]

            def wview(c0, n):
                return win_d[:, c0:c0 + n].rearrange("(kc p) n -> p kc n", p=128)
            K.op(POOL, lambda: g_.memset(vS[:, :, :, 64:128], 1.0), [], [b_v])
            K.op(POOL, lambda: g_.memset(qT[:], 0.0), [], [b_q])
            pl_dma(wq[:], wview(OQ, 512), w=[b_wq])
            pl_dma(wkk[:], wview(OK_, 512), w=[b_wk])
            pl_dma(wv[:], wview(OV, 512), w=[b_wv])
            K.op(POOL, lambda: g_.memset(wf[:], 0.0), [], [b_wf])
            K.op(POOL, lambda: g_.memset(caug[:], 0.0), [], [b_caug])
            for g3 in range(3):
                pl_dma(wf[:, :, g3 * 32:g3 * 32 + NH], wview(OF_, 8), w=[b_wf])
            rot["l"] = list(range(8))
            cnt = 0
            for n in range(NQ):
                i = nb()
                for kc in range(8):
                    mm(ps[i][0:72, 0:QB], wf[:, kc, :], hT[:, kc, n * QB:(n + 1) * QB], kc == 0, kc == 7,
                       [b_wf] + (b_hT[n * TQ:(n + 1) * TQ] + b_hT2[n * TQ:(n + 1) * TQ]), [pb[i]], kc == 7)
                K.op(ACT, lambda: a_.activation(out=ef[:, n * QB:(n + 1) * QB], in_=ps[i][0:72, 0:QB], func=AF.Exp,
                                                bias=nbf[0:72, 1:2], scale=-1.0), [pb[i], b_nbf], [b_ef])
            K.op(ACT, lambda: a_.activation(out=Lf[:], in_=ef[:], func=AF.Ln, bias=1.0, scale=1.0), [b_ef], [b_L])
            K.op(DVE, lambda: v_.tensor_tensor_scan(out=ef[:], data0=ones_c[0:72, 0:1].to_broadcast([72, S]), data1=Lf[:],
                                                    initial=0.0, op0=ALU.mult, op1=ALU.add), [b_L, b_const, b_ef], [b_ef])
            K.op(DVE, lambda: v_.tensor_scalar(out=Lf[:], in0=ef[:], scalar1=-8.0, scalar2=None, op0=ALU.mult), [b_ef, b_L], [b_L])
            K.op(DVE, lambda: v_.tensor_copy(out=tmpb[:], in_=Lf[:]), [b_L], [b_tmpb])
            K.op(DVE, lambda: v_.tensor_copy(out=caug[0:NH, :], in_=tmpb[0:NH, :]), [b_tmpb], [b_caug])
            K.op(DVE, lambda: v_.tensor_tensor(out=Lf[:], in0=Lf[:], in1=tmpb[:], op=ALU.subtract), [b_L, b_tmpb], [b_L])
            K.op(DVE, lambda: v_.tensor_copy(out=tmpb[:], in_=Lf[:]), [b_L, b_caug], [b_tmpb])
            K.op(DVE, lambda: v_.tensor_copy(out=caug[32:32 + NH, :], in_=tmpb[32:32 + NH, :]), [b_tmpb], [b_caug])
            K.op(DVE, lambda: v_.tensor_tensor(out=Lf[:], in0=Lf[:], in1=tmpb[:], op=ALU.subtract), [b_L, b_tmpb], [b_L])
            K.op(DVE, lambda: v_.tensor_copy(out=caug[64:64 + NH, :], in_=Lf[64:64 + NH, :]), [b_L], [b_caug])
            for (wt, bw, dst, bd, isq) in ((wq, b_wq, qT, b_q, True), (wkk, b_wk, kT, b_k, False)):
                for j in range(4):
                    for n in range(NQ):
                        i = nb()
                        for kc in range(8):
                            mm(ps[i][:, 0:QB], wt[:, kc, j * 128:(j + 1) * 128], hT[:, kc, n * QB:(n + 1) * QB], kc == 0, kc == 7,
                               [bw] + (b_hT[n * TQ:(n + 1) * TQ] + b_hT2[n * TQ:(n + 1) * TQ]), [pb[i]], kc == 7)
                        cols = slice(n * QB, (n + 1) * QB)
                        if isq:
                            K.op(ACT, lambda: a_.copy(out=qT[0:64, j, 0, cols], in_=ps[i][0:64, 0:QB]), [pb[i]], [bd])
                            K.op(DVE, lambda: v_.tensor_copy(out=qT[64:128, j, 1, cols], in_=ps[i][64:128, 0:QB]), [pb[i]], [bd])
                        elif cnt % 2 == 0:
                            K.op(ACT, lambda: a_.copy(out=kT[:, j, cols], in_=ps[i][:, 0:QB]), [pb[i]], [bd])
                        else:
                            K.op(DVE, lambda: v_.tensor_copy(out=kT[:, j, cols], in_=ps[i][:, 0:QB]), [pb[i]], [b_k2])
                        cnt += 1
            for t in range(NT_S):
                i = nb()
                for kc in range(8):
                    mm(ps[i][:, :], hT[:, kc, t * 128:(t + 1) * 128], wv[:, kc, :], kc == 0, kc == 7, [b_wv, b_hT[t], b_hT2[t]], [pb[i]], kc == 7)
                o_v = vS[:, t, :, :].rearrange("p j (c w) -> p j c w", w=64)[:, :, 0::2, :]
                i_v = ps[i][:, :].rearrange("p (j c w) -> p j c w", c=2, w=64)
                if t % 2 == 0:
                    K.op(ACT, lambda: a_.copy(out=o_v, in_=i_v), [pb[i]], [b_v])
                else:
                    K.op(DVE, lambda: v_.tensor_copy(out=o_v, in_=i_v), [pb[i]], [b_v2])
            i = nb()
            pT1 = ps[i][:, 0:NT_S * NH].rearrange("p (a b) -> p a b", b=NH)
            for t in range(NT_S):
                tr(pT1[:, t, :], ef[0:NH, t * 128:(t + 1) * 128], ident_f[0:NH, 0:NH], [b_ef, b_const], [pb[i]], t == NT_S - 1)
            K.op(DVE, lambda: v_.tensor_copy(out=cumLT[:], in_=pT1), [pb[i]], [b_cumLT])

            K.barrier()
            sv3 = al.off
            al.off = off_wq
            PT = [al.alloc("PT", [128, QB], BF16) for _ in range(6)]
            Rb = al.alloc("Rb", [128, QB], BF16)
            Rs = al.alloc("Rs", [128, QB], F32)
            al.off = sv3
            rot["l"] = [4, 5, 6, 7]
            pcount = 0
            LAG = 3
            grp = {"n": 0}
            pend_backs = []

            def att_front(j, q, t, half, nt, gpar):
                nonlocal pcount
                d = t - q * TQ
                q0 = max(d, 0) * 128
                h = 2 * j + half
                rows = slice(half * 64, half * 64 + 64)
                i = nb()
                mm(ps[i][:, q0:QB], kT[:, j, t * 128:(t + 1) * 128], qT[:, j, half, q * QB + q0:(q + 1) * QB],
                   True, False, [b_q, b_k, b_k2], [pb[i]], False)
                mm(ps[i][:, q0:QB], cbt[:, 512 + h * 128:512 + (h + 1) * 128], caug[:, q * QB + q0:(q + 1) * QB],
                   False, d < 0, [b_const, b_caug], [pb[i]], d < 0)
                if d >= 0:
                    mm(ps[i][:, q0:q0 + 128], ident_b, negm_b, False, True, [b_const], [pb[i]], True)
                pt = PT[pcount % 6]
                bpt = b_PT[pcount % 6]
                pcount += 1
                K.op(ACT, lambda: a_.activation(out=pt[:, q0:QB], in_=ps[i][:, q0:QB], func=AF.Exp,
                                                bias=cumLT[:, t, h:h + 1], scale=0.125), [pb[i], b_cumLT], [bpt])

                def back():
                    yi = half + 2 * gpar
                    ya_, yb_ = 2 * gpar, 2 * gpar + 1
                    lo = 0 if half == 0 else 64
                    mm(ps[yi][:, q0:QB], vS[:, t, j, lo:lo + 128], pt[:, q0:QB], t == 0, t == nt - 1,
                       [b_v, b_v2, bpt], [pb[yi]], True)
                    if t == nt - 1 and half == 1:
                        K.op(DVE, lambda: v_.reciprocal(out=Rs[64:128, :], in_=ps[ya_][64:128, 0:QB]), [pb[ya_], b_Rs], [b_Rs])
                        K.op(DVE, lambda: v_.reciprocal(out=Rs[0:64, :], in_=ps[yb_][0:64, 0:QB]), [pb[yb_], b_Rs], [b_Rs])
                        K.op(DVE, lambda: v_.tensor_copy(out=Rb[:], in_=Rs[:]), [b_Rs, b_Rb], [b_Rb])
                        isw = nb()
                        mm(ps[isw][:, 0:QB], swap_b, Rb[:, :], True, True, [b_const, b_Rb], [pb[isw]], True)
                        K.op(ACT, lambda: a_.copy(out=Rs[:], in_=ps[isw][:, 0:QB]), [pb[isw]], [b_Rs])
                        K.op(DVE, lambda: v_.tensor_tensor(out=yaT[0:64, j, q * QB:(q + 1) * QB], in0=ps[ya_][0:64, 0:QB],
                                                           in1=Rs[0:64, :], op=ALU.mult), [pb[ya_], b_Rs], [b_yaT])
                        K.op(DVE, lambda: v_.tensor_tensor(out=yaT[64:128, j, q * QB:(q + 1) * QB], in0=ps[yb_][64:128, 0:QB],
                                                           in1=Rs[64:128, :], op=ALU.mult), [pb[yb_], b_Rs], [b_yaT])
                return back

            for j in range(4):
                for q in range(NQ):
                    nt = q * TQ + TQ
                    gpar = grp["n"] % 2
                    grp["n"] += 1
                    precast(PC_PER)
                    for t in range(nt):
                        for half in range(2):
                            pend_backs.append(att_front(j, q, t, half, nt, gpar))
                            if len(pend_backs) > LAG:
                                pend_backs.pop(0)()
            while pend_backs:
                pend_backs.pop(0)()
            K.barrier()

            al.release(mU)
            xp = [al.alloc("xp", [128, S + 4], F32) for _ in range(2)]
            uu = [al.alloc("uu", [128, S], F32) for _ in range(2)]
            ub = [al.alloc("ub", [128, S], BF16) for _ in range(2)]
            gg = [al.alloc("gg", [128, S], BF16) for _ in range(2)]
            thr = al.alloc("thr", [128, S], F32)
            thi = al.alloc("thi", [128, S], F32)
            e2 = al.alloc("e2", [128, S], F32)
            wx = [al.alloc("wx", [128, 8, 128], BF16) for _ in range(2)]
            wg = [al.alloc("wg", [128, 8, 128], BF16) for _ in range(2)]
            wrg = [al.alloc("wrg", [128, 128], BF16) for _ in range(2)]
            wig = [al.alloc("wig", [128, 128], BF16) for _ in range(2)]
            gt = [[al.alloc("gt", [128, QB], F32) for _ in range(2)] for _ in range(2)]
            b_xp, b_uu, b_ub, b_gg = [Buf(), Buf()], [Buf(), Buf()], [Buf(), Buf()], [Buf(), Buf()]
            b_thr, b_thi, b_e2 = Buf(), Buf(), Buf()
            b_w4 = [[Buf() for _ in range(4)] for _ in range(2)]
            b_gt = [[Buf() for _ in range(2)] for _ in range(2)]
            rot["l"] = list(range(8))
            for s2_ in range(2):
                K.op(DVE, lambda: v_.memset(xp[s2_][:, 0:4], 0.0), [], [b_xp[s2_]])

            def load_w4(c):
                s_ = c % 2
                pl_dma(wx[s_][:], wview(OX + c * 128, 128), w=[b_w4[s_][0]])
                pl_dma(wg[s_][:], wview(OG + c * 128, 128), w=[b_w4[s_][1]])
                pl_dma(wrg[s_][:], wrg_d[c, :, :], w=[b_w4[s_][2]])
                pl_dma(wig[s_][:], wig_d[c, :, :], w=[b_w4[s_][3]])

            gcnt4 = {"n": 0}

            def m4_A(c):
                zero_fill(-(-NBLK // (NSEQ * 8)))
                s_ = c % 2
                bwx, bwg_, bwrg, bwig = b_w4[s_]
                xp_, uu_, ub_, gg_ = xp[s_], uu[s_], ub[s_], gg[s_]
                bxp, buu, bub, bgg = b_xp[s_], b_uu[s_], b_ub[s_], b_gg[s_]
                for n in range(NQ):
                    i = nb()
                    for kc in range(8):
                        mm(ps[i][:, 0:QB], wx[s_][:, kc, :], hT[:, kc, n * QB:(n + 1) * QB], kc == 0, kc == 7,
                           [bwx] + (b_hT[n * TQ:(n + 1) * TQ] + b_hT2[n * TQ:(n + 1) * TQ]), [pb[i]], kc == 7)
                    K.op(ACT, lambda: a_.copy(out=xp_[:, 3 + n * QB:3 + (n + 1) * QB], in_=ps[i][:, 0:QB]), [pb[i]], [bxp])
                K.op(DVE, lambda: v_.tensor_scalar(out=uu_[:], in0=xp_[:, 3:3 + S], scalar1=fm[:, 4, c:c + 1], scalar2=fm[:, 5, c:c + 1],
                                                   op0=ALU.mult, op1=ALU.add), [bxp, b_fm], [buu])
                for jj in range(3):
                    K.op(DVE, lambda: v_.scalar_tensor_tensor(out=uu_[:], in0=xp_[:, jj:jj + S], scalar=fm[:, 1 + jj, c:c + 1], in1=uu_[:],
                                                              op0=ALU.mult, op1=ALU.add), [bxp, b_fm, buu], [buu])
                K.op(POOL, lambda: g_.tensor_copy(out=ub_[:], in_=uu_[:]), [buu], [bub])
                for n in range(NQ):
                    i = nb()
                    for kc in range(8):
                        mm(ps[i][:, 0:QB], wg[s_][:, kc, :], hT[:, kc, n * QB:(n + 1) * QB], kc == 0, kc == 7,
                           [bwg_] + (b_hT[n * TQ:(n + 1) * TQ] + b_hT2[n * TQ:(n + 1) * TQ]), [pb[i]], kc == 7)
                    g0, g1 = gt[gcnt4["n"] % 2]
                    bg0, bg1 = b_gt[gcnt4["n"] % 2]
                    gcnt4["n"] += 1
                    pg = ps[i][:, 0:QB]
                    K.op(ACT, lambda: a_.activation(out=g0[:], in_=pg, func=AF.Square), [pb[i]], [bg0])
                    K.op(DVE, lambda: v_.tensor_scalar(out=g0[:], in0=g0[:], scalar1=0.044715, scalar2=1.0, op0=ALU.mult, op1=ALU.add),
                         [bg0], [bg0])
                    K.op(DVE, lambda: v_.tensor_tensor(out=g0[:], in0=g0[:], in1=pg, op=ALU.mult), [bg0, pb[i]], [bg0])
                    K.op(ACT, lambda: a_.activation(out=g1[:], in_=g0[:], func=AF.Tanh, scale=0.7978845608028654), [bg0], [bg1])
                    K.op(DVE, lambda: v_.scalar_tensor_tensor(out=gg_[:, n * QB:(n + 1) * QB], in0=g1[:], scalar=1.0, in1=pg,
                                                              op0=ALU.add, op1=ALU.mult), [bg1, pb[i]], [bgg])

            def m4_B(c):
                s_ = c % 2
                bwx, bwg_, bwrg, bwig = b_w4[s_]
                uu_, ub_, gg_ = uu[s_], ub[s_], gg[s_]
                buu, bub, bgg = b_uu[s_], b_ub[s_], b_gg[s_]
                for n in range(NQ):
                    i = nb()
                    mm(ps[i][:, 0:QB], wrg[s_][:, :], ub_[:, n * QB:(n + 1) * QB], True, True, [bwrg, bub], [pb[i]], True)
                    K.op(ACT, lambda: a_.activation(out=thr[:, n * QB:(n + 1) * QB], in_=ps[i][:, 0:QB], func=AF.Tanh,
                                                    bias=hbrg[:, c:c + 1], scale=0.5), [pb[i], b_sm], [b_thr])
                    i2 = nb()
                    mm(ps[i2][:, 0:QB], wig[s_][:, :], ub_[:, n * QB:(n + 1) * QB], True, True, [bwig, bub], [pb[i2]], True)
                    K.op(ACT, lambda: a_.activation(out=thi[:, n * QB:(n + 1) * QB], in_=ps[i2][:, 0:QB], func=AF.Tanh,
                                                    bias=hbig[:, c:c + 1], scale=0.5), [pb[i2], b_sm], [b_thi])
                K.op(ACT, lambda: a_.activation(out=e2[:], in_=thr[:], func=AF.Exp, bias=cneg[:, c:c + 1], scale=cneg[:, c:c + 1]),
                     [b_thr, b_sm], [b_e2])
                K.op(ACT, lambda: a_.activation(out=thr[:], in_=thr[:], func=AF.Exp, bias=hcneg[:, c:c + 1], scale=hcneg[:, c:c + 1]),
                     [b_thr, b_sm], [b_thr])
                K.op(DVE, lambda: v_.tensor_scalar(out=e2[:], in0=e2[:], scalar1=1.0 - 1.0e-7, scalar2=None, op0=ALU.min), [b_e2], [b_e2])
                K.op(ACT, lambda: a_.activation(out=e2[:], in_=e2[:], func=AF.Sqrt, bias=1.0, scale=-1.0), [b_e2], [b_e2])
                K.op(DVE, lambda: v_.scalar_tensor_tensor(out=thi[:], in0=thi[:], scalar=1.0, in1=e2[:], op0=ALU.add, op1=ALU.mult),
                     [b_thi, b_e2], [b_thi])
                K.op(DVE, lambda: v_.scalar_tensor_tensor(out=thi[:], in0=thi[:], scalar=0.5, in1=uu_[:], op0=ALU.mult, op1=ALU.mult),
                     [b_thi, buu], [b_thi])
                K.op(DVE, lambda: v_.tensor_tensor_scan(out=e2[:], data0=thr[:], data1=thi[:], initial=0.0, op0=ALU.mult, op1=ALU.add),
                     [b_thr, b_thi, b_e2], [b_e2])
                K.op(DVE, lambda: v_.scalar_tensor_tensor(out=yrT[:, c, :], in0=gg_[:], scalar=0.5, in1=e2[:], op0=ALU.mult, op1=ALU.mult),
                     [bgg, b_e2], [b_yrT])

            load_w4(0)
            load_w4(1)
            m4_A(0)
            for c in range(8):
                if c + 1 < 8:
                    m4_A(c + 1)
                m4_B(c)
                if c + 2 < 8:
                    load_w4(c + 2)
            K.barrier()

            al.release(mU)
            mgT = al.alloc("mgT", [128, 8, S], BF16)
            b_mg = [Buf() for _ in range(NT_S)]
            m5 = al.mark()
            w5 = [al.alloc("w5", [128, 28, 128], BF16) for _ in range(2)]
            b_w5 = [[Buf() for _ in range(4)] for _ in range(2)]
            sa = [[al.alloc("sa", [128, QB], F32) for _ in range(4)] for _ in range(2)]
            b_sa = [[Buf() for _ in range(4)] for _ in range(2)]
            rot["l"] = list(range(8))

            def load_w5(m):
                s_ = m % 2
                cs_ = slice(m * 128, (m + 1) * 128)
                pl_dma(w5[s_][:, 0:4, :], wba_d[:, cs_].rearrange("(kc p) n -> p kc n", p=128), w=[b_w5[s_][0]])
                pl_dma(w5[s_][:, 4:12, :], wbr_d[:, cs_].rearrange("(kc p) n -> p kc n", p=128), w=[b_w5[s_][1]])
                pl_dma(w5[s_][:, 12:20, :], wview(OGA + m * 128, 128), w=[b_w5[s_][2]])
                pl_dma(w5[s_][:, 20:28, :], wview(OGR + m * 128, 128), w=[b_w5[s_][3]])
            load_w5(0)
            scount = 0
            for m in range(8):
                s_ = m % 2
                bwa, bwr_, bwga, bwgr = b_w5[s_]
                if m + 1 < 8:
                    load_w5(m + 1)
                for n in range(NQ):
                    cols = slice(n * QB, (n + 1) * QB)
                    hb = (b_hT[n * TQ:(n + 1) * TQ] + b_hT2[n * TQ:(n + 1) * TQ])
                    iA, iR, iGA, iGR = nb(), nb(), nb(), nb()
                    for kc in range(4):
                        mm(ps[iA][:, 0:QB], w5[s_][:, kc, :], yaT[:, kc, cols], kc == 0, kc == 3, [bwa, b_yaT], [pb[iA]], kc == 3)
                    for kc in range(8):
                        mm(ps[iGA][:, 0:QB], w5[s_][:, 12 + kc, :], hT[:, kc, cols], kc == 0, kc == 7, [bwga] + hb, [pb[iGA]], kc == 7)
                    for kc in range(8):
                        mm(ps[iR][:, 0:QB], w5[s_][:, 4 + kc, :], yrT[:, kc, cols], kc == 0, kc == 7, [bwr_, b_yrT], [pb[iR]], kc == 7)
                    for kc in range(8):
                        mm(ps[iGR][:, 0:QB], w5[s_][:, 20 + kc, :], hT[:, kc, cols], kc == 0, kc == 7, [bwgr] + hb, [pb[iGR]], kc == 7)
                    s0, s1, s2, s3 = sa[scount % 2]
                    c0, c1, c2, c3 = b_sa[scount % 2]
                    scount += 1
                    K.op(ACT, lambda: a_.activation(out=s0[:], in_=ps[iGA][:, 0:QB], func=AF.Sigmoid), [pb[iGA]], [c0])
                    K.op(DVE, lambda: v_.tensor_tensor(out=s1[:], in0=s0[:], in1=ps[iA][:, 0:QB], op=ALU.mult), [c0, pb[iA]], [c1])
                    K.op(ACT, lambda: a_.activation(out=s2[:], in_=ps[iGR][:, 0:QB], func=AF.Sigmoid), [pb[iGR]], [c2])
                    K.op(DVE, lambda: v_.tensor_tensor(out=s3[:], in0=s2[:], in1=ps[iR][:, 0:QB], op=ALU.mult), [c2, pb[iR]], [c3])
                    K.op(POOL, lambda: g_.tensor_tensor(out=mgT[:, m, cols], in0=s1[:], in1=s3[:], op=ALU.add), [c1, c3],
                         b_mg[n * TQ:(n + 1) * TQ])
            K.barrier()

            al.release(m5)
            offB = al.off
            useA = (mU - off_hT) >= 80 * 1024
            if useA:
                al.off = off_hT
                al.limit = mU
            wo = al.alloc("wo", [128, 8, D], BF16)
            h2Tb = [al.alloc("h2Tb", [128, 8, QB], BF16) for _ in range(2)]
            xs2 = [al.alloc("xs2", [128, D], F32) for _ in range(2)]
            x1 = [al.alloc("x1", [128, D], F32) for _ in range(2)]
            h2 = [al.alloc("h2", [128, D], F32) for _ in range(2)]
            h2T = [al.alloc("h2T", [128, 8, 128], F32) for _ in range(2)]
            shs = [al.alloc("shs", [128, D], BF16) for _ in range(2)]
            h2b = [al.alloc("h2b", [128, D], BF16) for _ in range(2)]
            if useA:
                al.off = offB
                al.limit = LIMIT
            wsg = al.alloc("wsg", [128, 8, FF], BF16)
            wsu = al.alloc("wsu", [128, 8, FF], BF16)
            wsd = al.alloc("wsd", [128, 2, D], BF16)
            wr = al.alloc("wr", [128, 8, E], F32)
            bcv5 = al.alloc("bcv5", [128, 2, D], F32)
            b_bcv5 = Buf()
            gmb = al.alloc("gmb", [128, D], F32)
            gsfb = al.alloc("gsfb", [128, D], F32)
            shfb = al.alloc("shfb", [128, D], F32)
            sg = [al.alloc("sg", [128, QB], F32) for _ in range(2)]
            actT = al.alloc("actT", [128, 2, QB], BF16)
            rs = [al.alloc("rs", [128, 8], F32) for _ in range(2)]
            rt_ = [al.alloc("rt", [128, 6, E], F32) for _ in range(2)]
            g8 = [al.alloc("g8", [128, 8, 8], F32) for _ in range(2)]
            r8 = [al.alloc("r8", [128, 4, 8], F32) for _ in range(2)]
            i8 = [al.alloc("i8", [128, 8], U32) for _ in range(2)]
            mkb = [al.alloc("mkb", [128, E], BF16) for _ in range(2)]
            b_wo, b_ws, b_wr, b_bc5 = Buf(), Buf(), Buf(), Buf()
            b_xs2, b_x1, b_h2, b_h2b, b_h2T = [Buf(), Buf()], [Buf(), Buf()], [Buf(), Buf()], [Buf(), Buf()], [Buf(), Buf()]
            b_h2Tb, b_shs, b_sg, b_act, b_rs, b_rt = [Buf(), Buf()], [Buf(), Buf()], [Buf(), Buf()], Buf(), [Buf(), Buf()], [Buf(), Buf()]
            b_jk5 = Buf()
            jk5 = al.alloc("jk5", [128, D], BF16)
            pl_dma(wo[:], wout_d.ap().rearrange("(kc p) n -> p kc n", p=128), w=[b_wo])
            b_wsg, b_wsu, b_wsd = Buf(), Buf(), Buf()
            pl_dma(wsg[:], wsg_d.ap().rearrange("(kc p) n -> p kc n", p=128), w=[b_wsg])
            pl_dma(wsu[:], wsu_d.ap().rearrange("(kc p) n -> p kc n", p=128), w=[b_wsu])
            pl_dma(wsd[:], wsd_d.ap().rearrange("(kc p) n -> p kc n", p=128), w=[b_wsd])
            sp_dma(wr[:], wr_d.ap().rearrange("(kc p) n -> p kc n", p=128), w=[b_wr])
            sp_dma(bcv5[:], bcv_d[:, 0:2, :], w=[b_bcv5])
            sp_dma(gmb[:], modd[b:b + 1, 2 * D:3 * D].partition_broadcast(128), r=[b_modd], w=[b_bc5])
            sp_dma(gsfb[:], modd[b:b + 1, 4 * D:5 * D].partition_broadcast(128), r=[b_modd], w=[b_bc5])
            sp_dma(shfb[:], modd[b:b + 1, 3 * D:4 * D].partition_broadcast(128), r=[b_modd], w=[b_bc5])
            K.op(DVE, lambda: v_.tensor_tensor(out=gmb[:], in0=gmb[:], in1=bcv5[:, 0, :], op=ALU.mult), [b_bc5, b_bcv5], [b_bc5])
            K.op(DVE, lambda: v_.scalar_tensor_tensor(out=gsfb[:], in0=gsfb[:], scalar=1.0, in1=bcv5[:, 1, :], op0=ALU.add, op1=ALU.mult),
                 [b_bc5, b_bcv5], [b_bc5])
            rot["l"] = list(range(8))
            def m5_A(t):
                n, tt = t // TQ, t % TQ
                T = b * NT_S + t
                r0 = tok0 + t * 128
                p_ = t % 2
                xb, x1b, h2_, h2b_, h2T_, rs_ = xs2[p_], x1[p_], h2[p_], h2b[p_], h2T[p_], rs[p_]
                bxb, bx1, bh2, bh2b, bh2T, brs = b_xs2[p_], b_x1[p_], b_h2[p_], b_h2b[p_], b_h2T[p_], b_rs[p_]
                sp_dma(xb[:], x_d[r0:r0 + 128, :], w=[bxb])
                io = [nb(), nb()]
                for hf in range(2):
                    for kc in range(8):
                        mm(ps[io[hf]][:, :], mgT[:, kc, t * 128:(t + 1) * 128], wo[:, kc, hf * 512:(hf + 1) * 512], kc == 0, kc == 7,
                           [b_mg[t], b_wo], [pb[io[hf]]], kc == 7)
                for hf in range(2):
                    K.op(ACT, lambda: a_.activation(out=jk5[:, hf * 512:(hf + 1) * 512], in_=ps[io[hf]][:, :], func=AF.Square,
                                                    accum_out=rs_[:, hf:hf + 1]), [pb[io[hf]]], [b_jk5, brs])
                K.op(DVE, lambda: v_.tensor_tensor(out=rs_[:, 2:3], in0=rs_[:, 0:1], in1=rs_[:, 1:2], op=ALU.add), [brs], [brs])
                K.op(ACT, lambda: a_.activation(out=rs_[:, 3:4], in_=rs_[:, 2:3], func=AF.Sqrt, bias=EPS, scale=1.0 / D), [brs], [brs])
                K.op(DVE, lambda: v_.reciprocal(out=rs_[:, 4:5], in_=rs_[:, 3:4]), [brs], [brs])
                for hf in range(2):
                    cs_ = slice(hf * 512, (hf + 1) * 512)
                    K.op(DVE, lambda: v_.scalar_tensor_tensor(out=x1b[:, cs_], in0=ps[io[hf]][:, :], scalar=rs_[:, 4:5], in1=gmb[:, cs_],
                                                              op0=ALU.mult, op1=ALU.mult), [pb[io[hf]], brs, b_bc5], [bx1])
                K.op(POOL, lambda: g_.tensor_tensor(out=x1b[:], in0=x1b[:], in1=xb[:], op=ALU.add), [bx1, bxb], [bx1])
                sp_dma(x1_d[r0:r0 + 128, :], x1b[:], r=[bx1])
                K.op(ACT, lambda: a_.activation(out=jk5[:], in_=x1b[:], func=AF.Square, accum_out=rs_[:, 5:6]), [bx1], [b_jk5, brs])
                K.op(ACT, lambda: a_.activation(out=rs_[:, 6:7], in_=rs_[:, 5:6], func=AF.Sqrt, bias=EPS, scale=1.0 / D), [brs], [brs])
                K.op(DVE, lambda: v_.reciprocal(out=rs_[:, 7:8], in_=rs_[:, 6:7]), [brs], [brs])
                K.op(DVE, lambda: v_.scalar_tensor_tensor(out=h2_[:], in0=x1b[:], scalar=rs_[:, 7:8], in1=gsfb[:], op0=ALU.mult, op1=ALU.mult),
                     [bx1, brs, b_bc5], [bh2])
                K.op(POOL, lambda: g_.tensor_tensor(out=h2_[:], in0=h2_[:], in1=shfb[:], op=ALU.add), [bh2, b_bc5], [bh2])
                K.op(POOL, lambda: g_.tensor_copy(out=h2b_[:], in_=h2_[:]), [bh2], [bh2b])
                sp_dma(h2_d[r0:r0 + 128, :], h2b_[:], r=[bh2b])

            def m5_B(t):
                n, tt = t // TQ, t % TQ
                hb_ = h2Tb[n % 2]
                bhb = b_h2Tb[n % 2]
                T = b * NT_S + t
                p_ = t % 2
                h2_, h2T_ = h2[p_], h2T[p_]
                bh2, bh2T = b_h2[p_], b_h2T[p_]
                it = [nb(), nb()]
                for kc in range(8):
                    ib = it[kc // 4]
                    tr(ps[ib][:, (kc % 4) * 128:(kc % 4 + 1) * 128], h2_[:, kc * 128:(kc + 1) * 128], ident_f, [bh2, b_const], [pb[ib]],
                       kc % 4 == 3)
                for hf in range(2):
                    K.op(ACT, lambda: a_.copy(out=h2T_[:, hf * 4:(hf + 1) * 4, :], in_=ps[it[hf]][:, :].rearrange("p (a b) -> p a b", b=128)),
                         [pb[it[hf]]], [bh2T])
                K.op(POOL, lambda: g_.tensor_copy(out=hb_[:, :, tt * 128:(tt + 1) * 128], in_=h2T_[:]), [bh2T], [bhb])
                il = nb()
                for kc in range(8):
                    mm(ps[il][:, 0:E], h2T_[:, kc, :], wr[:, kc, :], kc == 0, kc == 7, [bh2T, b_wr], [pb[il]], kc == 7)
                R_, g8_, r8_, i8_, mk_ = rt_[p_], g8[p_], r8[p_], i8[p_], mkb[p_]
                brt = b_rt[p_]
                sc, sel, selm, mkf, wfull, tmp = (R_[:, k_, :] for k_ in range(6))
                K.op(ACT, lambda: a_.activation(out=sc, in_=ps[il][:, 0:E], func=AF.Sigmoid), [pb[il]], [brt])
                K.op(DVE, lambda: v_.tensor_tensor(out=sel, in0=sc, in1=rbias[:], op=ALU.add), [brt, b_const], [brt])
                for gg in range(8):
                    K.op(DVE, lambda: v_.max(out=g8_[:, gg, :], in_=sel[:, gg * 8:(gg + 1) * 8]), [brt], [brt])
                K.op(DVE, lambda: v_.tensor_tensor(out=r8_[:, 0, :], in0=g8_[:, :, 0], in1=g8_[:, :, 1], op=ALU.add), [brt], [brt])
                K.op(DVE, lambda: v_.max(out=r8_[:, 1, :], in_=r8_[:, 0, :]), [brt], [brt])
                K.op(DVE, lambda: v_.tensor_scalar(out=r8_[:, 2, :], in0=r8_[:, 0, :], scalar1=r8_[:, 1, 3:4], scalar2=-BIG,
                                                   op0=ALU.is_lt, op1=ALU.mult), [brt], [brt])
                K.op(DVE, lambda: v_.tensor_tensor(out=selm.rearrange("p (a b) -> p a b", b=8), in0=sel.rearrange("p (a b) -> p a b", b=8),
                                                   in1=r8_[:, 2, :].unsqueeze(2).to_broadcast([128, 8, 8]), op=ALU.add), [brt], [brt])
                K.op(DVE, lambda: v_.max(out=r8_[:, 3, :], in_=selm), [brt], [brt])
                K.op(DVE, lambda: v_.max_index(out=i8_[:], in_max=r8_[:, 3, :], in_values=selm), [brt], [brt])
                K.op(DVE, lambda: v_.tensor_scalar(out=mkf, in0=selm, scalar1=r8_[:, 3, 5:6], scalar2=None, op0=ALU.is_ge), [brt], [brt])
                K.op(DVE, lambda: v_.tensor_tensor(out=wfull, in0=sc, in1=mkf, op=ALU.mult), [brt], [brt])
                K.op(DVE, lambda: v_.tensor_reduce(out=r8_[:, 2, 0:1], in_=wfull, axis=AX.X, op=ALU.add), [brt], [brt])
                K.op(DVE, lambda: v_.reciprocal(out=r8_[:, 2, 1:2], in_=r8_[:, 2, 0:1]), [brt], [brt])
                K.op(POOL, lambda: g_.tensor_copy(out=mk_[:], in_=mkf), [brt], [brt])
                ik = nb()
                mm(ps[ik][:, 0:E], triu_b, mk_[:], True, True, [brt, b_const], [pb[ik]], False)
                mm(ps[ik][:, E:2 * E], ones_b, mk_[:], True, True, [brt, b_const], [pb[ik]], True)
                K.op(DVE, lambda: v_.tensor_tensor(out=tmp, in0=ps[ik][:, 0:E], in1=run[:], op=ALU.add), [pb[ik], b_run, brt], [brt])
                K.op(DVE, lambda: v_.tensor_tensor(out=rankm[:, T, :], in0=tmp, in1=mkf, op=ALU.mult), [brt], [b_route])
                K.op(DVE, lambda: v_.tensor_tensor(out=run[:], in0=run[:], in1=ps[ik][:, E:2 * E], op=ALU.add), [pb[ik], b_run], [b_run])
                K.op(DVE, lambda: v_.tensor_copy(out=eidx[:, T, :], in_=i8_[:]), [brt], [b_route])
                for k_ in range(TOPK):
                    K.op(DVE, lambda: v_.scalar_tensor_tensor(out=tmp, in0=iota64, scalar=eidx[:, T, k_:k_ + 1], in1=wfull,
                                                              op0=ALU.is_equal, op1=ALU.mult, accum_out=wk[:, T, k_:k_ + 1]),
                         [brt, b_route, b_const], [brt, b_route])
                K.op(DVE, lambda: v_.tensor_scalar(out=wkn[:, T, 0:TOPK], in0=wk[:, T, 0:TOPK], scalar1=r8_[:, 2, 1:2], scalar2=2.5,
                                                   op0=ALU.mult, op1=ALU.mult), [brt, b_route], [b_route])

            def m5_SH(n):
                hb_ = h2Tb[n % 2]
                bhb = b_h2Tb[n % 2]
                ig = [nb(), nb()]
                iu = [nb(), nb()]
                for c in range(2):
                    for kc in range(8):
                        mm(ps[ig[c]][:, 0:QB], wsg[:, kc, c * 128:(c + 1) * 128], hb_[:, kc, :], kc == 0, kc == 7, [b_wsg, bhb], [pb[ig[c]]], kc == 7)
                    for kc in range(8):
                        mm(ps[iu[c]][:, 0:QB], wsu[:, kc, c * 128:(c + 1) * 128], hb_[:, kc, :], kc == 0, kc == 7, [b_wsu, bhb], [pb[iu[c]]], kc == 7)
                    sg_ = sg[c]
                    K.op(ACT, lambda: a_.activation(out=sg_[:], in_=ps[ig[c]][:, 0:QB], func=AF.Sigmoid), [pb[ig[c]]], [b_sg[c]])
                    K.op(DVE, lambda: v_.tensor_tensor(out=sg_[:], in0=sg_[:], in1=ps[ig[c]][:, 0:QB], op=ALU.mult), [b_sg[c], pb[ig[c]]], [b_sg[c]])
                    K.op(DVE, lambda: v_.tensor_tensor(out=actT[:, c, :], in0=sg_[:], in1=ps[iu[c]][:, 0:QB], op=ALU.mult),
                         [b_sg[c], pb[iu[c]]], [b_act])
                for tt in range(TQ):
                    t = n * TQ + tt
                    r0 = tok0 + t * 128
                    iy = [nb(), nb()]
                    for hf in range(2):
                        for c in range(2):
                            mm(ps[iy[hf]][:, :], actT[:, c, tt * 128:(tt + 1) * 128], wsd[:, c, hf * 512:(hf + 1) * 512], c == 0, c == 1,
                               [b_act, b_wsd], [pb[iy[hf]]], c == 1)
                    sh_ = shs[t % 2]
                    K.op(ACT, lambda: a_.copy(out=sh_[:, 0:512], in_=ps[iy[0]][:, :]), [pb[iy[0]]], [b_shs[t % 2]])
                    K.op(DVE, lambda: v_.tensor_copy(out=sh_[:, 512:1024], in_=ps[iy[1]][:, :]), [pb[iy[1]]], [b_shs[t % 2]])
                    sp_dma(sh_d[r0:r0 + 128, :], sh_[:], r=[b_shs[t % 2]])

            m5_A(0)
            for t in range(NT_S):
                if t + 1 < NT_S:
                    m5_A(t + 1)
                m5_B(t)
                if t % TQ == TQ - 1:
                    m5_SH(t // TQ)
            K.barrier()

        al.release(mU)
        al.off = mU
        pe_ = al.alloc("pend", [128, 4, E], F32)
        pei = al.alloc("pei", [128, E], I32)
        ebf = al.alloc("ebf", [128, 2, NBLK], F32)
        djk = al.alloc("djk", [128, E], F32)
        rkp = al.alloc("rkp", [128, E], F32)
        destf = al.alloc("destf", [128, NT, 8], F32)
        b_pe, b_eb, b_dj, b_rkp, b_destf, b_desti = Buf(), Buf(), Buf(), Buf(), Buf(), Buf()
        b_desti_t = [Buf() for _ in range(NT)]
        K.op(DVE, lambda: v_.tensor_scalar(out=pe_[:, 3, :], in0=run[:], scalar1=float(CB - 1), scalar2=None, op0=ALU.add), [b_run], [b_pe])
        K.op(DVE, lambda: v_.tensor_copy(out=pei[:], in_=pe_[:, 3, :]), [b_pe], [b_pe])
        K.op(DVE, lambda: v_.tensor_single_scalar(out=pei[:], in_=pei[:], scalar=8, op=ALU.arith_shift_right), [b_pe], [b_pe])
        K.op(DVE, lambda: v_.tensor_copy(out=pe_[:, 0, :], in_=pei[:]), [b_pe], [b_pe])
        K.op(DVE, lambda: v_.tensor_tensor_scan(out=pe_[:, 1, :], data0=ones_c[:, 0:1].to_broadcast([128, E]), data1=pe_[:, 0, :],
                                                initial=0.0, op0=ALU.mult, op1=ALU.add), [b_pe, b_const], [b_pe])
        K.op(DVE, lambda: v_.tensor_tensor(out=pe_[:, 2, :], in0=pe_[:, 1, :], in1=pe_[:, 0, :], op=ALU.subtract), [b_pe], [b_pe])
        K.op(DVE, lambda: v_.tensor_scalar(out=pe_[:, 2, :], in0=pe_[:, 2, :], scalar1=float(CB), scalar2=None, op0=ALU.mult), [b_pe], [b_pe])
        for T in range(NT):
            K.op(DVE, lambda: v_.tensor_tensor(out=rkp[:], in0=rankm[:, T, :], in1=pe_[:, 2, :], op=ALU.add), [b_route, b_pe, b_rkp], [b_rkp])
            for k_ in range(TOPK):
                K.op(DVE, lambda: v_.scalar_tensor_tensor(out=djk[:], in0=iota64, scalar=eidx[:, T, k_:k_ + 1], in1=rkp[:],
                                                          op0=ALU.is_equal, op1=ALU.mult, accum_out=destf[:, T, k_:k_ + 1]),
                     [b_route, b_rkp, b_const], [b_dj, b_destf])
            K.op(DVE, lambda: v_.tensor_copy(out=desti[:, T, 0:TOPK], in_=destf[:, T, 0:TOPK]), [b_destf], [b_desti_t[T]])
        hg = [al.alloc("hg", [128, D], BF16) for _ in range(3)]
        b_hg = [Buf() for _ in range(3)]
        b_xs = Buf()
        for T in range(NT):
            hb_ = hg[T % 3]
            sp_dma(hb_[:], h2_d[T * 128:(T + 1) * 128, :], w=[b_hg[T % 3]])
            for k_ in range(TOPK):
                K.dma(K.qpool, lambda: g_.indirect_dma_start(out=xs_d[:, :], out_offset=bass.IndirectOffsetOnAxis(ap=desti[:, T, k_:k_ + 1], axis=0),
                                                             in_=hb_[:], in_offset=None, bounds_check=reg_slot, oob_is_err=False),
                      [b_hg[T % 3], b_desti_t[T], b_xs0], [], [b_xs])
        K.op(DVE, lambda: v_.memset(ebf[:, 0, :], 0.0), [], [b_eb])
        for e_ in range(E):
            K.op(DVE, lambda: v_.scalar_tensor_tensor(out=ebf[:, 0, :], in0=iotab, scalar=pe_[:, 1, e_:e_ + 1], in1=ebf[:, 0, :],
                                                      op0=ALU.is_ge, op1=ALU.add), [b_pe, b_const, b_eb], [b_eb])
        K.op(DVE, lambda: v_.tensor_scalar(out=ebf[:, 0, :], in0=ebf[:, 0, :], scalar1=float(E - 1), scalar2=128.0, op0=ALU.min, op1=ALU.mult),
             [b_eb], [b_eb])
        K.op(DVE, lambda: v_.tensor_scalar(out=ebf[:, 1, :], in0=ebf[:, 0, :], scalar1=pidx, scalar2=None, op0=ALU.add), [b_eb, b_const], [b_eb])
        if skip_reload and NBLK > 2:
            K.op(DVE, lambda: v_.tensor_tensor(out=ebf[:, 0, 2:NBLK], in0=ebf[:, 0, 2:NBLK], in1=ebf[:, 1, 0:NBLK - 2], op=ALU.subtract),
                 [b_eb], [b_eb])
            K.op(DVE, lambda: v_.tensor_scalar(out=ebf[:, 0, 2:NBLK], in0=ebf[:, 0, 2:NBLK], scalar1=pidx, scalar2=None, op0=ALU.add),
                 [b_eb, b_const], [b_eb])
            K.op(DVE, lambda: v_.tensor_scalar(out=ebf[:, 0, 2:NBLK], in0=ebf[:, 0, 2:NBLK], scalar1=0.0, scalar2=1.0e6,
                                               op0=ALU.is_equal, op1=ALU.mult), [b_eb], [b_eb])
            K.op(DVE, lambda: v_.tensor_tensor(out=ebf[:, 1, 2:NBLK], in0=ebf[:, 1, 2:NBLK], in1=ebf[:, 0, 2:NBLK], op=ALU.add), [b_eb], [b_eb])
        K.op(DVE, lambda: v_.tensor_copy(out=widx[:], in_=ebf[:, 1, :]), [b_eb], [b_widx])
        K.barrier()

        al.off = mU
        wE = [[al.alloc("wE", [128, 2048], BF16) for _ in range(3)] for _ in range(2)]
        b_wE = [[Buf() for _ in range(3)] for _ in range(2)]
        xsb = [al.alloc("xsb", [128, 2, D], BF16) for _ in range(3)]
        xTe = [al.alloc("xTe", [128, 8, CB], BF16) for _ in range(3)]
        sge = [al.alloc("sge", [128, 2 * CB], F32) for _ in range(2)]
        acte = [al.alloc("acte", [128, 2 * CB], BF16) for _ in range(2)]
        ysb = [al.alloc("ysb", [128, D], BF16) for _ in range(4)]
        b_xsb, b_xTe, b_sge, b_acte = [Buf(), Buf(), Buf()], [Buf(), Buf(), Buf()], [Buf(), Buf()], [Buf(), Buf()]
        b_ysb = [Buf() for _ in range(4)]
        b_ys = Buf()
        rot["l"] = list(range(8))
        wsrc = wpb_d

        def load_wE(blk, which):
            s_ = blk % 2
            for m in which:
                K.dma(K.qpool, lambda: g_.indirect_dma_start(out=wE[s_][m][:], out_offset=None, in_=wsrc[m][:, :],
                                                             in_offset=bass.IndirectOffsetOnAxis(ap=widx[:, blk:blk + 1], axis=0),
                                                             bounds_check=reg_w, oob_is_err=False),
                      [b_widx, b_wcast], [b_wE[s_][m]])

        def load_xs(blk):
            p_ = blk % 3
            sp_dma(xsb[p_][:], xs_d[blk * CB:(blk + 1) * CB, :].rearrange("(s p) d -> p s d", p=128), r=[b_xs], w=[b_xsb[p_]])

        def stage_T(blk):
            p_ = blk % 3
            for s2 in range(2):
                i = nb()
                pT = ps[i][:, :].bitcast(BF16)
                for kc in range(8):
                    tr(pT[:, kc * 128:(kc + 1) * 128], xsb[p_][:, s2, kc * 128:(kc + 1) * 128], ident_b, [b_xsb[p_], b_const], [pb[i]], kc == 7)
                o_ap = xTe[p_][:, :, s2 * 128:(s2 + 1) * 128]
                i_ap = pT.rearrange("p (a b) -> p a b", b=128)
                if s2 == 0:
                    K.op(ACT, lambda: a_.copy(out=o_ap, in_=i_ap), [pb[i]], [b_xTe[p_]])
                else:
                    K.op(DVE, lambda: v_.tensor_copy(out=o_ap, in_=i_ap), [pb[i]], [b_xTe[p_]])

        def stage_GU(blk):
            s_ = blk % 2
            p_ = blk % 2
            x_ = blk % 3
            wgE, wuE, wdE = wE[s_]
            bwg, bwu, bwd = b_wE[s_]
            ig_, iu_ = nb(), nb()
            for c in range(2):
                for kc in range(8):
                    mm(ps[ig_][:, c * CB:(c + 1) * CB], wgE[:, kc * FF + c * 128: kc * FF + (c + 1) * 128], xTe[x_][:, kc, :], kc == 0, kc == 7,
                       [bwg, b_xTe[x_]], [pb[ig_]], kc == 7)
            for c in range(2):
                for kc in range(8):
                    mm(ps[iu_][:, c * CB:(c + 1) * CB], wuE[:, kc * FF + c * 128: kc * FF + (c + 1) * 128], xTe[x_][:, kc, :], kc == 0, kc == 7,
                       [bwu, b_xTe[x_]], [pb[iu_]], kc == 7)
            K.op(ACT, lambda: a_.activation(out=sge[p_][:], in_=ps[ig_][:, :], func=AF.Sigmoid), [pb[ig_]], [b_sge[p_]])
            K.op(DVE, lambda: v_.tensor_tensor(out=sge[p_][:], in0=sge[p_][:], in1=ps[ig_][:, :], op=ALU.mult), [b_sge[p_], pb[ig_]], [b_sge[p_]])
            K.op(DVE, lambda: v_.tensor_tensor(out=acte[p_][:], in0=sge[p_][:], in1=ps[iu_][:, :], op=ALU.mult), [b_sge[p_], pb[iu_]], [b_acte[p_]])

        ycnt = {"n": 0}

        def stage_D(blk):
            s_ = blk % 2
            p_ = blk % 2
            wdE = wE[s_][2]
            bwd = b_wE[s_][2]
            for s2 in range(2):
                yb = ysb[ycnt["n"] % 4]
                byb = b_ysb[ycnt["n"] % 4]
                ycnt["n"] += 1
                for hf in range(2):
                    i = nb()
                    for c in range(2):
                        mm(ps[i][:, :], acte[p_][:, c * CB + s2 * 128: c * CB + (s2 + 1) * 128], wdE[:, c * D + hf * 512: c * D + (hf + 1) * 512],
                           c == 0, c == 1, [b_acte[p_], bwd], [pb[i]], c == 1)
                    if hf == 0:
                        K.op(ACT, lambda: a_.copy(out=yb[:, 0:512], in_=ps[i][:, :]), [pb[i]], [byb])
                    else:
                        K.op(DVE, lambda: v_.tensor_copy(out=yb[:, 512:1024], in_=ps[i][:, :]), [pb[i]], [byb])
                r0 = blk * CB + s2 * 128
                sp_dma(ys_d[r0:r0 + 128, :], yb[:], r=[byb], sw=[b_ys])

        precast(3 * E)
        load_wE(0, (0, 1, 2))
        if NBLK > 1:
            load_wE(1, (0, 1, 2))
        for b0 in range(min(3, NBLK)):
            load_xs(b0)
        stage_T(0)
        if NBLK > 1:
            stage_T(1)
        stage_GU(0)
        for blk in range(NBLK):
            if blk + 2 < NBLK:
                load_wE(blk + 2, (0, 1))
                stage_T(blk + 2)
                if blk + 3 < NBLK:
                    load_xs(blk + 3)
            if blk >= 1:
                stage_D(blk - 1)
                if blk + 1 < NBLK:
                    load_wE(blk + 1, (2,))
            if blk + 1 < NBLK:
                stage_GU(blk + 1)
        stage_D(NBLK - 1)
        K.barrier()

        al.off = mU
        gfb = al.alloc("gfb", [128, NSEQ, D], F32)
        bcvc = al.alloc("bcvc", [128, D], F32)
        b_bcvc = Buf()
        sp_dma(bcvc[:], bcv_d[:, 2, :], w=[b_bcvc])
        b_gfb = Buf()
        for b in range(NSEQ):
            sp_dma(gfb[:, b, :], modd[b:b + 1, 5 * D:6 * D].partition_broadcast(128), r=[b_modd], w=[b_gfb])
            K.op(DVE, lambda: v_.tensor_tensor(out=gfb[:, b, :], in0=gfb[:, b, :], in1=bcvc[:], op=ALU.mult), [b_gfb, b_bcvc], [b_gfb])
        x1c = [al.alloc("x1c", [128, D], F32) for _ in range(2)]
        zc = [al.alloc("zc", [128, D], F32) for _ in range(2)]
        yg = [al.alloc("yg", [128, D], BF16) for _ in range(12)]
        shc = [al.alloc("shc", [128, D], BF16) for _ in range(2)]
        b_shc = [Buf(), Buf()]
        jkc = al.alloc("jkc", [128, D], BF16)
        rc_ = [al.alloc("rc", [128, 4], F32) for _ in range(2)]
        b_x1c, b_zc, b_rc = [Buf(), Buf()], [Buf(), Buf()], [Buf(), Buf()]
        b_yg = [Buf() for _ in range(12)]
        b_jkc = Buf()
        b_out = Buf()
        z2 = [al.alloc("z2", [128, D], F32) for _ in range(2)]
        b_z2 = [Buf(), Buf()]
        z3 = [al.alloc("z3", [128, D], F32) for _ in range(2)]
        b_z3 = [Buf(), Buf()]
        gc = {"n": 0}

        dg = [al.alloc("dg", [128, 128], BF16) for _ in range(12)]
        b_dg = [Buf() for _ in range(12)]
        rot["l"] = list(range(8))
        cps = {}

        def c_A(T):
            p_ = T % 2
            r0 = T * 128
            sp_dma(x1c[p_][:], x1_d[r0:r0 + 128, :], w=[b_x1c[p_]])
            sp_dma(shc[p_][:], sh_d[r0:r0 + 128, :], w=[b_shc[p_]])
            ys_ = []
            for k_ in range(TOPK):
                y_ = yg[gc["n"] % 12]
                by = b_yg[gc["n"] % 12]
                d_ = dg[gc["n"] % 12]
                bd = b_dg[gc["n"] % 12]
                gc["n"] += 1
                K.dma(K.qpool, lambda: g_.indirect_dma_start(out=y_[:], out_offset=None, in_=ys_d[:, :],
                                                             in_offset=bass.IndirectOffsetOnAxis(ap=desti[:, T, k_:k_ + 1], axis=0),
                                                             bounds_check=reg_slot, oob_is_err=False),
                      [b_ys, b_desti_t[T]], [by])
                K.op(ACT, lambda: a_.activation(out=d_[:], in_=ident_f, func=AF.Identity, scale=wkn[:, T, k_:k_ + 1]),
                     [b_const, b_route], [bd])
                ys_.append((y_, by, d_, bd))
            banks = [nb(), nb()]
            cps[T] = banks
            for hf in range(2):
                i = banks[hf]
                cs_ = slice(hf * 512, (hf + 1) * 512)
                mm(ps[i][:, :], ident_b, shc[p_][:, cs_], True, False, [b_const, b_shc[p_]], [pb[i]], False)
                for k_ in range(TOPK):
                    y_, by, d_, bd = ys_[k_]
                    mm(ps[i][:, :], d_[:], y_[:, cs_], False, k_ == TOPK - 1, [bd, by], [pb[i]], k_ == TOPK - 1)

        def c_B(T):
            b = T // NT_S
            p_ = T % 2
            r0 = T * 128
            banks = cps.pop(T)
            for hf in range(2):
                K.op(ACT, lambda: a_.activation(out=jkc[:, hf * 512:(hf + 1) * 512], in_=ps[banks[hf]][:, :], func=AF.Square,
                                                accum_out=rc_[p_][:, hf:hf + 1]), [pb[banks[hf]]], [b_jkc, b_rc[p_]])
            K.op(DVE, lambda: v_.tensor_tensor(out=rc_[p_][:, 3:4], in0=rc_[p_][:, 0:1], in1=rc_[p_][:, 1:2], op=ALU.add), [b_rc[p_]], [b_rc[p_]])
            K.op(ACT, lambda: a_.activation(out=rc_[p_][:, 1:2], in_=rc_[p_][:, 3:4], func=AF.Sqrt, bias=EPS, scale=1.0 / D), [b_rc[p_]], [b_rc[p_]])
            K.op(DVE, lambda: v_.reciprocal(out=rc_[p_][:, 2:3], in_=rc_[p_][:, 1:2]), [b_rc[p_]], [b_rc[p_]])
            for hf in range(2):
                cs_ = slice(hf * 512, (hf + 1) * 512)
                K.op(DVE, lambda: v_.scalar_tensor_tensor(out=zc[p_][:, cs_], in0=ps[banks[hf]][:, :], scalar=rc_[p_][:, 2:3], in1=gfb[:, b, cs_],
                                                          op0=ALU.mult, op1=ALU.mult), [pb[banks[hf]], b_rc[p_], b_gfb], [b_zc[p_]])
            K.op(DVE, lambda: v_.tensor_tensor(out=zc[p_][:], in0=zc[p_][:], in1=x1c[p_][:], op=ALU.add), [b_zc[p_], b_x1c[p_]], [b_zc[p_]])
            sp_dma(out_d[r0:r0 + 128, :], zc[p_][:], r=[b_zc[p_]], sw=[b_out])

        c_A(0)
        for T in range(NT):
            if T + 1 < NT:
                c_A(T + 1)
            c_B(T)
        K.barrier()
    return nc


def _bf16():
    import ml_dtypes
    return ml_dtypes.bfloat16


def make_consts(NBLK):
    NCF = 128 + 128 + 64 + NBLK + 2
    cf = np.zeros((128, NCF), np.float32)
    cf[:, 0:128] = np.eye(128, dtype=np.float32)
    cf[127, 128:256] = 1.0
    cf[:, 256:320] = np.arange(64, dtype=np.float32)[None, :]
    cf[:, 320:320 + NBLK] = np.arange(NBLK, dtype=np.float32)[None, :]
    cf[:, 320 + NBLK] = np.arange(128, dtype=np.float32)
    cf[:, 321 + NBLK] = 1.0
    cb = np.zeros((128, 1792), np.float32)
    cb[:, 0:128] = np.eye(128)
    k = np.arange(128)[:, None]
    m = np.arange(128)[None, :]
    cb[:, 128:256] = (k < m)
    cb[:, 256:384] = (m >= k)
    cb[:, 384:512] = 1.0
    for h in range(NH):
        for g3 in range(3):
            cb[g3 * 32 + h, 512 + h * 128:512 + (h + 1) * 128] = 1.0
    cb[:, 1536:1664] = np.where(m < k, -30000.0, 0.0)
    cb[:, 1664:1792] = (m == (k + 64) % 128)
    return cf, cb.astype(_bf16())


def prep_shared(inp):
    f = np.float32
    sh = {}
    sh["w_ada"] = np.ascontiguousarray(inp["w_ada"][0], f)
    fm = np.zeros((128, 9, 8), f)

    def fmaj(v):
        return np.asarray(v, f).reshape(8, 128).T
    fm[:, 0, :] = fmaj(inp["g_pre_mix"][0])
    for j in range(4):
        fm[:, 1 + j, :] = fmaj(inp["w_conv"][0, j])
    fm[:, 5, :] = fmaj(inp["b_conv"][0])
    fm[:, 6, :] = fmaj(inp["b_rg"][0])
    fm[:, 7, :] = fmaj(inp["b_ig"][0])
    fm[:, 8, :] = fmaj(inp["rglru_lambda"][0])
    sh["fm"] = fm
    bcv = np.zeros((128, 3, D), f)
    bcv[:, 0, :] = np.asarray(inp["g_post_mix"][0], f)[None, :]
    bcv[:, 1, :] = np.asarray(inp["g_pre_ffn"][0], f)[None, :]
    bcv[:, 2, :] = np.asarray(inp["g_post_ffn"][0], f)[None, :]
    sh["bcv"] = bcv
    sh["rbias"] = np.ascontiguousarray(np.broadcast_to(np.asarray(inp["router_bias"][0], f)[None, :], (128, E)))
    sh["b_forget"] = np.asarray(inp["b_forget"][0], f).reshape(NH, 1)
    sh["w_in"] = np.ascontiguousarray(inp["w_in"][0], f)
    for nm, src in (("wrg_bd", inp["w_rg"][0]), ("wig_bd", inp["w_ig"][0])):
        bd = np.zeros((8, 128, 128), f)
        for c in range(8):
            bd[c, 0:64, 0:64] = src[2 * c]
            bd[c, 64:128, 64:128] = src[2 * c + 1]
        sh[nm] = bd
    sh["w_ba"] = np.ascontiguousarray(inp["w_branch_attn"][0], f)
    sh["w_br"] = np.ascontiguousarray(inp["w_branch_rnn"][0], f)
    sh["w_out"] = np.ascontiguousarray(inp["w_out"][0], f)
    sh["w_router"] = np.ascontiguousarray(inp["w_router"][0], f)
    sh["w_sg"] = np.ascontiguousarray(inp["w_sh_gate"][0], f)
    sh["w_su"] = np.ascontiguousarray(inp["w_sh_up"][0], f)
    sh["w_sd"] = np.ascontiguousarray(inp["w_sh_down"][0], f)
    wg = np.asarray(inp["w_exp_gate"][0], f).reshape(E, 8, 128, FF).transpose(0, 2, 1, 3)
    sh["wpg"] = np.ascontiguousarray(wg).reshape(E * 128, 2048)
    wu = np.asarray(inp["w_exp_up"][0], f).reshape(E, 8, 128, FF).transpose(0, 2, 1, 3)
    sh["wpu"] = np.ascontiguousarray(wu).reshape(E * 128, 2048)
    wd = np.asarray(inp["w_exp_down"][0], f).reshape(E, 2, 128, D).transpose(0, 2, 1, 3)
    sh["wpd"] = np.ascontiguousarray(wd).reshape(E * 128, 2048)
    return sh


def prep_core(inp, sh, core, NSEQ, S, NBLK):
    f = np.float32
    m = dict(sh)
    xs = np.asarray(inp["x"][core * NSEQ:(core + 1) * NSEQ], f).reshape(NSEQ * S, D)
    m["x"] = np.ascontiguousarray(xs)
    c = np.asarray(inp["c"][core * NSEQ:(core + 1) * NSEQ], f)
    m["csT"] = np.ascontiguousarray(c.T.reshape(8, 128, NSEQ).transpose(1, 0, 2))
    m["b_ada_rep"] = np.ascontiguousarray(np.broadcast_to(np.asarray(inp["b_ada"][0], f)[None, :], (NSEQ, 6 * D)))
    cf, cb = make_consts(NBLK)
    m["cf"] = cf
    m["cb"] = cb
    return m


def kernel(**inputs):
    B, S = inputs["x"].shape[0], inputs["x"].shape[1]
    NSEQ = B // NCORES
    NTOK = NSEQ * S
    NBLK = (NTOK * TOPK) // CB + E
    nc = build(NSEQ, S, skip_reload=True)
    sh = prep_shared(inputs)
    in_maps = [prep_core(inputs, sh, i, NSEQ, S, NBLK) for i in range(NCORES)]
    res = run_bass_kernel_spmd(nc, in_maps, core_ids=list(range(NCORES)))
    outs = [np.asarray(r["out"], np.float32).reshape(NSEQ, S, D) for r in res.results]
    return np.concatenate(outs, axis=0)
```
